# Optimizing a Trainium2 kernel written in Bass

```python
import math
import jax, jax.numpy as jnp
from jax import lax
import numpy as np

D_MODEL = 1024
BATCH = 16
SEQ = 4096
DEPTH = 1

CTX_LEN = 256
GRID_W = 64
HEAD_DIM = 64
RWKV_WIDTH = D_MODEL // 2
RWKV_HEADS = RWKV_WIDTH // HEAD_DIM
DIFF_WIDTH = D_MODEL - RWKV_WIDTH
DIFF_HEADS = DIFF_WIDTH // (2 * HEAD_DIM)
DECAY_LORA = 64
AAA_LORA = 64
GATE_LORA = 128
DIR_LORA = DECAY_LORA + AAA_LORA + GATE_LORA
RWKV_IN = 3 * RWKV_WIDTH + 2 * DIR_LORA
DIFF_IN = 3 * DIFF_WIDTH
IN_WIDTH = RWKV_IN + DIFF_IN
AXIS_DIM = HEAD_DIM // 2
ROPE_THETA = 10000.0
DIFF_SCALE = HEAD_DIM ** -0.5
N_GROUPS = 4
EXPERTS_PER_GROUP = 8
N_EXPERTS = N_GROUPS * EXPERTS_PER_GROUP
TOP_K_IN_GROUP = 2
EXPERT_FF = 512
ROUTE_BLOCK = 128
ATTN_BLOCK = 128
NORM_EPS = 1e-6
GN_EPS = 64e-5
N_MOD = 6

kernel_name = "hybrid_rwkv7_diffattn_hmoe_dit_block"


def _rms(x, w, eps=NORM_EPS):
    xf = x.astype(jnp.float32)
    y = xf * lax.rsqrt(jnp.mean(xf * xf, axis=-1, keepdims=True) + eps)
    return (y * w).astype(x.dtype)


def _axial_rope_tables(seq_len):
    rows = seq_len // GRID_W
    row_id = jnp.repeat(jnp.arange(rows), GRID_W).astype(jnp.float32)
    col_id = jnp.tile(jnp.arange(GRID_W), rows).astype(jnp.float32)
    inv = ROPE_THETA ** (-jnp.arange(0, AXIS_DIM, 2, dtype=jnp.float32) / AXIS_DIM)
    ar = row_id[:, None] * inv
    ac = col_id[:, None] * inv
    ang = jnp.concatenate([ar, ar, ac, ac], axis=-1)
    return jnp.cos(ang), jnp.sin(ang)


def _rotate_half_axial(x):
    x1, x2, x3, x4 = jnp.split(x, 4, axis=-1)
    return jnp.concatenate([-x2, x1, -x4, x3], axis=-1)


def _apply_rope(x, cos, sin):
    c = cos[:, None, None, :]
    s = sin[:, None, None, :]
    return (x * c + _rotate_half_axial(x) * s).astype(x.dtype)


def _centred_shift(z, w):
    zp = jnp.pad(z, ((0, 0), (1, 1), (0, 0)))
    return w[0] * zp[:, :-2] + w[1] * zp[:, 1:-1] + w[2] * zp[:, 2:]


def _rwkv_direction(z, d, w0, w_up, a0, a_up, g_up, k_k, k_a, r_k):
    B, T, _ = z.shape
    heads = lambda t: t.reshape(B, T, RWKV_HEADS, HEAD_DIM)
    r, k, v, lora = jnp.split(z, [RWKV_WIDTH, 2 * RWKV_WIDTH, 3 * RWKV_WIDTH], axis=-1)
    lora_d = lora[..., d * DIR_LORA:(d + 1) * DIR_LORA]
    lw, la, lg = jnp.split(lora_d, [DECAY_LORA, DECAY_LORA + AAA_LORA], axis=-1)
    kkf = heads(k * k_k).astype(jnp.float32)
    kk = kkf / jnp.maximum(jnp.sqrt(jnp.sum(kkf * kkf, axis=-1, keepdims=True)), 1e-12)
    w_raw = (w0[d] + jnp.tanh(lw) @ w_up[d]).astype(jnp.float32)
    w_log = -jax.nn.softplus(-w_raw) - 0.5
    decay = jnp.exp(-jnp.exp(w_log))
    a = jax.nn.sigmoid(a0[d] + la @ a_up[d])
    g = jax.nn.sigmoid(lg) @ g_up[d]
    kd = k * (1 + (a - 1) * k_a)
    rh, kh, vh, ah = heads(r), heads(kd), heads(v), heads(a)
    bonus = jnp.sum(rh * kh * r_k, axis=-1, keepdims=True) * vh
    scan_in = (rh, heads(decay), kh, vh, -kk, kk * ah)
    return scan_in, g, bonus


def _rwkv7_scan(scan_in, s0, reverse, emit):
    xs = tuple(jnp.moveaxis(t.astype(jnp.float32), 1, 0) for t in scan_in)

    def step(S, inp):
        r, w, k, v, a, b = inp
        sa = jnp.einsum('bhij,bhj->bhi', S, a)
        S = S * w[:, :, None, :] + sa[..., None] * b[:, :, None, :] + v[..., None] * k[:, :, None, :]
        y = jnp.einsum('bhij,bhj->bhi', S, r) if emit else None
        return S, y

    S, ys = lax.scan(step, s0, xs, reverse=reverse)
    return S, (jnp.moveaxis(ys, 0, 1) if emit else None)


def _rwkv_readout(y, bonus, g, ln_w, ln_b):
    B, T = y.shape[:2]
    mu = jnp.mean(y, axis=-1, keepdims=True)
    var = jnp.mean(jnp.square(y - mu), axis=-1, keepdims=True)
    yn = ((y - mu) * lax.rsqrt(var + GN_EPS)).reshape(B, T, RWKV_WIDTH) * ln_w + ln_b
    return ((yn + bonus.reshape(B, T, RWKV_WIDTH)) * g).astype(g.dtype)


def _rwkv_group(rx, rc, w0, w_up, a0, a_up, g_up, k_k, k_a, r_k, ln_w, ln_b, need_ctx_out):
    B = rx.shape[0]
    out_x, out_c = None, None
    for d, reverse in ((0, False), (1, True)):
        sx, gx, bx = _rwkv_direction(rx, d, w0, w_up, a0, a_up, g_up, k_k, k_a, r_k)
        sc, gc, bc = _rwkv_direction(rc, d, w0, w_up, a0, a_up, g_up, k_k, k_a, r_k)
        s0 = jnp.zeros((B, RWKV_HEADS, HEAD_DIM, HEAD_DIM), jnp.float32)
        s_ctx, yc = _rwkv7_scan(sc, s0, reverse, need_ctx_out)
        _, yx = _rwkv7_scan(sx, s_ctx, reverse, True)
        ox = _rwkv_readout(yx, bx, gx, ln_w, ln_b)
        out_x = ox if out_x is None else out_x + ox
        if need_ctx_out:
            oc = _rwkv_readout(yc, bc, gc, ln_w, ln_b)
            out_c = oc if out_c is None else out_c + oc
    return out_x, out_c


def _diff_softmax_attend(q, k, v, lam):
    s = jnp.einsum('bhmqd,bhmkd->bhmqk', q, k, preferred_element_type=jnp.float32) * DIFF_SCALE
    p = jax.nn.softmax(s, axis=-1)
    a = p[:, :, 0] - lam * p[:, :, 1]
    return jnp.einsum('bhqk,bhkd->bhqd', a.astype(v.dtype), v)


def _latent_diff_attention(q, k_all, v_all, lam):
    B, H, _, T, dh = q.shape
    nb = T // ATTN_BLOCK
    qb = q.reshape(B, H, 2, nb, ATTN_BLOCK, dh).transpose(3, 0, 1, 2, 4, 5)
    out = lax.map(lambda qq: _diff_softmax_attend(qq, k_all, v_all, lam), qb)
    return out.transpose(1, 0, 3, 2, 4).reshape(B, T, H, 2 * dh)


def _diff_group(dx, dc, cos, sin, q_norm_w, k_norm_w, lam_q1, lam_k1, lam_q2, lam_k2, subln_w,
                lam_init, need_ctx_out):
    def split_heads(z):
        B, T, _ = z.shape
        q, k, v = jnp.split(z, 3, axis=-1)
        q = _rms(q.reshape(B, T, DIFF_HEADS, 2, HEAD_DIM), q_norm_w)
        k = _rms(k.reshape(B, T, DIFF_HEADS, 2, HEAD_DIM), k_norm_w)
        v = v.reshape(B, T, DIFF_HEADS, 2 * HEAD_DIM)
        return q, k, v

    qx, kx, vx = split_heads(dx)
    qc, kc, vc = split_heads(dc)
    qx = _apply_rope(qx, cos, sin)
    kx = _apply_rope(kx, cos, sin)
    to_bh = lambda z: z.transpose(0, 2, 3, 1, 4)
    k_all = jnp.concatenate([to_bh(kc), to_bh(kx)], axis=3)
    v_all = jnp.concatenate([vc, vx], axis=1).transpose(0, 2, 1, 3)
    lam = (jnp.exp(jnp.sum((lam_q1 * lam_k1).astype(jnp.float32)))
           - jnp.exp(jnp.sum((lam_q2 * lam_k2).astype(jnp.float32))) + lam_init)
    B, T = dx.shape[:2]
    ox = _latent_diff_attention(to_bh(qx), k_all, v_all, lam)
    ox = (_rms(ox, subln_w) * (1.0 - lam_init)).reshape(B, T, DIFF_WIDTH)
    oc = None
    if need_ctx_out:
        Cn = dc.shape[1]
        oc = _diff_softmax_attend(to_bh(qc), to_bh(kc), vc.transpose(0, 2, 1, 3), lam)
        oc = (_rms(oc.transpose(0, 2, 1, 3), subln_w) * (1.0 - lam_init)).reshape(B, Cn, DIFF_WIDTH)
    return ox, oc


def _dispatch_experts(hf, experts, weights, w_gate, w_up, w_down):
    n, d = hf.shape
    m = n * TOP_K_IN_GROUP
    flat_e = experts.reshape(m).astype(jnp.int32)
    flat_w = weights.reshape(m)
    order = jnp.argsort(flat_e)
    sorted_e = flat_e[order]
    tok = (order // TOP_K_IN_GROUP).astype(jnp.int32)
    counts = jnp.bincount(flat_e, length=N_EXPERTS)
    padded = (counts + ROUTE_BLOCK - 1) // ROUTE_BLOCK * ROUTE_BLOCK
    pad_end = jnp.cumsum(padded)
    pad_start = pad_end - padded
    start = jnp.cumsum(counts) - counts
    dest = pad_start[sorted_e] + jnp.arange(m, dtype=jnp.int32) - start[sorted_e]
    n_blocks = (m + N_EXPERTS * (ROUTE_BLOCK - 1) + ROUTE_BLOCK - 1) // ROUTE_BLOCK
    n_slots = n_blocks * ROUTE_BLOCK
    slot_tok = jnp.full((n_slots,), n, jnp.int32).at[dest].set(tok)
    slot_w = jnp.zeros((n_slots,), jnp.float32).at[dest].set(flat_w[order])
    block_expert = jnp.minimum(
        jnp.searchsorted(pad_end, jnp.arange(n_blocks, dtype=jnp.int32) * ROUTE_BLOCK, side='right'),
        N_EXPERTS - 1)
    h_pad = jnp.concatenate([hf, jnp.zeros((1, d), hf.dtype)], axis=0)

    def block_fn(args):
        toks, e = args
        xb = h_pad[toks]
        hid = jax.nn.silu(xb @ w_gate[e]) * (xb @ w_up[e])
        return hid @ w_down[e]

    ys = lax.map(block_fn, (slot_tok.reshape(n_blocks, ROUTE_BLOCK), block_expert))
    contrib = ys.reshape(n_slots, d).astype(jnp.float32) * slot_w[:, None]
    out = jnp.zeros((n + 1, d), jnp.float32).at[slot_tok].add(contrib)
    return out[:n].astype(hf.dtype)


def _hier_moe(h, w_group, b_group, w_expert, b_expert, w_gate, w_up, w_down):
    shape = h.shape
    hf = h.reshape(-1, shape[-1])
    n = hf.shape[0]
    g_logits = (hf @ w_group + b_group).astype(jnp.float32)
    g_sel = jnp.argmax(g_logits, axis=-1).astype(jnp.int32)
    g_prob = jnp.take_along_axis(jax.nn.softmax(g_logits, axis=-1), g_sel[:, None], axis=1)
    e_logits = (hf @ w_expert + b_expert).astype(jnp.float32).reshape(n, N_GROUPS, EXPERTS_PER_GROUP)
    e_logits = jnp.take_along_axis(e_logits, g_sel[:, None, None], axis=1)[:, 0]
    top_v, top_i = lax.top_k(e_logits, TOP_K_IN_GROUP)
    weights = jax.nn.softmax(top_v, axis=-1) * g_prob
    experts = g_sel[:, None] * EXPERTS_PER_GROUP + top_i
    y = _dispatch_experts(hf, experts, weights, w_gate, w_up, w_down)
    return y.reshape(shape)


def setup_inputs(seed: int = 0) -> dict:
    key = jax.random.key(seed)
    keys = jax.random.split(key, 40)
    L, D, RW = DEPTH, D_MODEL, RWKV_WIDTH

    def nrm(j, shape, s):
        return s * jax.random.normal(keys[j], shape, jnp.float32)

    return {
        "x": nrm(0, (BATCH, SEQ, D), 1.0),
        "c": nrm(1, (BATCH, D), 1.0),
        "ctx": nrm(2, (BATCH, CTX_LEN, D), 1.0),
        "c_ctx": nrm(3, (D,), 1.0),
        "norm1_w": 1.0 + nrm(4, (L, D), 0.02),
        "norm2_w": 1.0 + nrm(5, (L, D), 0.02),
        "w_mod": nrm(6, (L, D, N_MOD * D), 0.5 * D ** -0.5),
        "b_mod": nrm(7, (L, N_MOD * D), 0.02),
        "w_in": nrm(8, (L, D, IN_WIDTH), D ** -0.5),
        "shift_w": jnp.array([0.3, 1.0, 0.3], jnp.float32)[None, :, None] + nrm(9, (L, 3, RWKV_IN), 0.05),
        "rwkv_w0": jax.random.uniform(keys[10], (L, 2, RW), jnp.float32, -6.0, -1.0),
        "rwkv_w_up": nrm(11, (L, 2, DECAY_LORA, RW), 0.1),
        "rwkv_a0": nrm(12, (L, 2, RW), 0.1),
        "rwkv_a_up": nrm(13, (L, 2, AAA_LORA, RW), 0.1),
        "rwkv_g_up": nrm(14, (L, 2, GATE_LORA, RW), GATE_LORA ** -0.5),
        "rwkv_k_k": 0.85 + nrm(15, (L, RW), 0.02),
        "rwkv_k_a": 1.0 + nrm(16, (L, RW), 0.02),
        "rwkv_r_k": nrm(17, (L, RWKV_HEADS, HEAD_DIM), 0.1),
        "rwkv_ln_w": 1.0 + nrm(18, (L, RW), 0.02),
        "rwkv_ln_b": nrm(19, (L, RW), 0.02),
        "q_norm_w": 1.0 + nrm(20, (L, HEAD_DIM), 0.02),
        "k_norm_w": 1.0 + nrm(21, (L, HEAD_DIM), 0.02),
        "lam_q1": nrm(22, (L, HEAD_DIM), 0.1),
        "lam_k1": nrm(23, (L, HEAD_DIM), 0.1),
        "lam_q2": nrm(24, (L, HEAD_DIM), 0.1),
        "lam_k2": nrm(25, (L, HEAD_DIM), 0.1),
        "subln_w": 1.0 + nrm(26, (L, 2 * HEAD_DIM), 0.02),
        "w_out": nrm(27, (L, D, D), D ** -0.5),
        "w_group": nrm(28, (L, D, N_GROUPS), D ** -0.5),
        "b_group": nrm(29, (L, N_GROUPS), 0.01),
        "w_expert": nrm(30, (L, D, N_EXPERTS), D ** -0.5),
        "b_expert": nrm(31, (L, N_EXPERTS), 0.01),
        "moe_w_gate": nrm(32, (L, N_EXPERTS, D, EXPERT_FF), D ** -0.5),
        "moe_w_up": nrm(33, (L, N_EXPERTS, D, EXPERT_FF), D ** -0.5),
        "moe_w_down": nrm(34, (L, N_EXPERTS, EXPERT_FF, D), EXPERT_FF ** -0.5),
    }


def reference(x, c, ctx, c_ctx, norm1_w, norm2_w, w_mod, b_mod, w_in, shift_w,
              rwkv_w0, rwkv_w_up, rwkv_a0, rwkv_a_up, rwkv_g_up, rwkv_k_k, rwkv_k_a, rwkv_r_k,
              rwkv_ln_w, rwkv_ln_b, q_norm_w, k_norm_w, lam_q1, lam_k1, lam_q2, lam_k2, subln_w,
              w_out, w_group, b_group, w_expert, b_expert, moe_w_gate, moe_w_up, moe_w_down):
    T = x.shape[1]
    cos, sin = _axial_rope_tables(T)
    for i in range(DEPTH):
        last = i == DEPTH - 1
        lam_init = 0.8 - 0.6 * math.exp(-0.3 * i)
        mod_x = jax.nn.silu(c) @ w_mod[i] + b_mod[i]
        mod_c = jax.nn.silu(c_ctx) @ w_mod[i] + b_mod[i]
        sh1, sc1, g1, sh2, sc2, g2 = jnp.split(mod_x[:, None, :], N_MOD, axis=-1)
        csh1, csc1, cg1, csh2, csc2, cg2 = jnp.split(mod_c, N_MOD, axis=-1)

        hx = _rms(x, norm1_w[i]) * (1 + sc1) + sh1
        hc = _rms(ctx, norm1_w[i]) * (1 + csc1) + csh1
        px = hx @ w_in[i]
        pc = hc @ w_in[i]

        rx = _centred_shift(px[..., :RWKV_IN], shift_w[i])
        rc = _centred_shift(pc[..., :RWKV_IN], shift_w[i])
        o_rx, o_rc = _rwkv_group(rx, rc, rwkv_w0[i], rwkv_w_up[i], rwkv_a0[i], rwkv_a_up[i],
                                 rwkv_g_up[i], rwkv_k_k[i], rwkv_k_a[i], rwkv_r_k[i],
                                 rwkv_ln_w[i], rwkv_ln_b[i], not last)
        o_dx, o_dc = _diff_group(px[..., RWKV_IN:], pc[..., RWKV_IN:], cos, sin, q_norm_w[i],
                                 k_norm_w[i], lam_q1[i], lam_k1[i], lam_q2[i], lam_k2[i],
                                 subln_w[i], lam_init, not last)

        x = x + g1 * (jnp.concatenate([o_rx, o_dx], axis=-1) @ w_out[i])
        h2 = _rms(x, norm2_w[i]) * (1 + sc2) + sh2
        x = x + g2 * _hier_moe(h2, w_group[i], b_group[i], w_expert[i], b_expert[i],
                               moe_w_gate[i], moe_w_up[i], moe_w_down[i])
        if not last:
            ctx = ctx + cg1 * (jnp.concatenate([o_rc, o_dc], axis=-1) @ w_out[i])
            hc2 = _rms(ctx, norm2_w[i]) * (1 + csc2) + csh2
            ctx = ctx + cg2 * _hier_moe(hc2, w_group[i], b_group[i], w_expert[i], b_expert[i],
                                        moe_w_gate[i], moe_w_up[i], moe_w_down[i])
    return x
```

```python
import copy
import math
from contextlib import ExitStack

import numpy as np
import concourse.bass as bass
import concourse.mybir as mybir
from concourse.bass_utils import run_bass_kernel_spmd

F32 = mybir.dt.float32
BF16 = mybir.dt.bfloat16
AF = mybir.ActivationFunctionType
ALU = mybir.AluOpType
AX = mybir.AxisListType

D = 1024
NB = 2
RW = 512
INW = 3584
NE = 32
FF = 512
SUB = 4


class Buf:
    __slots__ = ("name", "w", "r")

    def __init__(self, name):
        self.name = name
        self.w = None
        self.r = []


class DSem:
    def __init__(self, h, q):
        self.h = h
        self.q = q
        self.total = 0


class K:
    def __init__(self, nc, es):
        self.nc = nc
        self.es = es
        self.E = {"pe": nc.tensor, "act": nc.scalar, "dve": nc.vector, "pool": nc.gpsimd, "sp": nc.sync}
        self.sem = {e: es.enter_context(nc.semaphore("c_" + e)) for e in ("pe", "act", "dve", "pool")}
        self.cnt = {e: 0 for e in self.sem}
        self.seen = {e: {} for e in self.E}
        self.dsems = []
        self.bufs = []
        self.dry = False
        self.inloop = False
        self.used = set()
        self.nbuf = 0
        self.phase_no = 0

    def buf(self, name=None):
        self.nbuf += 1
        b = Buf(name or ("b%d" % self.nbuf))
        self.bufs.append(b)
        return b

    def dsem(self, q, name):
        d = DSem(self.es.enter_context(self.nc.semaphore("d_%s_%d" % (name, self.phase_no))), q)
        self.dsems.append(d)
        return d

    def sb(self, name, shape, dt):
        return self.es.enter_context(self.nc.sbuf_tensor("%s_%d" % (name, self.phase_no), shape, dt))

    def ps(self, name, shape, dt=F32):
        return self.es.enter_context(self.nc.psum_tensor("%s_%d" % (name, self.phase_no), shape, dt))

    def _wait(self, e, tok):
        kind, src, n = tok
        key = src if kind == "E" else id(src)
        if kind == "E" and src == e and e == "pe":
            return
        if kind == "D":
            n = src.total
        if self.seen[e].get(key, -1) >= n:
            return
        self.seen[e][key] = n
        if self.dry:
            self.used.add((e, key))
            return
        h = self.sem[src] if kind == "E" else src.h
        if self.inloop:
            R = self.regs[(e, key)]
            delta = n - self.cur[(e, key)]
            if delta != 0:
                self.E[e].reg_add(R, R, delta)
            self.cur[(e, key)] = n
            self.E[e].wait_ge(h, R)
        else:
            self.E[e].wait_ge(h, n)

    def _sync(self, e, reads, writes):
        for b in reads:
            if b.w is not None:
                self._wait(e, b.w)
        for b in writes:
            if b.w is not None:
                self._wait(e, b.w)
            for t in b.r:
                self._wait(e, t)

    def op(self, e, fn, reads=(), writes=()):
        self._sync(e, reads, writes)
        self.cnt[e] += 1
        tok = ("E", e, self.cnt[e])
        if not self.dry:
            fn(self.E[e]).then_inc(self.sem[e], 1)
        self.seen[e][e] = max(self.seen[e].get(e, -1), 0)
        for b in reads:
            b.r.append(tok)
        for b in writes:
            b.w = tok
            b.r = []
        return tok

    def dma(self, ds, out, in_, reads=(), writes=()):
        q = ds.q
        self._sync(q, reads, writes)
        ds.total += 16
        tok = ("D", ds, ds.total)
        if not self.dry:
            self.E[q].dma_start(out=out, in_=in_).then_inc(ds.h, 16)
        for b in reads:
            b.r.append(tok)
        for b in writes:
            b.w = tok
            b.r = []
        return tok

    def drain(self):
        for d in self.dsems:
            if d.total > 0:
                self._wait(d.q, ("D", d, d.total))

    def barrier(self):
        self.drain()
        if not self.dry:
            self.nc.all_engine_barrier()
        for b in self.bufs:
            b.w = None
            b.r = []

    def _keycount(self, key):
        if isinstance(key, str):
            return self.cnt[key]
        for d in self.dsems:
            if id(d) == key:
                return d.total
        raise KeyError(key)

    def _snap_bufs(self, shift):
        def sh(tok):
            kind, src, n = tok
            return (kind, src, n - shift[src if kind == "E" else id(src)])
        return [(None if b.w is None else sh(b.w), [sh(t) for t in b.r]) for b in self.bufs]

    def _load_bufs(self, states):
        for b, (w, r) in zip(self.bufs, states):
            b.w = w
            b.r = list(r)

    def loop(self, n_iter, body, static=False):
        self.barrier()
        for e in self.seen:
            self.seen[e] = {}
        if n_iter == 1 or static:
            for i in range(n_iter):
                body(i)
                self.barrier()
            return
        c0 = dict(self.cnt)
        d0 = [d.total for d in self.dsems]
        nb0 = len(self.bufs)

        def rewind():
            self.cnt = dict(c0)
            for d, t in zip(self.dsems, d0):
                d.total = t
            for e in self.seen:
                self.seen[e] = {}

        self.dry = True
        self.used = set()
        body(0)
        P = {e: self.cnt[e] - c0[e] for e in self.cnt}
        for d, t in zip(self.dsems, d0):
            P[id(d)] = d.total - t
        carried = self._snap_bufs(P)
        rewind()
        self._load_bufs(carried)
        self.used = set()
        body(0)
        self.drain()
        used = sorted(self.used, key=str)
        rewind()
        self._load_bufs(carried)
        self.dry = False
        self.regs = {}
        self.cur = {}
        base = {}
        for (e, key) in used:
            self.nbuf += 1
            R = self.E[e].alloc_register("w_%s_%d" % (e, self.nbuf))
            base[(e, key)] = self._keycount(key)
            self.E[e].reg_mov(R, base[(e, key)])
            self.regs[(e, key)] = R
            self.cur[(e, key)] = base[(e, key)]
        with self.nc.Fori(0, n_iter) as it:
            self.inloop = True
            body(it)
            for (e, key) in used:
                delta = base[(e, key)] + P[key] - self.cur[(e, key)]
                if delta != 0:
                    self.E[e].reg_add(self.regs[(e, key)], self.regs[(e, key)], delta)
            self.inloop = False
        for (e, key) in used:
            self.E[e].free_register(self.regs[(e, key)])
        for e in self.cnt:
            self.cnt[e] += (n_iter - 1) * P[e]
        for d in self.dsems:
            d.total += (n_iter - 1) * P[id(d)]
        shift = {key: -(n_iter - 1) * P[key] for key in P}
        self._load_bufs(self._snap_bufs(shift))
        for e in self.seen:
            self.seen[e] = {}
        self.barrier()

    def phase_begin(self, pes):
        self.es = pes
        self.phase_no += 1
        self._mark = (len(self.dsems), len(self.bufs))

    def phase_end(self, es):
        self.barrier()
        self.es = es
        del self.dsems[self._mark[0]:]
        del self.bufs[self._mark[1]:]


def _bc(ap, shape):
    return ap.to_broadcast(shape)


C_ID = 0
C_IDA = 128
C_IDB = 256
C_BONES = 384
C_ONES = 512
C_ROT = 640
C_SEL = 768
C_ID3 = 1152
NCONST = 1160


def make_consts():
    c = np.zeros((128, NCONST), np.float32)
    c[:, C_ID:C_ID + 128] = np.eye(128)
    c[:64, C_IDA:C_IDA + 64] = np.eye(64)
    c[64:, C_IDB + 64:C_IDB + 128] = np.eye(64)
    c[:64, C_BONES:C_BONES + 64] = 1.0
    c[64:, C_BONES + 64:C_BONES + 128] = 1.0
    c[:, C_ONES:C_ONES + 128] = 1.0
    R = np.zeros((128, 128), np.float32)
    for blk in range(2):
        o = blk * 64
        for i in range(16):
            R[o + 16 + i, o + i] = -1.0
            R[o + i, o + 16 + i] = 1.0
            R[o + 48 + i, o + 32 + i] = -1.0
            R[o + 32 + i, o + 48 + i] = 1.0
    c[:, C_ROT:C_ROT + 128] = R
    for b in range(3):
        c[b, C_SEL + b * 128:C_SEL + (b + 1) * 128] = 1.0
    c[:3, C_ID3:C_ID3 + 3] = np.eye(3)
    return c


def rope_tables(TX, TCX):
    T = TX + TCX
    rows = TX // 64
    row_id = np.repeat(np.arange(rows), 64).astype(np.float32)
    col_id = np.tile(np.arange(64), rows).astype(np.float32)
    inv = (10000.0 ** (-np.arange(0, 32, 2, dtype=np.float32) / 32)).astype(np.float32)
    ar = row_id[:, None] * inv
    ac = col_id[:, None] * inv
    ang = np.concatenate([ar, ar, ac, ac], axis=-1)
    cos = np.ones((T, 64), np.float32)
    sin = np.zeros((T, 64), np.float32)
    cos[TCX:] = np.cos(ang)
    sin[TCX:] = np.sin(ang)
    cs = np.concatenate([cos.T, cos.T], axis=0)
    sn = np.concatenate([sin.T, sin.T], axis=0)
    return np.ascontiguousarray(cs), np.ascontiguousarray(sn)


def colform(v, n):
    return np.ascontiguousarray(np.asarray(v, np.float32).reshape(n, 128).T)


def geom(TX, TCX):
    T = TX + TCX
    NT = T // 128
    TP = T + 4
    blocks = []
    p = 0
    while p < TCX:
        n = min(512, TCX - p)
        blocks.append((p, n, p + 1))
        p += n
    p = 0
    while p < TX:
        n = min(512, TX - p)
        blocks.append((TCX + p, n, TCX + 3 + p))
        p += n
    return T, NT, TP, blocks


def build_program(TX, TCX, phases="ABCDEFG", debug=()):
    T, NT, TP, blocks = geom(TX, TCX)
    NTC = TCX // 128
    nc = bass.Bass("TRN2", target_bir_lowering=False)
    dram = {}

    def din(name, shape, dt=F32):
        dram[name] = nc.dram_tensor(name, list(shape), dt, kind="ExternalInput").ap()
        return dram[name]

    def dscr(name, shape, dt=F32):
        kind = "ExternalOutput" if name in debug else "Internal"
        dram[name] = nc.dram_tensor(name, list(shape), dt, kind=kind).ap()
        return dram[name]

    seq = din("seq", [NB, T, D])
    csT = din("csT", [128, 8, 3])
    consts_d = din("consts", [128, NCONST])
    w_mod = din("w_mod", [D, 6 * D])
    b_mod = din("b_mod", [1, 6 * D])
    n1w = din("n1w", [128, 8])
    n2w = din("n2w", [128, 8])
    w_in = din("w_in", [D, INW])
    shift_w = din("shift_w", [3, 2048])
    rw_w0 = din("rw_w0", [128, 2, 4])
    rw_a0 = din("rw_a0", [128, 2, 4])
    rw_wup = din("rw_wup", [2, 64, RW])
    rw_aup = din("rw_aup", [2, 64, RW])
    rw_gup = din("rw_gup", [2, 128, RW])
    rw_kk = din("rw_kk", [128, 4])
    rw_ka = din("rw_ka", [128, 4])
    rw_rk = din("rw_rk", [128, 4])
    rw_lnw = din("rw_lnw", [128, 4])
    rw_lnb = din("rw_lnb", [128, 4])
    qnw = din("qnw", [128, 1])
    knw = din("knw", [128, 1])
    lamv = din("lamv", [1, 256])
    sublnw = din("sublnw", [128, 1])
    w_out = din("w_out", [D, D])
    w_rt = din("w_rt", [D, 36])
    b_rt = din("b_rt", [1, 36])
    moe_g = din("moe_g", [NE, D, FF])
    moe_u = din("moe_u", [NE, D, FF])
    moe_d = din("moe_d", [NE, FF, D])
    ropec = din("ropec", [128, T])
    ropes = din("ropes", [128, T])
    out_d = nc.dram_tensor("out", [NB, TX, D], F32, kind="ExternalOutput").ap()

    P_d = dscr("P_d", [NB, INW, T])
    cols_d = dscr("cols_d", [2, NB, 4, 128, T + 1, 4], BF16)
    w_dd = dscr("w_dd", [2, NB, 4, 128, T])
    rows_d = dscr("rows_d", [2, 6, T, NB * 4, 128], BF16)
    v_d = dscr("v_d", [2, T, NB * 4, 64], BF16)
    g_d = dscr("g_d", [2, NB, 4, 128, T], BF16)
    bon_d = dscr("bon_d", [2, NB, 4, 128, T], BF16)
    y_d = dscr("y_d", [2, 2, T + 2, NB * 4, 64], BF16)
    qT_d = dscr("qT_d", [NB, 4, 128, T], BF16)
    kT_d = dscr("kT_d", [NB, 4, 128, T], BF16)
    vt_d = dscr("vt_d", [NB, T, 512], BF16)
    cat_d = dscr("cat_d", [NB, D, TX], BF16)
    x1_d = dscr("x1_d", [NB, TX, D])
    h2T_d = dscr("h2T_d", [NB, D, TX], BF16)
    wd_d = dscr("wd_d", [NB, TX, NE])

    with ExitStack() as es:
        k = K(nc, es)
        consts = k.sb("consts_sb", [128, NCONST], F32)
        cbf = k.sb("cbf", [128, 768], BF16)
        modT = k.sb("modT", [128, 48, 3], F32)
        A1 = k.sb("A1", [128, 8, 3], F32)
        A2 = k.sb("A2", [128, 8, 3], F32)
        G1 = k.sb("G1", [128, NB, D], F32)
        G2 = k.sb("G2", [128, NB, D], F32)
        eps_t = k.sb("eps_t", [128, 1], F32)
        b_consts = k.buf("consts")
        b_mod_ = k.buf("mod")
        ds_c = k.dsem("sp", "c")
        k.dma(ds_c, consts[:], consts_d[:, :], writes=[b_consts])
        k.op("dve", lambda e: e.tensor_copy(cbf[:], consts[:, 0:768]), reads=[b_consts], writes=[b_consts])
        k.op("dve", lambda e: e.memset(eps_t[:], 1e-6), writes=[b_consts])
        ident_bf = cbf[:, C_ID:C_ID + 128]
        identA_bf = cbf[:, C_IDA:C_IDA + 128]
        identB_bf = cbf[:, C_IDB:C_IDB + 128]
        bones_bf = cbf[:, C_BONES:C_BONES + 128]
        ones_bf = cbf[:, C_ONES:C_ONES + 128]
        rot_bf = cbf[:, C_ROT:C_ROT + 128]
        k.barrier()

        if "A" in phases:
            with ExitStack() as pes:
                k.phase_begin(pes)
                silT = k.sb("silT", [128, 8, 3], F32)
                modrow = k.sb("modrow", [3, 6 * D], F32)
                bmr = k.sb("bmr", [3, 6 * D], F32)
                n1c = k.sb("n1c", [128, 8], F32)
                n2c = k.sb("n2c", [128, 8], F32)
                wm = [k.sb("wm%d" % i, [128, 8, 1024], F32) for i in range(2)]
                pa = [k.ps("pa%d" % i, [3, 512]) for i in range(2)]
                pc = k.ps("pc", [128, 48, 3])
                pg = [k.ps("pg%d" % i, [128, 512]) for i in range(2)]
                b_sil, b_bmr, b_pc = k.buf(), k.buf(), k.buf()
                b_wm = [k.buf(), k.buf()]
                b_pa = [k.buf(), k.buf()]
                b_pg = [k.buf(), k.buf()]
                ds_a = k.dsem("sp", "a")
                ds_w = [k.dsem("sp", "wm0"), k.dsem("sp", "wm1")]
                k.dma(ds_a, silT[:], csT[:, :, :], writes=[b_sil])
                k.dma(ds_a, bmr[:], b_mod.partition_broadcast(3), writes=[b_bmr])
                k.dma(ds_a, n1c[:], n1w[:, :], writes=[b_bmr])
                k.dma(ds_a, n2c[:], n2w[:, :], writes=[b_bmr])
                k.op("act", lambda e: e.activation(out=silT[:], in_=silT[:], func=AF.Silu), reads=[b_sil], writes=[b_sil])
                wmv = w_mod.rearrange("(kc p) n -> p kc n", p=128)
                for m in range(6):
                    s = m % 2
                    k.dma(ds_w[s], wm[s][:], wmv[:, :, m * 1024:(m + 1) * 1024], writes=[b_wm[s]])
                    for blk in range(2):
                        pb = (m * 2 + blk) % 2
                        for kc in range(8):
                            k.op("pe", lambda e, kc=kc, s=s, blk=blk, pb=pb: e.matmul(
                                pa[pb][:], silT[:, kc, :], wm[s][:, kc, blk * 512:(blk + 1) * 512],
                                start=(kc == 0), stop=(kc == 7)), reads=[b_sil, b_wm[s]], writes=[b_pa[pb]])
                        c0 = m * 1024 + blk * 512
                        k.op("dve", lambda e, pb=pb, c0=c0: e.tensor_tensor(
                            out=modrow[:, c0:c0 + 512], in0=pa[pb][:], in1=bmr[:, c0:c0 + 512], op=ALU.add),
                            reads=[b_pa[pb], b_bmr], writes=[b_mod_])
                for f in range(48):
                    k.op("pe", lambda e, f=f: e.matmul(pc[:, f, :], modrow[0:3, f * 128:(f + 1) * 128],
                                                      consts[0:3, C_ID3:C_ID3 + 3], start=True, stop=True),
                         reads=[b_mod_, b_consts], writes=[b_pc])
                k.op("dve", lambda e: e.tensor_copy(modT[:], pc[:]), reads=[b_pc], writes=[b_mod_])
                k.op("dve", lambda e: e.scalar_tensor_tensor(
                    out=A1[:], in0=modT[:, 8:16, :], scalar=1.0, in1=_bc(n1c[:].unsqueeze(2), [128, 8, 3]),
                    op0=ALU.add, op1=ALU.mult), reads=[b_mod_, b_bmr], writes=[b_mod_])
                k.op("dve", lambda e: e.scalar_tensor_tensor(
                    out=A2[:], in0=modT[:, 32:40, :], scalar=1.0, in1=_bc(n2c[:].unsqueeze(2), [128, 8, 3]),
                    op0=ALU.add, op1=ALU.mult), reads=[b_mod_, b_bmr], writes=[b_mod_])
                i = 0
                for gi, Gt in ((2, G1), (5, G2)):
                    for b in range(NB):
                        for blk in range(2):
                            pb = i % 2
                            i += 1
                            c0 = gi * 1024 + blk * 512
                            k.op("pe", lambda e, b=b, c0=c0, pb=pb: e.matmul(
                                pg[pb][:], consts[0:3, C_SEL + b * 128:C_SEL + (b + 1) * 128],
                                modrow[0:3, c0:c0 + 512], start=True, stop=True),
                                reads=[b_mod_, b_consts], writes=[b_pg[pb]])
                            k.op("act", lambda e, Gt=Gt, b=b, blk=blk, pb=pb: e.activation(
                                out=Gt[:, b, blk * 512:(blk + 1) * 512], in_=pg[pb][:], func=AF.Copy),
                                reads=[b_pg[pb]], writes=[b_mod_])
                k.barrier()
                k.phase_end(es)
        B1 = modT[:, 0:8, :]
        B2 = modT[:, 24:32, :]

        if "dbgA" in debug:
            pass

        if "B" in phases:
            with ExitStack() as pes:
                k.phase_begin(pes)
                hT = k.sb("hT", [128, 8, TP], BF16)
                xt = [k.sb("xt%d" % i, [128, D], F32) for i in range(2)]
                sq = k.sb("sq", [128, D], F32)
                ss = k.sb("ss", [128, 4], F32)
                xn = [k.sb("xn%d" % i, [128, D], BF16) for i in range(2)]
                pt = [k.ps("pt%d" % i, [128, 8, 128], BF16) for i in range(2)]
                wst = [k.sb("wst%d" % i, [128, 8, 128], F32) for i in range(2)]
                wbf = [k.sb("wbf%d" % i, [128, 8, 3, 128], BF16) for i in range(2)]
                swb = k.sb("swb", [128, 3, 2048], F32)
                pp = [k.ps("pp%d" % i, [128, 512]) for i in range(4)]
                ev = [k.sb("ev%d" % i, [128, 512], F32) for i in range(4)]
                b_hT = k.buf("hT")
                b_xt, b_xn, b_pt = [k.buf(), k.buf()], [k.buf(), k.buf()], [k.buf(), k.buf()]
                b_sq, b_ss, b_swb = k.buf(), k.buf(), k.buf()
                b_wst, b_wbf = [k.buf(), k.buf()], [k.buf(), k.buf()]
                b_pp, b_ev = [k.buf() for _ in range(4)], [k.buf() for _ in range(4)]
                ds_x = [k.dsem("sp", "x0"), k.dsem("sp", "x1")]
                ds_ws = [k.dsem("sp", "ws0"), k.dsem("sp", "ws1")]
                ds_sw = k.dsem("sp", "sw")
                ds_ev = [k.dsem("pool", "ev%d" % i) for i in range(4)]
                k.dma(ds_sw, swb[:].rearrange("p a b -> p (a b)"),
                      shift_w.rearrange("a b -> (a b)").unsqueeze(0).partition_broadcast(128)
                      if False else shift_w.rearrange("(o a) b -> o (a b)", o=1).partition_broadcast(128),
                      writes=[b_swb])
                k.op("pool", lambda e: e.memset(hT[:], 0.0), writes=[b_hT])
                w_in_v = w_in.rearrange("(kc p) n -> p kc n", p=128)
                for b in range(NB):
                    for q in range(NT):
                        s = q % 2
                        sel = 2 if q < NTC else b
                        col0 = (q * 128 + 1) if q < NTC else (q * 128 + 3)
                        k.dma(ds_x[s], xt[s][:], seq[b, q * 128:(q + 1) * 128, :], writes=[b_xt[s]])
                        k.op("act", lambda e, s=s: e.activation(out=sq[:], in_=xt[s][:], func=AF.Square),
                             reads=[b_xt[s]], writes=[b_sq])
                        k.op("dve", lambda e: e.tensor_reduce(out=ss[:, 0:1], in_=sq[:], axis=AX.X, op=ALU.add),
                             reads=[b_sq], writes=[b_ss])
                        k.op("act", lambda e: e.activation(out=ss[:, 1:2], in_=ss[:, 0:1], func=AF.Sqrt,
                                                           bias=eps_t[:, 0:1], scale=1.0 / D),
                             reads=[b_ss, b_consts], writes=[b_ss])
                        k.op("dve", lambda e: e.reciprocal(ss[:, 2:3], ss[:, 1:2]), reads=[b_ss], writes=[b_ss])
                        k.op("dve", lambda e, s=s: e.tensor_scalar(out=xn[s][:], in0=xt[s][:], scalar1=ss[:, 2:3],
                                                                   scalar2=None, op0=ALU.mult),
                             reads=[b_xt[s], b_ss], writes=[b_xn[s]])
                        for kc in range(8):
                            k.op("pe", lambda e, s=s, kc=kc: e.transpose(out=pt[s][:, kc, :],
                                                                         in_=xn[s][:, kc * 128:(kc + 1) * 128],
                                                                         identity=ident_bf),
                                 reads=[b_xn[s], b_consts], writes=[b_pt[s]])
                        for kc in range(8):
                            k.op("act", lambda e, s=s, kc=kc, sel=sel, col0=col0: e.activation(
                                out=hT[:, kc, col0:col0 + 128], in_=pt[s][:, kc, :], func=AF.Identity,
                                bias=B1[:, kc, sel:sel + 1], scale=A1[:, kc, sel:sel + 1]),
                                reads=[b_pt[s], b_mod_], writes=[b_hT])
                    ie = 0
                    for c in range(28):
                        s = c % 2
                        k.dma(ds_ws[s], wst[s][:], w_in_v[:, :, c * 128:(c + 1) * 128], writes=[b_wst[s]])
                        ntap = 3 if c < 16 else 1
                        if c < 16:
                            for j in range(3):
                                eng = "pool" if j == 1 else "dve"
                                k.op(eng, lambda e, s=s, j=j, c=c: e.tensor_tensor(
                                    out=wbf[s][:, :, j, :], in0=wst[s][:],
                                    in1=_bc(swb[:, j, c * 128:(c + 1) * 128].unsqueeze(1), [128, 8, 128]),
                                    op=ALU.mult), reads=[b_wst[s], b_swb], writes=[b_wbf[s]])
                        else:
                            k.op("dve", lambda e, s=s: e.tensor_copy(wbf[s][:, :, 1, :], wst[s][:]),
                                 reads=[b_wst[s]], writes=[b_wbf[s]])
                        for (pos0, N, colb) in blocks:
                            pi = ie % 4
                            ie += 1
                            taps = (0, 1, 2) if c < 16 else (1,)
                            nmm = len(taps) * 8
                            im = 0
                            for j in taps:
                                for kc in range(8):
                                    k.op("pe", lambda e, pi=pi, s=s, kc=kc, j=j, colb=colb, N=N, im=im, nmm=nmm: e.matmul(
                                        pp[pi][:, 0:N], wbf[s][:, kc, j, :], hT[:, kc, colb + j - 1:colb + j - 1 + N],
                                        start=(im == 0), stop=(im == nmm - 1)),
                                        reads=[b_wbf[s], b_hT], writes=[b_pp[pi]])
                                    im += 1
                            eng = "act" if pi % 2 == 0 else "dve"
                            if eng == "act":
                                k.op("act", lambda e, pi=pi, N=N: e.activation(out=ev[pi][:, 0:N], in_=pp[pi][:, 0:N], func=AF.Copy),
                                     reads=[b_pp[pi]], writes=[b_ev[pi]])
                            else:
                                k.op("dve", lambda e, pi=pi, N=N: e.tensor_copy(ev[pi][:, 0:N], pp[pi][:, 0:N]),
                                     reads=[b_pp[pi]], writes=[b_ev[pi]])
                            k.dma(ds_ev[pi], P_d[b, c * 128:(c + 1) * 128, pos0:pos0 + N], ev[pi][:, 0:N],
                                  reads=[b_ev[pi]])
                    k.barrier()
                k.phase_end(es)

        if "C" in phases:
            with ExitStack() as pes:
                k.phase_begin(pes)
                PB = [k.sb("PB%d" % i, [128, 16, 512], F32) for i in range(2)]
                b_PB = [k.buf(), k.buf()]
                ds_pb = [k.dsem("sp", "pb0"), k.dsem("sp", "pb1")]
                ds_st = k.dsem("sp", "cst")
                ds_o = [k.dsem("pool", "co%d" % i) for i in range(4)]
                wst_c = k.sb("wst_c", [128, 512], F32)
                Wwa = k.sb("Wwa", [128, 2, 512], BF16)
                Wg = k.sb("Wg", [128, 2, 512], BF16)
                colc = k.sb("colc", [128, 40], F32)
                b_w = k.buf("cw")
                for d in range(2):
                    k.dma(ds_st, wst_c[0:64, :], rw_wup[d], writes=[b_w])
                    k.dma(ds_st, wst_c[64:128, :], rw_aup[d], writes=[b_w])
                    k.op("dve", lambda e, d=d: e.tensor_copy(Wwa[:, d, :], wst_c[:]), reads=[b_w], writes=[b_w])
                    k.dma(ds_st, wst_c[:], rw_gup[d], writes=[b_w])
                    k.op("dve", lambda e, d=d: e.tensor_copy(Wg[:, d, :], wst_c[:]), reads=[b_w], writes=[b_w])
                k.dma(ds_st, colc[:, 0:8], rw_w0.rearrange("p a b -> p (a b)"), writes=[b_w])
                k.dma(ds_st, colc[:, 8:16], rw_a0.rearrange("p a b -> p (a b)"), writes=[b_w])
                k.dma(ds_st, colc[:, 16:20], rw_kk[:, :], writes=[b_w])
                k.dma(ds_st, colc[:, 20:24], rw_ka[:, :], writes=[b_w])
                k.dma(ds_st, colc[:, 24:28], rw_rk[:, :], writes=[b_w])
                k.op("dve", lambda e: e.tensor_scalar(out=colc[:, 28:32], in0=colc[:, 20:24], scalar1=-1.0, scalar2=1.0,
                                                      op0=ALU.mult, op1=ALU.add), reads=[b_w], writes=[b_w])
                Vbf = k.sb("Vbf", [128, 4, 512], BF16)
                RH = k.sb("RH", [128, 4, 514], F32)
                CAx = k.sb("CAx", [128, 4], BF16)
                b_RH = k.buf("RH")
                ds_rh = k.dsem("sp", "rh")
                k.op("pool", lambda e: e.memset(CAx[:], 0.0), writes=[b_RH])
                TLs = [k.sb("TL%d" % i, [128, 512], BF16) for i in range(2)]
                SGs = [k.sb("SG%d" % i, [128, 512], BF16) for i in range(2)]
                f32ts = [{n: k.sb("c_%s%d" % (n, i), [128, 512], F32) for n in ("sgw", "dec", "Aa", "kkf", "sd", "kkn", "tmpk", "kd")} for i in range(2)]
                bfts = [{n: k.sb("c_%s%d" % (n, i), [128, 512], BF16) for n in ("Gg", "kk2", "bsc", "kdb", "rkr", "bon")} for i in range(2)]
                CAs = [k.sb("CA%d" % i, [128, 512, 4], BF16) for i in range(2)]
                rowsbs = [k.sb("rowsb%d" % i, [128, 4, 128], BF16) for i in range(2)]
                vrow = k.sb("vrow", [128, 4, 128], BF16)
                pw, pa_, pg_, pss, pbo = (k.ps(n, [128, 512]) for n in ("pw", "pa_", "pg_", "pss", "pbo"))
                prow = k.ps("prow", [128, 4, 128])
                pv = k.ps("pv", [128, 4, 128])
                bbs = [{n: k.buf(n) for n in ("TL", "SG", "sgw", "dec", "Aa", "kkf", "sd", "kkn", "tmpk", "kd", "Gg", "kk2",
                                              "bsc", "kdb", "rkr", "bon", "CA", "rowsb")} for i in range(2)]
                bbg = {n: k.buf(n) for n in ("Vbf", "vrow", "pw", "pa_", "pg_", "pss", "pbo", "prow", "pv")}
                for i in range(2):
                    bbs[i].update(bbg)
                bb = bbs[0]
                for i in range(2):
                    k.op("pool", lambda e, i=i: e.memset(CAs[i][:], 0.0), writes=[bbs[i]["CA"]])
                ihp = 0
                idd = 0
                irow = 0
                ztile = k.sb("ztile", [128, 1024], BF16)
                b_z = k.buf("z")
                k.op("pool", lambda e: e.memset(ztile[:], 0.0), writes=[b_z])
                for d in range(2):
                    zv = rows_d[d, 2:4].rearrange("r t s c -> (r t) (s c)")
                    for i in range(2 * T // 128):
                        k.dma(ds_o[i % 4], zv[i * 128:(i + 1) * 128, :], ztile[:], reads=[b_z])
                ib = 0
                for b in range(NB):
                    for (pos0, N, _c) in blocks:
                        s = ib % 2
                        ib += 1
                        P = PB[s]
                        bP = b_PB[s]
                        k.dma(ds_pb[s], P[:, :, 0:N], P_d[b, 0:2048, pos0:pos0 + N].rearrange("(c p) n -> p c n", p=128),
                              writes=[bP])
                        k.op("pool", lambda e: e.memset(RH[:], 0.0), writes=[b_RH])
                        lo = max(pos0 - 1, 0)
                        hi = min(pos0 + N + 1, T)
                        co = lo - (pos0 - 1)
                        k.dma(ds_rh, RH[:, :, co:co + hi - lo], P_d[b, 0:512, lo:hi].rearrange("(c p) n -> p c n", p=128), writes=[b_RH])
                        k.op("pool", lambda e, P=P, N=N: e.tensor_copy(Vbf[:, :, 0:N], P[:, 8:12, 0:N]), reads=[bP], writes=[bb["Vbf"]])
                        for j in range(N // 128):
                            for hp in range(4):
                                k.op("pe", lambda e, hp=hp, j=j: e.matmul(pv[:, hp, :], Vbf[:, hp, j * 128:(j + 1) * 128], ident_bf,
                                                                         start=True, stop=True),
                                     reads=[bb["Vbf"], b_consts], writes=[bb["pv"]])
                            k.op("act", lambda e: e.activation(out=vrow[:], in_=pv[:], func=AF.Copy), reads=[bb["pv"]], writes=[bb["vrow"]])
                            p0 = pos0 + j * 128
                            for ab in range(2):
                                k.dma(ds_o[ab], v_d[ab, p0:p0 + 128, b * 4:(b + 1) * 4, :], vrow[:, :, ab * 64:(ab + 1) * 64],
                                      reads=[bb["vrow"]])
                        for d in range(2):
                            TL = TLs[idd % 2]
                            SG = SGs[idd % 2]
                            bbd = bbs[idd % 2]
                            idd += 1
                            k.op("act", lambda e, P=P, N=N, d=d, TL=TL: e.activation(out=TL[0:64, 0:N], in_=P[0:64, 12 + 2 * d, 0:N], func=AF.Tanh),
                                 reads=[bP], writes=[bbd["TL"]])
                            k.op("dve", lambda e, P=P, N=N, d=d: e.tensor_copy(TL[64:128, 0:N], P[64:128, 12 + 2 * d, 0:N]),
                                 reads=[bP], writes=[bbd["TL"]])
                            k.op("act", lambda e, P=P, N=N, d=d: e.activation(out=SG[:, 0:N], in_=P[:, 13 + 2 * d, 0:N], func=AF.Sigmoid),
                                 reads=[bP], writes=[bbd["SG"]])
                            for hp in range(4):
                                hs = slice(hp * 128, (hp + 1) * 128)
                                t = f32ts[ihp % 2]
                                u = bfts[ihp % 2]
                                bb = dict(bbs[ihp % 2])
                                bb["TL"] = bbd["TL"]
                                bb["SG"] = bbd["SG"]
                                CA = CAs[ihp % 2]
                                ihp += 1
                                k.op("pe", lambda e, d=d, hs=hs, N=N: e.matmul(pw[:, 0:N], Wwa[0:64, d, hs], TL[0:64, 0:N], start=True, stop=True),
                                     reads=[b_w, bb["TL"]], writes=[bb["pw"]])
                                k.op("pe", lambda e, d=d, hs=hs, N=N: e.matmul(pa_[:, 0:N], Wwa[64:128, d, hs], TL[64:128, 0:N], start=True, stop=True),
                                     reads=[b_w, bb["TL"]], writes=[bb["pa_"]])
                                k.op("pe", lambda e, d=d, hs=hs, N=N: e.matmul(pg_[:, 0:N], Wg[:, d, hs], SG[:, 0:N], start=True, stop=True),
                                     reads=[b_w, bb["SG"]], writes=[bb["pg_"]])
                                ci = d * 4 + hp
                                k.op("act", lambda e, N=N, ci=ci: e.activation(out=t["sgw"][:, 0:N], in_=pw[:, 0:N], func=AF.Sigmoid,
                                                                               bias=colc[:, ci:ci + 1], scale=1.0),
                                     reads=[bb["pw"], b_w], writes=[bb["sgw"]])
                                k.op("act", lambda e, N=N: e.activation(out=t["dec"][:, 0:N], in_=t["sgw"][:, 0:N], func=AF.Exp,
                                                                        scale=-math.exp(-0.5)),
                                     reads=[bb["sgw"]], writes=[bb["dec"]])
                                k.dma(ds_o[2], w_dd[d, b, hp, :, pos0:pos0 + N], t["dec"][:, 0:N], reads=[bb["dec"]])
                                k.op("act", lambda e, N=N, ci=ci: e.activation(out=t["Aa"][:, 0:N], in_=pa_[:, 0:N], func=AF.Sigmoid,
                                                                               bias=colc[:, 8 + ci:9 + ci], scale=1.0),
                                     reads=[bb["pa_"], b_w], writes=[bb["Aa"]])
                                k.op("act", lambda e, N=N: e.activation(out=u["Gg"][:, 0:N], in_=pg_[:, 0:N], func=AF.Copy),
                                     reads=[bb["pg_"]], writes=[bb["Gg"]])
                                k.dma(ds_o[3], g_d[d, b, hp, :, pos0:pos0 + N], u["Gg"][:, 0:N], reads=[bb["Gg"]])
                                kk_ = P[:, 4 + hp, 0:N]
                                r_ = P[:, hp, 0:N]
                                v_ = P[:, 8 + hp, 0:N]
                                k.op("dve", lambda e, N=N, hp=hp, kk_=kk_: e.tensor_scalar(out=t["kkf"][:, 0:N], in0=kk_, scalar1=colc[:, 16 + hp:17 + hp],
                                                                                        scalar2=None, op0=ALU.mult),
                                     reads=[bP, b_w], writes=[bb["kkf"]])
                                k.op("pool", lambda e, N=N: e.tensor_tensor(out=u["kk2"][:, 0:N], in0=t["kkf"][:, 0:N], in1=t["kkf"][:, 0:N], op=ALU.mult),
                                     reads=[bb["kkf"]], writes=[bb["kk2"]])
                                k.op("pe", lambda e, N=N: e.matmul(pss[:, 0:N], bones_bf, u["kk2"][:, 0:N], start=True, stop=True),
                                     reads=[bb["kk2"], b_consts], writes=[bb["pss"]])
                                k.op("act", lambda e, N=N: e.activation(out=t["sd"][:, 0:N], in_=pss[:, 0:N], func=AF.Sqrt),
                                     reads=[bb["pss"]], writes=[bb["sd"]])
                                k.op("dve", lambda e, N=N: e.tensor_scalar(out=t["sd"][:, 0:N], in0=t["sd"][:, 0:N], scalar1=1e-12, scalar2=None, op0=ALU.max),
                                     reads=[bb["sd"]], writes=[bb["sd"]])
                                k.op("dve", lambda e, N=N: e.reciprocal(t["sd"][:, 0:N], t["sd"][:, 0:N]), reads=[bb["sd"]], writes=[bb["sd"]])
                                k.op("dve", lambda e, N=N: e.tensor_tensor(out=t["kkn"][:, 0:N], in0=t["kkf"][:, 0:N], in1=t["sd"][:, 0:N], op=ALU.mult),
                                     reads=[bb["kkf"], bb["sd"]], writes=[bb["kkn"]])
                                k.op("dve", lambda e, N=N: e.tensor_tensor(out=u["bsc"][:, 0:N], in0=t["kkn"][:, 0:N], in1=t["Aa"][:, 0:N], op=ALU.mult),
                                     reads=[bb["kkn"], bb["Aa"]], writes=[bb["bsc"]])
                                k.op("pool", lambda e, N=N, hp=hp: e.tensor_scalar(out=t["tmpk"][:, 0:N], in0=t["Aa"][:, 0:N], scalar1=colc[:, 20 + hp:21 + hp],
                                                                                 scalar2=colc[:, 28 + hp:29 + hp], op0=ALU.mult, op1=ALU.add),
                                     reads=[bb["Aa"], b_w], writes=[bb["tmpk"]])
                                k.op("pool", lambda e, N=N, kk_=kk_: e.tensor_tensor(out=t["kd"][:, 0:N], in0=kk_, in1=t["tmpk"][:, 0:N], op=ALU.mult),
                                     reads=[bP, bb["tmpk"]], writes=[bb["kd"]])
                                k.op("act", lambda e, N=N: e.activation(out=u["kdb"][:, 0:N], in_=t["kd"][:, 0:N], func=AF.Copy),
                                     reads=[bb["kd"]], writes=[bb["kdb"]])
                                k.op("dve", lambda e, N=N, hp=hp, r_=r_: e.scalar_tensor_tensor(out=u["rkr"][:, 0:N], in0=r_, scalar=colc[:, 24 + hp:25 + hp],
                                                                                             in1=t["kd"][:, 0:N], op0=ALU.mult, op1=ALU.mult),
                                     reads=[bP, bb["kd"], b_w], writes=[bb["rkr"]])
                                k.op("pe", lambda e, N=N: e.matmul(pbo[:, 0:N], bones_bf, u["rkr"][:, 0:N], start=True, stop=True),
                                     reads=[bb["rkr"], b_consts], writes=[bb["pbo"]])
                                k.op("dve", lambda e, N=N, v_=v_: e.tensor_tensor(out=u["bon"][:, 0:N], in0=pbo[:, 0:N], in1=v_, op=ALU.mult),
                                     reads=[bb["pbo"], bP], writes=[bb["bon"]])
                                k.dma(ds_o[0], bon_d[d, b, hp, :, pos0:pos0 + N], u["bon"][:, 0:N], reads=[bb["bon"]])
                                k.op("pool", lambda e, N=N: e.tensor_scalar(out=CA[0:64, 0:N, 0], in0=t["kkn"][0:64, 0:N], scalar1=-1.0, scalar2=None, op0=ALU.mult),
                                     reads=[bb["kkn"]], writes=[bb["CA"]])
                                k.op("pool", lambda e, N=N: e.tensor_scalar(out=CA[64:128, 0:N, 1], in0=t["kkn"][64:128, 0:N], scalar1=-1.0, scalar2=None, op0=ALU.mult),
                                     reads=[bb["kkn"]], writes=[bb["CA"]])
                                ro = 0 if d == 0 else 2
                                k.op("act", lambda e, N=N, hp=hp, ro=ro: e.activation(out=CA[0:64, 0:N, 2], in_=RH[0:64, hp, ro:ro + N], func=AF.Copy),
                                     reads=[b_RH], writes=[bb["CA"]])
                                k.op("act", lambda e, N=N, hp=hp, ro=ro: e.activation(out=CA[64:128, 0:N, 3], in_=RH[64:128, hp, ro:ro + N], func=AF.Copy),
                                     reads=[b_RH], writes=[bb["CA"]])
                                if d == 0 and pos0 + N == T:
                                    k.op("act", lambda e, N=N, hp=hp: e.activation(out=CAx[0:64, 2:3], in_=RH[0:64, hp, N:N + 1], func=AF.Copy),
                                         reads=[b_RH], writes=[b_RH])
                                    k.op("act", lambda e, N=N, hp=hp: e.activation(out=CAx[64:128, 3:4], in_=RH[64:128, hp, N:N + 1], func=AF.Copy),
                                         reads=[b_RH], writes=[b_RH])
                                    k.dma(ds_rh, cols_d[0, b, hp, :, T, :], CAx[:], reads=[b_RH])
                                k.dma(ds_o[1], cols_d[d, b, hp, :, pos0:pos0 + N, :], CA[:, 0:N, :], reads=[bb["CA"]])
                                for j in range(N // 128):
                                    js = slice(j * 128, (j + 1) * 128)
                                    rowsb = rowsbs[irow % 2]
                                    bb["rowsb"] = bbs[irow % 2]["rowsb"]
                                    irow += 1
                                    k.op("pe", lambda e, js=js: e.matmul(prow[:, 0, :], u["bsc"][:, js], identA_bf, start=True, stop=True),
                                         reads=[bb["bsc"], b_consts], writes=[bb["prow"]])
                                    k.op("pe", lambda e, js=js: e.matmul(prow[:, 1, :], u["bsc"][:, js], identB_bf, start=True, stop=True),
                                         reads=[bb["bsc"], b_consts], writes=[bb["prow"]])
                                    k.op("pe", lambda e, js=js: e.matmul(prow[:, 2, :], u["kdb"][:, js], identA_bf, start=True, stop=True),
                                         reads=[bb["kdb"], b_consts], writes=[bb["prow"]])
                                    k.op("pe", lambda e, js=js: e.matmul(prow[:, 3, :], u["kdb"][:, js], identB_bf, start=True, stop=True),
                                         reads=[bb["kdb"], b_consts], writes=[bb["prow"]])
                                    k.op("dve", lambda e: e.tensor_copy(rowsb[:], prow[:]), reads=[bb["prow"]], writes=[bb["rowsb"]])
                                    p0 = pos0 + j * 128
                                    k.dma(ds_o[2], rows_d[d, 0:2, p0:p0 + 128, b * 4 + hp, :].rearrange("r t c -> t r c"), rowsb[:, 0:2, :],
                                          reads=[bb["rowsb"]])
                                    k.dma(ds_o[3], rows_d[d, 4:6, p0:p0 + 128, b * 4 + hp, :].rearrange("r t c -> t r c"), rowsb[:, 2:4, :],
                                          reads=[bb["rowsb"]])
                k.barrier()
                k.phase_end(es)

        if "D" in phases:
            with ExitStack() as pes:
                k.phase_begin(pes)
                CH = 16
                ST = k.sb("ST", [128, 2, 8, 64], F32)
                T1 = k.sb("T1", [128, 2, 8, 64], F32)
                STb = k.sb("STb", [128, 2, 8, 64], BF16)
                colsAR = [k.sb("colsAR%d" % g, [128, 8, CH, 4], BF16) for g in range(2)]
                wcol = [k.sb("wcol%d" % g, [128, 8, CH], F32) for g in range(2)]
                rowsL = [k.sb("rowsL%d" % g, [6, CH, 8, 128], BF16) for g in range(2)]
                stage = [k.sb("stage%d" % g, [6, CH, 8, 64], BF16) for g in range(2)]
                ps1 = [k.ps("ps1%d" % g, [4, 8, 64]) for g in range(2)]
                ps2 = [k.ps("ps2%d" % g, [128, 8, 64]) for g in range(2)]
                b_ST, b_T1, b_STb = [k.buf(), k.buf()], [k.buf(), k.buf()], [k.buf(), k.buf()]
                b_cols, b_wc = [k.buf(), k.buf()], [k.buf(), k.buf()]
                b_rows, b_stv, b_sty = [k.buf(), k.buf()], [k.buf(), k.buf()], [k.buf(), k.buf()]
                b_ps1, b_ps2 = [k.buf(), k.buf()], [k.buf(), k.buf()]
                ds_g = [k.dsem("sp", "dg0"), k.dsem("pool", "dg1")]
                ds_y = [k.dsem("sp", "dy0"), k.dsem("pool", "dy1")]
                k.op("dve", lambda e: e.memset(ST[:], 0.0), writes=b_ST)
                k.op("dve", lambda e: e.memset(STb[:], 0.0), writes=b_STb)
                for g in range(2):
                    k.op("pool", lambda e, g=g: e.memset(stage[g][:], 0.0), writes=[b_stv[g], b_sty[g]])
                cols_v = [cols_d[g].rearrange("b h p t c -> p (b h) t c") for g in range(2)]
                wdd_v = [w_dd[g].rearrange("b h p t -> p (b h) t") for g in range(2)]

                def dsl(start, size):
                    if isinstance(start, int):
                        return slice(start, start + size)
                    return bass.ds(start, size)

                def scan_body(cbase, n):
                    def body(it):
                        cidx = [cbase + it, (cbase + n - 1) - it]
                        for g in range(2):
                            p0 = cidx[g] * CH
                            k.dma(ds_g[g], colsAR[g][:], cols_v[g][:, :, dsl(p0, CH), :], writes=[b_cols[g]])
                            k.dma(ds_g[g], wcol[g][:], wdd_v[g][:, :, dsl(p0, CH)], writes=[b_wc[g]])
                            k.dma(ds_g[g], rowsL[g][:], rows_d[g, :, dsl(p0, CH), :, :], writes=[b_rows[g]])
                            k.dma(ds_g[g], stage[g][4:6], v_d[:, dsl(p0, CH), :, :], writes=[b_stv[g]])
                        for st_ in range(CH):
                            tl = [st_, CH - 1 - st_]
                            for g in range(2):
                                for pr in range(8):
                                    k.op("pe", lambda e, g=g, pr=pr, t_=tl[g]: e.matmul(
                                        ps1[g][0:4, pr, :], colsAR[g][:, pr, t_, :], STb[:, g, pr, :], start=True, stop=True),
                                        reads=[b_cols[g], b_STb[g]], writes=[b_ps1[g]])
                            for g in range(2):
                                k.op("act", lambda e, g=g, t_=tl[g]: e.activation(
                                    out=stage[g][0:4, t_, :, :], in_=ps1[g][0:4, :, :], func=AF.Copy),
                                    reads=[b_ps1[g]], writes=[b_sty[g]])
                            for g in range(2):
                                k.op("pool", lambda e, g=g, t_=tl[g]: e.tensor_tensor(
                                    out=T1[:, g], in0=ST[:, g], in1=_bc(wcol[g][:, :, t_:t_ + 1], [128, 8, 64]), op=ALU.mult),
                                    reads=[b_ST[g], b_wc[g]], writes=[b_T1[g]])
                            for g in range(2):
                                for pr in range(8):
                                    k.op("pe", lambda e, g=g, pr=pr, t_=tl[g]: e.matmul(
                                        ps2[g][:, pr, :], rowsL[g][0:6, t_, pr, :], stage[g][0:6, t_, pr, :],
                                        start=True, stop=True),
                                        reads=[b_rows[g], b_stv[g], b_sty[g]], writes=[b_ps2[g]])
                            for g in range(2):
                                k.op("dve", lambda e, g=g: e.tensor_tensor(out=STb[:, g], in0=T1[:, g], in1=ps2[g][:], op=ALU.add),
                                     reads=[b_T1[g], b_ps2[g]], writes=[b_STb[g]])
                            for g in range(2):
                                k.op("dve", lambda e, g=g: e.tensor_tensor(out=ST[:, g], in0=T1[:, g], in1=ps2[g][:], op=ALU.add),
                                     reads=[b_T1[g], b_ps2[g]], writes=[b_ST[g]])
                        for g in range(2):
                            p0 = cidx[g] * CH
                            k.dma(ds_y[g], y_d[g, :, dsl(p0 + 1, CH), :, :], stage[g][2:4], reads=[b_sty[g]])
                    return body

                k.loop(TCX // CH, scan_body(0, TCX // CH))
                k.loop(TX // CH, scan_body(TCX // CH, TX // CH))
                for g, pv_ in ((0, T), (1, TCX - 1)):
                    k.dma(ds_g[g], colsAR[g][:, :, 0:1, :], cols_v[g][:, :, pv_:pv_ + 1, :], writes=[b_cols[g]])
                    for pr in range(8):
                        k.op("pe", lambda e, g=g, pr=pr: e.matmul(ps1[g][0:4, pr, :], colsAR[g][:, pr, 0, :], STb[:, g, pr, :],
                                                                  start=True, stop=True),
                             reads=[b_cols[g], b_STb[g]], writes=[b_ps1[g]])
                    k.op("act", lambda e, g=g: e.activation(out=stage[g][0:4, 0, :, :], in_=ps1[g][0:4, :, :], func=AF.Copy),
                         reads=[b_ps1[g]], writes=[b_sty[g]])
                    k.dma(ds_y[g], y_d[g, :, pv_ + 1:pv_ + 2, :, :], stage[g][2:4, 0:1], reads=[b_sty[g]])
                k.phase_end(es)

        xblocks = [(p, n) for (p, n, _c) in blocks if p >= TCX]
        if "E" in phases:
            with ExitStack() as pes:
                k.phase_begin(pes)
                Gt = k.sb("Gt", [128, 2, 4, 512], BF16)
                Bt = k.sb("Bt", [128, 2, 4, 512], BF16)
                Yt = k.sb("Yt", [128, 2, 4, 2, 64], BF16)
                Yf = k.sb("Yf", [128, 16, 64], F32)
                cen = k.sb("cen", [128, 16, 64], F32)
                sqe = k.sb("sqe", [128, 16, 64], F32)
                st4 = k.sb("st4", [128, 4, 16], F32)
                yh = k.sb("yh", [128, 2, 512], BF16)
                Zn = k.sb("Zn", [128, 2, 4, 128], F32)
                catR = k.sb("catR", [128, 4, 512], BF16)
                lnc = k.sb("lnc", [128, 8], F32)
                gne = k.sb("gne", [128, 1], F32)
                pte = k.ps("pte", [128, 8, 128], BF16)
                bG, bB, bY, bYf, bcen, bsq, bst, byh, bZn, bcat, bln, bpte = (k.buf() for _ in range(12))
                ds_e = [k.dsem("sp", "e%d" % i) for i in range(3)]
                ds_eo = k.dsem("pool", "eo")
                k.dma(ds_e[2], lnc[:, 0:4], rw_lnw[:, :], writes=[bln])
                k.dma(ds_e[2], lnc[:, 4:8], rw_lnb[:, :], writes=[bln])
                k.op("dve", lambda e: e.memset(gne[:], 64e-5), writes=[bln])
                for b in range(NB):
                    for (pos0, N) in xblocks:
                        for d in range(2):
                            k.dma(ds_e[0], Gt[:, d, :, 0:N], g_d[d, b, :, :, pos0:pos0 + N].rearrange("h p t -> p h t"), writes=[bG])
                            k.dma(ds_e[0], Bt[:, d, :, 0:N], bon_d[d, b, :, :, pos0:pos0 + N].rearrange("h p t -> p h t"), writes=[bB])
                        for j in range(N // 128):
                            pos = pos0 + j * 128
                            for d in range(2):
                                sl0 = pos + 2 if d == 0 else pos
                                for ab in range(2):
                                    k.dma(ds_e[1], Yt[:, d, :, ab, :], y_d[d, ab, sl0:sl0 + 128, b * 4:(b + 1) * 4, :], writes=[bY])
                            k.op("act", lambda e: e.activation(out=Yf[:], in_=Yt[:].rearrange("p d h a i -> p (d h a) i"), func=AF.Copy),
                                 reads=[bY], writes=[bYf])
                            k.op("dve", lambda e: e.tensor_reduce(out=st4[:, 0, :], in_=Yf[:], axis=AX.X, op=ALU.add), reads=[bYf], writes=[bst])
                            k.op("dve", lambda e: e.tensor_scalar(out=st4[:, 1, :], in0=st4[:, 0, :], scalar1=-1.0 / 64, scalar2=None, op0=ALU.mult),
                                 reads=[bst], writes=[bst])
                            k.op("dve", lambda e: e.tensor_tensor(out=cen[:], in0=Yf[:], in1=_bc(st4[:, 1, :].unsqueeze(2), [128, 16, 64]), op=ALU.add),
                                 reads=[bYf, bst], writes=[bcen])
                            k.op("pool", lambda e: e.tensor_tensor(out=sqe[:], in0=cen[:], in1=cen[:], op=ALU.mult), reads=[bcen], writes=[bsq])
                            k.op("dve", lambda e: e.tensor_reduce(out=st4[:, 2, :], in_=sqe[:], axis=AX.X, op=ALU.add), reads=[bsq], writes=[bst])
                            k.op("act", lambda e: e.activation(out=st4[:, 3, :], in_=st4[:, 2, :], func=AF.Sqrt, bias=gne[:, 0:1], scale=1.0 / 64),
                                 reads=[bst, bln], writes=[bst])
                            k.op("dve", lambda e: e.reciprocal(st4[:, 3, :], st4[:, 3, :]), reads=[bst], writes=[bst])
                            k.op("dve", lambda e: e.tensor_tensor(out=yh[:].rearrange("p d (g i) -> p (d g) i", i=64), in0=cen[:],
                                                                  in1=_bc(st4[:, 3, :].unsqueeze(2), [128, 16, 64]), op=ALU.mult),
                                 reads=[bcen, bst], writes=[byh])
                            for d in range(2):
                                for hp in range(4):
                                    k.op("pe", lambda e, d=d, hp=hp: e.transpose(out=pte[:, d * 4 + hp, :], in_=yh[:, d, hp * 128:(hp + 1) * 128],
                                                                                 identity=ident_bf), reads=[byh, b_consts], writes=[bpte])
                            for d in range(2):
                                for hp in range(4):
                                    k.op("act", lambda e, d=d, hp=hp: e.activation(out=Zn[:, d, hp, :], in_=pte[:, d * 4 + hp, :], func=AF.Identity,
                                                                                   bias=lnc[:, 4 + hp:5 + hp], scale=lnc[:, hp:hp + 1]),
                                         reads=[bpte, bln], writes=[bZn])
                            js = slice(j * 128, (j + 1) * 128)
                            k.op("dve", lambda e, js=js: e.tensor_tensor(out=Zn[:], in0=Zn[:], in1=Bt[:, :, :, js], op=ALU.add), reads=[bZn, bB], writes=[bZn])
                            k.op("pool", lambda e, js=js: e.tensor_tensor(out=Zn[:], in0=Zn[:], in1=Gt[:, :, :, js], op=ALU.mult), reads=[bZn, bG], writes=[bZn])
                            k.op("dve", lambda e, js=js: e.tensor_tensor(out=catR[:, :, js], in0=Zn[:, 0], in1=Zn[:, 1], op=ALU.add), reads=[bZn], writes=[bcat])
                        k.dma(ds_eo, cat_d[b, 0:512, pos0 - TCX:pos0 - TCX + N].rearrange("(h p) t -> p h t", p=128), catR[:, :, 0:N], reads=[bcat])
                k.phase_end(es)

        if "F" in phases:
            with ExitStack() as pes:
                k.phase_begin(pes)
                PD = [k.sb("PD%d" % i, [128, 12, 512], F32) for i in range(2)]
                cosb = [k.sb("cosb%d" % i, [128, 512], F32) for i in range(2)]
                sinb = [k.sb("sinb%d" % i, [128, 512], F32) for i in range(2)]
                x2 = k.sb("x2", [128, 512], BF16)
                sdf = k.sb("sdf", [128, 512], F32)
                XQ = k.sb("XQ", [128, 512], F32)
                XQb = k.sb("XQb", [128, 512], BF16)
                t1f = k.sb("t1f", [128, 512], F32)
                t2f = k.sb("t2f", [128, 512], F32)
                qo = [k.sb("qo%d" % i, [128, 512], BF16) for i in range(2)]
                Vb = k.sb("Vb", [128, 4, 512], BF16)
                vtk = [k.sb("vtk%d" % i, [128, 512], BF16) for i in range(2)]
                nwc = k.sb("nwc", [128, 2], F32)
                pssf = k.ps("pssf", [128, 512])
                prot = k.ps("prot", [128, 512])
                pvf = k.ps("pvf", [128, 4, 128])
                bPD, bcs = [k.buf(), k.buf()], [k.buf(), k.buf()]
                bx2, bsd, bXQ, bXQb, bt1, bt2, bVb, bnw, bpss, bprot, bpv = (k.buf() for _ in range(11))
                bqo, bvtk = [k.buf(), k.buf()], [k.buf(), k.buf()]
                ds_f = [k.dsem("sp", "f0"), k.dsem("sp", "f1")]
                ds_fw = k.dsem("sp", "fw")
                ds_fo = [k.dsem("pool", "fo0"), k.dsem("pool", "fo1")]
                ds_fv = [k.dsem("pool", "fv0"), k.dsem("pool", "fv1")]
                k.dma(ds_fw, nwc[:, 0:1], qnw[:, :], writes=[bnw])
                k.dma(ds_fw, nwc[:, 1:2], knw[:, :], writes=[bnw])
                ib = 0
                iq = 0
                iv = 0
                for b in range(NB):
                    for (pos0, N, _c) in blocks:
                        s_ = ib % 2
                        ib += 1
                        P = PD[s_]
                        k.dma(ds_f[s_], P[:, :, 0:N], P_d[b, 2048:3584, pos0:pos0 + N].rearrange("(c p) n -> p c n", p=128), writes=[bPD[s_]])
                        k.dma(ds_f[s_], cosb[s_][:, 0:N], ropec[:, pos0:pos0 + N], writes=[bcs[s_]])
                        k.dma(ds_f[s_], sinb[s_][:, 0:N], ropes[:, pos0:pos0 + N], writes=[bcs[s_]])
                        for c in range(8):
                            if c < 4 and pos0 < TCX:
                                continue
                            X = P[:, c, 0:N]
                            wi = 0 if c < 4 else 1
                            k.op("pool", lambda e, X=X, N=N: e.tensor_tensor(out=x2[:, 0:N], in0=X, in1=X, op=ALU.mult), reads=[bPD[s_]], writes=[bx2])
                            k.op("pe", lambda e, N=N: e.matmul(pssf[:, 0:N], bones_bf, x2[:, 0:N], start=True, stop=True), reads=[bx2, b_consts], writes=[bpss])
                            k.op("act", lambda e, N=N: e.activation(out=sdf[:, 0:N], in_=pssf[:, 0:N], func=AF.Sqrt, bias=eps_t[:, 0:1], scale=1.0 / 64),
                                 reads=[bpss, b_consts], writes=[bsd])
                            k.op("dve", lambda e, N=N: e.reciprocal(sdf[:, 0:N], sdf[:, 0:N]), reads=[bsd], writes=[bsd])
                            k.op("dve", lambda e, X=X, N=N, wi=wi: e.scalar_tensor_tensor(out=XQ[:, 0:N], in0=X, scalar=nwc[:, wi:wi + 1], in1=sdf[:, 0:N],
                                                                                         op0=ALU.mult, op1=ALU.mult),
                                 reads=[bPD[s_], bsd, bnw], writes=[bXQ])
                            k.op("act", lambda e, N=N: e.activation(out=XQb[:, 0:N], in_=XQ[:, 0:N], func=AF.Copy), reads=[bXQ], writes=[bXQb])
                            k.op("pe", lambda e, N=N: e.matmul(prot[:, 0:N], rot_bf, XQb[:, 0:N], start=True, stop=True), reads=[bXQb, b_consts], writes=[bprot])
                            k.op("pool", lambda e, N=N, s_=s_: e.tensor_tensor(out=t1f[:, 0:N], in0=XQ[:, 0:N], in1=cosb[s_][:, 0:N], op=ALU.mult),
                                 reads=[bXQ, bcs[s_]], writes=[bt1])
                            k.op("dve", lambda e, N=N, s_=s_: e.tensor_tensor(out=t2f[:, 0:N], in0=prot[:, 0:N], in1=sinb[s_][:, 0:N], op=ALU.mult),
                                 reads=[bprot, bcs[s_]], writes=[bt2])
                            qs = iq % 2
                            iq += 1
                            k.op("dve", lambda e, N=N, qs=qs: e.tensor_tensor(out=qo[qs][:, 0:N], in0=t1f[:, 0:N], in1=t2f[:, 0:N], op=ALU.add),
                                 reads=[bt1, bt2], writes=[bqo[qs]])
                            dst = qT_d[b, c, :, pos0:pos0 + N] if c < 4 else kT_d[b, c - 4, :, pos0:pos0 + N]
                            k.dma(ds_fo[qs], dst, qo[qs][:, 0:N], reads=[bqo[qs]])
                        k.op("act", lambda e, P=P, N=N: e.activation(out=Vb[:, :, 0:N], in_=P[:, 8:12, 0:N], func=AF.Copy), reads=[bPD[s_]], writes=[bVb])
                        for j in range(N // 128):
                            for h in range(4):
                                k.op("pe", lambda e, h=h, j=j: e.matmul(pvf[:, h, :], Vb[:, h, j * 128:(j + 1) * 128], ident_bf, start=True, stop=True),
                                     reads=[bVb, b_consts], writes=[bpv])
                            vs = iv % 2
                            iv += 1
                            k.op("act", lambda e, vs=vs: e.activation(out=vtk[vs][:], in_=pvf[:].rearrange("p h c -> p (h c)"), func=AF.Copy),
                                 reads=[bpv], writes=[bvtk[vs]])
                            p0 = pos0 + j * 128
                            k.dma(ds_fv[vs], vt_d[b, p0:p0 + 128, :], vtk[vs][:], reads=[bvtk[vs]])
                k.phase_end(es)

            with ExitStack() as pes:
                k.phase_begin(pes)
                LAM_INIT = 0.8 - 0.6 * math.exp(-0.3 * 0)
                KT = [k.sb("KT%d" % i, [128, T], BF16) for i in range(2)]
                VT = [k.sb("VT%d" % i, [128, NT, 128], BF16) for i in range(2)]
                QT = [k.sb("QT%d" % i, [128, 512], BF16) for i in range(2)]
                pT = [[k.sb("pT%d%d" % (m, i), [128, 512], BF16) for i in range(2)] for m in range(2)]
                lamt = k.sb("lamt", [1, 256], F32)
                lamw = k.sb("lamw", [1, 136], F32)
                nlamc = k.sb("nlamc", [128, 1], F32)
                slw = k.sb("slw", [128, 1], F32)
                o0 = k.sb("o0", [128, 512], F32)
                o1 = k.sb("o1", [128, 512], F32)
                rz = k.sb("rz", [128, 512], F32)
                od2 = k.sb("od2", [128, 512], BF16)
                res = [k.sb("res%d" % i, [128, 512], BF16) for i in range(2)]
                sT = [[k.ps("sT%d%d" % (m, i), [128, 512]) for i in range(2)] for m in range(2)]
                Oa = [k.ps("Oa%d" % m, [128, 512]) for m in range(2)]
                Za = [k.ps("Za%d" % m, [128, 512]) for m in range(2)]
                bKT, bVT, bQT = [k.buf(), k.buf()], [k.buf(), k.buf()], [k.buf(), k.buf()]
                bpT = [[k.buf(), k.buf()], [k.buf(), k.buf()]]
                bsT = [[k.buf(), k.buf()], [k.buf(), k.buf()]]
                bO, bZ = [k.buf(), k.buf()], [k.buf(), k.buf()]
                blam, bo0, bo1, brz, bod2 = (k.buf() for _ in range(5))
                bres = [k.buf(), k.buf()]
                ds_kv = [k.dsem("sp", "kv0"), k.dsem("sp", "kv1")]
                ds_q = [k.dsem("sp", "q0"), k.dsem("sp", "q1")]
                ds_l = k.dsem("sp", "lam")
                ds_ro = [k.dsem("pool", "ro0"), k.dsem("pool", "ro1")]
                k.dma(ds_l, lamt[:], lamv[:, :], writes=[blam])
                k.dma(ds_l, slw[:], sublnw[:, :], writes=[blam])
                k.op("dve", lambda e: e.tensor_tensor(out=lamw[:, 0:64], in0=lamt[:, 0:64], in1=lamt[:, 64:128], op=ALU.mult), reads=[blam], writes=[blam])
                k.op("dve", lambda e: e.tensor_tensor(out=lamw[:, 64:128], in0=lamt[:, 128:192], in1=lamt[:, 192:256], op=ALU.mult), reads=[blam], writes=[blam])
                k.op("dve", lambda e: e.tensor_reduce(out=lamw[:, 128:129], in_=lamw[:, 0:64], axis=AX.X, op=ALU.add), reads=[blam], writes=[blam])
                k.op("dve", lambda e: e.tensor_reduce(out=lamw[:, 129:130], in_=lamw[:, 64:128], axis=AX.X, op=ALU.add), reads=[blam], writes=[blam])
                k.op("act", lambda e: e.activation(out=lamw[:, 130:132], in_=lamw[:, 128:130], func=AF.Exp), reads=[blam], writes=[blam])
                k.op("dve", lambda e: e.tensor_tensor(out=lamw[:, 132:133], in0=lamw[:, 131:132], in1=lamw[:, 130:131], op=ALU.subtract), reads=[blam], writes=[blam])
                k.op("dve", lambda e: e.tensor_scalar(out=lamw[:, 133:134], in0=lamw[:, 132:133], scalar1=-LAM_INIT, scalar2=None, op0=ALU.add),
                     reads=[blam], writes=[blam])
                k.op("pe", lambda e: e.matmul(Za[0][:, 0:1], consts[0:1, C_ONES:C_ONES + 128], lamw[0:1, 133:134], start=True, stop=True),
                     reads=[blam, b_consts], writes=[bZ[0]])
                k.op("dve", lambda e: e.tensor_copy(nlamc[:], Za[0][:, 0:1]), reads=[bZ[0]], writes=[blam])
                k.op("dve", lambda e: e.tensor_scalar(out=slw[:], in0=slw[:], scalar1=1.0 - LAM_INIT, scalar2=None, op0=ALU.mult), reads=[blam], writes=[blam])
                ih = 0
                iqb = 0
                ipt = [0, 0]
                for b in range(NB):
                    for h in range(4):
                        hs = ih % 2
                        ih += 1
                        k.dma(ds_kv[hs], KT[hs][:], kT_d[b, h, :, :], writes=[bKT[hs]])
                        k.dma(ds_kv[hs], VT[hs][:], vt_d[b, :, h * 128:(h + 1) * 128].rearrange("(n p) c -> p n c", p=128), writes=[bVT[hs]])
                        for (pos0, N) in xblocks:
                            qs = iqb % 2
                            iqb += 1
                            k.dma(ds_q[qs], QT[qs][:, 0:N], qT_d[b, h, :, pos0:pos0 + N], writes=[bQT[qs]])
                            items = [(kt, m) for kt in range(NT) for m in range(2)]
                            slots = []
                            for (kt, m) in items:
                                slots.append(ipt[m] % 2)
                                ipt[m] += 1

                            def score(ix):
                                kt, m = items[ix]
                                i_ = slots[ix]
                                ms = slice(64 * m, 64 * m + 64)
                                k.op("pe", lambda e: e.matmul(sT[m][i_][:, 0:N], KT[hs][ms, kt * 128:(kt + 1) * 128], QT[qs][ms, 0:N],
                                                              start=True, stop=True),
                                     reads=[bKT[hs], bQT[qs]], writes=[bsT[m][i_]])

                            LOOK = 2
                            for ix in range(min(LOOK, len(items))):
                                score(ix)
                            for ix, (kt, m) in enumerate(items):
                                i_ = slots[ix]
                                k.op("act", lambda e, m=m, i_=i_: e.activation(out=pT[m][i_][:, 0:N], in_=sT[m][i_][:, 0:N], func=AF.Exp, scale=0.125),
                                     reads=[bsT[m][i_]], writes=[bpT[m][i_]])
                                if ix + LOOK < len(items):
                                    score(ix + LOOK)
                                k.op("pe", lambda e, m=m, i_=i_, kt=kt: e.matmul(
                                    Oa[m][:, 0:N], VT[hs][:, kt, :], pT[m][i_][:, 0:N], start=(kt == 0), stop=(kt == NT - 1)),
                                    reads=[bVT[hs], bpT[m][i_]], writes=[bO[m]])
                                k.op("pe", lambda e, m=m, i_=i_, kt=kt: e.matmul(
                                    Za[m][:, 0:N], ones_bf, pT[m][i_][:, 0:N], start=(kt == 0), stop=(kt == NT - 1)),
                                    reads=[bpT[m][i_], b_consts], writes=[bZ[m]])
                            k.op("dve", lambda e, N=N: e.reciprocal(rz[:, 0:N], Za[0][:, 0:N]), reads=[bZ[0]], writes=[brz])
                            k.op("dve", lambda e, N=N: e.tensor_tensor(out=o0[:, 0:N], in0=Oa[0][:, 0:N], in1=rz[:, 0:N], op=ALU.mult), reads=[bO[0], brz], writes=[bo0])
                            k.op("dve", lambda e, N=N: e.reciprocal(rz[:, 0:N], Za[1][:, 0:N]), reads=[bZ[1]], writes=[brz])
                            k.op("dve", lambda e, N=N: e.tensor_tensor(out=o1[:, 0:N], in0=Oa[1][:, 0:N], in1=rz[:, 0:N], op=ALU.mult), reads=[bO[1], brz], writes=[bo1])
                            k.op("dve", lambda e, N=N: e.scalar_tensor_tensor(out=o0[:, 0:N], in0=o1[:, 0:N], scalar=nlamc[:, 0:1], in1=o0[:, 0:N],
                                                                               op0=ALU.mult, op1=ALU.add), reads=[bo1, bo0, blam], writes=[bo0])
                            k.op("pool", lambda e, N=N: e.tensor_tensor(out=od2[:, 0:N], in0=o0[:, 0:N], in1=o0[:, 0:N], op=ALU.mult), reads=[bo0], writes=[bod2])
                            k.op("pe", lambda e, N=N: e.matmul(sT[0][0][:, 0:N], ones_bf, od2[:, 0:N], start=True, stop=True),
                                 reads=[bod2, b_consts], writes=[bsT[0][0]])
                            k.op("act", lambda e, N=N: e.activation(out=rz[:, 0:N], in_=sT[0][0][:, 0:N], func=AF.Sqrt, bias=eps_t[:, 0:1], scale=1.0 / 128),
                                 reads=[bsT[0][0], b_consts], writes=[brz])
                            k.op("dve", lambda e, N=N: e.reciprocal(rz[:, 0:N], rz[:, 0:N]), reads=[brz], writes=[brz])
                            rs = iqb % 2
                            k.op("dve", lambda e, N=N, rs=rs: e.scalar_tensor_tensor(out=res[rs][:, 0:N], in0=o0[:, 0:N], scalar=slw[:, 0:1], in1=rz[:, 0:N],
                                                                                     op0=ALU.mult, op1=ALU.mult), reads=[bo0, brz, blam], writes=[bres[rs]])
                            k.dma(ds_ro[rs], cat_d[b, 512 + h * 128:512 + (h + 1) * 128, pos0 - TCX:pos0 - TCX + N], res[rs][:, 0:N], reads=[bres[rs]])
                k.phase_end(es)

        if "G" in phases:
            with ExitStack() as pes:
                k.phase_begin(pes)
                wo_st = k.sb("wo_st", [128, 4, D], F32)
                woutb = k.sb("woutb", [128, 8, D], BF16)
                wrt = k.sb("wrt", [128, 8, 36], F32)
                brt = k.sb("brt", [128, 36], F32)
                xt2 = [k.sb("xt2%d" % i, [128, D], F32) for i in range(2)]
                catT = [k.sb("catT%d" % i, [128, 8, 128], BF16) for i in range(2)]
                tmpo = k.sb("tmpo", [128, D], F32)
                x1 = [k.sb("x1%d" % i, [128, D], F32) for i in range(2)]
                sq2 = k.sb("sq2", [128, D], F32)
                ss2 = k.sb("ss2", [128, 4], F32)
                xn2 = k.sb("xn2", [128, D], F32)
                h2f = k.sb("h2f", [128, 8, 128], F32)
                h2b = [k.sb("h2b%d" % i, [128, 8, 128], BF16) for i in range(2)]
                Lg = k.sb("Lg", [128, 36], F32)
                rt = k.sb("rt", [128, 16], F32)
                goh = k.sb("goh", [128, 4], F32)
                em = k.sb("em", [128, 4, 8], F32)
                em2 = k.sb("em2", [128, 32], F32)
                m1 = k.sb("m1", [128, 32], F32)
                m2 = k.sb("m2", [128, 32], F32)
                Wdt = [k.sb("Wdt%d" % i, [128, 32], F32) for i in range(2)]
                po = [k.ps("po%d" % i, [128, 512]) for i in range(2)]
                ptf = k.ps("ptf", [128, 8, 128])
                pl = k.ps("pl", [128, 36])
                bw, bpo, bptf, bpl, btmp, bsq2, bss2, bxn2, bh2f, bL, brt_ = (k.buf() for _ in range(11))
                bxt, bcatT, bx1, bh2b, bWd = ([k.buf(), k.buf()] for _ in range(5))
                bpo = [k.buf(), k.buf()]
                ds_w = k.dsem("sp", "gw")
                ds_i = [k.dsem("sp", "gi0"), k.dsem("sp", "gi1")]
                ds_o1 = [k.dsem("pool", "go0"), k.dsem("pool", "go1")]
                wov = w_out.rearrange("(kc p) n -> p kc n", p=128)
                for hf in range(2):
                    k.dma(ds_w, wo_st[:], wov[:, hf * 4:(hf + 1) * 4, :], writes=[bw])
                    k.op("dve", lambda e, hf=hf: e.tensor_copy(woutb[:, hf * 4:(hf + 1) * 4, :], wo_st[:]), reads=[bw], writes=[bw])
                k.dma(ds_w, wrt[:], w_rt.rearrange("(kc p) n -> p kc n", p=128), writes=[bw])
                k.dma(ds_w, brt[:], b_rt.partition_broadcast(128), writes=[bw])
                it_ = 0
                for b in range(NB):
                    for xp in range(0, TX, 128):
                        s_ = it_ % 2
                        it_ += 1
                        k.dma(ds_i[s_], xt2[s_][:], seq[b, TCX + xp:TCX + xp + 128, :], writes=[bxt[s_]])
                        k.dma(ds_i[s_], catT[s_][:], cat_d[b, :, xp:xp + 128].rearrange("(c p) t -> p c t", p=128), writes=[bcatT[s_]])
                        for hf in range(2):
                            for kc in range(8):
                                k.op("pe", lambda e, hf=hf, kc=kc, s_=s_: e.matmul(po[hf][:], catT[s_][:, kc, :], woutb[:, kc, hf * 512:(hf + 1) * 512],
                                                                                   start=(kc == 0), stop=(kc == 7)),
                                     reads=[bcatT[s_], bw], writes=[bpo[hf]])
                            hsl = slice(hf * 512, (hf + 1) * 512)
                            k.op("dve", lambda e, hf=hf, hsl=hsl, b=b: e.tensor_tensor(out=tmpo[:, hsl], in0=po[hf][:], in1=G1[:, b, hsl], op=ALU.mult),
                                 reads=[bpo[hf], b_mod_], writes=[btmp])
                            k.op("pool", lambda e, hsl=hsl, s_=s_: e.tensor_tensor(out=x1[s_][:, hsl], in0=tmpo[:, hsl], in1=xt2[s_][:, hsl], op=ALU.add),
                                 reads=[btmp, bxt[s_]], writes=[bx1[s_]])
                        k.dma(ds_o1[s_], x1_d[b, xp:xp + 128, :], x1[s_][:], reads=[bx1[s_]])
                        k.op("act", lambda e, s_=s_: e.activation(out=sq2[:], in_=x1[s_][:], func=AF.Square), reads=[bx1[s_]], writes=[bsq2])
                        k.op("dve", lambda e: e.tensor_reduce(out=ss2[:, 0:1], in_=sq2[:], axis=AX.X, op=ALU.add), reads=[bsq2], writes=[bss2])
                        k.op("act", lambda e: e.activation(out=ss2[:, 1:2], in_=ss2[:, 0:1], func=AF.Sqrt, bias=eps_t[:, 0:1], scale=1.0 / D),
                             reads=[bss2, b_consts], writes=[bss2])
                        k.op("dve", lambda e: e.reciprocal(ss2[:, 2:3], ss2[:, 1:2]), reads=[bss2], writes=[bss2])
                        k.op("dve", lambda e, s_=s_: e.tensor_scalar(out=xn2[:], in0=x1[s_][:], scalar1=ss2[:, 2:3], scalar2=None, op0=ALU.mult),
                             reads=[bx1[s_], bss2], writes=[bxn2])
                        for kc in range(8):
                            k.op("pe", lambda e, kc=kc: e.transpose(out=ptf[:, kc, :], in_=xn2[:, kc * 128:(kc + 1) * 128], identity=consts[:, C_ID:C_ID + 128]),
                                 reads=[bxn2, b_consts], writes=[bptf])
                        for kc in range(8):
                            k.op("act", lambda e, kc=kc, b=b: e.activation(out=h2f[:, kc, :], in_=ptf[:, kc, :], func=AF.Identity,
                                                                           bias=B2[:, kc, b:b + 1], scale=A2[:, kc, b:b + 1]),
                                 reads=[bptf, b_mod_], writes=[bh2f])
                        k.op("pool", lambda e, s_=s_: e.tensor_copy(h2b[s_][:], h2f[:]), reads=[bh2f], writes=[bh2b[s_]])
                        k.dma(ds_o1[s_], h2T_d[b, :, xp:xp + 128].rearrange("(c p) t -> p c t", p=128), h2b[s_][:], reads=[bh2b[s_]])
                        for kc in range(8):
                            k.op("pe", lambda e, kc=kc: e.matmul(pl[:], h2f[:, kc, :], wrt[:, kc, :], start=(kc == 0), stop=(kc == 7)),
                                 reads=[bh2f, bw], writes=[bpl])
                        k.op("dve", lambda e: e.tensor_tensor(out=Lg[:], in0=pl[:], in1=brt[:], op=ALU.add), reads=[bpl, bw], writes=[bL])
                        R_ = [bL, brt_]
                        k.op("dve", lambda e: e.tensor_reduce(out=rt[:, 0:1], in_=Lg[:, 0:4], axis=AX.X, op=ALU.max), reads=R_, writes=[brt_])
                        k.op("dve", lambda e: e.tensor_scalar(out=goh[:], in0=Lg[:, 0:4], scalar1=rt[:, 0:1], scalar2=None, op0=ALU.subtract), reads=R_, writes=[brt_])
                        k.op("act", lambda e: e.activation(out=em2[:, 0:4], in_=goh[:], func=AF.Exp), reads=R_, writes=[brt_])
                        k.op("dve", lambda e: e.tensor_reduce(out=rt[:, 1:2], in_=em2[:, 0:4], axis=AX.X, op=ALU.add), reads=R_, writes=[brt_])
                        k.op("dve", lambda e: e.reciprocal(rt[:, 2:3], rt[:, 1:2]), reads=R_, writes=[brt_])
                        k.op("dve", lambda e: e.tensor_scalar(out=goh[:], in0=Lg[:, 0:4], scalar1=rt[:, 0:1], scalar2=None, op0=ALU.is_equal), reads=R_, writes=[brt_])
                        k.op("dve", lambda e: e.tensor_scalar(out=goh[:], in0=goh[:], scalar1=-1.0, scalar2=1e30, op0=ALU.add, op1=ALU.mult), reads=R_, writes=[brt_])
                        k.op("dve", lambda e: e.tensor_tensor(out=em[:], in0=Lg[:, 4:36].rearrange("p (g x) -> p g x", x=8),
                                                              in1=_bc(goh[:].unsqueeze(2), [128, 4, 8]), op=ALU.add), reads=R_, writes=[brt_])
                        emf = em[:].rearrange("p g x -> p (g x)")
                        k.op("dve", lambda e: e.tensor_reduce(out=rt[:, 3:4], in_=emf, axis=AX.X, op=ALU.max), reads=R_, writes=[brt_])
                        k.op("dve", lambda e: e.tensor_scalar(out=m1[:], in0=emf, scalar1=rt[:, 3:4], scalar2=None, op0=ALU.is_equal), reads=R_, writes=[brt_])
                        k.op("dve", lambda e: e.scalar_tensor_tensor(out=em2[:], in0=m1[:], scalar=-1e30, in1=emf, op0=ALU.mult, op1=ALU.add), reads=R_, writes=[brt_])
                        k.op("dve", lambda e: e.tensor_reduce(out=rt[:, 4:5], in_=em2[:], axis=AX.X, op=ALU.max), reads=R_, writes=[brt_])
                        k.op("dve", lambda e: e.tensor_scalar(out=m2[:], in0=em2[:], scalar1=rt[:, 4:5], scalar2=None, op0=ALU.is_equal), reads=R_, writes=[brt_])
                        k.op("dve", lambda e: e.tensor_tensor(out=rt[:, 5:6], in0=rt[:, 4:5], in1=rt[:, 3:4], op=ALU.subtract), reads=R_, writes=[brt_])
                        k.op("act", lambda e: e.activation(out=rt[:, 6:7], in_=rt[:, 5:6], func=AF.Exp), reads=R_, writes=[brt_])
                        k.op("dve", lambda e: e.tensor_scalar(out=rt[:, 7:8], in0=rt[:, 6:7], scalar1=1.0, scalar2=None, op0=ALU.add), reads=R_, writes=[brt_])
                        k.op("dve", lambda e: e.reciprocal(rt[:, 8:9], rt[:, 7:8]), reads=R_, writes=[brt_])
                        k.op("dve", lambda e: e.tensor_tensor(out=rt[:, 9:10], in0=rt[:, 8:9], in1=rt[:, 2:3], op=ALU.mult), reads=R_, writes=[brt_])
                        k.op("dve", lambda e: e.tensor_tensor(out=rt[:, 10:11], in0=rt[:, 9:10], in1=rt[:, 6:7], op=ALU.mult), reads=R_, writes=[brt_])
                        k.op("dve", lambda e: e.tensor_scalar(out=m1[:], in0=m1[:], scalar1=rt[:, 9:10], scalar2=None, op0=ALU.mult), reads=R_, writes=[brt_])
                        k.op("dve", lambda e, s_=s_: e.scalar_tensor_tensor(out=Wdt[s_][:], in0=m2[:], scalar=rt[:, 10:11], in1=m1[:], op0=ALU.mult, op1=ALU.add),
                             reads=R_, writes=[bWd[s_]])
                        k.dma(ds_o1[s_], wd_d[b, xp:xp + 128, :], Wdt[s_][:], reads=[bWd[s_]])
                k.phase_end(es)

            with ExitStack() as pes:
                k.phase_begin(pes)
                TB = min(1024, TX)
                TBC = TB // 128
                NTB = TB // 512
                h2T = k.sb("h2T", [128, 8, TB], BF16)
                acc = k.sb("acc", [128, TBC, D], F32)
                Wd = k.sb("Wd", [128, TBC, NE], F32)
                x1h = k.sb("x1h", [128, 4, D], F32)
                stg = [k.sb("stg%d" % i, [128, 2048], F32) for i in range(3)]
                wgb = [k.sb("wgb%d" % i, [128, 8, FF], BF16) for i in range(2)]
                wub = [k.sb("wub%d" % i, [128, 8, FF], BF16) for i in range(2)]
                wdb = [k.sb("wdb%d" % i, [128, 4, D], BF16) for i in range(2)]
                sg = [k.sb("sg%d" % i, [128, 512], F32) for i in range(2)]
                hid = [k.sb("hid%d" % i, [128, 4, 512], BF16) for i in range(2)]
                pg = [k.ps("pg%d" % i, [128, 512]) for i in range(2)]
                pu = [k.ps("pu%d" % i, [128, 512]) for i in range(2)]
                py = [k.ps("py%d" % i, [128, 512]) for i in range(2)]
                bh2T, bacc, bWd_, bx1h = (k.buf() for _ in range(4))
                bstg = [k.buf() for _ in range(3)]
                bwg, bwu, bwd_, bsg, bhid, bpg, bpu, bpy = ([k.buf(), k.buf()] for _ in range(8))
                ds_m = k.dsem("sp", "mi")
                ds_s = [k.dsem("sp", "ms%d" % i) for i in range(3)]
                ds_x1 = k.dsem("sp", "mx")
                ds_out = k.dsem("pool", "mo")

                def dsl2(start, size):
                    if isinstance(start, int):
                        return slice(start, start + size)
                    return bass.ds(start, size)

                def moe_body(b):
                    def body(it):
                        off = it * TB
                        k.dma(ds_m, h2T[:], h2T_d[b, :, dsl2(off, TB)].rearrange("(c p) t -> p c t", p=128), writes=[bh2T])
                        k.dma(ds_m, Wd[:], wd_d[b, dsl2(off, TB), :].rearrange("(n p) e -> p n e", p=128), writes=[bWd_])
                        k.op("pool", lambda e: e.memset(acc[:], 0.0), writes=[bacc])
                        ist = 0
                        cnt = [0, 0, 0]
                        for ex in range(NE):
                            ws = ex % 2
                            srcs = []
                            gv = moe_g[ex].rearrange("(kc p) f -> p kc f", p=128)
                            uv = moe_u[ex].rearrange("(kc p) f -> p kc f", p=128)
                            dv = moe_d[ex].rearrange("(fc p) n -> p fc n", p=128)
                            for hf in range(2):
                                srcs.append((gv[:, hf * 4:(hf + 1) * 4, :], wgb[ws][:, hf * 4:(hf + 1) * 4, :], bwg[ws], "p (a f) -> p a f", 4))
                                srcs.append((uv[:, hf * 4:(hf + 1) * 4, :], wub[ws][:, hf * 4:(hf + 1) * 4, :], bwu[ws], "p (a f) -> p a f", 4))
                            for hf in range(2):
                                srcs.append((dv[:, hf * 2:(hf + 1) * 2, :], wdb[ws][:, hf * 2:(hf + 1) * 2, :], bwd_[ws], "p (a f) -> p a f", 2))
                            for (src, dst, bdst, pat, a_) in srcs:
                                si = ist % 3
                                ist += 1
                                k.dma(ds_s[si], stg[si][:].rearrange(pat, a=a_), src, writes=[bstg[si]])
                                k.op("pool", lambda e, si=si, dst=dst, pat=pat, a_=a_: e.tensor_copy(dst, stg[si][:].rearrange(pat, a=a_)),
                                     reads=[bstg[si]], writes=[bdst])
                            for tb in range(NTB):
                                tsl = slice(tb * 512, (tb + 1) * 512)
                                hs_ = cnt[0] % 2
                                cnt[0] += 1
                                for fc in range(4):
                                    pi = cnt[1] % 2
                                    cnt[1] += 1
                                    fsl = slice(fc * 128, (fc + 1) * 128)
                                    for kc in range(8):
                                        k.op("pe", lambda e, pi=pi, ws=ws, kc=kc, fsl=fsl, tsl=tsl: e.matmul(pg[pi][:], wgb[ws][:, kc, fsl], h2T[:, kc, tsl],
                                                                                                             start=(kc == 0), stop=(kc == 7)),
                                             reads=[bwg[ws], bh2T], writes=[bpg[pi]])
                                    for kc in range(8):
                                        k.op("pe", lambda e, pi=pi, ws=ws, kc=kc, fsl=fsl, tsl=tsl: e.matmul(pu[pi][:], wub[ws][:, kc, fsl], h2T[:, kc, tsl],
                                                                                                             start=(kc == 0), stop=(kc == 7)),
                                             reads=[bwu[ws], bh2T], writes=[bpu[pi]])
                                    k.op("act", lambda e, pi=pi: e.activation(out=sg[pi][:], in_=pg[pi][:], func=AF.Silu), reads=[bpg[pi]], writes=[bsg[pi]])
                                    k.op("dve", lambda e, pi=pi, hs_=hs_, fc=fc: e.tensor_tensor(out=hid[hs_][:, fc, :], in0=sg[pi][:], in1=pu[pi][:], op=ALU.mult),
                                         reads=[bsg[pi], bpu[pi]], writes=[bhid[hs_]])
                                for tc in range(4):
                                    ch = tb * 4 + tc
                                    for hf in range(2):
                                        yi = cnt[2] % 2
                                        cnt[2] += 1
                                        for fc in range(4):
                                            k.op("pe", lambda e, yi=yi, hs_=hs_, fc=fc, tc=tc, ws=ws, hf=hf: e.matmul(
                                                py[yi][:], hid[hs_][:, fc, tc * 128:(tc + 1) * 128], wdb[ws][:, fc, hf * 512:(hf + 1) * 512],
                                                start=(fc == 0), stop=(fc == 3)), reads=[bhid[hs_], bwd_[ws]], writes=[bpy[yi]])
                                        k.op("dve", lambda e, yi=yi, ch=ch, hf=hf, ex=ex: e.scalar_tensor_tensor(
                                            out=acc[:, ch, hf * 512:(hf + 1) * 512], in0=py[yi][:], scalar=Wd[:, ch, ex:ex + 1],
                                            in1=acc[:, ch, hf * 512:(hf + 1) * 512], op0=ALU.mult, op1=ALU.add),
                                            reads=[bpy[yi], bWd_, bacc], writes=[bacc])
                        for hq in range(TBC // 4):
                            k.dma(ds_x1, x1h[:], x1_d[b, dsl2(off + hq * 512, 512), :].rearrange("(n p) d -> p n d", p=128), writes=[bx1h])
                            asl = acc[:, hq * 4:(hq + 1) * 4, :]
                            k.op("dve", lambda e, asl=asl: e.tensor_tensor(out=asl, in0=asl, in1=_bc(G2[:, b, :].unsqueeze(1), [128, 4, D]), op=ALU.mult),
                                 reads=[bacc, b_mod_], writes=[bacc])
                            k.op("pool", lambda e, asl=asl: e.tensor_tensor(out=asl, in0=asl, in1=x1h[:], op=ALU.add), reads=[bacc, bx1h], writes=[bacc])
                            k.dma(ds_out, out_d[b, dsl2(off + hq * 512, 512), :].rearrange("(n p) d -> p n d", p=128), asl, reads=[bacc])
                    return body

                for b in range(NB):
                    k.loop(TX // TB, moe_body(b), static=True)
                k.phase_end(es)
        k.barrier()
    return nc, dram


def core_inputs(inp, b0, TX, TCX, shared=None):
    f = lambda a: np.ascontiguousarray(np.asarray(a, np.float32))
    if shared is None:
        shared = {}
        cs, sn = rope_tables(TX, TCX)
        shared["ropec"], shared["ropes"] = cs, sn
        shared["consts"] = make_consts()
        shared["w_mod"] = f(inp["w_mod"][0])
        shared["b_mod"] = f(inp["b_mod"][0]).reshape(1, -1)
        shared["n1w"] = colform(inp["norm1_w"][0], 8)
        shared["n2w"] = colform(inp["norm2_w"][0], 8)
        shared["w_in"] = f(inp["w_in"][0])
        shared["shift_w"] = f(inp["shift_w"][0])
        shared["rw_w0"] = f(np.asarray(inp["rwkv_w0"][0]).reshape(2, 4, 128).transpose(2, 0, 1))
        shared["rw_a0"] = f(np.asarray(inp["rwkv_a0"][0]).reshape(2, 4, 128).transpose(2, 0, 1))
        shared["rw_wup"] = f(inp["rwkv_w_up"][0])
        shared["rw_aup"] = f(inp["rwkv_a_up"][0])
        shared["rw_gup"] = f(inp["rwkv_g_up"][0])
        shared["rw_kk"] = colform(inp["rwkv_k_k"][0], 4)
        shared["rw_ka"] = colform(inp["rwkv_k_a"][0], 4)
        shared["rw_rk"] = colform(np.asarray(inp["rwkv_r_k"][0]).reshape(-1), 4)
        shared["rw_lnw"] = colform(inp["rwkv_ln_w"][0], 4)
        shared["rw_lnb"] = colform(inp["rwkv_ln_b"][0], 4)
        shared["qnw"] = f(np.tile(np.asarray(inp["q_norm_w"][0]), 2).reshape(128, 1))
        shared["knw"] = f(np.tile(np.asarray(inp["k_norm_w"][0]), 2).reshape(128, 1))
        shared["lamv"] = f(np.concatenate([np.asarray(inp[n][0]) for n in ("lam_q1", "lam_k1", "lam_q2", "lam_k2")]).reshape(1, 256))
        shared["sublnw"] = f(np.asarray(inp["subln_w"][0]).reshape(128, 1))
        shared["w_out"] = f(inp["w_out"][0])
        shared["w_rt"] = f(np.concatenate([np.asarray(inp["w_group"][0]), np.asarray(inp["w_expert"][0])], axis=1))
        shared["b_rt"] = f(np.concatenate([np.asarray(inp["b_group"][0]), np.asarray(inp["b_expert"][0])]).reshape(1, 36))
        shared["moe_g"] = f(inp["moe_w_gate"][0])
        shared["moe_u"] = f(inp["moe_w_up"][0])
        shared["moe_d"] = f(inp["moe_w_down"][0])
    m = dict(shared)
    x = np.asarray(inp["x"][b0:b0 + NB], np.float32)
    ctx = np.asarray(inp["ctx"][b0:b0 + NB], np.float32)
    m["seq"] = np.ascontiguousarray(np.concatenate([ctx, x], axis=1))
    cc = np.concatenate([np.asarray(inp["c"][b0:b0 + NB], np.float32), np.asarray(inp["c_ctx"], np.float32)[None]], axis=0)
    m["csT"] = np.ascontiguousarray(cc.reshape(3, 8, 128).transpose(2, 1, 0))
    return m, shared


TX_FULL, TCX_FULL = 4096, 256
_CACHE = {}


def kernel(**inputs):
    inp = {k_: np.asarray(v) for k_, v in inputs.items()}
    B = inp["x"].shape[0]
    ncores = B // NB
    if "nc" not in _CACHE:
        _CACHE["nc"] = build_program(TX_FULL, TCX_FULL)
    nc, dram = _CACHE["nc"]
    in_maps = []
    shared = None
    for c in range(ncores):
        m, shared = core_inputs(inp, c * NB, TX_FULL, TCX_FULL, shared)
        in_maps.append({k_: v for k_, v in m.items() if k_ in dram})
    res = run_bass_kernel_spmd(nc, in_maps, core_ids=list(range(ncores)))
    out = np.concatenate([np.asarray(r["out"]) for r in res.results], axis=0)
    return out.astype(np.float32, copy=False)
```

```python
import copy
import math
from contextlib import ExitStack

import numpy as np
import concourse.bass as bass
import concourse.mybir as mybir
from concourse.bass_utils import run_bass_kernel_spmd

F32 = mybir.dt.float32
BF16 = mybir.dt.bfloat16
AF = mybir.ActivationFunctionType
ALU = mybir.AluOpType
AX = mybir.AxisListType

D = 1024
NB = 2
RW = 512
INW = 3584
NE = 32
FF = 512
SUB = 4


class Buf:
    __slots__ = ("name", "w", "r")

    def __init__(self, name):
        self.name = name
        self.w = None
        self.r = []


class DSem:
    def __init__(self, h, q):
        self.h = h
        self.q = q
        self.total = 0


class K:
    def __init__(self, nc, es):
        self.nc = nc
        self.es = es
        self.E = {"pe": nc.tensor, "act": nc.scalar, "dve": nc.vector, "pool": nc.gpsimd, "sp": nc.sync}
        self.sem = {e: es.enter_context(nc.semaphore("c_" + e)) for e in ("pe", "act", "dve", "pool")}
        self.cnt = {e: 0 for e in self.sem}
        self.seen = {e: {} for e in self.E}
        self.dsems = []
        self.bufs = []
        self.dry = False
        self.inloop = False
        self.used = set()
        self.nbuf = 0
        self.phase_no = 0

    def buf(self, name=None):
        self.nbuf += 1
        b = Buf(name or ("b%d" % self.nbuf))
        self.bufs.append(b)
        return b

    def dsem(self, q, name):
        d = DSem(self.es.enter_context(self.nc.semaphore("d_%s_%d" % (name, self.phase_no))), q)
        self.dsems.append(d)
        return d

    def sb(self, name, shape, dt):
        return self.es.enter_context(self.nc.sbuf_tensor("%s_%d" % (name, self.phase_no), shape, dt))

    def ps(self, name, shape, dt=F32):
        return self.es.enter_context(self.nc.psum_tensor("%s_%d" % (name, self.phase_no), shape, dt))

    def _wait(self, e, tok):
        kind, src, n = tok
        key = src if kind == "E" else id(src)
        if kind == "E" and src == e and e == "pe":
            return
        if kind == "D":
            n = src.total
        if self.seen[e].get(key, -1) >= n:
            return
        self.seen[e][key] = n
        if self.dry:
            self.used.add((e, key))
            return
        h = self.sem[src] if kind == "E" else src.h
        if self.inloop:
            R = self.regs[(e, key)]
            delta = n - self.cur[(e, key)]
            if delta != 0:
                self.E[e].reg_add(R, R, delta)
            self.cur[(e, key)] = n
            self.E[e].wait_ge(h, R)
        else:
            self.E[e].wait_ge(h, n)

    def _sync(self, e, reads, writes):
        best = {}
        def add(tok):
            kind, src, n = tok
            key = (kind, src if kind == "E" else id(src))
            if key not in best or best[key][2] < n:
                best[key] = tok
        for b in reads:
            if b.w is not None:
                add(b.w)
        for b in writes:
            if b.w is not None:
                add(b.w)
            for t in b.r:
                add(t)
        for key in sorted(best, key=str):
            self._wait(e, best[key])

    def op(self, e, fn, reads=(), writes=()):
        self._sync(e, reads, writes)
        self.cnt[e] += 1
        tok = ("E", e, self.cnt[e])
        if not self.dry:
            fn(self.E[e]).then_inc(self.sem[e], 1)
        self.seen[e][e] = max(self.seen[e].get(e, -1), 0)
        for b in reads:
            b.r = [t for t in b.r if not (t[0] == "E" and t[1] == e)] + [tok]
        for b in writes:
            b.w = tok
            b.r = []
        return tok

    def dma(self, ds, out, in_, reads=(), writes=()):
        q = ds.q
        self._sync(q, reads, writes)
        ds.total += 16
        tok = ("D", ds, ds.total)
        if not self.dry:
            self.E[q].dma_start(out=out, in_=in_).then_inc(ds.h, 16)
        for b in reads:
            b.r.append(tok)
        for b in writes:
            b.w = tok
            b.r = []
        return tok

    def drain(self):
        for d in self.dsems:
            if d.total > 0:
                self._wait(d.q, ("D", d, d.total))

    def barrier(self):
        self.drain()
        if not self.dry:
            self.nc.all_engine_barrier()
        for b in self.bufs:
            b.w = None
            b.r = []

    def _keycount(self, key):
        if isinstance(key, str):
            return self.cnt[key]
        for d in self.dsems:
            if id(d) == key:
                return d.total
        raise KeyError(key)

    def _snap_bufs(self, shift):
        def sh(tok):
            kind, src, n = tok
            return (kind, src, n - shift[src if kind == "E" else id(src)])
        return [(None if b.w is None else sh(b.w), [sh(t) for t in b.r]) for b in self.bufs]

    def _load_bufs(self, states):
        for b, (w, r) in zip(self.bufs, states):
            b.w = w
            b.r = list(r)

    def loop(self, n_iter, body, static=False):
        self.barrier()
        for e in self.seen:
            self.seen[e] = {}
        if n_iter == 1 or static:
            for i in range(n_iter):
                body(i)
                self.barrier()
            return
        c0 = dict(self.cnt)
        d0 = [d.total for d in self.dsems]
        nb0 = len(self.bufs)

        def rewind():
            self.cnt = dict(c0)
            for d, t in zip(self.dsems, d0):
                d.total = t
            for e in self.seen:
                self.seen[e] = {}

        self.dry = True
        self.used = set()
        body(0)
        P = {e: self.cnt[e] - c0[e] for e in self.cnt}
        for d, t in zip(self.dsems, d0):
            P[id(d)] = d.total - t
        carried = self._snap_bufs(P)
        rewind()
        self._load_bufs(carried)
        self.used = set()
        body(0)
        self.drain()
        used = sorted(self.used, key=str)
        rewind()
        self._load_bufs(carried)
        self.dry = False
        self.regs = {}
        self.cur = {}
        base = {}
        for (e, key) in used:
            self.nbuf += 1
            R = self.E[e].alloc_register("w_%s_%d" % (e, self.nbuf))
            base[(e, key)] = self._keycount(key)
            self.E[e].reg_mov(R, base[(e, key)])
            self.regs[(e, key)] = R
            self.cur[(e, key)] = base[(e, key)]
        with self.nc.Fori(0, n_iter, hint_back_edge=True) as it:
            self.inloop = True
            body(it)
            for (e, key) in used:
                delta = base[(e, key)] + P[key] - self.cur[(e, key)]
                if delta != 0:
                    self.E[e].reg_add(self.regs[(e, key)], self.regs[(e, key)], delta)
            self.inloop = False
        for (e, key) in used:
            self.E[e].free_register(self.regs[(e, key)])
        for e in self.cnt:
            self.cnt[e] += (n_iter - 1) * P[e]
        for d in self.dsems:
            d.total += (n_iter - 1) * P[id(d)]
        shift = {key: -(n_iter - 1) * P[key] for key in P}
        self._load_bufs(self._snap_bufs(shift))
        for e in self.seen:
            self.seen[e] = {}
        self.barrier()

    def phase_begin(self, pes):
        self.es = pes
        self.phase_no += 1
        self._mark = (len(self.dsems), len(self.bufs))

    def phase_end(self, es):
        self.barrier()
        self.es = es
        del self.dsems[self._mark[0]:]
        del self.bufs[self._mark[1]:]


def _bc(ap, shape):
    return ap.to_broadcast(shape)


C_ID = 0
C_IDA = 128
C_IDB = 256
C_BONES = 384
C_ONES = 512
C_ROT = 640
C_SEL = 768
C_ID3 = 1152
NCONST = 1160


def make_consts():
    c = np.zeros((128, NCONST), np.float32)
    c[:, C_ID:C_ID + 128] = np.eye(128)
    c[:64, C_IDA:C_IDA + 64] = np.eye(64)
    c[64:, C_IDB + 64:C_IDB + 128] = np.eye(64)
    c[:64, C_BONES:C_BONES + 64] = 1.0
    c[64:, C_BONES + 64:C_BONES + 128] = 1.0
    c[:, C_ONES:C_ONES + 128] = 1.0
    R = np.zeros((128, 128), np.float32)
    for blk in range(2):
        o = blk * 64
        for i in range(16):
            R[o + 16 + i, o + i] = -1.0
            R[o + i, o + 16 + i] = 1.0
            R[o + 48 + i, o + 32 + i] = -1.0
            R[o + 32 + i, o + 48 + i] = 1.0
    c[:, C_ROT:C_ROT + 128] = R
    for b in range(3):
        c[b, C_SEL + b * 128:C_SEL + (b + 1) * 128] = 1.0
    c[:3, C_ID3:C_ID3 + 3] = np.eye(3)
    return c


def rope_tables(TX, TCX):
    T = TX + TCX
    rows = TX // 64
    row_id = np.repeat(np.arange(rows), 64).astype(np.float32)
    col_id = np.tile(np.arange(64), rows).astype(np.float32)
    inv = (10000.0 ** (-np.arange(0, 32, 2, dtype=np.float32) / 32)).astype(np.float32)
    ar = row_id[:, None] * inv
    ac = col_id[:, None] * inv
    ang = np.concatenate([ar, ar, ac, ac], axis=-1)
    cos = np.ones((T, 64), np.float32)
    sin = np.zeros((T, 64), np.float32)
    cos[TCX:] = np.cos(ang)
    sin[TCX:] = np.sin(ang)
    cs = np.concatenate([cos.T, cos.T], axis=0)
    sn = np.concatenate([sin.T, sin.T], axis=0)
    return np.ascontiguousarray(cs), np.ascontiguousarray(sn)


def colform(v, n):
    return np.ascontiguousarray(np.asarray(v, np.float32).reshape(n, 128).T)


def geom(TX, TCX):
    T = TX + TCX
    NT = T // 128
    TP = T + 4
    blocks = []
    p = 0
    while p < TCX:
        n = min(512, TCX - p)
        blocks.append((p, n, p + 1))
        p += n
    p = 0
    while p < TX:
        n = min(512, TX - p)
        blocks.append((TCX + p, n, TCX + 3 + p))
        p += n
    return T, NT, TP, blocks


def build_program(TX, TCX, phases="ABCDEFG", debug=()):
    T, NT, TP, blocks = geom(TX, TCX)
    NTC = TCX // 128
    nc = bass.Bass("TRN2", target_bir_lowering=False)
    dram = {}

    def din(name, shape, dt=F32):
        dram[name] = nc.dram_tensor(name, list(shape), dt, kind="ExternalInput").ap()
        return dram[name]

    def dscr(name, shape, dt=F32):
        kind = "ExternalOutput" if name in debug else "Internal"
        dram[name] = nc.dram_tensor(name, list(shape), dt, kind=kind).ap()
        return dram[name]

    seq = din("seq", [NB, T, D])
    csT = din("csT", [128, 8, 3])
    consts_d = din("consts", [128, NCONST])
    w_mod = din("w_mod", [D, 6 * D])
    b_mod = din("b_mod", [1, 6 * D])
    n1w = din("n1w", [128, 8])
    n2w = din("n2w", [128, 8])
    w_in = din("w_in", [D, INW])
    shift_w = din("shift_w", [3, 2048])
    rw_w0 = din("rw_w0", [128, 2, 4])
    rw_a0 = din("rw_a0", [128, 2, 4])
    rw_wup = din("rw_wup", [2, 64, RW])
    rw_aup = din("rw_aup", [2, 64, RW])
    rw_gup = din("rw_gup", [2, 128, RW])
    rw_kk = din("rw_kk", [128, 4])
    rw_ka = din("rw_ka", [128, 4])
    rw_rk = din("rw_rk", [128, 4])
    rw_lnw = din("rw_lnw", [128, 4])
    rw_lnb = din("rw_lnb", [128, 4])
    qnw = din("qnw", [128, 1])
    knw = din("knw", [128, 1])
    lamv = din("lamv", [1, 256])
    sublnw = din("sublnw", [128, 1])
    w_out = din("w_out", [D, D])
    w_rt = din("w_rt", [D, 36])
    b_rt = din("b_rt", [1, 36])
    moe_g = din("moe_g", [NE, D, FF])
    moe_u = din("moe_u", [NE, D, FF])
    moe_d = din("moe_d", [NE, FF, D])
    ropec = din("ropec", [128, T])
    ropes = din("ropes", [128, T])
    out_d = nc.dram_tensor("out", [NB, TX, D], F32, kind="ExternalOutput").ap()

    P_d = dscr("P_d", [NB, INW, T])
    NCH = T // 16
    cols_d = dscr("cols_d", [2, 128, NCH + 1, NB, 4, 16, 6], BF16)
    rows_d = dscr("rows_d", [2, 6, T, NB * 4, 128], BF16)
    v_d = dscr("v_d", [2, T, NB * 4, 64], BF16)
    g_d = dscr("g_d", [2, NB, 4, 128, T], BF16)
    bon_d = dscr("bon_d", [2, NB, 4, 128, T], BF16)
    y_d = dscr("y_d", [2, 2, T + 2, NB * 4, 64], BF16)
    qT_d = dscr("qT_d", [NB, 4, 128, T], BF16)
    kT_d = dscr("kT_d", [NB, 4, 128, T], BF16)
    vt_d = dscr("vt_d", [NB, T, 512], BF16)
    cat_d = dscr("cat_d", [NB, D, TX], BF16)
    x1_d = dscr("x1_d", [NB, TX, D])
    h2T_d = dscr("h2T_d", [NB, D, TX], BF16)
    wd_d = dscr("wd_d", [NB, TX, NE])

    with ExitStack() as es:
        k = K(nc, es)
        consts = k.sb("consts_sb", [128, NCONST], F32)
        cbf = k.sb("cbf", [128, 768], BF16)
        modT = k.sb("modT", [128, 48, 3], F32)
        A1 = k.sb("A1", [128, 8, 3], F32)
        A2 = k.sb("A2", [128, 8, 3], F32)
        G1 = k.sb("G1", [128, NB, D], F32)
        G2 = k.sb("G2", [128, NB, D], F32)
        eps_t = k.sb("eps_t", [128, 1], F32)
        b_consts = k.buf("consts")
        b_mod_ = k.buf("mod")
        ds_c = k.dsem("sp", "c")
        k.dma(ds_c, consts[:], consts_d[:, :], writes=[b_consts])
        k.op("dve", lambda e: e.tensor_copy(cbf[:], consts[:, 0:768]), reads=[b_consts], writes=[b_consts])
        k.op("dve", lambda e: e.memset(eps_t[:], 1e-6), writes=[b_consts])
        ident_bf = cbf[:, C_ID:C_ID + 128]
        identA_bf = cbf[:, C_IDA:C_IDA + 128]
        identB_bf = cbf[:, C_IDB:C_IDB + 128]
        bones_bf = cbf[:, C_BONES:C_BONES + 128]
        ones_bf = cbf[:, C_ONES:C_ONES + 128]
        rot_bf = cbf[:, C_ROT:C_ROT + 128]
        k.barrier()

        if "A" in phases:
            with ExitStack() as pes:
                k.phase_begin(pes)
                silT = k.sb("silT", [128, 8, 3], F32)
                modrow = k.sb("modrow", [3, 6 * D], F32)
                bmr = k.sb("bmr", [3, 6 * D], F32)
                n1c = k.sb("n1c", [128, 8], F32)
                n2c = k.sb("n2c", [128, 8], F32)
                wm = [k.sb("wm%d" % i, [128, 8, 1024], F32) for i in range(2)]
                pa = [k.ps("pa%d" % i, [3, 512]) for i in range(2)]
                pc = k.ps("pc", [128, 48, 3])
                pg = [k.ps("pg%d" % i, [128, 512]) for i in range(2)]
                b_sil, b_bmr, b_pc = k.buf(), k.buf(), k.buf()
                b_wm = [k.buf(), k.buf()]
                b_pa = [k.buf(), k.buf()]
                b_pg = [k.buf(), k.buf()]
                ds_a = k.dsem("sp", "a")
                ds_w = [k.dsem("sp", "wm0"), k.dsem("sp", "wm1")]
                k.dma(ds_a, silT[:], csT[:, :, :], writes=[b_sil])
                k.dma(ds_a, bmr[:], b_mod.partition_broadcast(3), writes=[b_bmr])
                k.dma(ds_a, n1c[:], n1w[:, :], writes=[b_bmr])
                k.dma(ds_a, n2c[:], n2w[:, :], writes=[b_bmr])
                k.op("act", lambda e: e.activation(out=silT[:], in_=silT[:], func=AF.Silu), reads=[b_sil], writes=[b_sil])
                wmv = w_mod.rearrange("(kc p) n -> p kc n", p=128)
                for m in range(6):
                    s = m % 2
                    k.dma(ds_w[s], wm[s][:], wmv[:, :, m * 1024:(m + 1) * 1024], writes=[b_wm[s]])
                    for blk in range(2):
                        pb = (m * 2 + blk) % 2
                        for kc in range(8):
                            k.op("pe", lambda e, kc=kc, s=s, blk=blk, pb=pb: e.matmul(
                                pa[pb][:], silT[:, kc, :], wm[s][:, kc, blk * 512:(blk + 1) * 512],
                                start=(kc == 0), stop=(kc == 7)), reads=[b_sil, b_wm[s]], writes=[b_pa[pb]])
                        c0 = m * 1024 + blk * 512
                        k.op("dve", lambda e, pb=pb, c0=c0: e.tensor_tensor(
                            out=modrow[:, c0:c0 + 512], in0=pa[pb][:], in1=bmr[:, c0:c0 + 512], op=ALU.add),
                            reads=[b_pa[pb], b_bmr], writes=[b_mod_])
                for f in range(48):
                    k.op("pe", lambda e, f=f: e.matmul(pc[:, f, :], modrow[0:3, f * 128:(f + 1) * 128],
                                                      consts[0:3, C_ID3:C_ID3 + 3], start=True, stop=True),
                         reads=[b_mod_, b_consts], writes=[b_pc])
                k.op("dve", lambda e: e.tensor_copy(modT[:], pc[:]), reads=[b_pc], writes=[b_mod_])
                k.op("dve", lambda e: e.scalar_tensor_tensor(
                    out=A1[:], in0=modT[:, 8:16, :], scalar=1.0, in1=_bc(n1c[:].unsqueeze(2), [128, 8, 3]),
                    op0=ALU.add, op1=ALU.mult), reads=[b_mod_, b_bmr], writes=[b_mod_])
                k.op("dve", lambda e: e.scalar_tensor_tensor(
                    out=A2[:], in0=modT[:, 32:40, :], scalar=1.0, in1=_bc(n2c[:].unsqueeze(2), [128, 8, 3]),
                    op0=ALU.add, op1=ALU.mult), reads=[b_mod_, b_bmr], writes=[b_mod_])
                i = 0
                for gi, Gt in ((2, G1), (5, G2)):
                    for b in range(NB):
                        for blk in range(2):
                            pb = i % 2
                            i += 1
                            c0 = gi * 1024 + blk * 512
                            k.op("pe", lambda e, b=b, c0=c0, pb=pb: e.matmul(
                                pg[pb][:], consts[0:3, C_SEL + b * 128:C_SEL + (b + 1) * 128],
                                modrow[0:3, c0:c0 + 512], start=True, stop=True),
                                reads=[b_mod_, b_consts], writes=[b_pg[pb]])
                            k.op("act", lambda e, Gt=Gt, b=b, blk=blk, pb=pb: e.activation(
                                out=Gt[:, b, blk * 512:(blk + 1) * 512], in_=pg[pb][:], func=AF.Copy),
                                reads=[b_pg[pb]], writes=[b_mod_])
                k.barrier()
                k.phase_end(es)
        B1 = modT[:, 0:8, :]
        B2 = modT[:, 24:32, :]

        if "dbgA" in debug:
            pass

        if "B" in phases:
            with ExitStack() as pes:
                k.phase_begin(pes)
                hT = k.sb("hT", [128, 8, TP], BF16)
                xt = [k.sb("xt%d" % i, [128, D], F32) for i in range(2)]
                sq = k.sb("sq", [128, D], F32)
                ss = k.sb("ss", [128, 4], F32)
                xn = [k.sb("xn%d" % i, [128, D], BF16) for i in range(2)]
                pt = [k.ps("pt%d" % i, [128, 8, 128], BF16) for i in range(2)]
                wst = [k.sb("wst%d" % i, [128, 8, 128], F32) for i in range(2)]
                wbf = [k.sb("wbf%d" % i, [128, 8, 3, 128], BF16) for i in range(2)]
                swb = k.sb("swb", [128, 3, 2048], F32)
                pp = [k.ps("pp%d" % i, [128, 512]) for i in range(4)]
                ev = [k.sb("ev%d" % i, [128, 512], F32) for i in range(4)]
                b_hT = k.buf("hT")
                b_xt, b_xn, b_pt = [k.buf(), k.buf()], [k.buf(), k.buf()], [k.buf(), k.buf()]
                b_sq, b_ss, b_swb = k.buf(), k.buf(), k.buf()
                b_wst, b_wbf = [k.buf(), k.buf()], [k.buf(), k.buf()]
                b_pp, b_ev = [k.buf() for _ in range(4)], [k.buf() for _ in range(4)]
                ds_x = [k.dsem("sp", "x0"), k.dsem("sp", "x1")]
                ds_ws = [k.dsem("sp", "ws0"), k.dsem("sp", "ws1")]
                ds_sw = k.dsem("sp", "sw")
                ds_ev = [k.dsem("pool", "ev%d" % i) for i in range(4)]
                k.dma(ds_sw, swb[:].rearrange("p a b -> p (a b)"),
                      shift_w.rearrange("a b -> (a b)").unsqueeze(0).partition_broadcast(128)
                      if False else shift_w.rearrange("(o a) b -> o (a b)", o=1).partition_broadcast(128),
                      writes=[b_swb])
                k.op("pool", lambda e: e.memset(hT[:], 0.0), writes=[b_hT])
                w_in_v = w_in.rearrange("(kc p) n -> p kc n", p=128)
                for b in range(NB):
                    for q in range(NT):
                        s = q % 2
                        sel = 2 if q < NTC else b
                        col0 = (q * 128 + 1) if q < NTC else (q * 128 + 3)
                        k.dma(ds_x[s], xt[s][:], seq[b, q * 128:(q + 1) * 128, :], writes=[b_xt[s]])
                        k.op("act", lambda e, s=s: e.activation(out=sq[:], in_=xt[s][:], func=AF.Square),
                             reads=[b_xt[s]], writes=[b_sq])
                        k.op("dve", lambda e: e.tensor_reduce(out=ss[:, 0:1], in_=sq[:], axis=AX.X, op=ALU.add),
                             reads=[b_sq], writes=[b_ss])
                        k.op("act", lambda e: e.activation(out=ss[:, 1:2], in_=ss[:, 0:1], func=AF.Sqrt,
                                                           bias=eps_t[:, 0:1], scale=1.0 / D),
                             reads=[b_ss, b_consts], writes=[b_ss])
                        k.op("dve", lambda e: e.reciprocal(ss[:, 2:3], ss[:, 1:2]), reads=[b_ss], writes=[b_ss])
                        k.op("dve", lambda e, s=s: e.tensor_scalar(out=xn[s][:], in0=xt[s][:], scalar1=ss[:, 2:3],
                                                                   scalar2=None, op0=ALU.mult),
                             reads=[b_xt[s], b_ss], writes=[b_xn[s]])
                        for kc in range(8):
                            k.op("pe", lambda e, s=s, kc=kc: e.transpose(out=pt[s][:, kc, :],
                                                                         in_=xn[s][:, kc * 128:(kc + 1) * 128],
                                                                         identity=ident_bf),
                                 reads=[b_xn[s], b_consts], writes=[b_pt[s]])
                        for kc in range(8):
                            k.op("act", lambda e, s=s, kc=kc, sel=sel, col0=col0: e.activation(
                                out=hT[:, kc, col0:col0 + 128], in_=pt[s][:, kc, :], func=AF.Identity,
                                bias=B1[:, kc, sel:sel + 1], scale=A1[:, kc, sel:sel + 1]),
                                reads=[b_pt[s], b_mod_], writes=[b_hT])
                    ie = 0
                    for c in range(28):
                        s = c % 2
                        k.dma(ds_ws[s], wst[s][:], w_in_v[:, :, c * 128:(c + 1) * 128], writes=[b_wst[s]])
                        ntap = 3 if c < 16 else 1
                        if c < 16:
                            for j in range(3):
                                eng = "pool" if j == 1 else "dve"
                                k.op(eng, lambda e, s=s, j=j, c=c: e.tensor_tensor(
                                    out=wbf[s][:, :, j, :], in0=wst[s][:],
                                    in1=_bc(swb[:, j, c * 128:(c + 1) * 128].unsqueeze(1), [128, 8, 128]),
                                    op=ALU.mult), reads=[b_wst[s], b_swb], writes=[b_wbf[s]])
                        else:
                            k.op("dve", lambda e, s=s: e.tensor_copy(wbf[s][:, :, 1, :], wst[s][:]),
                                 reads=[b_wst[s]], writes=[b_wbf[s]])
                        for (pos0, N, colb) in blocks:
                            pi = ie % 4
                            ie += 1
                            taps = (0, 1, 2) if c < 16 else (1,)
                            nmm = len(taps) * 8
                            im = 0
                            for j in taps:
                                for kc in range(8):
                                    k.op("pe", lambda e, pi=pi, s=s, kc=kc, j=j, colb=colb, N=N, im=im, nmm=nmm: e.matmul(
                                        pp[pi][:, 0:N], wbf[s][:, kc, j, :], hT[:, kc, colb + j - 1:colb + j - 1 + N],
                                        start=(im == 0), stop=(im == nmm - 1)),
                                        reads=[b_wbf[s], b_hT], writes=[b_pp[pi]])
                                    im += 1
                            eng = "act" if pi % 2 == 0 else "dve"
                            if eng == "act":
                                k.op("act", lambda e, pi=pi, N=N: e.activation(out=ev[pi][:, 0:N], in_=pp[pi][:, 0:N], func=AF.Copy),
                                     reads=[b_pp[pi]], writes=[b_ev[pi]])
                            else:
                                k.op("dve", lambda e, pi=pi, N=N: e.tensor_copy(ev[pi][:, 0:N], pp[pi][:, 0:N]),
                                     reads=[b_pp[pi]], writes=[b_ev[pi]])
                            k.dma(ds_ev[pi], P_d[b, c * 128:(c + 1) * 128, pos0:pos0 + N], ev[pi][:, 0:N],
                                  reads=[b_ev[pi]])
                    k.barrier()
                k.phase_end(es)

        if "C" in phases:
            with ExitStack() as pes:
                k.phase_begin(pes)
                PB = [k.sb("PB%d" % i, [128, 16, 512], F32) for i in range(2)]
                b_PB = [k.buf(), k.buf()]
                ds_pb = [k.dsem("sp", "pb0"), k.dsem("sp", "pb1")]
                ds_st = k.dsem("sp", "cst")
                ds_o = [k.dsem("pool", "co%d" % i) for i in range(4)]
                wst_c = k.sb("wst_c", [128, 512], F32)
                Wwa = k.sb("Wwa", [128, 2, 512], BF16)
                Wg = k.sb("Wg", [128, 2, 512], BF16)
                colc = k.sb("colc", [128, 40], F32)
                b_w = k.buf("cw")
                for d in range(2):
                    k.dma(ds_st, wst_c[0:64, :], rw_wup[d], writes=[b_w])
                    k.dma(ds_st, wst_c[64:128, :], rw_aup[d], writes=[b_w])
                    k.op("dve", lambda e, d=d: e.tensor_copy(Wwa[:, d, :], wst_c[:]), reads=[b_w], writes=[b_w])
                    k.dma(ds_st, wst_c[:], rw_gup[d], writes=[b_w])
                    k.op("dve", lambda e, d=d: e.tensor_copy(Wg[:, d, :], wst_c[:]), reads=[b_w], writes=[b_w])
                k.dma(ds_st, colc[:, 0:8], rw_w0.rearrange("p a b -> p (a b)"), writes=[b_w])
                k.dma(ds_st, colc[:, 8:16], rw_a0.rearrange("p a b -> p (a b)"), writes=[b_w])
                k.dma(ds_st, colc[:, 16:20], rw_kk[:, :], writes=[b_w])
                k.dma(ds_st, colc[:, 20:24], rw_ka[:, :], writes=[b_w])
                k.dma(ds_st, colc[:, 24:28], rw_rk[:, :], writes=[b_w])
                k.op("dve", lambda e: e.tensor_scalar(out=colc[:, 28:32], in0=colc[:, 20:24], scalar1=-1.0, scalar2=1.0,
                                                      op0=ALU.mult, op1=ALU.add), reads=[b_w], writes=[b_w])
                Vbf = k.sb("Vbf", [128, 4, 512], BF16)
                RH = k.sb("RH", [128, 4, 514], F32)
                CAx = k.sb("CAx", [128, 6], BF16)
                b_RH = k.buf("RH")
                ds_rh = k.dsem("sp", "rh")
                k.op("pool", lambda e: e.memset(CAx[:], 0.0), writes=[b_RH])
                TLs = [k.sb("TL%d" % i, [128, 512], BF16) for i in range(2)]
                SGs = [k.sb("SG%d" % i, [128, 512], BF16) for i in range(2)]
                f32ts = [{n: k.sb("c_%s%d" % (n, i), [128, 512], F32) for n in ("sgw", "dec", "Aa", "kkf", "sd", "kkn", "tmpk", "kd")} for i in range(2)]
                bfts = [{n: k.sb("c_%s%d" % (n, i), [128, 512], BF16) for n in ("Gg", "kk2", "bsc", "kdb", "rkr", "bon")} for i in range(2)]
                CAb = k.sb("CAb", [128, 32, 4, 16, 6], BF16)
                CAw = CAb[:].bitcast(F32)
                bCA = k.buf("CAb")
                rowsbs = [k.sb("rowsb%d" % i, [128, 4, 128], BF16) for i in range(2)]
                vrow = k.sb("vrow", [128, 4, 128], BF16)
                pw, pa_, pg_, pss, pbo = (k.ps(n, [128, 512]) for n in ("pw", "pa_", "pg_", "pss", "pbo"))
                prow = k.ps("prow", [128, 4, 128])
                pv = k.ps("pv", [128, 4, 128])
                bbs = [{n: k.buf(n) for n in ("TL", "SG", "sgw", "dec", "Aa", "kkf", "sd", "kkn", "tmpk", "kd", "Gg", "kk2",
                                              "bsc", "kdb", "rkr", "bon", "CA", "rowsb")} for i in range(2)]
                bbg = {n: k.buf(n) for n in ("Vbf", "vrow", "pw", "pa_", "pg_", "pss", "pbo", "prow", "pv")}
                for i in range(2):
                    bbs[i].update(bbg)
                bb = bbs[0]
                k.op("pool", lambda e: e.memset(CAb[:], 0.0), writes=[bCA])
                ihp = 0
                idd = 0
                irow = 0
                ztile = k.sb("ztile", [128, 1024], BF16)
                b_z = k.buf("z")
                k.op("pool", lambda e: e.memset(ztile[:], 0.0), writes=[b_z])
                for d in range(2):
                    zv = rows_d[d, 2:4].rearrange("r t s c -> (r t) (s c)")
                    for i in range(2 * T // 128):
                        k.dma(ds_o[i % 4], zv[i * 128:(i + 1) * 128, :], ztile[:], reads=[b_z])
                ib = 0
                for b in range(NB):
                    for (pos0, N, _c) in blocks:
                        s = ib % 2
                        ib += 1
                        P = PB[s]
                        bP = b_PB[s]
                        k.dma(ds_pb[s], P[:, :, 0:N], P_d[b, 0:2048, pos0:pos0 + N].rearrange("(c p) n -> p c n", p=128),
                              writes=[bP])
                        k.op("pool", lambda e: e.memset(RH[:], 0.0), writes=[b_RH])
                        lo = max(pos0 - 1, 0)
                        hi = min(pos0 + N + 1, T)
                        co = lo - (pos0 - 1)
                        k.dma(ds_rh, RH[:, :, co:co + hi - lo], P_d[b, 0:512, lo:hi].rearrange("(c p) n -> p c n", p=128), writes=[b_RH])
                        k.op("pool", lambda e, P=P, N=N: e.tensor_copy(Vbf[:, :, 0:N], P[:, 8:12, 0:N]), reads=[bP], writes=[bb["Vbf"]])
                        for j in range(N // 128):
                            for hp in range(4):
                                k.op("pe", lambda e, hp=hp, j=j: e.matmul(pv[:, hp, :], Vbf[:, hp, j * 128:(j + 1) * 128], ident_bf,
                                                                         start=True, stop=True),
                                     reads=[bb["Vbf"], b_consts], writes=[bb["pv"]])
                            k.op("act", lambda e: e.activation(out=vrow[:], in_=pv[:], func=AF.Copy), reads=[bb["pv"]], writes=[bb["vrow"]])
                            p0 = pos0 + j * 128
                            for ab in range(2):
                                k.dma(ds_o[ab], v_d[ab, p0:p0 + 128, b * 4:(b + 1) * 4, :], vrow[:, :, ab * 64:(ab + 1) * 64],
                                      reads=[bb["vrow"]])
                        for d in range(2):
                            TL = TLs[idd % 2]
                            SG = SGs[idd % 2]
                            bbd = bbs[idd % 2]
                            idd += 1
                            k.op("act", lambda e, P=P, N=N, d=d, TL=TL: e.activation(out=TL[0:64, 0:N], in_=P[0:64, 12 + 2 * d, 0:N], func=AF.Tanh),
                                 reads=[bP], writes=[bbd["TL"]])
                            k.op("dve", lambda e, P=P, N=N, d=d: e.tensor_copy(TL[64:128, 0:N], P[64:128, 12 + 2 * d, 0:N]),
                                 reads=[bP], writes=[bbd["TL"]])
                            k.op("act", lambda e, P=P, N=N, d=d: e.activation(out=SG[:, 0:N], in_=P[:, 13 + 2 * d, 0:N], func=AF.Sigmoid),
                                 reads=[bP], writes=[bbd["SG"]])
                            for hp in range(4):
                                hs = slice(hp * 128, (hp + 1) * 128)
                                t = f32ts[ihp % 2]
                                u = bfts[ihp % 2]
                                bb = dict(bbs[ihp % 2])
                                bb["TL"] = bbd["TL"]
                                bb["SG"] = bbd["SG"]
                                bb["CA"] = bCA
                                nch = N // 16
                                c16 = lambda ap: ap.rearrange("p (c s) -> p c s", s=16)
                                ihp += 1
                                k.op("pe", lambda e, d=d, hs=hs, N=N: e.matmul(pw[:, 0:N], Wwa[0:64, d, hs], TL[0:64, 0:N], start=True, stop=True),
                                     reads=[b_w, bb["TL"]], writes=[bb["pw"]])
                                k.op("pe", lambda e, d=d, hs=hs, N=N: e.matmul(pa_[:, 0:N], Wwa[64:128, d, hs], TL[64:128, 0:N], start=True, stop=True),
                                     reads=[b_w, bb["TL"]], writes=[bb["pa_"]])
                                k.op("pe", lambda e, d=d, hs=hs, N=N: e.matmul(pg_[:, 0:N], Wg[:, d, hs], SG[:, 0:N], start=True, stop=True),
                                     reads=[b_w, bb["SG"]], writes=[bb["pg_"]])
                                ci = d * 4 + hp
                                k.op("act", lambda e, N=N, ci=ci: e.activation(out=t["sgw"][:, 0:N], in_=pw[:, 0:N], func=AF.Sigmoid,
                                                                               bias=colc[:, ci:ci + 1], scale=1.0),
                                     reads=[bb["pw"], b_w], writes=[bb["sgw"]])
                                k.op("act", lambda e, N=N: e.activation(out=t["dec"][:, 0:N], in_=t["sgw"][:, 0:N], func=AF.Exp,
                                                                        scale=-math.exp(-0.5)),
                                     reads=[bb["sgw"]], writes=[bb["dec"]])
                                k.op("dve", lambda e, N=N: e.tensor_copy(CAw[:, 0:nch, hp, :, 2], c16(t["dec"][:, 0:N])),
                                     reads=[bb["dec"]], writes=[bCA])
                                k.op("act", lambda e, N=N, ci=ci: e.activation(out=t["Aa"][:, 0:N], in_=pa_[:, 0:N], func=AF.Sigmoid,
                                                                               bias=colc[:, 8 + ci:9 + ci], scale=1.0),
                                     reads=[bb["pa_"], b_w], writes=[bb["Aa"]])
                                k.op("act", lambda e, N=N: e.activation(out=u["Gg"][:, 0:N], in_=pg_[:, 0:N], func=AF.Copy),
                                     reads=[bb["pg_"]], writes=[bb["Gg"]])
                                k.dma(ds_o[3], g_d[d, b, hp, :, pos0:pos0 + N], u["Gg"][:, 0:N], reads=[bb["Gg"]])
                                kk_ = P[:, 4 + hp, 0:N]
                                r_ = P[:, hp, 0:N]
                                v_ = P[:, 8 + hp, 0:N]
                                k.op("dve", lambda e, N=N, hp=hp, kk_=kk_: e.tensor_scalar(out=t["kkf"][:, 0:N], in0=kk_, scalar1=colc[:, 16 + hp:17 + hp],
                                                                                        scalar2=None, op0=ALU.mult),
                                     reads=[bP, b_w], writes=[bb["kkf"]])
                                k.op("pool", lambda e, N=N: e.tensor_tensor(out=u["kk2"][:, 0:N], in0=t["kkf"][:, 0:N], in1=t["kkf"][:, 0:N], op=ALU.mult),
                                     reads=[bb["kkf"]], writes=[bb["kk2"]])
                                k.op("pe", lambda e, N=N: e.matmul(pss[:, 0:N], bones_bf, u["kk2"][:, 0:N], start=True, stop=True),
                                     reads=[bb["kk2"], b_consts], writes=[bb["pss"]])
                                k.op("act", lambda e, N=N: e.activation(out=t["sd"][:, 0:N], in_=pss[:, 0:N], func=AF.Sqrt),
                                     reads=[bb["pss"]], writes=[bb["sd"]])
                                k.op("dve", lambda e, N=N: e.tensor_scalar(out=t["sd"][:, 0:N], in0=t["sd"][:, 0:N], scalar1=1e-12, scalar2=None, op0=ALU.max),
                                     reads=[bb["sd"]], writes=[bb["sd"]])
                                k.op("dve", lambda e, N=N: e.reciprocal(t["sd"][:, 0:N], t["sd"][:, 0:N]), reads=[bb["sd"]], writes=[bb["sd"]])
                                k.op("dve", lambda e, N=N: e.tensor_tensor(out=t["kkn"][:, 0:N], in0=t["kkf"][:, 0:N], in1=t["sd"][:, 0:N], op=ALU.mult),
                                     reads=[bb["kkf"], bb["sd"]], writes=[bb["kkn"]])
                                k.op("dve", lambda e, N=N: e.tensor_tensor(out=u["bsc"][:, 0:N], in0=t["kkn"][:, 0:N], in1=t["Aa"][:, 0:N], op=ALU.mult),
                                     reads=[bb["kkn"], bb["Aa"]], writes=[bb["bsc"]])
                                k.op("pool", lambda e, N=N, hp=hp: e.tensor_scalar(out=t["tmpk"][:, 0:N], in0=t["Aa"][:, 0:N], scalar1=colc[:, 20 + hp:21 + hp],
                                                                                 scalar2=colc[:, 28 + hp:29 + hp], op0=ALU.mult, op1=ALU.add),
                                     reads=[bb["Aa"], b_w], writes=[bb["tmpk"]])
                                k.op("pool", lambda e, N=N, kk_=kk_: e.tensor_tensor(out=t["kd"][:, 0:N], in0=kk_, in1=t["tmpk"][:, 0:N], op=ALU.mult),
                                     reads=[bP, bb["tmpk"]], writes=[bb["kd"]])
                                k.op("act", lambda e, N=N: e.activation(out=u["kdb"][:, 0:N], in_=t["kd"][:, 0:N], func=AF.Copy),
                                     reads=[bb["kd"]], writes=[bb["kdb"]])
                                k.op("dve", lambda e, N=N, hp=hp, r_=r_: e.scalar_tensor_tensor(out=u["rkr"][:, 0:N], in0=r_, scalar=colc[:, 24 + hp:25 + hp],
                                                                                             in1=t["kd"][:, 0:N], op0=ALU.mult, op1=ALU.mult),
                                     reads=[bP, bb["kd"], b_w], writes=[bb["rkr"]])
                                k.op("pe", lambda e, N=N: e.matmul(pbo[:, 0:N], bones_bf, u["rkr"][:, 0:N], start=True, stop=True),
                                     reads=[bb["rkr"], b_consts], writes=[bb["pbo"]])
                                k.op("dve", lambda e, N=N, v_=v_: e.tensor_tensor(out=u["bon"][:, 0:N], in0=pbo[:, 0:N], in1=v_, op=ALU.mult),
                                     reads=[bb["pbo"], bP], writes=[bb["bon"]])
                                k.dma(ds_o[0], bon_d[d, b, hp, :, pos0:pos0 + N], u["bon"][:, 0:N], reads=[bb["bon"]])
                                k.op("pool", lambda e, N=N: e.tensor_scalar(out=CAb[0:64, 0:nch, hp, :, 0], in0=c16(t["kkn"][0:64, 0:N]), scalar1=-1.0, scalar2=None, op0=ALU.mult),
                                     reads=[bb["kkn"]], writes=[bb["CA"]])
                                k.op("pool", lambda e, N=N: e.tensor_scalar(out=CAb[64:128, 0:nch, hp, :, 1], in0=c16(t["kkn"][64:128, 0:N]), scalar1=-1.0, scalar2=None, op0=ALU.mult),
                                     reads=[bb["kkn"]], writes=[bb["CA"]])
                                ro = 0 if d == 0 else 2
                                k.op("act", lambda e, N=N, hp=hp, ro=ro: e.activation(out=CAb[0:64, 0:nch, hp, :, 2], in_=c16(RH[0:64, hp, ro:ro + N]), func=AF.Copy),
                                     reads=[b_RH], writes=[bb["CA"]])
                                k.op("act", lambda e, N=N, hp=hp, ro=ro: e.activation(out=CAb[64:128, 0:nch, hp, :, 3], in_=c16(RH[64:128, hp, ro:ro + N]), func=AF.Copy),
                                     reads=[b_RH], writes=[bb["CA"]])
                                if d == 0 and pos0 + N == T:
                                    k.op("act", lambda e, N=N, hp=hp: e.activation(out=CAx[0:64, 2:3], in_=RH[0:64, hp, N:N + 1], func=AF.Copy),
                                         reads=[b_RH], writes=[b_RH])
                                    k.op("act", lambda e, N=N, hp=hp: e.activation(out=CAx[64:128, 3:4], in_=RH[64:128, hp, N:N + 1], func=AF.Copy),
                                         reads=[b_RH], writes=[b_RH])
                                    k.dma(ds_rh, cols_d[0, :, NCH, b, hp, 0, :], CAx[:], reads=[b_RH])
                                if hp == 3:
                                    k.dma(ds_o[1], cols_d[d, :, pos0 // 16:pos0 // 16 + nch, b, :, :, :], CAb[:, 0:nch], reads=[bCA])
                                for j in range(N // 128):
                                    js = slice(j * 128, (j + 1) * 128)
                                    rowsb = rowsbs[irow % 2]
                                    bb["rowsb"] = bbs[irow % 2]["rowsb"]
                                    irow += 1
                                    k.op("pe", lambda e, js=js: e.matmul(prow[:, 0, :], u["bsc"][:, js], identA_bf, start=True, stop=True),
                                         reads=[bb["bsc"], b_consts], writes=[bb["prow"]])
                                    k.op("pe", lambda e, js=js: e.matmul(prow[:, 1, :], u["bsc"][:, js], identB_bf, start=True, stop=True),
                                         reads=[bb["bsc"], b_consts], writes=[bb["prow"]])
                                    k.op("pe", lambda e, js=js: e.matmul(prow[:, 2, :], u["kdb"][:, js], identA_bf, start=True, stop=True),
                                         reads=[bb["kdb"], b_consts], writes=[bb["prow"]])
                                    k.op("pe", lambda e, js=js: e.matmul(prow[:, 3, :], u["kdb"][:, js], identB_bf, start=True, stop=True),
                                         reads=[bb["kdb"], b_consts], writes=[bb["prow"]])
                                    k.op("dve", lambda e: e.tensor_copy(rowsb[:], prow[:]), reads=[bb["prow"]], writes=[bb["rowsb"]])
                                    p0 = pos0 + j * 128
                                    k.dma(ds_o[2], rows_d[d, 0:2, p0:p0 + 128, b * 4 + hp, :].rearrange("r t c -> t r c"), rowsb[:, 0:2, :],
                                          reads=[bb["rowsb"]])
                                    k.dma(ds_o[3], rows_d[d, 4:6, p0:p0 + 128, b * 4 + hp, :].rearrange("r t c -> t r c"), rowsb[:, 2:4, :],
                                          reads=[bb["rowsb"]])
                k.barrier()
                k.phase_end(es)

        if "D" in phases:
            with ExitStack() as pes:
                k.phase_begin(pes)
                CH = 16
                ST = k.sb("ST", [128, 2, 8, 64], F32)
                T1 = k.sb("T1", [128, 2, 8, 64], F32)
                STb = k.sb("STb", [128, 2, 8, 64], BF16)
                colsAR = [k.sb("colsAR%d" % g, [128, 1, 8, CH, 6], BF16) for g in range(2)]
                wv = [colsAR[g][:].bitcast(F32) for g in range(2)]
                rowsL = [k.sb("rowsL%d" % g, [6, CH, 8, 128], BF16) for g in range(2)]
                stage = [k.sb("stage%d" % g, [6, CH, 8, 64], BF16) for g in range(2)]
                ps1 = [k.ps("ps1%d" % g, [4, 8, 64]) for g in range(2)]
                ps2 = [k.ps("ps2%d" % g, [128, 8, 64]) for g in range(2)]
                b_ST, b_T1, b_STb = [k.buf(), k.buf()], [k.buf(), k.buf()], [k.buf(), k.buf()]
                b_cols, b_wc = [k.buf(), k.buf()], [k.buf(), k.buf()]
                b_rows, b_stv, b_sty = [k.buf(), k.buf()], [k.buf(), k.buf()], [k.buf(), k.buf()]
                b_ps1, b_ps2 = [k.buf(), k.buf()], [k.buf(), k.buf()]
                ds_g = [k.dsem("sp", "dg0"), k.dsem("pool", "dg1")]
                ds_y = [k.dsem("sp", "dy0"), k.dsem("pool", "dy1")]
                k.op("dve", lambda e: e.memset(ST[:], 0.0), writes=b_ST)
                k.op("dve", lambda e: e.memset(STb[:], 0.0), writes=b_STb)
                for g in range(2):
                    k.op("pool", lambda e, g=g: e.memset(stage[g][:], 0.0), writes=[b_stv[g], b_sty[g]])
                cols_v = [cols_d[g].rearrange("p c b h s x -> p c (b h) s x") for g in range(2)]

                def dsl(start, size):
                    if isinstance(start, int):
                        return slice(start, start + size)
                    return bass.ds(start, size)

                def scan_body(cbase, n):
                    def body(it):
                        cidx = [cbase + it, (cbase + n - 1) - it]
                        for g in range(2):
                            p0 = cidx[g] * CH
                            k.dma(ds_g[g], colsAR[g][:], cols_v[g][:, dsl(cidx[g], 1)], writes=[b_cols[g], b_wc[g]])
                            k.dma(ds_g[g], rowsL[g][:], rows_d[g, :, dsl(p0, CH), :, :], writes=[b_rows[g]])
                            k.dma(ds_g[g], stage[g][4:6], v_d[:, dsl(p0, CH), :, :], writes=[b_stv[g]])
                        for st_ in range(CH):
                            tl = [st_, CH - 1 - st_]
                            for g in range(2):
                                for pr in range(8):
                                    k.op("pe", lambda e, g=g, pr=pr, t_=tl[g]: e.matmul(
                                        ps1[g][0:4, pr, :], colsAR[g][:, 0, pr, t_, 0:4], STb[:, g, pr, :], start=True, stop=True),
                                        reads=[b_cols[g], b_STb[g]], writes=[b_ps1[g]])
                            for g in range(2):
                                k.op("act", lambda e, g=g, t_=tl[g]: e.activation(
                                    out=stage[g][0:4, t_, :, :], in_=ps1[g][0:4, :, :], func=AF.Copy),
                                    reads=[b_ps1[g]], writes=[b_sty[g]])
                            for g in range(2):
                                k.op("pool", lambda e, g=g, t_=tl[g]: e.tensor_tensor(
                                    out=T1[:, g], in0=ST[:, g], in1=_bc(wv[g][:, 0, :, t_, 2:3], [128, 8, 64]), op=ALU.mult),
                                    reads=[b_ST[g], b_wc[g]], writes=[b_T1[g]])
                            for g in range(2):
                                for pr in range(8):
                                    k.op("pe", lambda e, g=g, pr=pr, t_=tl[g]: e.matmul(
                                        ps2[g][:, pr, :], rowsL[g][0:6, t_, pr, :], stage[g][0:6, t_, pr, :],
                                        start=True, stop=True),
                                        reads=[b_rows[g], b_stv[g], b_sty[g]], writes=[b_ps2[g]])
                            for g in range(2):
                                k.op("dve", lambda e, g=g: e.tensor_tensor(out=STb[:, g], in0=T1[:, g], in1=ps2[g][:], op=ALU.add),
                                     reads=[b_T1[g], b_ps2[g]], writes=[b_STb[g]])
                                k.op("dve", lambda e, g=g: e.tensor_tensor(out=ST[:, g], in0=T1[:, g], in1=ps2[g][:], op=ALU.add),
                                     reads=[b_T1[g], b_ps2[g]], writes=[b_ST[g]])
                        for g in range(2):
                            p0 = cidx[g] * CH
                            k.dma(ds_y[g], y_d[g, :, dsl(p0 + 1, CH), :, :], stage[g][2:4], reads=[b_sty[g]])
                    return body

                k.loop(TCX // CH, scan_body(0, TCX // CH))
                k.loop(TX // CH, scan_body(TCX // CH, TX // CH))
                for g, pv_ in ((0, T), (1, TCX - 1)):
                    k.dma(ds_g[g], colsAR[g][:], cols_v[g][:, pv_ // 16:pv_ // 16 + 1], writes=[b_cols[g]])
                    for pr in range(8):
                        k.op("pe", lambda e, g=g, pr=pr: e.matmul(ps1[g][0:4, pr, :], colsAR[g][:, 0, pr, pv_ % 16, 0:4], STb[:, g, pr, :],
                                                                  start=True, stop=True),
                             reads=[b_cols[g], b_STb[g]], writes=[b_ps1[g]])
                    k.op("act", lambda e, g=g: e.activation(out=stage[g][0:4, 0, :, :], in_=ps1[g][0:4, :, :], func=AF.Copy),
                         reads=[b_ps1[g]], writes=[b_sty[g]])
                    k.dma(ds_y[g], y_d[g, :, pv_ + 1:pv_ + 2, :, :], stage[g][2:4, 0:1], reads=[b_sty[g]])
                k.phase_end(es)

        xblocks = [(p, n) for (p, n, _c) in blocks if p >= TCX]
        if "E" in phases:
            with ExitStack() as pes:
                k.phase_begin(pes)
                Gt = k.sb("Gt", [128, 2, 4, 512], BF16)
                Bt = k.sb("Bt", [128, 2, 4, 512], BF16)
                Yt = k.sb("Yt", [128, 2, 4, 2, 64], BF16)
                Yf = k.sb("Yf", [128, 16, 64], F32)
                cen = k.sb("cen", [128, 16, 64], F32)
                sqe = k.sb("sqe", [128, 16, 64], F32)
                st4 = k.sb("st4", [128, 4, 16], F32)
                yh = k.sb("yh", [128, 2, 512], BF16)
                Zn = k.sb("Zn", [128, 2, 4, 128], F32)
                catR = k.sb("catR", [128, 4, 512], BF16)
                lnc = k.sb("lnc", [128, 8], F32)
                gne = k.sb("gne", [128, 1], F32)
                pte = k.ps("pte", [128, 8, 128], BF16)
                bG, bB, bY, bYf, bcen, bsq, bst, byh, bZn, bcat, bln, bpte = (k.buf() for _ in range(12))
                ds_e = [k.dsem("sp", "e%d" % i) for i in range(3)]
                ds_eo = k.dsem("pool", "eo")
                k.dma(ds_e[2], lnc[:, 0:4], rw_lnw[:, :], writes=[bln])
                k.dma(ds_e[2], lnc[:, 4:8], rw_lnb[:, :], writes=[bln])
                k.op("dve", lambda e: e.memset(gne[:], 64e-5), writes=[bln])
                for b in range(NB):
                    for (pos0, N) in xblocks:
                        for d in range(2):
                            k.dma(ds_e[0], Gt[:, d, :, 0:N], g_d[d, b, :, :, pos0:pos0 + N].rearrange("h p t -> p h t"), writes=[bG])
                            k.dma(ds_e[0], Bt[:, d, :, 0:N], bon_d[d, b, :, :, pos0:pos0 + N].rearrange("h p t -> p h t"), writes=[bB])
                        for j in range(N // 128):
                            pos = pos0 + j * 128
                            for d in range(2):
                                sl0 = pos + 2 if d == 0 else pos
                                for ab in range(2):
                                    k.dma(ds_e[1], Yt[:, d, :, ab, :], y_d[d, ab, sl0:sl0 + 128, b * 4:(b + 1) * 4, :], writes=[bY])
                            k.op("act", lambda e: e.activation(out=Yf[:], in_=Yt[:].rearrange("p d h a i -> p (d h a) i"), func=AF.Copy),
                                 reads=[bY], writes=[bYf])
                            k.op("dve", lambda e: e.tensor_reduce(out=st4[:, 0, :], in_=Yf[:], axis=AX.X, op=ALU.add), reads=[bYf], writes=[bst])
                            k.op("dve", lambda e: e.tensor_scalar(out=st4[:, 1, :], in0=st4[:, 0, :], scalar1=-1.0 / 64, scalar2=None, op0=ALU.mult),
                                 reads=[bst], writes=[bst])
                            k.op("dve", lambda e: e.tensor_tensor(out=cen[:], in0=Yf[:], in1=_bc(st4[:, 1, :].unsqueeze(2), [128, 16, 64]), op=ALU.add),
                                 reads=[bYf, bst], writes=[bcen])
                            k.op("pool", lambda e: e.tensor_tensor(out=sqe[:], in0=cen[:], in1=cen[:], op=ALU.mult), reads=[bcen], writes=[bsq])
                            k.op("dve", lambda e: e.tensor_reduce(out=st4[:, 2, :], in_=sqe[:], axis=AX.X, op=ALU.add), reads=[bsq], writes=[bst])
                            k.op("act", lambda e: e.activation(out=st4[:, 3, :], in_=st4[:, 2, :], func=AF.Sqrt, bias=gne[:, 0:1], scale=1.0 / 64),
                                 reads=[bst, bln], writes=[bst])
                            k.op("dve", lambda e: e.reciprocal(st4[:, 3, :], st4[:, 3, :]), reads=[bst], writes=[bst])
                            k.op("dve", lambda e: e.tensor_tensor(out=yh[:].rearrange("p d (g i) -> p (d g) i", i=64), in0=cen[:],
                                                                  in1=_bc(st4[:, 3, :].unsqueeze(2), [128, 16, 64]), op=ALU.mult),
                                 reads=[bcen, bst], writes=[byh])
                            for d in range(2):
                                for hp in range(4):
                                    k.op("pe", lambda e, d=d, hp=hp: e.transpose(out=pte[:, d * 4 + hp, :], in_=yh[:, d, hp * 128:(hp + 1) * 128],
                                                                                 identity=ident_bf), reads=[byh, b_consts], writes=[bpte])
                            for d in range(2):
                                for hp in range(4):
                                    k.op("act", lambda e, d=d, hp=hp: e.activation(out=Zn[:, d, hp, :], in_=pte[:, d * 4 + hp, :], func=AF.Identity,
                                                                                   bias=lnc[:, 4 + hp:5 + hp], scale=lnc[:, hp:hp + 1]),
                                         reads=[bpte, bln], writes=[bZn])
                            js = slice(j * 128, (j + 1) * 128)
                            k.op("dve", lambda e, js=js: e.tensor_tensor(out=Zn[:], in0=Zn[:], in1=Bt[:, :, :, js], op=ALU.add), reads=[bZn, bB], writes=[bZn])
                            k.op("pool", lambda e, js=js: e.tensor_tensor(out=Zn[:], in0=Zn[:], in1=Gt[:, :, :, js], op=ALU.mult), reads=[bZn, bG], writes=[bZn])
                            k.op("dve", lambda e, js=js: e.tensor_tensor(out=catR[:, :, js], in0=Zn[:, 0], in1=Zn[:, 1], op=ALU.add), reads=[bZn], writes=[bcat])
                        k.dma(ds_eo, cat_d[b, 0:512, pos0 - TCX:pos0 - TCX + N].rearrange("(h p) t -> p h t", p=128), catR[:, :, 0:N], reads=[bcat])
                k.phase_end(es)

        if "F" in phases:
            with ExitStack() as pes:
                k.phase_begin(pes)
                PD = [k.sb("PD%d" % i, [128, 12, 512], F32) for i in range(2)]
                cosb = [k.sb("cosb%d" % i, [128, 512], F32) for i in range(2)]
                sinb = [k.sb("sinb%d" % i, [128, 512], F32) for i in range(2)]
                x2 = k.sb("x2", [128, 512], BF16)
                sdf = k.sb("sdf", [128, 512], F32)
                XQ = k.sb("XQ", [128, 512], F32)
                XQb = k.sb("XQb", [128, 512], BF16)
                t1f = k.sb("t1f", [128, 512], F32)
                t2f = k.sb("t2f", [128, 512], F32)
                qo = [k.sb("qo%d" % i, [128, 512], BF16) for i in range(2)]
                Vb = k.sb("Vb", [128, 4, 512], BF16)
                vtk = [k.sb("vtk%d" % i, [128, 512], BF16) for i in range(2)]
                nwc = k.sb("nwc", [128, 2], F32)
                pssf = k.ps("pssf", [128, 512])
                prot = k.ps("prot", [128, 512])
                pvf = k.ps("pvf", [128, 4, 128])
                bPD, bcs = [k.buf(), k.buf()], [k.buf(), k.buf()]
                bx2, bsd, bXQ, bXQb, bt1, bt2, bVb, bnw, bpss, bprot, bpv = (k.buf() for _ in range(11))
                bqo, bvtk = [k.buf(), k.buf()], [k.buf(), k.buf()]
                ds_f = [k.dsem("sp", "f0"), k.dsem("sp", "f1")]
                ds_fw = k.dsem("sp", "fw")
                ds_fo = [k.dsem("pool", "fo0"), k.dsem("pool", "fo1")]
                ds_fv = [k.dsem("pool", "fv0"), k.dsem("pool", "fv1")]
                k.dma(ds_fw, nwc[:, 0:1], qnw[:, :], writes=[bnw])
                k.dma(ds_fw, nwc[:, 1:2], knw[:, :], writes=[bnw])
                ib = 0
                iq = 0
                iv = 0
                for b in range(NB):
                    for (pos0, N, _c) in blocks:
                        s_ = ib % 2
                        ib += 1
                        P = PD[s_]
                        k.dma(ds_f[s_], P[:, :, 0:N], P_d[b, 2048:3584, pos0:pos0 + N].rearrange("(c p) n -> p c n", p=128), writes=[bPD[s_]])
                        k.dma(ds_f[s_], cosb[s_][:, 0:N], ropec[:, pos0:pos0 + N], writes=[bcs[s_]])
                        k.dma(ds_f[s_], sinb[s_][:, 0:N], ropes[:, pos0:pos0 + N], writes=[bcs[s_]])
                        for c in range(8):
                            if c < 4 and pos0 < TCX:
                                continue
                            X = P[:, c, 0:N]
                            wi = 0 if c < 4 else 1
                            k.op("pool", lambda e, X=X, N=N: e.tensor_tensor(out=x2[:, 0:N], in0=X, in1=X, op=ALU.mult), reads=[bPD[s_]], writes=[bx2])
                            k.op("pe", lambda e, N=N: e.matmul(pssf[:, 0:N], bones_bf, x2[:, 0:N], start=True, stop=True), reads=[bx2, b_consts], writes=[bpss])
                            k.op("act", lambda e, N=N: e.activation(out=sdf[:, 0:N], in_=pssf[:, 0:N], func=AF.Sqrt, bias=eps_t[:, 0:1], scale=1.0 / 64),
                                 reads=[bpss, b_consts], writes=[bsd])
                            k.op("dve", lambda e, N=N: e.reciprocal(sdf[:, 0:N], sdf[:, 0:N]), reads=[bsd], writes=[bsd])
                            k.op("dve", lambda e, X=X, N=N, wi=wi: e.scalar_tensor_tensor(out=XQ[:, 0:N], in0=X, scalar=nwc[:, wi:wi + 1], in1=sdf[:, 0:N],
                                                                                         op0=ALU.mult, op1=ALU.mult),
                                 reads=[bPD[s_], bsd, bnw], writes=[bXQ])
                            k.op("act", lambda e, N=N: e.activation(out=XQb[:, 0:N], in_=XQ[:, 0:N], func=AF.Copy), reads=[bXQ], writes=[bXQb])
                            k.op("pe", lambda e, N=N: e.matmul(prot[:, 0:N], rot_bf, XQb[:, 0:N], start=True, stop=True), reads=[bXQb, b_consts], writes=[bprot])
                            k.op("pool", lambda e, N=N, s_=s_: e.tensor_tensor(out=t1f[:, 0:N], in0=XQ[:, 0:N], in1=cosb[s_][:, 0:N], op=ALU.mult),
                                 reads=[bXQ, bcs[s_]], writes=[bt1])
                            k.op("dve", lambda e, N=N, s_=s_: e.tensor_tensor(out=t2f[:, 0:N], in0=prot[:, 0:N], in1=sinb[s_][:, 0:N], op=ALU.mult),
                                 reads=[bprot, bcs[s_]], writes=[bt2])
                            qs = iq % 2
                            iq += 1
                            k.op("dve", lambda e, N=N, qs=qs: e.tensor_tensor(out=qo[qs][:, 0:N], in0=t1f[:, 0:N], in1=t2f[:, 0:N], op=ALU.add),
                                 reads=[bt1, bt2], writes=[bqo[qs]])
                            dst = qT_d[b, c, :, pos0:pos0 + N] if c < 4 else kT_d[b, c - 4, :, pos0:pos0 + N]
                            k.dma(ds_fo[qs], dst, qo[qs][:, 0:N], reads=[bqo[qs]])
                        k.op("act", lambda e, P=P, N=N: e.activation(out=Vb[:, :, 0:N], in_=P[:, 8:12, 0:N], func=AF.Copy), reads=[bPD[s_]], writes=[bVb])
                        for j in range(N // 128):
                            for h in range(4):
                                k.op("pe", lambda e, h=h, j=j: e.matmul(pvf[:, h, :], Vb[:, h, j * 128:(j + 1) * 128], ident_bf, start=True, stop=True),
                                     reads=[bVb, b_consts], writes=[bpv])
                            vs = iv % 2
                            iv += 1
                            k.op("act", lambda e, vs=vs: e.activation(out=vtk[vs][:], in_=pvf[:].rearrange("p h c -> p (h c)"), func=AF.Copy),
                                 reads=[bpv], writes=[bvtk[vs]])
                            p0 = pos0 + j * 128
                            k.dma(ds_fv[vs], vt_d[b, p0:p0 + 128, :], vtk[vs][:], reads=[bvtk[vs]])
                k.phase_end(es)

            with ExitStack() as pes:
                k.phase_begin(pes)
                LAM_INIT = 0.8 - 0.6 * math.exp(-0.3 * 0)
                KT = [k.sb("KT%d" % i, [128, T], BF16) for i in range(2)]
                VT = [k.sb("VT%d" % i, [128, NT, 128], BF16) for i in range(2)]
                QT = [k.sb("QT%d" % i, [128, 512], BF16) for i in range(2)]
                pT = [[k.sb("pT%d%d" % (m, i), [128, 512], BF16) for i in range(2)] for m in range(2)]
                lamt = k.sb("lamt", [1, 256], F32)
                lamw = k.sb("lamw", [1, 136], F32)
                nlamc = k.sb("nlamc", [128, 1], F32)
                slw = k.sb("slw", [128, 1], F32)
                o0 = k.sb("o0", [128, 512], F32)
                o1 = k.sb("o1", [128, 512], F32)
                rz = k.sb("rz", [128, 512], F32)
                od2 = k.sb("od2", [128, 512], BF16)
                res = [k.sb("res%d" % i, [128, 512], BF16) for i in range(2)]
                sT = [[k.ps("sT%d%d" % (m, i), [128, 512]) for i in range(2)] for m in range(2)]
                Oa = [k.ps("Oa%d" % m, [128, 512]) for m in range(2)]
                Za = [k.ps("Za%d" % m, [128, 512]) for m in range(2)]
                bKT, bVT, bQT = [k.buf(), k.buf()], [k.buf(), k.buf()], [k.buf(), k.buf()]
                bpT = [[k.buf(), k.buf()], [k.buf(), k.buf()]]
                bsT = [[k.buf(), k.buf()], [k.buf(), k.buf()]]
                bO, bZ = [k.buf(), k.buf()], [k.buf(), k.buf()]
                blam, bo0, bo1, brz, bod2 = (k.buf() for _ in range(5))
                bres = [k.buf(), k.buf()]
                ds_kv = [k.dsem("sp", "kv0"), k.dsem("sp", "kv1")]
                ds_q = [k.dsem("sp", "q0"), k.dsem("sp", "q1")]
                ds_l = k.dsem("sp", "lam")
                ds_ro = [k.dsem("pool", "ro0"), k.dsem("pool", "ro1")]
                k.dma(ds_l, lamt[:], lamv[:, :], writes=[blam])
                k.dma(ds_l, slw[:], sublnw[:, :], writes=[blam])
                k.op("dve", lambda e: e.tensor_tensor(out=lamw[:, 0:64], in0=lamt[:, 0:64], in1=lamt[:, 64:128], op=ALU.mult), reads=[blam], writes=[blam])
                k.op("dve", lambda e: e.tensor_tensor(out=lamw[:, 64:128], in0=lamt[:, 128:192], in1=lamt[:, 192:256], op=ALU.mult), reads=[blam], writes=[blam])
                k.op("dve", lambda e: e.tensor_reduce(out=lamw[:, 128:129], in_=lamw[:, 0:64], axis=AX.X, op=ALU.add), reads=[blam], writes=[blam])
                k.op("dve", lambda e: e.tensor_reduce(out=lamw[:, 129:130], in_=lamw[:, 64:128], axis=AX.X, op=ALU.add), reads=[blam], writes=[blam])
                k.op("act", lambda e: e.activation(out=lamw[:, 130:132], in_=lamw[:, 128:130], func=AF.Exp), reads=[blam], writes=[blam])
                k.op("dve", lambda e: e.tensor_tensor(out=lamw[:, 132:133], in0=lamw[:, 131:132], in1=lamw[:, 130:131], op=ALU.subtract), reads=[blam], writes=[blam])
                k.op("dve", lambda e: e.tensor_scalar(out=lamw[:, 133:134], in0=lamw[:, 132:133], scalar1=-LAM_INIT, scalar2=None, op0=ALU.add),
                     reads=[blam], writes=[blam])
                k.op("pe", lambda e: e.matmul(Za[0][:, 0:1], consts[0:1, C_ONES:C_ONES + 128], lamw[0:1, 133:134], start=True, stop=True),
                     reads=[blam, b_consts], writes=[bZ[0]])
                k.op("dve", lambda e: e.tensor_copy(nlamc[:], Za[0][:, 0:1]), reads=[bZ[0]], writes=[blam])
                k.op("dve", lambda e: e.tensor_scalar(out=slw[:], in0=slw[:], scalar1=1.0 - LAM_INIT, scalar2=None, op0=ALU.mult), reads=[blam], writes=[blam])
                ih = 0
                iqb = 0
                ipt = [0, 0]
                for b in range(NB):
                    for h in range(4):
                        hs = ih % 2
                        ih += 1
                        k.dma(ds_kv[hs], KT[hs][:], kT_d[b, h, :, :], writes=[bKT[hs]])
                        k.dma(ds_kv[hs], VT[hs][:], vt_d[b, :, h * 128:(h + 1) * 128].rearrange("(n p) c -> p n c", p=128), writes=[bVT[hs]])
                        for (pos0, N) in xblocks:
                            qs = iqb % 2
                            iqb += 1
                            k.dma(ds_q[qs], QT[qs][:, 0:N], qT_d[b, h, :, pos0:pos0 + N], writes=[bQT[qs]])
                            items = [(kt, m) for kt in range(NT) for m in range(2)]
                            slots = []
                            for (kt, m) in items:
                                slots.append(ipt[m] % 2)
                                ipt[m] += 1

                            def score(ix):
                                kt, m = items[ix]
                                i_ = slots[ix]
                                ms = slice(64 * m, 64 * m + 64)
                                k.op("pe", lambda e: e.matmul(sT[m][i_][:, 0:N], KT[hs][ms, kt * 128:(kt + 1) * 128], QT[qs][ms, 0:N],
                                                              start=True, stop=True),
                                     reads=[bKT[hs], bQT[qs]], writes=[bsT[m][i_]])

                            LOOK = 2
                            for ix in range(min(LOOK, len(items))):
                                score(ix)
                            for ix, (kt, m) in enumerate(items):
                                i_ = slots[ix]
                                k.op("act", lambda e, m=m, i_=i_: e.activation(out=pT[m][i_][:, 0:N], in_=sT[m][i_][:, 0:N], func=AF.Exp, scale=0.125),
                                     reads=[bsT[m][i_]], writes=[bpT[m][i_]])
                                if ix + LOOK < len(items):
                                    score(ix + LOOK)
                                k.op("pe", lambda e, m=m, i_=i_, kt=kt: e.matmul(
                                    Oa[m][:, 0:N], VT[hs][:, kt, :], pT[m][i_][:, 0:N], start=(kt == 0), stop=(kt == NT - 1)),
                                    reads=[bVT[hs], bpT[m][i_]], writes=[bO[m]])
                                k.op("pe", lambda e, m=m, i_=i_, kt=kt: e.matmul(
                                    Za[m][:, 0:N], ones_bf, pT[m][i_][:, 0:N], start=(kt == 0), stop=(kt == NT - 1)),
                                    reads=[bpT[m][i_], b_consts], writes=[bZ[m]])
                            k.op("dve", lambda e, N=N: e.reciprocal(rz[:, 0:N], Za[0][:, 0:N]), reads=[bZ[0]], writes=[brz])
                            k.op("dve", lambda e, N=N: e.tensor_tensor(out=o0[:, 0:N], in0=Oa[0][:, 0:N], in1=rz[:, 0:N], op=ALU.mult), reads=[bO[0], brz], writes=[bo0])
                            k.op("dve", lambda e, N=N: e.reciprocal(rz[:, 0:N], Za[1][:, 0:N]), reads=[bZ[1]], writes=[brz])
                            k.op("dve", lambda e, N=N: e.tensor_tensor(out=o1[:, 0:N], in0=Oa[1][:, 0:N], in1=rz[:, 0:N], op=ALU.mult), reads=[bO[1], brz], writes=[bo1])
                            k.op("dve", lambda e, N=N: e.scalar_tensor_tensor(out=o0[:, 0:N], in0=o1[:, 0:N], scalar=nlamc[:, 0:1], in1=o0[:, 0:N],
                                                                               op0=ALU.mult, op1=ALU.add), reads=[bo1, bo0, blam], writes=[bo0])
                            k.op("pool", lambda e, N=N: e.tensor_tensor(out=od2[:, 0:N], in0=o0[:, 0:N], in1=o0[:, 0:N], op=ALU.mult), reads=[bo0], writes=[bod2])
                            k.op("pe", lambda e, N=N: e.matmul(sT[0][0][:, 0:N], ones_bf, od2[:, 0:N], start=True, stop=True),
                                 reads=[bod2, b_consts], writes=[bsT[0][0]])
                            k.op("act", lambda e, N=N: e.activation(out=rz[:, 0:N], in_=sT[0][0][:, 0:N], func=AF.Sqrt, bias=eps_t[:, 0:1], scale=1.0 / 128),
                                 reads=[bsT[0][0], b_consts], writes=[brz])
                            k.op("dve", lambda e, N=N: e.reciprocal(rz[:, 0:N], rz[:, 0:N]), reads=[brz], writes=[brz])
                            rs = iqb % 2
                            k.op("dve", lambda e, N=N, rs=rs: e.scalar_tensor_tensor(out=res[rs][:, 0:N], in0=o0[:, 0:N], scalar=slw[:, 0:1], in1=rz[:, 0:N],
                                                                                     op0=ALU.mult, op1=ALU.mult), reads=[bo0, brz, blam], writes=[bres[rs]])
                            k.dma(ds_ro[rs], cat_d[b, 512 + h * 128:512 + (h + 1) * 128, pos0 - TCX:pos0 - TCX + N], res[rs][:, 0:N], reads=[bres[rs]])
                k.phase_end(es)

        if "G" in phases:
            with ExitStack() as pes:
                k.phase_begin(pes)
                wo_st = k.sb("wo_st", [128, 4, D], F32)
                woutb = k.sb("woutb", [128, 8, D], BF16)
                wrt = k.sb("wrt", [128, 8, 36], F32)
                brt = k.sb("brt", [128, 36], F32)
                xt2 = [k.sb("xt2%d" % i, [128, D], F32) for i in range(2)]
                catT = [k.sb("catT%d" % i, [128, 8, 128], BF16) for i in range(2)]
                tmpo = k.sb("tmpo", [128, D], F32)
                x1 = [k.sb("x1%d" % i, [128, D], F32) for i in range(2)]
                sq2 = k.sb("sq2", [128, D], F32)
                ss2 = k.sb("ss2", [128, 4], F32)
                xn2 = k.sb("xn2", [128, D], F32)
                h2f = k.sb("h2f", [128, 8, 128], F32)
                h2b = [k.sb("h2b%d" % i, [128, 8, 128], BF16) for i in range(2)]
                Lg = k.sb("Lg", [128, 36], F32)
                rt = k.sb("rt", [128, 16], F32)
                goh = k.sb("goh", [128, 4], F32)
                em = k.sb("em", [128, 4, 8], F32)
                em2 = k.sb("em2", [128, 32], F32)
                m1 = k.sb("m1", [128, 32], F32)
                m2 = k.sb("m2", [128, 32], F32)
                Wdt = [k.sb("Wdt%d" % i, [128, 32], F32) for i in range(2)]
                po = [k.ps("po%d" % i, [128, 512]) for i in range(2)]
                ptf = k.ps("ptf", [128, 8, 128])
                pl = k.ps("pl", [128, 36])
                bw, bpo, bptf, bpl, btmp, bsq2, bss2, bxn2, bh2f, bL, brt_ = (k.buf() for _ in range(11))
                bxt, bcatT, bx1, bh2b, bWd = ([k.buf(), k.buf()] for _ in range(5))
                bpo = [k.buf(), k.buf()]
                ds_w = k.dsem("sp", "gw")
                ds_i = [k.dsem("sp", "gi0"), k.dsem("sp", "gi1")]
                ds_o1 = [k.dsem("pool", "go0"), k.dsem("pool", "go1")]
                wov = w_out.rearrange("(kc p) n -> p kc n", p=128)
                for hf in range(2):
                    k.dma(ds_w, wo_st[:], wov[:, hf * 4:(hf + 1) * 4, :], writes=[bw])
                    k.op("dve", lambda e, hf=hf: e.tensor_copy(woutb[:, hf * 4:(hf + 1) * 4, :], wo_st[:]), reads=[bw], writes=[bw])
                k.dma(ds_w, wrt[:], w_rt.rearrange("(kc p) n -> p kc n", p=128), writes=[bw])
                k.dma(ds_w, brt[:], b_rt.partition_broadcast(128), writes=[bw])
                it_ = 0
                for b in range(NB):
                    for xp in range(0, TX, 128):
                        s_ = it_ % 2
                        it_ += 1
                        k.dma(ds_i[s_], xt2[s_][:], seq[b, TCX + xp:TCX + xp + 128, :], writes=[bxt[s_]])
                        k.dma(ds_i[s_], catT[s_][:], cat_d[b, :, xp:xp + 128].rearrange("(c p) t -> p c t", p=128), writes=[bcatT[s_]])
                        for hf in range(2):
                            for kc in range(8):
                                k.op("pe", lambda e, hf=hf, kc=kc, s_=s_: e.matmul(po[hf][:], catT[s_][:, kc, :], woutb[:, kc, hf * 512:(hf + 1) * 512],
                                                                                   start=(kc == 0), stop=(kc == 7)),
                                     reads=[bcatT[s_], bw], writes=[bpo[hf]])
                            hsl = slice(hf * 512, (hf + 1) * 512)
                            k.op("dve", lambda e, hf=hf, hsl=hsl, b=b: e.tensor_tensor(out=tmpo[:, hsl], in0=po[hf][:], in1=G1[:, b, hsl], op=ALU.mult),
                                 reads=[bpo[hf], b_mod_], writes=[btmp])
                            k.op("pool", lambda e, hsl=hsl, s_=s_: e.tensor_tensor(out=x1[s_][:, hsl], in0=tmpo[:, hsl], in1=xt2[s_][:, hsl], op=ALU.add),
                                 reads=[btmp, bxt[s_]], writes=[bx1[s_]])
                        k.dma(ds_o1[s_], x1_d[b, xp:xp + 128, :], x1[s_][:], reads=[bx1[s_]])
                        k.op("act", lambda e, s_=s_: e.activation(out=sq2[:], in_=x1[s_][:], func=AF.Square), reads=[bx1[s_]], writes=[bsq2])
                        k.op("dve", lambda e: e.tensor_reduce(out=ss2[:, 0:1], in_=sq2[:], axis=AX.X, op=ALU.add), reads=[bsq2], writes=[bss2])
                        k.op("act", lambda e: e.activation(out=ss2[:, 1:2], in_=ss2[:, 0:1], func=AF.Sqrt, bias=eps_t[:, 0:1], scale=1.0 / D),
                             reads=[bss2, b_consts], writes=[bss2])
                        k.op("dve", lambda e: e.reciprocal(ss2[:, 2:3], ss2[:, 1:2]), reads=[bss2], writes=[bss2])
                        k.op("dve", lambda e, s_=s_: e.tensor_scalar(out=xn2[:], in0=x1[s_][:], scalar1=ss2[:, 2:3], scalar2=None, op0=ALU.mult),
                             reads=[bx1[s_], bss2], writes=[bxn2])
                        for kc in range(8):
                            k.op("pe", lambda e, kc=kc: e.transpose(out=ptf[:, kc, :], in_=xn2[:, kc * 128:(kc + 1) * 128], identity=consts[:, C_ID:C_ID + 128]),
                                 reads=[bxn2, b_consts], writes=[bptf])
                        for kc in range(8):
                            k.op("act", lambda e, kc=kc, b=b: e.activation(out=h2f[:, kc, :], in_=ptf[:, kc, :], func=AF.Identity,
                                                                           bias=B2[:, kc, b:b + 1], scale=A2[:, kc, b:b + 1]),
                                 reads=[bptf, b_mod_], writes=[bh2f])
                        k.op("pool", lambda e, s_=s_: e.tensor_copy(h2b[s_][:], h2f[:]), reads=[bh2f], writes=[bh2b[s_]])
                        k.dma(ds_o1[s_], h2T_d[b, :, xp:xp + 128].rearrange("(c p) t -> p c t", p=128), h2b[s_][:], reads=[bh2b[s_]])
                        for kc in range(8):
                            k.op("pe", lambda e, kc=kc: e.matmul(pl[:], h2f[:, kc, :], wrt[:, kc, :], start=(kc == 0), stop=(kc == 7)),
                                 reads=[bh2f, bw], writes=[bpl])
                        k.op("dve", lambda e: e.tensor_tensor(out=Lg[:], in0=pl[:], in1=brt[:], op=ALU.add), reads=[bpl, bw], writes=[bL])
                        R_ = [bL, brt_]
                        k.op("dve", lambda e: e.tensor_reduce(out=rt[:, 0:1], in_=Lg[:, 0:4], axis=AX.X, op=ALU.max), reads=R_, writes=[brt_])
                        k.op("dve", lambda e: e.tensor_scalar(out=goh[:], in0=Lg[:, 0:4], scalar1=rt[:, 0:1], scalar2=None, op0=ALU.subtract), reads=R_, writes=[brt_])
                        k.op("act", lambda e: e.activation(out=em2[:, 0:4], in_=goh[:], func=AF.Exp), reads=R_, writes=[brt_])
                        k.op("dve", lambda e: e.tensor_reduce(out=rt[:, 1:2], in_=em2[:, 0:4], axis=AX.X, op=ALU.add), reads=R_, writes=[brt_])
                        k.op("dve", lambda e: e.reciprocal(rt[:, 2:3], rt[:, 1:2]), reads=R_, writes=[brt_])
                        k.op("dve", lambda e: e.tensor_scalar(out=goh[:], in0=Lg[:, 0:4], scalar1=rt[:, 0:1], scalar2=None, op0=ALU.is_equal), reads=R_, writes=[brt_])
                        k.op("dve", lambda e: e.tensor_scalar(out=goh[:], in0=goh[:], scalar1=-1.0, scalar2=1e30, op0=ALU.add, op1=ALU.mult), reads=R_, writes=[brt_])
                        k.op("dve", lambda e: e.tensor_tensor(out=em[:], in0=Lg[:, 4:36].rearrange("p (g x) -> p g x", x=8),
                                                              in1=_bc(goh[:].unsqueeze(2), [128, 4, 8]), op=ALU.add), reads=R_, writes=[brt_])
                        emf = em[:].rearrange("p g x -> p (g x)")
                        k.op("dve", lambda e: e.tensor_reduce(out=rt[:, 3:4], in_=emf, axis=AX.X, op=ALU.max), reads=R_, writes=[brt_])
                        k.op("dve", lambda e: e.tensor_scalar(out=m1[:], in0=emf, scalar1=rt[:, 3:4], scalar2=None, op0=ALU.is_equal), reads=R_, writes=[brt_])
                        k.op("dve", lambda e: e.scalar_tensor_tensor(out=em2[:], in0=m1[:], scalar=-1e30, in1=emf, op0=ALU.mult, op1=ALU.add), reads=R_, writes=[brt_])
                        k.op("dve", lambda e: e.tensor_reduce(out=rt[:, 4:5], in_=em2[:], axis=AX.X, op=ALU.max), reads=R_, writes=[brt_])
                        k.op("dve", lambda e: e.tensor_scalar(out=m2[:], in0=em2[:], scalar1=rt[:, 4:5], scalar2=None, op0=ALU.is_equal), reads=R_, writes=[brt_])
                        k.op("dve", lambda e: e.tensor_tensor(out=rt[:, 5:6], in0=rt[:, 4:5], in1=rt[:, 3:4], op=ALU.subtract), reads=R_, writes=[brt_])
                        k.op("act", lambda e: e.activation(out=rt[:, 6:7], in_=rt[:, 5:6], func=AF.Exp), reads=R_, writes=[brt_])
                        k.op("dve", lambda e: e.tensor_scalar(out=rt[:, 7:8], in0=rt[:, 6:7], scalar1=1.0, scalar2=None, op0=ALU.add), reads=R_, writes=[brt_])
                        k.op("dve", lambda e: e.reciprocal(rt[:, 8:9], rt[:, 7:8]), reads=R_, writes=[brt_])
                        k.op("dve", lambda e: e.tensor_tensor(out=rt[:, 9:10], in0=rt[:, 8:9], in1=rt[:, 2:3], op=ALU.mult), reads=R_, writes=[brt_])
                        k.op("dve", lambda e: e.tensor_tensor(out=rt[:, 10:11], in0=rt[:, 9:10], in1=rt[:, 6:7], op=ALU.mult), reads=R_, writes=[brt_])
                        k.op("dve", lambda e: e.tensor_scalar(out=m1[:], in0=m1[:], scalar1=rt[:, 9:10], scalar2=None, op0=ALU.mult), reads=R_, writes=[brt_])
                        k.op("dve", lambda e, s_=s_: e.scalar_tensor_tensor(out=Wdt[s_][:], in0=m2[:], scalar=rt[:, 10:11], in1=m1[:], op0=ALU.mult, op1=ALU.add),
                             reads=R_, writes=[bWd[s_]])
                        k.dma(ds_o1[s_], wd_d[b, xp:xp + 128, :], Wdt[s_][:], reads=[bWd[s_]])
                k.phase_end(es)

            with ExitStack() as pes:
                k.phase_begin(pes)
                TB = min(1024, TX)
                TBC = TB // 128
                NTB = TB // 512
                h2T = k.sb("h2T", [128, 8, TB], BF16)
                acc = k.sb("acc", [128, TBC, D], F32)
                Wd = k.sb("Wd", [128, TBC, NE], F32)
                x1h = k.sb("x1h", [128, 4, D], F32)
                stg = [k.sb("stg%d" % i, [128, 2048], F32) for i in range(3)]
                wgb = [k.sb("wgb%d" % i, [128, 8, FF], BF16) for i in range(2)]
                wub = [k.sb("wub%d" % i, [128, 8, FF], BF16) for i in range(2)]
                wdb = [k.sb("wdb%d" % i, [128, 4, D], BF16) for i in range(2)]
                sg = [k.sb("sg%d" % i, [128, 512], F32) for i in range(2)]
                hid = [k.sb("hid%d" % i, [128, 4, 512], BF16) for i in range(2)]
                pg = [k.ps("pg%d" % i, [128, 512]) for i in range(2)]
                pu = [k.ps("pu%d" % i, [128, 512]) for i in range(2)]
                py = [k.ps("py%d" % i, [128, 512]) for i in range(2)]
                bh2T, bacc, bWd_, bx1h = (k.buf() for _ in range(4))
                bstg = [k.buf() for _ in range(3)]
                bwg, bwu, bwd_, bsg, bhid, bpg, bpu, bpy = ([k.buf(), k.buf()] for _ in range(8))
                ds_m = k.dsem("sp", "mi")
                ds_s = [k.dsem("sp", "ms%d" % i) for i in range(3)]
                ds_x1 = k.dsem("sp", "mx")
                ds_out = k.dsem("pool", "mo")

                def dsl2(start, size):
                    if isinstance(start, int):
                        return slice(start, start + size)
                    return bass.ds(start, size)

                def moe_body(b):
                    def body(it):
                        off = it * TB
                        k.dma(ds_m, h2T[:], h2T_d[b, :, dsl2(off, TB)].rearrange("(c p) t -> p c t", p=128), writes=[bh2T])
                        k.dma(ds_m, Wd[:], wd_d[b, dsl2(off, TB), :].rearrange("(n p) e -> p n e", p=128), writes=[bWd_])
                        k.op("pool", lambda e: e.memset(acc[:], 0.0), writes=[bacc])
                        ist = 0
                        cnt = [0, 0, 0]
                        for ex in range(NE):
                            ws = ex % 2
                            srcs = []
                            gv = moe_g[ex].rearrange("(kc p) f -> p kc f", p=128)
                            uv = moe_u[ex].rearrange("(kc p) f -> p kc f", p=128)
                            dv = moe_d[ex].rearrange("(fc p) n -> p fc n", p=128)
                            for hf in range(2):
                                srcs.append((gv[:, hf * 4:(hf + 1) * 4, :], wgb[ws][:, hf * 4:(hf + 1) * 4, :], bwg[ws], "p (a f) -> p a f", 4))
                                srcs.append((uv[:, hf * 4:(hf + 1) * 4, :], wub[ws][:, hf * 4:(hf + 1) * 4, :], bwu[ws], "p (a f) -> p a f", 4))
                            for hf in range(2):
                                srcs.append((dv[:, hf * 2:(hf + 1) * 2, :], wdb[ws][:, hf * 2:(hf + 1) * 2, :], bwd_[ws], "p (a f) -> p a f", 2))
                            for (src, dst, bdst, pat, a_) in srcs:
                                si = ist % 3
                                ist += 1
                                k.dma(ds_s[si], stg[si][:].rearrange(pat, a=a_), src, writes=[bstg[si]])
                                k.op("pool", lambda e, si=si, dst=dst, pat=pat, a_=a_: e.tensor_copy(dst, stg[si][:].rearrange(pat, a=a_)),
                                     reads=[bstg[si]], writes=[bdst])
                            for tb in range(NTB):
                                tsl = slice(tb * 512, (tb + 1) * 512)
                                hs_ = cnt[0] % 2
                                cnt[0] += 1
                                for fc in range(4):
                                    pi = cnt[1] % 2
                                    cnt[1] += 1
                                    fsl = slice(fc * 128, (fc + 1) * 128)
                                    for kc in range(8):
                                        k.op("pe", lambda e, pi=pi, ws=ws, kc=kc, fsl=fsl, tsl=tsl: e.matmul(pg[pi][:], wgb[ws][:, kc, fsl], h2T[:, kc, tsl],
                                                                                                             start=(kc == 0), stop=(kc == 7)),
                                             reads=[bwg[ws], bh2T], writes=[bpg[pi]])
                                    for kc in range(8):
                                        k.op("pe", lambda e, pi=pi, ws=ws, kc=kc, fsl=fsl, tsl=tsl: e.matmul(pu[pi][:], wub[ws][:, kc, fsl], h2T[:, kc, tsl],
                                                                                                             start=(kc == 0), stop=(kc == 7)),
                                             reads=[bwu[ws], bh2T], writes=[bpu[pi]])
                                    k.op("act", lambda e, pi=pi: e.activation(out=sg[pi][:], in_=pg[pi][:], func=AF.Silu), reads=[bpg[pi]], writes=[bsg[pi]])
                                    k.op("dve", lambda e, pi=pi, hs_=hs_, fc=fc: e.tensor_tensor(out=hid[hs_][:, fc, :], in0=sg[pi][:], in1=pu[pi][:], op=ALU.mult),
                                         reads=[bsg[pi], bpu[pi]], writes=[bhid[hs_]])
                                for tc in range(4):
                                    ch = tb * 4 + tc
                                    for hf in range(2):
                                        yi = cnt[2] % 2
                                        cnt[2] += 1
                                        for fc in range(4):
                                            k.op("pe", lambda e, yi=yi, hs_=hs_, fc=fc, tc=tc, ws=ws, hf=hf: e.matmul(
                                                py[yi][:], hid[hs_][:, fc, tc * 128:(tc + 1) * 128], wdb[ws][:, fc, hf * 512:(hf + 1) * 512],
                                                start=(fc == 0), stop=(fc == 3)), reads=[bhid[hs_], bwd_[ws]], writes=[bpy[yi]])
                                        k.op("dve", lambda e, yi=yi, ch=ch, hf=hf, ex=ex: e.scalar_tensor_tensor(
                                            out=acc[:, ch, hf * 512:(hf + 1) * 512], in0=py[yi][:], scalar=Wd[:, ch, ex:ex + 1],
                                            in1=acc[:, ch, hf * 512:(hf + 1) * 512], op0=ALU.mult, op1=ALU.add),
                                            reads=[bpy[yi], bWd_, bacc], writes=[bacc])
                        for hq in range(TBC // 4):
                            k.dma(ds_x1, x1h[:], x1_d[b, dsl2(off + hq * 512, 512), :].rearrange("(n p) d -> p n d", p=128), writes=[bx1h])
                            asl = acc[:, hq * 4:(hq + 1) * 4, :]
                            k.op("dve", lambda e, asl=asl: e.tensor_tensor(out=asl, in0=asl, in1=_bc(G2[:, b, :].unsqueeze(1), [128, 4, D]), op=ALU.mult),
                                 reads=[bacc, b_mod_], writes=[bacc])
                            k.op("pool", lambda e, asl=asl: e.tensor_tensor(out=asl, in0=asl, in1=x1h[:], op=ALU.add), reads=[bacc, bx1h], writes=[bacc])
                            k.dma(ds_out, out_d[b, dsl2(off + hq * 512, 512), :].rearrange("(n p) d -> p n d", p=128), asl, reads=[bacc])
                    return body

                for b in range(NB):
                    k.loop(TX // TB, moe_body(b), static=True)
                k.phase_end(es)
        k.barrier()
    return nc, dram


def core_inputs(inp, b0, TX, TCX, shared=None):
    f = lambda a: np.ascontiguousarray(np.asarray(a, np.float32))
    if shared is None:
        shared = {}
        cs, sn = rope_tables(TX, TCX)
        shared["ropec"], shared["ropes"] = cs, sn
        shared["consts"] = make_consts()
        shared["w_mod"] = f(inp["w_mod"][0])
        shared["b_mod"] = f(inp["b_mod"][0]).reshape(1, -1)
        shared["n1w"] = colform(inp["norm1_w"][0], 8)
        shared["n2w"] = colform(inp["norm2_w"][0], 8)
        shared["w_in"] = f(inp["w_in"][0])
        shared["shift_w"] = f(inp["shift_w"][0])
        shared["rw_w0"] = f(np.asarray(inp["rwkv_w0"][0]).reshape(2, 4, 128).transpose(2, 0, 1))
        shared["rw_a0"] = f(np.asarray(inp["rwkv_a0"][0]).reshape(2, 4, 128).transpose(2, 0, 1))
        shared["rw_wup"] = f(inp["rwkv_w_up"][0])
        shared["rw_aup"] = f(inp["rwkv_a_up"][0])
        shared["rw_gup"] = f(inp["rwkv_g_up"][0])
        shared["rw_kk"] = colform(inp["rwkv_k_k"][0], 4)
        shared["rw_ka"] = colform(inp["rwkv_k_a"][0], 4)
        shared["rw_rk"] = colform(np.asarray(inp["rwkv_r_k"][0]).reshape(-1), 4)
        shared["rw_lnw"] = colform(inp["rwkv_ln_w"][0], 4)
        shared["rw_lnb"] = colform(inp["rwkv_ln_b"][0], 4)
        shared["qnw"] = f(np.tile(np.asarray(inp["q_norm_w"][0]), 2).reshape(128, 1))
        shared["knw"] = f(np.tile(np.asarray(inp["k_norm_w"][0]), 2).reshape(128, 1))
        shared["lamv"] = f(np.concatenate([np.asarray(inp[n][0]) for n in ("lam_q1", "lam_k1", "lam_q2", "lam_k2")]).reshape(1, 256))
        shared["sublnw"] = f(np.asarray(inp["subln_w"][0]).reshape(128, 1))
        shared["w_out"] = f(inp["w_out"][0])
        shared["w_rt"] = f(np.concatenate([np.asarray(inp["w_group"][0]), np.asarray(inp["w_expert"][0])], axis=1))
        shared["b_rt"] = f(np.concatenate([np.asarray(inp["b_group"][0]), np.asarray(inp["b_expert"][0])]).reshape(1, 36))
        shared["moe_g"] = f(inp["moe_w_gate"][0])
        shared["moe_u"] = f(inp["moe_w_up"][0])
        shared["moe_d"] = f(inp["moe_w_down"][0])
    m = dict(shared)
    x = np.asarray(inp["x"][b0:b0 + NB], np.float32)
    ctx = np.asarray(inp["ctx"][b0:b0 + NB], np.float32)
    m["seq"] = np.ascontiguousarray(np.concatenate([ctx, x], axis=1))
    cc = np.concatenate([np.asarray(inp["c"][b0:b0 + NB], np.float32), np.asarray(inp["c_ctx"], np.float32)[None]], axis=0)
    m["csT"] = np.ascontiguousarray(cc.reshape(3, 8, 128).transpose(2, 1, 0))
    return m, shared


TX_FULL, TCX_FULL = 4096, 256
_CACHE = {}


def kernel(**inputs):
    inp = {k_: np.asarray(v) for k_, v in inputs.items()}
    B = inp["x"].shape[0]
    ncores = B // NB
    if "nc" not in _CACHE:
        _CACHE["nc"] = build_program(TX_FULL, TCX_FULL)
    nc, dram = _CACHE["nc"]
    in_maps = []
    shared = None
    for c in range(ncores):
        m, shared = core_inputs(inp, c * NB, TX_FULL, TCX_FULL, shared)
        in_maps.append({k_: v for k_, v in m.items() if k_ in dram})
    res = run_bass_kernel_spmd(nc, in_maps, core_ids=list(range(ncores)))
    out = np.concatenate([np.asarray(r["out"]) for r in res.results], axis=0)
    return out.astype(np.float32, copy=False)
```

```python
import copy
import math
from contextlib import ExitStack

import numpy as np
import concourse.bass as bass
import concourse.mybir as mybir
from concourse.bass_utils import run_bass_kernel_spmd

F32 = mybir.dt.float32
BF16 = mybir.dt.bfloat16
AF = mybir.ActivationFunctionType
ALU = mybir.AluOpType
AX = mybir.AxisListType

D = 1024
NB = 2
RW = 512
INW = 3584
NE = 32
FF = 512
SUB = 4


class Buf:
    __slots__ = ("name", "w", "r")

    def __init__(self, name):
        self.name = name
        self.w = None
        self.r = []


class DSem:
    def __init__(self, h, q):
        self.h = h
        self.q = q
        self.total = 0


class K:
    def __init__(self, nc, es):
        self.nc = nc
        self.es = es
        self.E = {"pe": nc.tensor, "act": nc.scalar, "dve": nc.vector, "pool": nc.gpsimd, "sp": nc.sync}
        self.sem = {e: es.enter_context(nc.semaphore("c_" + e)) for e in ("pe", "act", "dve", "pool")}
        self.cnt = {e: 0 for e in self.sem}
        self.seen = {e: {} for e in self.E}
        self.dsems = []
        self.bufs = []
        self.dry = False
        self.inloop = False
        self.used = set()
        self.nbuf = 0
        self.phase_no = 0

    def buf(self, name=None):
        self.nbuf += 1
        b = Buf(name or ("b%d" % self.nbuf))
        self.bufs.append(b)
        return b

    def dsem(self, q, name):
        d = DSem(self.es.enter_context(self.nc.semaphore("d_%s_%d" % (name, self.phase_no))), q)
        self.dsems.append(d)
        return d

    def sb(self, name, shape, dt):
        return self.es.enter_context(self.nc.sbuf_tensor("%s_%d" % (name, self.phase_no), shape, dt))

    def ps(self, name, shape, dt=F32):
        return self.es.enter_context(self.nc.psum_tensor("%s_%d" % (name, self.phase_no), shape, dt))

    def _wait(self, e, tok):
        kind, src, n = tok
        key = src if kind == "E" else id(src)
        if kind == "E" and src == e and e == "pe":
            return
        if kind == "D":
            n = src.total
        if self.seen[e].get(key, -1) >= n:
            return
        self.seen[e][key] = n
        if self.dry:
            self.used.add((e, key))
            return
        h = self.sem[src] if kind == "E" else src.h
        if self.inloop:
            R = self.regs[(e, key)]
            delta = n - self.cur[(e, key)]
            if delta != 0:
                self.E[e].reg_add(R, R, delta)
            self.cur[(e, key)] = n
            self.E[e].wait_ge(h, R)
        else:
            self.E[e].wait_ge(h, n)

    def _sync(self, e, reads, writes):
        best = {}
        def add(tok):
            kind, src, n = tok
            key = (kind, src if kind == "E" else id(src))
            if key not in best or best[key][2] < n:
                best[key] = tok
        for b in reads:
            if b.w is not None:
                add(b.w)
        for b in writes:
            if b.w is not None:
                add(b.w)
            for t in b.r:
                add(t)
        for key in sorted(best, key=str):
            self._wait(e, best[key])

    def op(self, e, fn, reads=(), writes=()):
        self._sync(e, reads, writes)
        self.cnt[e] += 1
        tok = ("E", e, self.cnt[e])
        if not self.dry:
            fn(self.E[e]).then_inc(self.sem[e], 1)
        self.seen[e][e] = max(self.seen[e].get(e, -1), 0)
        for b in reads:
            b.r = [t for t in b.r if not (t[0] == "E" and t[1] == e)] + [tok]
        for b in writes:
            b.w = tok
            b.r = []
        return tok

    def dma(self, ds, out, in_, reads=(), writes=()):
        q = ds.q
        self._sync(q, reads, writes)
        ds.total += 16
        tok = ("D", ds, ds.total)
        if not self.dry:
            self.E[q].dma_start(out=out, in_=in_).then_inc(ds.h, 16)
        for b in reads:
            b.r.append(tok)
        for b in writes:
            b.w = tok
            b.r = []
        return tok

    def drain(self):
        for d in self.dsems:
            if d.total > 0:
                self._wait(d.q, ("D", d, d.total))

    def barrier(self):
        self.drain()
        if not self.dry:
            self.nc.all_engine_barrier()
        for b in self.bufs:
            b.w = None
            b.r = []

    def _keycount(self, key):
        if isinstance(key, str):
            return self.cnt[key]
        for d in self.dsems:
            if id(d) == key:
                return d.total
        raise KeyError(key)

    def _snap_bufs(self, shift):
        def sh(tok):
            kind, src, n = tok
            return (kind, src, n - shift[src if kind == "E" else id(src)])
        return [(None if b.w is None else sh(b.w), [sh(t) for t in b.r]) for b in self.bufs]

    def _load_bufs(self, states):
        for b, (w, r) in zip(self.bufs, states):
            b.w = w
            b.r = list(r)

    def loop(self, n_iter, body, static=False):
        self.barrier()
        for e in self.seen:
            self.seen[e] = {}
        if n_iter == 1 or static:
            for i in range(n_iter):
                body(i)
                self.barrier()
            return
        c0 = dict(self.cnt)
        d0 = [d.total for d in self.dsems]
        nb0 = len(self.bufs)

        def rewind():
            self.cnt = dict(c0)
            for d, t in zip(self.dsems, d0):
                d.total = t
            for e in self.seen:
                self.seen[e] = {}

        self.dry = True
        self.used = set()
        body(0)
        P = {e: self.cnt[e] - c0[e] for e in self.cnt}
        for d, t in zip(self.dsems, d0):
            P[id(d)] = d.total - t
        carried = self._snap_bufs(P)
        rewind()
        self._load_bufs(carried)
        self.used = set()
        body(0)
        self.drain()
        used = sorted(self.used, key=str)
        rewind()
        self._load_bufs(carried)
        self.dry = False
        self.regs = {}
        self.cur = {}
        base = {}
        for (e, key) in used:
            self.nbuf += 1
            R = self.E[e].alloc_register("w_%s_%d" % (e, self.nbuf))
            base[(e, key)] = self._keycount(key)
            self.E[e].reg_mov(R, base[(e, key)])
            self.regs[(e, key)] = R
            self.cur[(e, key)] = base[(e, key)]
        with self.nc.Fori(0, n_iter, hint_back_edge=True) as it:
            self.inloop = True
            body(it)
            for (e, key) in used:
                delta = base[(e, key)] + P[key] - self.cur[(e, key)]
                if delta != 0:
                    self.E[e].reg_add(self.regs[(e, key)], self.regs[(e, key)], delta)
            self.inloop = False
        for (e, key) in used:
            self.E[e].free_register(self.regs[(e, key)])
        for e in self.cnt:
            self.cnt[e] += (n_iter - 1) * P[e]
        for d in self.dsems:
            d.total += (n_iter - 1) * P[id(d)]
        shift = {key: -(n_iter - 1) * P[key] for key in P}
        self._load_bufs(self._snap_bufs(shift))
        for e in self.seen:
            self.seen[e] = {}
        self.barrier()

    def phase_begin(self, pes):
        self.es = pes
        self.phase_no += 1
        self._mark = (len(self.dsems), len(self.bufs))

    def phase_end(self, es):
        self.barrier()
        self.es = es
        del self.dsems[self._mark[0]:]
        del self.bufs[self._mark[1]:]


def _bc(ap, shape):
    return ap.to_broadcast(shape)


C_ID = 0
C_IDA = 128
C_IDB = 256
C_BONES = 384
C_ONES = 512
C_ROT = 640
C_SEL = 768
C_ID3 = 1152
NCONST = 1160


def make_consts():
    c = np.zeros((128, NCONST), np.float32)
    c[:, C_ID:C_ID + 128] = np.eye(128)
    c[:64, C_IDA:C_IDA + 64] = np.eye(64)
    c[64:, C_IDB + 64:C_IDB + 128] = np.eye(64)
    c[:64, C_BONES:C_BONES + 64] = 1.0
    c[64:, C_BONES + 64:C_BONES + 128] = 1.0
    c[:, C_ONES:C_ONES + 128] = 1.0
    R = np.zeros((128, 128), np.float32)
    for blk in range(2):
        o = blk * 64
        for i in range(16):
            R[o + 16 + i, o + i] = -1.0
            R[o + i, o + 16 + i] = 1.0
            R[o + 48 + i, o + 32 + i] = -1.0
            R[o + 32 + i, o + 48 + i] = 1.0
    c[:, C_ROT:C_ROT + 128] = R
    for b in range(3):
        c[b, C_SEL + b * 128:C_SEL + (b + 1) * 128] = 1.0
    c[:3, C_ID3:C_ID3 + 3] = np.eye(3)
    return c


def rope_tables(TX, TCX):
    T = TX + TCX
    rows = TX // 64
    row_id = np.repeat(np.arange(rows), 64).astype(np.float32)
    col_id = np.tile(np.arange(64), rows).astype(np.float32)
    inv = (10000.0 ** (-np.arange(0, 32, 2, dtype=np.float32) / 32)).astype(np.float32)
    ar = row_id[:, None] * inv
    ac = col_id[:, None] * inv
    ang = np.concatenate([ar, ar, ac, ac], axis=-1)
    cos = np.ones((T, 64), np.float32)
    sin = np.zeros((T, 64), np.float32)
    cos[TCX:] = np.cos(ang)
    sin[TCX:] = np.sin(ang)
    cs = np.concatenate([cos.T, cos.T], axis=0)
    sn = np.concatenate([sin.T, sin.T], axis=0)
    return np.ascontiguousarray(cs), np.ascontiguousarray(sn)


def colform(v, n):
    return np.ascontiguousarray(np.asarray(v, np.float32).reshape(n, 128).T)


def geom(TX, TCX):
    T = TX + TCX
    NT = T // 128
    TP = T + 4
    blocks = []
    p = 0
    while p < TCX:
        n = min(512, TCX - p)
        blocks.append((p, n, p + 1))
        p += n
    p = 0
    while p < TX:
        n = min(512, TX - p)
        blocks.append((TCX + p, n, TCX + 3 + p))
        p += n
    return T, NT, TP, blocks


def build_program(TX, TCX, phases="ABCDEFG", debug=()):
    T, NT, TP, blocks = geom(TX, TCX)
    NTC = TCX // 128
    nc = bass.Bass("TRN2", target_bir_lowering=False)
    dram = {}

    def din(name, shape, dt=F32):
        dram[name] = nc.dram_tensor(name, list(shape), dt, kind="ExternalInput").ap()
        return dram[name]

    def dscr(name, shape, dt=F32):
        kind = "ExternalOutput" if name in debug else "Internal"
        dram[name] = nc.dram_tensor(name, list(shape), dt, kind=kind).ap()
        return dram[name]

    seq = din("seq", [NB, T, D])
    csT = din("csT", [128, 8, 3])
    consts_d = din("consts", [128, NCONST])
    w_mod = din("w_mod", [D, 6 * D])
    b_mod = din("b_mod", [1, 6 * D])
    n1w = din("n1w", [128, 8])
    n2w = din("n2w", [128, 8])
    w_in = din("w_in", [D, INW])
    shift_w = din("shift_w", [3, 2048])
    rw_w0 = din("rw_w0", [128, 2, 4])
    rw_a0 = din("rw_a0", [128, 2, 4])
    rw_wup = din("rw_wup", [2, 64, RW])
    rw_aup = din("rw_aup", [2, 64, RW])
    rw_gup = din("rw_gup", [2, 128, RW])
    rw_kk = din("rw_kk", [128, 4])
    rw_ka = din("rw_ka", [128, 4])
    rw_rk = din("rw_rk", [128, 4])
    rw_lnw = din("rw_lnw", [128, 4])
    rw_lnb = din("rw_lnb", [128, 4])
    qnw = din("qnw", [128, 1])
    knw = din("knw", [128, 1])
    lamv = din("lamv", [1, 256])
    sublnw = din("sublnw", [128, 1])
    w_out = din("w_out", [D, D])
    w_rt = din("w_rt", [D, 36])
    b_rt = din("b_rt", [1, 36])
    moe_g = din("moe_g", [NE, D, FF])
    moe_u = din("moe_u", [NE, D, FF])
    moe_d = din("moe_d", [NE, FF, D])
    ropec = din("ropec", [128, T])
    ropes = din("ropes", [128, T])
    out_d = nc.dram_tensor("out", [NB, TX, D], F32, kind="ExternalOutput").ap()

    P_d = dscr("P_d", [NB, INW, T])
    NCH = T // 16
    cols_d = dscr("cols_d", [2, 128, NCH + 1, NB, 4, 16, 6], BF16)
    rows_d = dscr("rows_d", [2, 6, T, NB * 4, 128], BF16)
    v_d = dscr("v_d", [2, T, NB * 4, 64], BF16)
    g_d = dscr("g_d", [2, NB, 4, 128, T], BF16)
    bon_d = dscr("bon_d", [2, NB, 4, 128, T], BF16)
    y_d = dscr("y_d", [2, 2, T + 2, NB * 4, 64], BF16)
    qT_d = dscr("qT_d", [NB, 4, 128, T], BF16)
    kT_d = dscr("kT_d", [NB, 4, 128, T], BF16)
    vt_d = dscr("vt_d", [NB, T, 512], BF16)
    cat_d = dscr("cat_d", [NB, D, TX], BF16)
    x1_d = dscr("x1_d", [NB, TX, D])
    h2T_d = dscr("h2T_d", [NB, D, TX], BF16)
    wd_d = dscr("wd_d", [NB, TX, NE])

    with ExitStack() as es:
        k = K(nc, es)
        consts = k.sb("consts_sb", [128, NCONST], F32)
        cbf = k.sb("cbf", [128, 768], BF16)
        modT = k.sb("modT", [128, 48, 3], F32)
        A1 = k.sb("A1", [128, 8, 3], F32)
        A2 = k.sb("A2", [128, 8, 3], F32)
        G1 = k.sb("G1", [128, NB, D], F32)
        G2 = k.sb("G2", [128, NB, D], F32)
        eps_t = k.sb("eps_t", [128, 1], F32)
        b_consts = k.buf("consts")
        b_mod_ = k.buf("mod")
        ds_c = k.dsem("sp", "c")
        k.dma(ds_c, consts[:], consts_d[:, :], writes=[b_consts])
        k.op("dve", lambda e: e.tensor_copy(cbf[:], consts[:, 0:768]), reads=[b_consts], writes=[b_consts])
        k.op("dve", lambda e: e.memset(eps_t[:], 1e-6), writes=[b_consts])
        ident_bf = cbf[:, C_ID:C_ID + 128]
        identA_bf = cbf[:, C_IDA:C_IDA + 128]
        identB_bf = cbf[:, C_IDB:C_IDB + 128]
        bones_bf = cbf[:, C_BONES:C_BONES + 128]
        ones_bf = cbf[:, C_ONES:C_ONES + 128]
        rot_bf = cbf[:, C_ROT:C_ROT + 128]
        k.barrier()

        if "A" in phases:
            with ExitStack() as pes:
                k.phase_begin(pes)
                silT = k.sb("silT", [128, 8, 3], F32)
                modrow = k.sb("modrow", [3, 6 * D], F32)
                bmr = k.sb("bmr", [3, 6 * D], F32)
                n1c = k.sb("n1c", [128, 8], F32)
                n2c = k.sb("n2c", [128, 8], F32)
                wm = [k.sb("wm%d" % i, [128, 8, 1024], F32) for i in range(2)]
                pa = [k.ps("pa%d" % i, [3, 512]) for i in range(2)]
                pc = k.ps("pc", [128, 48, 3])
                pg = [k.ps("pg%d" % i, [128, 512]) for i in range(2)]
                b_sil, b_bmr, b_pc = k.buf(), k.buf(), k.buf()
                b_wm = [k.buf(), k.buf()]
                b_pa = [k.buf(), k.buf()]
                b_pg = [k.buf(), k.buf()]
                ds_a = k.dsem("sp", "a")
                ds_w = [k.dsem("sp", "wm0"), k.dsem("sp", "wm1")]
                k.dma(ds_a, silT[:], csT[:, :, :], writes=[b_sil])
                k.dma(ds_a, bmr[:], b_mod.partition_broadcast(3), writes=[b_bmr])
                k.dma(ds_a, n1c[:], n1w[:, :], writes=[b_bmr])
                k.dma(ds_a, n2c[:], n2w[:, :], writes=[b_bmr])
                k.op("act", lambda e: e.activation(out=silT[:], in_=silT[:], func=AF.Silu), reads=[b_sil], writes=[b_sil])
                wmv = w_mod.rearrange("(kc p) n -> p kc n", p=128)
                for m in range(6):
                    s = m % 2
                    k.dma(ds_w[s], wm[s][:], wmv[:, :, m * 1024:(m + 1) * 1024], writes=[b_wm[s]])
                    for blk in range(2):
                        pb = (m * 2 + blk) % 2
                        for kc in range(8):
                            k.op("pe", lambda e, kc=kc, s=s, blk=blk, pb=pb: e.matmul(
                                pa[pb][:], silT[:, kc, :], wm[s][:, kc, blk * 512:(blk + 1) * 512],
                                start=(kc == 0), stop=(kc == 7)), reads=[b_sil, b_wm[s]], writes=[b_pa[pb]])
                        c0 = m * 1024 + blk * 512
                        k.op("dve", lambda e, pb=pb, c0=c0: e.tensor_tensor(
                            out=modrow[:, c0:c0 + 512], in0=pa[pb][:], in1=bmr[:, c0:c0 + 512], op=ALU.add),
                            reads=[b_pa[pb], b_bmr], writes=[b_mod_])
                for f in range(48):
                    k.op("pe", lambda e, f=f: e.matmul(pc[:, f, :], modrow[0:3, f * 128:(f + 1) * 128],
                                                      consts[0:3, C_ID3:C_ID3 + 3], start=True, stop=True),
                         reads=[b_mod_, b_consts], writes=[b_pc])
                k.op("dve", lambda e: e.tensor_copy(modT[:], pc[:]), reads=[b_pc], writes=[b_mod_])
                k.op("dve", lambda e: e.scalar_tensor_tensor(
                    out=A1[:], in0=modT[:, 8:16, :], scalar=1.0, in1=_bc(n1c[:].unsqueeze(2), [128, 8, 3]),
                    op0=ALU.add, op1=ALU.mult), reads=[b_mod_, b_bmr], writes=[b_mod_])
                k.op("dve", lambda e: e.scalar_tensor_tensor(
                    out=A2[:], in0=modT[:, 32:40, :], scalar=1.0, in1=_bc(n2c[:].unsqueeze(2), [128, 8, 3]),
                    op0=ALU.add, op1=ALU.mult), reads=[b_mod_, b_bmr], writes=[b_mod_])
                i = 0
                for gi, Gt in ((2, G1), (5, G2)):
                    for b in range(NB):
                        for blk in range(2):
                            pb = i % 2
                            i += 1
                            c0 = gi * 1024 + blk * 512
                            k.op("pe", lambda e, b=b, c0=c0, pb=pb: e.matmul(
                                pg[pb][:], consts[0:3, C_SEL + b * 128:C_SEL + (b + 1) * 128],
                                modrow[0:3, c0:c0 + 512], start=True, stop=True),
                                reads=[b_mod_, b_consts], writes=[b_pg[pb]])
                            k.op("act", lambda e, Gt=Gt, b=b, blk=blk, pb=pb: e.activation(
                                out=Gt[:, b, blk * 512:(blk + 1) * 512], in_=pg[pb][:], func=AF.Copy),
                                reads=[b_pg[pb]], writes=[b_mod_])
                k.barrier()
                k.phase_end(es)
        B1 = modT[:, 0:8, :]
        B2 = modT[:, 24:32, :]

        if "dbgA" in debug:
            pass

        if "B" in phases:
            with ExitStack() as pes:
                k.phase_begin(pes)
                hT = k.sb("hT", [128, 8, TP], BF16)
                xt = [k.sb("xt%d" % i, [128, D], F32) for i in range(2)]
                sq = k.sb("sq", [128, D], F32)
                ss = k.sb("ss", [128, 4], F32)
                xn = [k.sb("xn%d" % i, [128, D], BF16) for i in range(2)]
                pt = [k.ps("pt%d" % i, [128, 8, 128], BF16) for i in range(2)]
                wst = [k.sb("wst%d" % i, [128, 8, 128], F32) for i in range(2)]
                wbf = [k.sb("wbf%d" % i, [128, 8, 3, 128], BF16) for i in range(2)]
                swb = k.sb("swb", [128, 3, 2048], F32)
                pp = [k.ps("pp%d" % i, [128, 512]) for i in range(4)]
                ev = [k.sb("ev%d" % i, [128, 512], F32) for i in range(4)]
                b_hT = k.buf("hT")
                b_xt, b_xn, b_pt = [k.buf(), k.buf()], [k.buf(), k.buf()], [k.buf(), k.buf()]
                b_sq, b_ss, b_swb = k.buf(), k.buf(), k.buf()
                b_wst, b_wbf = [k.buf(), k.buf()], [k.buf(), k.buf()]
                b_pp, b_ev = [k.buf() for _ in range(4)], [k.buf() for _ in range(4)]
                ds_x = [k.dsem("sp", "x0"), k.dsem("sp", "x1")]
                ds_ws = [k.dsem("sp", "ws0"), k.dsem("sp", "ws1")]
                ds_sw = k.dsem("sp", "sw")
                ds_ev = [k.dsem("pool", "ev%d" % i) for i in range(4)]
                k.dma(ds_sw, swb[:].rearrange("p a b -> p (a b)"),
                      shift_w.rearrange("a b -> (a b)").unsqueeze(0).partition_broadcast(128)
                      if False else shift_w.rearrange("(o a) b -> o (a b)", o=1).partition_broadcast(128),
                      writes=[b_swb])
                k.op("pool", lambda e: e.memset(hT[:], 0.0), writes=[b_hT])
                w_in_v = w_in.rearrange("(kc p) n -> p kc n", p=128)
                for b in range(NB):
                    for q in range(NT):
                        s = q % 2
                        sel = 2 if q < NTC else b
                        col0 = (q * 128 + 1) if q < NTC else (q * 128 + 3)
                        k.dma(ds_x[s], xt[s][:], seq[b, q * 128:(q + 1) * 128, :], writes=[b_xt[s]])
                        k.op("act", lambda e, s=s: e.activation(out=sq[:], in_=xt[s][:], func=AF.Square),
                             reads=[b_xt[s]], writes=[b_sq])
                        k.op("dve", lambda e: e.tensor_reduce(out=ss[:, 0:1], in_=sq[:], axis=AX.X, op=ALU.add),
                             reads=[b_sq], writes=[b_ss])
                        k.op("act", lambda e: e.activation(out=ss[:, 1:2], in_=ss[:, 0:1], func=AF.Sqrt,
                                                           bias=eps_t[:, 0:1], scale=1.0 / D),
                             reads=[b_ss, b_consts], writes=[b_ss])
                        k.op("dve", lambda e: e.reciprocal(ss[:, 2:3], ss[:, 1:2]), reads=[b_ss], writes=[b_ss])
                        k.op("dve", lambda e, s=s: e.tensor_scalar(out=xn[s][:], in0=xt[s][:], scalar1=ss[:, 2:3],
                                                                   scalar2=None, op0=ALU.mult),
                             reads=[b_xt[s], b_ss], writes=[b_xn[s]])
                        for kc in range(8):
                            k.op("pe", lambda e, s=s, kc=kc: e.transpose(out=pt[s][:, kc, :],
                                                                         in_=xn[s][:, kc * 128:(kc + 1) * 128],
                                                                         identity=ident_bf),
                                 reads=[b_xn[s], b_consts], writes=[b_pt[s]])
                        for kc in range(8):
                            k.op("act", lambda e, s=s, kc=kc, sel=sel, col0=col0: e.activation(
                                out=hT[:, kc, col0:col0 + 128], in_=pt[s][:, kc, :], func=AF.Identity,
                                bias=B1[:, kc, sel:sel + 1], scale=A1[:, kc, sel:sel + 1]),
                                reads=[b_pt[s], b_mod_], writes=[b_hT])
                    ie = 0
                    for c in range(28):
                        s = c % 2
                        k.dma(ds_ws[s], wst[s][:], w_in_v[:, :, c * 128:(c + 1) * 128], writes=[b_wst[s]])
                        ntap = 3 if c < 16 else 1
                        if c < 16:
                            for j in range(3):
                                eng = "pool" if j == 1 else "dve"
                                k.op(eng, lambda e, s=s, j=j, c=c: e.tensor_tensor(
                                    out=wbf[s][:, :, j, :], in0=wst[s][:],
                                    in1=_bc(swb[:, j, c * 128:(c + 1) * 128].unsqueeze(1), [128, 8, 128]),
                                    op=ALU.mult), reads=[b_wst[s], b_swb], writes=[b_wbf[s]])
                        else:
                            k.op("dve", lambda e, s=s: e.tensor_copy(wbf[s][:, :, 1, :], wst[s][:]),
                                 reads=[b_wst[s]], writes=[b_wbf[s]])
                        for (pos0, N, colb) in blocks:
                            pi = ie % 4
                            ie += 1
                            taps = (0, 1, 2) if c < 16 else (1,)
                            nmm = len(taps) * 8
                            im = 0
                            for j in taps:
                                for kc in range(8):
                                    k.op("pe", lambda e, pi=pi, s=s, kc=kc, j=j, colb=colb, N=N, im=im, nmm=nmm: e.matmul(
                                        pp[pi][:, 0:N], wbf[s][:, kc, j, :], hT[:, kc, colb + j - 1:colb + j - 1 + N],
                                        start=(im == 0), stop=(im == nmm - 1)),
                                        reads=[b_wbf[s], b_hT], writes=[b_pp[pi]])
                                    im += 1
                            eng = "act" if pi % 2 == 0 else "dve"
                            if eng == "act":
                                k.op("act", lambda e, pi=pi, N=N: e.activation(out=ev[pi][:, 0:N], in_=pp[pi][:, 0:N], func=AF.Copy),
                                     reads=[b_pp[pi]], writes=[b_ev[pi]])
                            else:
                                k.op("dve", lambda e, pi=pi, N=N: e.tensor_copy(ev[pi][:, 0:N], pp[pi][:, 0:N]),
                                     reads=[b_pp[pi]], writes=[b_ev[pi]])
                            k.dma(ds_ev[pi], P_d[b, c * 128:(c + 1) * 128, pos0:pos0 + N], ev[pi][:, 0:N],
                                  reads=[b_ev[pi]])
                    k.barrier()
                k.phase_end(es)

        if "C" in phases:
            with ExitStack() as pes:
                k.phase_begin(pes)
                PB = [k.sb("PB%d" % i, [128, 16, 512], F32) for i in range(2)]
                b_PB = [k.buf(), k.buf()]
                ds_pb = [k.dsem("sp", "pb0"), k.dsem("sp", "pb1")]
                ds_st = k.dsem("sp", "cst")
                ds_o = [k.dsem("pool", "co%d" % i) for i in range(4)]
                wst_c = k.sb("wst_c", [128, 512], F32)
                Wwa = k.sb("Wwa", [128, 2, 512], BF16)
                Wg = k.sb("Wg", [128, 2, 512], BF16)
                colc = k.sb("colc", [128, 40], F32)
                b_w = k.buf("cw")
                for d in range(2):
                    k.dma(ds_st, wst_c[0:64, :], rw_wup[d], writes=[b_w])
                    k.dma(ds_st, wst_c[64:128, :], rw_aup[d], writes=[b_w])
                    k.op("dve", lambda e, d=d: e.tensor_copy(Wwa[:, d, :], wst_c[:]), reads=[b_w], writes=[b_w])
                    k.dma(ds_st, wst_c[:], rw_gup[d], writes=[b_w])
                    k.op("dve", lambda e, d=d: e.tensor_copy(Wg[:, d, :], wst_c[:]), reads=[b_w], writes=[b_w])
                k.dma(ds_st, colc[:, 0:8], rw_w0.rearrange("p a b -> p (a b)"), writes=[b_w])
                k.dma(ds_st, colc[:, 8:16], rw_a0.rearrange("p a b -> p (a b)"), writes=[b_w])
                k.dma(ds_st, colc[:, 16:20], rw_kk[:, :], writes=[b_w])
                k.dma(ds_st, colc[:, 20:24], rw_ka[:, :], writes=[b_w])
                k.dma(ds_st, colc[:, 24:28], rw_rk[:, :], writes=[b_w])
                k.op("dve", lambda e: e.tensor_scalar(out=colc[:, 28:32], in0=colc[:, 20:24], scalar1=-1.0, scalar2=1.0,
                                                      op0=ALU.mult, op1=ALU.add), reads=[b_w], writes=[b_w])
                Vbf = k.sb("Vbf", [128, 4, 512], BF16)
                RH = k.sb("RH", [128, 4, 514], F32)
                CAx = k.sb("CAx", [128, 6], BF16)
                b_RH = k.buf("RH")
                ds_rh = k.dsem("sp", "rh")
                k.op("pool", lambda e: e.memset(CAx[:], 0.0), writes=[b_RH])
                TLs = [k.sb("TL%d" % i, [128, 512], BF16) for i in range(2)]
                SGs = [k.sb("SG%d" % i, [128, 512], BF16) for i in range(2)]
                f32ts = [{n: k.sb("c_%s%d" % (n, i), [128, 512], F32) for n in ("sgw", "dec", "Aa", "kkf", "sd", "kkn", "tmpk", "kd")} for i in range(2)]
                bfts = [{n: k.sb("c_%s%d" % (n, i), [128, 512], BF16) for n in ("Gg", "kk2", "bsc", "kdb", "rkr", "bon")} for i in range(2)]
                CAb = k.sb("CAb", [128, 32, 4, 16, 6], BF16)
                CAw = CAb[:].bitcast(F32)
                bCA = k.buf("CAb")
                rowsbs = [k.sb("rowsb%d" % i, [128, 4, 128], BF16) for i in range(2)]
                vrow = k.sb("vrow", [128, 4, 128], BF16)
                pw, pa_, pg_, pss, pbo = (k.ps(n, [128, 512]) for n in ("pw", "pa_", "pg_", "pss", "pbo"))
                prow = k.ps("prow", [128, 4, 128])
                pv = k.ps("pv", [128, 4, 128])
                bbs = [{n: k.buf(n) for n in ("TL", "SG", "sgw", "dec", "Aa", "kkf", "sd", "kkn", "tmpk", "kd", "Gg", "kk2",
                                              "bsc", "kdb", "rkr", "bon", "CA", "rowsb")} for i in range(2)]
                bbg = {n: k.buf(n) for n in ("Vbf", "vrow", "pw", "pa_", "pg_", "pss", "pbo", "prow", "pv")}
                for i in range(2):
                    bbs[i].update(bbg)
                bb = bbs[0]
                k.op("pool", lambda e: e.memset(CAb[:], 0.0), writes=[bCA])
                ihp = 0
                idd = 0
                irow = 0
                ztile = k.sb("ztile", [128, 1024], BF16)
                b_z = k.buf("z")
                k.op("pool", lambda e: e.memset(ztile[:], 0.0), writes=[b_z])
                for d in range(2):
                    zv = rows_d[d, 2:4].rearrange("r t s c -> (r t) (s c)")
                    for i in range(2 * T // 128):
                        k.dma(ds_o[i % 4], zv[i * 128:(i + 1) * 128, :], ztile[:], reads=[b_z])
                ib = 0
                for b in range(NB):
                    for (pos0, N, _c) in blocks:
                        s = ib % 2
                        ib += 1
                        P = PB[s]
                        bP = b_PB[s]
                        k.dma(ds_pb[s], P[:, :, 0:N], P_d[b, 0:2048, pos0:pos0 + N].rearrange("(c p) n -> p c n", p=128),
                              writes=[bP])
                        k.op("pool", lambda e: e.memset(RH[:], 0.0), writes=[b_RH])
                        lo = max(pos0 - 1, 0)
                        hi = min(pos0 + N + 1, T)
                        co = lo - (pos0 - 1)
                        k.dma(ds_rh, RH[:, :, co:co + hi - lo], P_d[b, 0:512, lo:hi].rearrange("(c p) n -> p c n", p=128), writes=[b_RH])
                        k.op("pool", lambda e, P=P, N=N: e.tensor_copy(Vbf[:, :, 0:N], P[:, 8:12, 0:N]), reads=[bP], writes=[bb["Vbf"]])
                        for j in range(N // 128):
                            for hp in range(4):
                                k.op("pe", lambda e, hp=hp, j=j: e.matmul(pv[:, hp, :], Vbf[:, hp, j * 128:(j + 1) * 128], ident_bf,
                                                                         start=True, stop=True),
                                     reads=[bb["Vbf"], b_consts], writes=[bb["pv"]])
                            k.op("act", lambda e: e.activation(out=vrow[:], in_=pv[:], func=AF.Copy), reads=[bb["pv"]], writes=[bb["vrow"]])
                            p0 = pos0 + j * 128
                            for ab in range(2):
                                k.dma(ds_o[ab], v_d[ab, p0:p0 + 128, b * 4:(b + 1) * 4, :], vrow[:, :, ab * 64:(ab + 1) * 64],
                                      reads=[bb["vrow"]])
                        for d in range(2):
                            TL = TLs[idd % 2]
                            SG = SGs[idd % 2]
                            bbd = bbs[idd % 2]
                            idd += 1
                            k.op("act", lambda e, P=P, N=N, d=d, TL=TL: e.activation(out=TL[0:64, 0:N], in_=P[0:64, 12 + 2 * d, 0:N], func=AF.Tanh),
                                 reads=[bP], writes=[bbd["TL"]])
                            k.op("dve", lambda e, P=P, N=N, d=d: e.tensor_copy(TL[64:128, 0:N], P[64:128, 12 + 2 * d, 0:N]),
                                 reads=[bP], writes=[bbd["TL"]])
                            k.op("act", lambda e, P=P, N=N, d=d: e.activation(out=SG[:, 0:N], in_=P[:, 13 + 2 * d, 0:N], func=AF.Sigmoid),
                                 reads=[bP], writes=[bbd["SG"]])
                            for hp in range(4):
                                hs = slice(hp * 128, (hp + 1) * 128)
                                t = f32ts[ihp % 2]
                                u = bfts[ihp % 2]
                                bb = dict(bbs[ihp % 2])
                                bb["TL"] = bbd["TL"]
                                bb["SG"] = bbd["SG"]
                                bb["CA"] = bCA
                                nch = N // 16
                                c16 = lambda ap: ap.rearrange("p (c s) -> p c s", s=16)
                                ihp += 1
                                k.op("pe", lambda e, d=d, hs=hs, N=N: e.matmul(pw[:, 0:N], Wwa[0:64, d, hs], TL[0:64, 0:N], start=True, stop=True),
                                     reads=[b_w, bb["TL"]], writes=[bb["pw"]])
                                k.op("pe", lambda e, d=d, hs=hs, N=N: e.matmul(pa_[:, 0:N], Wwa[64:128, d, hs], TL[64:128, 0:N], start=True, stop=True),
                                     reads=[b_w, bb["TL"]], writes=[bb["pa_"]])
                                k.op("pe", lambda e, d=d, hs=hs, N=N: e.matmul(pg_[:, 0:N], Wg[:, d, hs], SG[:, 0:N], start=True, stop=True),
                                     reads=[b_w, bb["SG"]], writes=[bb["pg_"]])
                                ci = d * 4 + hp
                                k.op("act", lambda e, N=N, ci=ci: e.activation(out=t["sgw"][:, 0:N], in_=pw[:, 0:N], func=AF.Sigmoid,
                                                                               bias=colc[:, ci:ci + 1], scale=1.0),
                                     reads=[bb["pw"], b_w], writes=[bb["sgw"]])
                                k.op("act", lambda e, N=N: e.activation(out=t["dec"][:, 0:N], in_=t["sgw"][:, 0:N], func=AF.Exp,
                                                                        scale=-math.exp(-0.5)),
                                     reads=[bb["sgw"]], writes=[bb["dec"]])
                                k.op("dve", lambda e, N=N: e.tensor_copy(CAw[:, 0:nch, hp, :, 2], c16(t["dec"][:, 0:N])),
                                     reads=[bb["dec"]], writes=[bCA])
                                k.op("act", lambda e, N=N, ci=ci: e.activation(out=t["Aa"][:, 0:N], in_=pa_[:, 0:N], func=AF.Sigmoid,
                                                                               bias=colc[:, 8 + ci:9 + ci], scale=1.0),
                                     reads=[bb["pa_"], b_w], writes=[bb["Aa"]])
                                k.op("act", lambda e, N=N: e.activation(out=u["Gg"][:, 0:N], in_=pg_[:, 0:N], func=AF.Copy),
                                     reads=[bb["pg_"]], writes=[bb["Gg"]])
                                k.dma(ds_o[3], g_d[d, b, hp, :, pos0:pos0 + N], u["Gg"][:, 0:N], reads=[bb["Gg"]])
                                kk_ = P[:, 4 + hp, 0:N]
                                r_ = P[:, hp, 0:N]
                                v_ = P[:, 8 + hp, 0:N]
                                k.op("dve", lambda e, N=N, hp=hp, kk_=kk_: e.tensor_scalar(out=t["kkf"][:, 0:N], in0=kk_, scalar1=colc[:, 16 + hp:17 + hp],
                                                                                        scalar2=None, op0=ALU.mult),
                                     reads=[bP, b_w], writes=[bb["kkf"]])
                                k.op("act", lambda e, N=N: e.activation(out=u["kk2"][:, 0:N], in_=t["kkf"][:, 0:N], func=AF.Square),
                                     reads=[bb["kkf"]], writes=[bb["kk2"]])
                                k.op("pe", lambda e, N=N: e.matmul(pss[:, 0:N], bones_bf, u["kk2"][:, 0:N], start=True, stop=True),
                                     reads=[bb["kk2"], b_consts], writes=[bb["pss"]])
                                k.op("act", lambda e, N=N: e.activation(out=t["sd"][:, 0:N], in_=pss[:, 0:N], func=AF.Sqrt),
                                     reads=[bb["pss"]], writes=[bb["sd"]])
                                k.op("dve", lambda e, N=N: e.tensor_scalar(out=t["sd"][:, 0:N], in0=t["sd"][:, 0:N], scalar1=1e-12, scalar2=None, op0=ALU.max),
                                     reads=[bb["sd"]], writes=[bb["sd"]])
                                k.op("dve", lambda e, N=N: e.reciprocal(t["sd"][:, 0:N], t["sd"][:, 0:N]), reads=[bb["sd"]], writes=[bb["sd"]])
                                k.op("dve", lambda e, N=N: e.tensor_tensor(out=t["kkn"][:, 0:N], in0=t["kkf"][:, 0:N], in1=t["sd"][:, 0:N], op=ALU.mult),
                                     reads=[bb["kkf"], bb["sd"]], writes=[bb["kkn"]])
                                k.op("dve", lambda e, N=N: e.tensor_tensor(out=u["bsc"][:, 0:N], in0=t["kkn"][:, 0:N], in1=t["Aa"][:, 0:N], op=ALU.mult),
                                     reads=[bb["kkn"], bb["Aa"]], writes=[bb["bsc"]])
                                k.op("dve", lambda e, N=N, hp=hp: e.tensor_scalar(out=t["tmpk"][:, 0:N], in0=t["Aa"][:, 0:N], scalar1=colc[:, 20 + hp:21 + hp],
                                                                                 scalar2=colc[:, 28 + hp:29 + hp], op0=ALU.mult, op1=ALU.add),
                                     reads=[bb["Aa"], b_w], writes=[bb["tmpk"]])
                                k.op("dve", lambda e, N=N, kk_=kk_: e.tensor_tensor(out=t["kd"][:, 0:N], in0=kk_, in1=t["tmpk"][:, 0:N], op=ALU.mult),
                                     reads=[bP, bb["tmpk"]], writes=[bb["kd"]])
                                k.op("act", lambda e, N=N: e.activation(out=u["kdb"][:, 0:N], in_=t["kd"][:, 0:N], func=AF.Copy),
                                     reads=[bb["kd"]], writes=[bb["kdb"]])
                                k.op("dve", lambda e, N=N, hp=hp, r_=r_: e.scalar_tensor_tensor(out=u["rkr"][:, 0:N], in0=r_, scalar=colc[:, 24 + hp:25 + hp],
                                                                                             in1=t["kd"][:, 0:N], op0=ALU.mult, op1=ALU.mult),
                                     reads=[bP, bb["kd"], b_w], writes=[bb["rkr"]])
                                k.op("pe", lambda e, N=N: e.matmul(pbo[:, 0:N], bones_bf, u["rkr"][:, 0:N], start=True, stop=True),
                                     reads=[bb["rkr"], b_consts], writes=[bb["pbo"]])
                                k.op("dve", lambda e, N=N, v_=v_: e.tensor_tensor(out=u["bon"][:, 0:N], in0=pbo[:, 0:N], in1=v_, op=ALU.mult),
                                     reads=[bb["pbo"], bP], writes=[bb["bon"]])
                                k.dma(ds_o[0], bon_d[d, b, hp, :, pos0:pos0 + N], u["bon"][:, 0:N], reads=[bb["bon"]])
                                k.op("dve", lambda e, N=N: e.tensor_scalar(out=CAb[0:64, 0:nch, hp, :, 0], in0=c16(t["kkn"][0:64, 0:N]), scalar1=-1.0, scalar2=None, op0=ALU.mult),
                                     reads=[bb["kkn"]], writes=[bb["CA"]])
                                k.op("dve", lambda e, N=N: e.tensor_scalar(out=CAb[64:128, 0:nch, hp, :, 1], in0=c16(t["kkn"][64:128, 0:N]), scalar1=-1.0, scalar2=None, op0=ALU.mult),
                                     reads=[bb["kkn"]], writes=[bb["CA"]])
                                ro = 0 if d == 0 else 2
                                k.op("act", lambda e, N=N, hp=hp, ro=ro: e.activation(out=CAb[0:64, 0:nch, hp, :, 2], in_=c16(RH[0:64, hp, ro:ro + N]), func=AF.Copy),
                                     reads=[b_RH], writes=[bb["CA"]])
                                k.op("act", lambda e, N=N, hp=hp, ro=ro: e.activation(out=CAb[64:128, 0:nch, hp, :, 3], in_=c16(RH[64:128, hp, ro:ro + N]), func=AF.Copy),
                                     reads=[b_RH], writes=[bb["CA"]])
                                if d == 0 and pos0 + N == T:
                                    k.op("act", lambda e, N=N, hp=hp: e.activation(out=CAx[0:64, 2:3], in_=RH[0:64, hp, N:N + 1], func=AF.Copy),
                                         reads=[b_RH], writes=[b_RH])
                                    k.op("act", lambda e, N=N, hp=hp: e.activation(out=CAx[64:128, 3:4], in_=RH[64:128, hp, N:N + 1], func=AF.Copy),
                                         reads=[b_RH], writes=[b_RH])
                                    k.dma(ds_rh, cols_d[0, :, NCH, b, hp, 0, :], CAx[:], reads=[b_RH])
                                if hp == 3:
                                    k.dma(ds_o[1], cols_d[d, :, pos0 // 16:pos0 // 16 + nch, b, :, :, :], CAb[:, 0:nch], reads=[bCA])
                                for j in range(N // 128):
                                    js = slice(j * 128, (j + 1) * 128)
                                    rowsb = rowsbs[irow % 2]
                                    bb["rowsb"] = bbs[irow % 2]["rowsb"]
                                    irow += 1
                                    k.op("pe", lambda e, js=js: e.matmul(prow[:, 0, :], u["bsc"][:, js], identA_bf, start=True, stop=True),
                                         reads=[bb["bsc"], b_consts], writes=[bb["prow"]])
                                    k.op("pe", lambda e, js=js: e.matmul(prow[:, 1, :], u["bsc"][:, js], identB_bf, start=True, stop=True),
                                         reads=[bb["bsc"], b_consts], writes=[bb["prow"]])
                                    k.op("pe", lambda e, js=js: e.matmul(prow[:, 2, :], u["kdb"][:, js], identA_bf, start=True, stop=True),
                                         reads=[bb["kdb"], b_consts], writes=[bb["prow"]])
                                    k.op("pe", lambda e, js=js: e.matmul(prow[:, 3, :], u["kdb"][:, js], identB_bf, start=True, stop=True),
                                         reads=[bb["kdb"], b_consts], writes=[bb["prow"]])
                                    k.op("dve", lambda e: e.tensor_copy(rowsb[:], prow[:]), reads=[bb["prow"]], writes=[bb["rowsb"]])
                                    p0 = pos0 + j * 128
                                    k.dma(ds_o[2], rows_d[d, 0:2, p0:p0 + 128, b * 4 + hp, :].rearrange("r t c -> t r c"), rowsb[:, 0:2, :],
                                          reads=[bb["rowsb"]])
                                    k.dma(ds_o[3], rows_d[d, 4:6, p0:p0 + 128, b * 4 + hp, :].rearrange("r t c -> t r c"), rowsb[:, 2:4, :],
                                          reads=[bb["rowsb"]])
                k.barrier()
                k.phase_end(es)

        if "D" in phases:
            with ExitStack() as pes:
                k.phase_begin(pes)
                CH = 16
                ST = k.sb("ST", [128, 2, 8, 64], F32)
                T1 = k.sb("T1", [128, 2, 8, 64], F32)
                STb = k.sb("STb", [128, 2, 8, 64], BF16)
                colsAR = [k.sb("colsAR%d" % g, [128, 1, 8, CH, 6], BF16) for g in range(2)]
                wv = [colsAR[g][:].bitcast(F32) for g in range(2)]
                rowsL = [k.sb("rowsL%d" % g, [6, CH, 8, 128], BF16) for g in range(2)]
                stage = [k.sb("stage%d" % g, [6, CH, 8, 64], BF16) for g in range(2)]
                ps1 = [k.ps("ps1%d" % g, [4, 8, 64]) for g in range(2)]
                ps2 = [k.ps("ps2%d" % g, [128, 8, 64]) for g in range(2)]
                b_ST, b_T1, b_STb = [k.buf(), k.buf()], [k.buf(), k.buf()], [k.buf(), k.buf()]
                b_cols, b_wc = [k.buf(), k.buf()], [k.buf(), k.buf()]
                b_rows, b_stv, b_sty = [k.buf(), k.buf()], [k.buf(), k.buf()], [k.buf(), k.buf()]
                b_ps1, b_ps2 = [k.buf(), k.buf()], [k.buf(), k.buf()]
                ds_g = [k.dsem("sp", "dg0"), k.dsem("pool", "dg1")]
                ds_gr = [k.dsem("sp", "dgr0"), k.dsem("pool", "dgr1")]
                ds_gv = [k.dsem("sp", "dgv0"), k.dsem("pool", "dgv1")]
                ds_y = [k.dsem("sp", "dy0"), k.dsem("pool", "dy1")]
                k.op("dve", lambda e: e.memset(ST[:], 0.0), writes=b_ST)
                k.op("dve", lambda e: e.memset(STb[:], 0.0), writes=b_STb)
                for g in range(2):
                    k.op("pool", lambda e, g=g: e.memset(stage[g][:], 0.0), writes=[b_stv[g], b_sty[g]])
                cols_v = [cols_d[g].rearrange("p c b h s x -> p c (b h) s x") for g in range(2)]

                def dsl(start, size):
                    if isinstance(start, int):
                        return slice(start, start + size)
                    return bass.ds(start, size)

                def scan_body(cbase, n):
                    def body(it):
                        cidx = [cbase + it, (cbase + n - 1) - it]
                        for g in range(2):
                            p0 = cidx[g] * CH
                            k.dma(ds_g[g], colsAR[g][:], cols_v[g][:, dsl(cidx[g], 1)], writes=[b_cols[g], b_wc[g]])
                            k.dma(ds_gr[g], rowsL[g][:], rows_d[g, :, dsl(p0, CH), :, :], writes=[b_rows[g]])
                            k.dma(ds_gv[g], stage[g][4:6], v_d[:, dsl(p0, CH), :, :], writes=[b_stv[g]])
                        for st_ in range(CH):
                            tl = [st_, CH - 1 - st_]
                            for g in range(2):
                                for pr in range(8):
                                    k.op("pe", lambda e, g=g, pr=pr, t_=tl[g]: e.matmul(
                                        ps1[g][0:4, pr, :], colsAR[g][:, 0, pr, t_, 0:4], STb[:, g, pr, :], start=True, stop=True),
                                        reads=[b_cols[g], b_STb[g]], writes=[b_ps1[g]])
                            for g in range(2):
                                k.op("act", lambda e, g=g, t_=tl[g]: e.activation(
                                    out=stage[g][0:4, t_, :, :], in_=ps1[g][0:4, :, :], func=AF.Copy),
                                    reads=[b_ps1[g]], writes=[b_sty[g]])
                            for g in range(2):
                                k.op("pool", lambda e, g=g, t_=tl[g]: e.tensor_tensor(
                                    out=T1[:, g], in0=ST[:, g], in1=_bc(wv[g][:, 0, :, t_, 2:3], [128, 8, 64]), op=ALU.mult),
                                    reads=[b_ST[g], b_wc[g]], writes=[b_T1[g]])
                            for g in range(2):
                                for pr in range(8):
                                    k.op("pe", lambda e, g=g, pr=pr, t_=tl[g]: e.matmul(
                                        ps2[g][:, pr, :], rowsL[g][0:6, t_, pr, :], stage[g][0:6, t_, pr, :],
                                        start=True, stop=True),
                                        reads=[b_rows[g], b_stv[g], b_sty[g]], writes=[b_ps2[g]])
                            for g in range(2):
                                k.op("dve", lambda e, g=g: e.tensor_tensor(out=STb[:, g], in0=T1[:, g], in1=ps2[g][:], op=ALU.add),
                                     reads=[b_T1[g], b_ps2[g]], writes=[b_STb[g]])
                                k.op("dve", lambda e, g=g: e.tensor_tensor(out=ST[:, g], in0=T1[:, g], in1=ps2[g][:], op=ALU.add),
                                     reads=[b_T1[g], b_ps2[g]], writes=[b_ST[g]])
                        for g in range(2):
                            p0 = cidx[g] * CH
                            k.dma(ds_y[g], y_d[g, :, dsl(p0 + 1, CH), :, :], stage[g][2:4], reads=[b_sty[g]])
                    return body

                k.loop(TCX // CH, scan_body(0, TCX // CH))
                k.loop(TX // CH, scan_body(TCX // CH, TX // CH))
                for g, pv_ in ((0, T), (1, TCX - 1)):
                    k.dma(ds_g[g], colsAR[g][:], cols_v[g][:, pv_ // 16:pv_ // 16 + 1], writes=[b_cols[g]])
                    for pr in range(8):
                        k.op("pe", lambda e, g=g, pr=pr: e.matmul(ps1[g][0:4, pr, :], colsAR[g][:, 0, pr, pv_ % 16, 0:4], STb[:, g, pr, :],
                                                                  start=True, stop=True),
                             reads=[b_cols[g], b_STb[g]], writes=[b_ps1[g]])
                    k.op("act", lambda e, g=g: e.activation(out=stage[g][0:4, 0, :, :], in_=ps1[g][0:4, :, :], func=AF.Copy),
                         reads=[b_ps1[g]], writes=[b_sty[g]])
                    k.dma(ds_y[g], y_d[g, :, pv_ + 1:pv_ + 2, :, :], stage[g][2:4, 0:1], reads=[b_sty[g]])
                k.phase_end(es)

        xblocks = [(p, n) for (p, n, _c) in blocks if p >= TCX]
        if "E" in phases:
            with ExitStack() as pes:
                k.phase_begin(pes)
                Gt = k.sb("Gt", [128, 2, 4, 512], BF16)
                Bt = k.sb("Bt", [128, 2, 4, 512], BF16)
                Yt = k.sb("Yt", [128, 2, 4, 2, 64], BF16)
                Yf = k.sb("Yf", [128, 16, 64], F32)
                cen = k.sb("cen", [128, 16, 64], F32)
                sqe = k.sb("sqe", [128, 16, 64], F32)
                st4 = k.sb("st4", [128, 4, 16], F32)
                yh = k.sb("yh", [128, 2, 512], BF16)
                Zn = k.sb("Zn", [128, 2, 4, 128], F32)
                catR = k.sb("catR", [128, 4, 512], BF16)
                lnc = k.sb("lnc", [128, 8], F32)
                gne = k.sb("gne", [128, 1], F32)
                pte = k.ps("pte", [128, 8, 128], BF16)
                bG, bB, bY, bYf, bcen, bsq, bst, byh, bZn, bcat, bln, bpte = (k.buf() for _ in range(12))
                ds_e = [k.dsem("sp", "e%d" % i) for i in range(3)]
                ds_eo = k.dsem("pool", "eo")
                k.dma(ds_e[2], lnc[:, 0:4], rw_lnw[:, :], writes=[bln])
                k.dma(ds_e[2], lnc[:, 4:8], rw_lnb[:, :], writes=[bln])
                k.op("dve", lambda e: e.memset(gne[:], 64e-5), writes=[bln])
                for b in range(NB):
                    for (pos0, N) in xblocks:
                        for d in range(2):
                            k.dma(ds_e[0], Gt[:, d, :, 0:N], g_d[d, b, :, :, pos0:pos0 + N].rearrange("h p t -> p h t"), writes=[bG])
                            k.dma(ds_e[0], Bt[:, d, :, 0:N], bon_d[d, b, :, :, pos0:pos0 + N].rearrange("h p t -> p h t"), writes=[bB])
                        for j in range(N // 128):
                            pos = pos0 + j * 128
                            for d in range(2):
                                sl0 = pos + 2 if d == 0 else pos
                                for ab in range(2):
                                    k.dma(ds_e[1], Yt[:, d, :, ab, :], y_d[d, ab, sl0:sl0 + 128, b * 4:(b + 1) * 4, :], writes=[bY])
                            k.op("act", lambda e: e.activation(out=Yf[:], in_=Yt[:].rearrange("p d h a i -> p (d h a) i"), func=AF.Copy),
                                 reads=[bY], writes=[bYf])
                            k.op("dve", lambda e: e.tensor_reduce(out=st4[:, 0, :], in_=Yf[:], axis=AX.X, op=ALU.add), reads=[bYf], writes=[bst])
                            k.op("dve", lambda e: e.tensor_scalar(out=st4[:, 1, :], in0=st4[:, 0, :], scalar1=-1.0 / 64, scalar2=None, op0=ALU.mult),
                                 reads=[bst], writes=[bst])
                            k.op("dve", lambda e: e.tensor_tensor(out=cen[:], in0=Yf[:], in1=_bc(st4[:, 1, :].unsqueeze(2), [128, 16, 64]), op=ALU.add),
                                 reads=[bYf, bst], writes=[bcen])
                            k.op("pool", lambda e: e.tensor_tensor(out=sqe[:], in0=cen[:], in1=cen[:], op=ALU.mult), reads=[bcen], writes=[bsq])
                            k.op("dve", lambda e: e.tensor_reduce(out=st4[:, 2, :], in_=sqe[:], axis=AX.X, op=ALU.add), reads=[bsq], writes=[bst])
                            k.op("act", lambda e: e.activation(out=st4[:, 3, :], in_=st4[:, 2, :], func=AF.Sqrt, bias=gne[:, 0:1], scale=1.0 / 64),
                                 reads=[bst, bln], writes=[bst])
                            k.op("dve", lambda e: e.reciprocal(st4[:, 3, :], st4[:, 3, :]), reads=[bst], writes=[bst])
                            k.op("dve", lambda e: e.tensor_tensor(out=yh[:].rearrange("p d (g i) -> p (d g) i", i=64), in0=cen[:],
                                                                  in1=_bc(st4[:, 3, :].unsqueeze(2), [128, 16, 64]), op=ALU.mult),
                                 reads=[bcen, bst], writes=[byh])
                            for d in range(2):
                                for hp in range(4):
                                    k.op("pe", lambda e, d=d, hp=hp: e.transpose(out=pte[:, d * 4 + hp, :], in_=yh[:, d, hp * 128:(hp + 1) * 128],
                                                                                 identity=ident_bf), reads=[byh, b_consts], writes=[bpte])
                            for d in range(2):
                                for hp in range(4):
                                    k.op("act", lambda e, d=d, hp=hp: e.activation(out=Zn[:, d, hp, :], in_=pte[:, d * 4 + hp, :], func=AF.Identity,
                                                                                   bias=lnc[:, 4 + hp:5 + hp], scale=lnc[:, hp:hp + 1]),
                                         reads=[bpte, bln], writes=[bZn])
                            js = slice(j * 128, (j + 1) * 128)
                            k.op("dve", lambda e, js=js: e.tensor_tensor(out=Zn[:], in0=Zn[:], in1=Bt[:, :, :, js], op=ALU.add), reads=[bZn, bB], writes=[bZn])
                            k.op("pool", lambda e, js=js: e.tensor_tensor(out=Zn[:], in0=Zn[:], in1=Gt[:, :, :, js], op=ALU.mult), reads=[bZn, bG], writes=[bZn])
                            k.op("dve", lambda e, js=js: e.tensor_tensor(out=catR[:, :, js], in0=Zn[:, 0], in1=Zn[:, 1], op=ALU.add), reads=[bZn], writes=[bcat])
                        k.dma(ds_eo, cat_d[b, 0:512, pos0 - TCX:pos0 - TCX + N].rearrange("(h p) t -> p h t", p=128), catR[:, :, 0:N], reads=[bcat])
                k.phase_end(es)

        if "F" in phases:
            with ExitStack() as pes:
                k.phase_begin(pes)
                PD = [k.sb("PD%d" % i, [128, 12, 512], F32) for i in range(2)]
                cosb = [k.sb("cosb%d" % i, [128, 512], F32) for i in range(2)]
                sinb = [k.sb("sinb%d" % i, [128, 512], F32) for i in range(2)]
                x2 = k.sb("x2", [128, 512], BF16)
                sdf = k.sb("sdf", [128, 512], F32)
                XQ = k.sb("XQ", [128, 512], F32)
                XQb = k.sb("XQb", [128, 512], BF16)
                t1f = k.sb("t1f", [128, 512], F32)
                t2f = k.sb("t2f", [128, 512], F32)
                qo = [k.sb("qo%d" % i, [128, 512], BF16) for i in range(2)]
                Vb = k.sb("Vb", [128, 4, 512], BF16)
                vtk = [k.sb("vtk%d" % i, [128, 512], BF16) for i in range(2)]
                nwc = k.sb("nwc", [128, 2], F32)
                pssf = k.ps("pssf", [128, 512])
                prot = k.ps("prot", [128, 512])
                pvf = k.ps("pvf", [128, 4, 128])
                bPD, bcs = [k.buf(), k.buf()], [k.buf(), k.buf()]
                bx2, bsd, bXQ, bXQb, bt1, bt2, bVb, bnw, bpss, bprot, bpv = (k.buf() for _ in range(11))
                bqo, bvtk = [k.buf(), k.buf()], [k.buf(), k.buf()]
                ds_f = [k.dsem("sp", "f0"), k.dsem("sp", "f1")]
                ds_fw = k.dsem("sp", "fw")
                ds_fo = [k.dsem("pool", "fo0"), k.dsem("pool", "fo1")]
                ds_fv = [k.dsem("pool", "fv0"), k.dsem("pool", "fv1")]
                k.dma(ds_fw, nwc[:, 0:1], qnw[:, :], writes=[bnw])
                k.dma(ds_fw, nwc[:, 1:2], knw[:, :], writes=[bnw])
                ib = 0
                iq = 0
                iv = 0
                for b in range(NB):
                    for (pos0, N, _c) in blocks:
                        s_ = ib % 2
                        ib += 1
                        P = PD[s_]
                        k.dma(ds_f[s_], P[:, :, 0:N], P_d[b, 2048:3584, pos0:pos0 + N].rearrange("(c p) n -> p c n", p=128), writes=[bPD[s_]])
                        k.dma(ds_f[s_], cosb[s_][:, 0:N], ropec[:, pos0:pos0 + N], writes=[bcs[s_]])
                        k.dma(ds_f[s_], sinb[s_][:, 0:N], ropes[:, pos0:pos0 + N], writes=[bcs[s_]])
                        for c in range(8):
                            if c < 4 and pos0 < TCX:
                                continue
                            X = P[:, c, 0:N]
                            wi = 0 if c < 4 else 1
                            k.op("pool", lambda e, X=X, N=N: e.tensor_tensor(out=x2[:, 0:N], in0=X, in1=X, op=ALU.mult), reads=[bPD[s_]], writes=[bx2])
                            k.op("pe", lambda e, N=N: e.matmul(pssf[:, 0:N], bones_bf, x2[:, 0:N], start=True, stop=True), reads=[bx2, b_consts], writes=[bpss])
                            k.op("act", lambda e, N=N: e.activation(out=sdf[:, 0:N], in_=pssf[:, 0:N], func=AF.Sqrt, bias=eps_t[:, 0:1], scale=1.0 / 64),
                                 reads=[bpss, b_consts], writes=[bsd])
                            k.op("dve", lambda e, N=N: e.reciprocal(sdf[:, 0:N], sdf[:, 0:N]), reads=[bsd], writes=[bsd])
                            k.op("dve", lambda e, X=X, N=N, wi=wi: e.scalar_tensor_tensor(out=XQ[:, 0:N], in0=X, scalar=nwc[:, wi:wi + 1], in1=sdf[:, 0:N],
                                                                                         op0=ALU.mult, op1=ALU.mult),
                                 reads=[bPD[s_], bsd, bnw], writes=[bXQ])
                            k.op("act", lambda e, N=N: e.activation(out=XQb[:, 0:N], in_=XQ[:, 0:N], func=AF.Copy), reads=[bXQ], writes=[bXQb])
                            k.op("pe", lambda e, N=N: e.matmul(prot[:, 0:N], rot_bf, XQb[:, 0:N], start=True, stop=True), reads=[bXQb, b_consts], writes=[bprot])
                            k.op("pool", lambda e, N=N, s_=s_: e.tensor_tensor(out=t1f[:, 0:N], in0=XQ[:, 0:N], in1=cosb[s_][:, 0:N], op=ALU.mult),
                                 reads=[bXQ, bcs[s_]], writes=[bt1])
                            k.op("dve", lambda e, N=N, s_=s_: e.tensor_tensor(out=t2f[:, 0:N], in0=prot[:, 0:N], in1=sinb[s_][:, 0:N], op=ALU.mult),
                                 reads=[bprot, bcs[s_]], writes=[bt2])
                            qs = iq % 2
                            iq += 1
                            k.op("dve", lambda e, N=N, qs=qs: e.tensor_tensor(out=qo[qs][:, 0:N], in0=t1f[:, 0:N], in1=t2f[:, 0:N], op=ALU.add),
                                 reads=[bt1, bt2], writes=[bqo[qs]])
                            dst = qT_d[b, c, :, pos0:pos0 + N] if c < 4 else kT_d[b, c - 4, :, pos0:pos0 + N]
                            k.dma(ds_fo[qs], dst, qo[qs][:, 0:N], reads=[bqo[qs]])
                        k.op("act", lambda e, P=P, N=N: e.activation(out=Vb[:, :, 0:N], in_=P[:, 8:12, 0:N], func=AF.Copy), reads=[bPD[s_]], writes=[bVb])
                        for j in range(N // 128):
                            for h in range(4):
                                k.op("pe", lambda e, h=h, j=j: e.matmul(pvf[:, h, :], Vb[:, h, j * 128:(j + 1) * 128], ident_bf, start=True, stop=True),
                                     reads=[bVb, b_consts], writes=[bpv])
                            vs = iv % 2
                            iv += 1
                            k.op("act", lambda e, vs=vs: e.activation(out=vtk[vs][:], in_=pvf[:].rearrange("p h c -> p (h c)"), func=AF.Copy),
                                 reads=[bpv], writes=[bvtk[vs]])
                            p0 = pos0 + j * 128
                            k.dma(ds_fv[vs], vt_d[b, p0:p0 + 128, :], vtk[vs][:], reads=[bvtk[vs]])
                k.phase_end(es)

            with ExitStack() as pes:
                k.phase_begin(pes)
                LAM_INIT = 0.8 - 0.6 * math.exp(-0.3 * 0)
                KT = [k.sb("KT%d" % i, [128, T], BF16) for i in range(2)]
                VT = [k.sb("VT%d" % i, [128, NT, 128], BF16) for i in range(2)]
                QT = [k.sb("QT%d" % i, [128, 512], BF16) for i in range(2)]
                pT = [[k.sb("pT%d%d" % (m, i), [128, 512], BF16) for i in range(2)] for m in range(2)]
                lamt = k.sb("lamt", [1, 256], F32)
                lamw = k.sb("lamw", [1, 136], F32)
                nlamc = k.sb("nlamc", [128, 1], F32)
                slw = k.sb("slw", [128, 1], F32)
                o0 = k.sb("o0", [128, 512], F32)
                o1 = k.sb("o1", [128, 512], F32)
                rz = k.sb("rz", [128, 512], F32)
                od2 = k.sb("od2", [128, 512], BF16)
                res = [k.sb("res%d" % i, [128, 512], BF16) for i in range(2)]
                sT = [[k.ps("sT%d%d" % (m, i), [128, 512]) for i in range(2)] for m in range(2)]
                Oa = [k.ps("Oa%d" % m, [128, 512]) for m in range(2)]
                Za = [k.ps("Za%d" % m, [128, 512]) for m in range(2)]
                bKT, bVT, bQT = [k.buf(), k.buf()], [k.buf(), k.buf()], [k.buf(), k.buf()]
                bpT = [[k.buf(), k.buf()], [k.buf(), k.buf()]]
                bsT = [[k.buf(), k.buf()], [k.buf(), k.buf()]]
                bO, bZ = [k.buf(), k.buf()], [k.buf(), k.buf()]
                blam, bo0, bo1, brz, bod2 = (k.buf() for _ in range(5))
                bres = [k.buf(), k.buf()]
                ds_kv = [k.dsem("sp", "kv0"), k.dsem("sp", "kv1")]
                ds_q = [k.dsem("sp", "q0"), k.dsem("sp", "q1")]
                ds_l = k.dsem("sp", "lam")
                ds_ro = [k.dsem("pool", "ro0"), k.dsem("pool", "ro1")]
                k.dma(ds_l, lamt[:], lamv[:, :], writes=[blam])
                k.dma(ds_l, slw[:], sublnw[:, :], writes=[blam])
                k.op("dve", lambda e: e.tensor_tensor(out=lamw[:, 0:64], in0=lamt[:, 0:64], in1=lamt[:, 64:128], op=ALU.mult), reads=[blam], writes=[blam])
                k.op("dve", lambda e: e.tensor_tensor(out=lamw[:, 64:128], in0=lamt[:, 128:192], in1=lamt[:, 192:256], op=ALU.mult), reads=[blam], writes=[blam])
                k.op("dve", lambda e: e.tensor_reduce(out=lamw[:, 128:129], in_=lamw[:, 0:64], axis=AX.X, op=ALU.add), reads=[blam], writes=[blam])
                k.op("dve", lambda e: e.tensor_reduce(out=lamw[:, 129:130], in_=lamw[:, 64:128], axis=AX.X, op=ALU.add), reads=[blam], writes=[blam])
                k.op("act", lambda e: e.activation(out=lamw[:, 130:132], in_=lamw[:, 128:130], func=AF.Exp), reads=[blam], writes=[blam])
                k.op("dve", lambda e: e.tensor_tensor(out=lamw[:, 132:133], in0=lamw[:, 131:132], in1=lamw[:, 130:131], op=ALU.subtract), reads=[blam], writes=[blam])
                k.op("dve", lambda e: e.tensor_scalar(out=lamw[:, 133:134], in0=lamw[:, 132:133], scalar1=-LAM_INIT, scalar2=None, op0=ALU.add),
                     reads=[blam], writes=[blam])
                k.op("pe", lambda e: e.matmul(Za[0][:, 0:1], consts[0:1, C_ONES:C_ONES + 128], lamw[0:1, 133:134], start=True, stop=True),
                     reads=[blam, b_consts], writes=[bZ[0]])
                k.op("dve", lambda e: e.tensor_copy(nlamc[:], Za[0][:, 0:1]), reads=[bZ[0]], writes=[blam])
                k.op("dve", lambda e: e.tensor_scalar(out=slw[:], in0=slw[:], scalar1=1.0 - LAM_INIT, scalar2=None, op0=ALU.mult), reads=[blam], writes=[blam])
                ih = 0
                iqb = 0
                ipt = [0, 0]
                for b in range(NB):
                    for h in range(4):
                        hs = ih % 2
                        ih += 1
                        k.dma(ds_kv[hs], KT[hs][:], kT_d[b, h, :, :], writes=[bKT[hs]])
                        k.dma(ds_kv[hs], VT[hs][:], vt_d[b, :, h * 128:(h + 1) * 128].rearrange("(n p) c -> p n c", p=128), writes=[bVT[hs]])
                        for (pos0, N) in xblocks:
                            qs = iqb % 2
                            iqb += 1
                            k.dma(ds_q[qs], QT[qs][:, 0:N], qT_d[b, h, :, pos0:pos0 + N], writes=[bQT[qs]])
                            items = [(kt, m) for kt in range(NT) for m in range(2)]
                            slots = []
                            for (kt, m) in items:
                                slots.append(ipt[m] % 2)
                                ipt[m] += 1

                            def score(ix):
                                kt, m = items[ix]
                                i_ = slots[ix]
                                ms = slice(64 * m, 64 * m + 64)
                                k.op("pe", lambda e: e.matmul(sT[m][i_][:, 0:N], KT[hs][ms, kt * 128:(kt + 1) * 128], QT[qs][ms, 0:N],
                                                              start=True, stop=True),
                                     reads=[bKT[hs], bQT[qs]], writes=[bsT[m][i_]])

                            LOOK = 2
                            for ix in range(min(LOOK, len(items))):
                                score(ix)
                            for ix, (kt, m) in enumerate(items):
                                i_ = slots[ix]
                                k.op("act", lambda e, m=m, i_=i_: e.activation(out=pT[m][i_][:, 0:N], in_=sT[m][i_][:, 0:N], func=AF.Exp, scale=0.125),
                                     reads=[bsT[m][i_]], writes=[bpT[m][i_]])
                                if ix + LOOK < len(items):
                                    score(ix + LOOK)
                                k.op("pe", lambda e, m=m, i_=i_, kt=kt: e.matmul(
                                    Oa[m][:, 0:N], VT[hs][:, kt, :], pT[m][i_][:, 0:N], start=(kt == 0), stop=(kt == NT - 1)),
                                    reads=[bVT[hs], bpT[m][i_]], writes=[bO[m]])
                                k.op("pe", lambda e, m=m, i_=i_, kt=kt: e.matmul(
                                    Za[m][:, 0:N], ones_bf, pT[m][i_][:, 0:N], start=(kt == 0), stop=(kt == NT - 1)),
                                    reads=[bpT[m][i_], b_consts], writes=[bZ[m]])
                            k.op("dve", lambda e, N=N: e.reciprocal(rz[:, 0:N], Za[0][:, 0:N]), reads=[bZ[0]], writes=[brz])
                            k.op("dve", lambda e, N=N: e.tensor_tensor(out=o0[:, 0:N], in0=Oa[0][:, 0:N], in1=rz[:, 0:N], op=ALU.mult), reads=[bO[0], brz], writes=[bo0])
                            k.op("dve", lambda e, N=N: e.reciprocal(rz[:, 0:N], Za[1][:, 0:N]), reads=[bZ[1]], writes=[brz])
                            k.op("dve", lambda e, N=N: e.tensor_tensor(out=o1[:, 0:N], in0=Oa[1][:, 0:N], in1=rz[:, 0:N], op=ALU.mult), reads=[bO[1], brz], writes=[bo1])
                            k.op("dve", lambda e, N=N: e.scalar_tensor_tensor(out=o0[:, 0:N], in0=o1[:, 0:N], scalar=nlamc[:, 0:1], in1=o0[:, 0:N],
                                                                               op0=ALU.mult, op1=ALU.add), reads=[bo1, bo0, blam], writes=[bo0])
                            k.op("pool", lambda e, N=N: e.tensor_tensor(out=od2[:, 0:N], in0=o0[:, 0:N], in1=o0[:, 0:N], op=ALU.mult), reads=[bo0], writes=[bod2])
                            k.op("pe", lambda e, N=N: e.matmul(sT[0][0][:, 0:N], ones_bf, od2[:, 0:N], start=True, stop=True),
                                 reads=[bod2, b_consts], writes=[bsT[0][0]])
                            k.op("act", lambda e, N=N: e.activation(out=rz[:, 0:N], in_=sT[0][0][:, 0:N], func=AF.Sqrt, bias=eps_t[:, 0:1], scale=1.0 / 128),
                                 reads=[bsT[0][0], b_consts], writes=[brz])
                            k.op("dve", lambda e, N=N: e.reciprocal(rz[:, 0:N], rz[:, 0:N]), reads=[brz], writes=[brz])
                            rs = iqb % 2
                            k.op("dve", lambda e, N=N, rs=rs: e.scalar_tensor_tensor(out=res[rs][:, 0:N], in0=o0[:, 0:N], scalar=slw[:, 0:1], in1=rz[:, 0:N],
                                                                                     op0=ALU.mult, op1=ALU.mult), reads=[bo0, brz, blam], writes=[bres[rs]])
                            k.dma(ds_ro[rs], cat_d[b, 512 + h * 128:512 + (h + 1) * 128, pos0 - TCX:pos0 - TCX + N], res[rs][:, 0:N], reads=[bres[rs]])
                k.phase_end(es)

        if "G" in phases:
            with ExitStack() as pes:
                k.phase_begin(pes)
                wo_st = k.sb("wo_st", [128, 4, D], F32)
                woutb = k.sb("woutb", [128, 8, D], BF16)
                wrt = k.sb("wrt", [128, 8, 36], F32)
                brt = k.sb("brt", [128, 36], F32)
                xt2 = [k.sb("xt2%d" % i, [128, D], F32) for i in range(2)]
                catT = [k.sb("catT%d" % i, [128, 8, 128], BF16) for i in range(2)]
                tmpo = k.sb("tmpo", [128, D], F32)
                x1 = [k.sb("x1%d" % i, [128, D], F32) for i in range(2)]
                sq2 = k.sb("sq2", [128, D], F32)
                ss2 = k.sb("ss2", [128, 4], F32)
                xn2 = k.sb("xn2", [128, D], F32)
                h2f = k.sb("h2f", [128, 8, 128], F32)
                h2b = [k.sb("h2b%d" % i, [128, 8, 128], BF16) for i in range(2)]
                Lg = k.sb("Lg", [128, 36], F32)
                rt = k.sb("rt", [128, 16], F32)
                goh = k.sb("goh", [128, 4], F32)
                em = k.sb("em", [128, 4, 8], F32)
                em2 = k.sb("em2", [128, 32], F32)
                m1 = k.sb("m1", [128, 32], F32)
                m2 = k.sb("m2", [128, 32], F32)
                Wdt = [k.sb("Wdt%d" % i, [128, 32], F32) for i in range(2)]
                po = [k.ps("po%d" % i, [128, 512]) for i in range(2)]
                ptf = k.ps("ptf", [128, 8, 128])
                pl = k.ps("pl", [128, 36])
                bw, bpo, bptf, bpl, btmp, bsq2, bss2, bxn2, bh2f, bL, brt_ = (k.buf() for _ in range(11))
                bxt, bcatT, bx1, bh2b, bWd = ([k.buf(), k.buf()] for _ in range(5))
                bpo = [k.buf(), k.buf()]
                ds_w = k.dsem("sp", "gw")
                ds_i = [k.dsem("sp", "gi0"), k.dsem("sp", "gi1")]
                ds_o1 = [k.dsem("pool", "go0"), k.dsem("pool", "go1")]
                wov = w_out.rearrange("(kc p) n -> p kc n", p=128)
                for hf in range(2):
                    k.dma(ds_w, wo_st[:], wov[:, hf * 4:(hf + 1) * 4, :], writes=[bw])
                    k.op("dve", lambda e, hf=hf: e.tensor_copy(woutb[:, hf * 4:(hf + 1) * 4, :], wo_st[:]), reads=[bw], writes=[bw])
                k.dma(ds_w, wrt[:], w_rt.rearrange("(kc p) n -> p kc n", p=128), writes=[bw])
                k.dma(ds_w, brt[:], b_rt.partition_broadcast(128), writes=[bw])
                it_ = 0
                for b in range(NB):
                    for xp in range(0, TX, 128):
                        s_ = it_ % 2
                        it_ += 1
                        k.dma(ds_i[s_], xt2[s_][:], seq[b, TCX + xp:TCX + xp + 128, :], writes=[bxt[s_]])
                        k.dma(ds_i[s_], catT[s_][:], cat_d[b, :, xp:xp + 128].rearrange("(c p) t -> p c t", p=128), writes=[bcatT[s_]])
                        for hf in range(2):
                            for kc in range(8):
                                k.op("pe", lambda e, hf=hf, kc=kc, s_=s_: e.matmul(po[hf][:], catT[s_][:, kc, :], woutb[:, kc, hf * 512:(hf + 1) * 512],
                                                                                   start=(kc == 0), stop=(kc == 7)),
                                     reads=[bcatT[s_], bw], writes=[bpo[hf]])
                            hsl = slice(hf * 512, (hf + 1) * 512)
                            k.op("dve", lambda e, hf=hf, hsl=hsl, b=b: e.tensor_tensor(out=tmpo[:, hsl], in0=po[hf][:], in1=G1[:, b, hsl], op=ALU.mult),
                                 reads=[bpo[hf], b_mod_], writes=[btmp])
                            k.op("pool", lambda e, hsl=hsl, s_=s_: e.tensor_tensor(out=x1[s_][:, hsl], in0=tmpo[:, hsl], in1=xt2[s_][:, hsl], op=ALU.add),
                                 reads=[btmp, bxt[s_]], writes=[bx1[s_]])
                        k.dma(ds_o1[s_], x1_d[b, xp:xp + 128, :], x1[s_][:], reads=[bx1[s_]])
                        k.op("act", lambda e, s_=s_: e.activation(out=sq2[:], in_=x1[s_][:], func=AF.Square), reads=[bx1[s_]], writes=[bsq2])
                        k.op("dve", lambda e: e.tensor_reduce(out=ss2[:, 0:1], in_=sq2[:], axis=AX.X, op=ALU.add), reads=[bsq2], writes=[bss2])
                        k.op("act", lambda e: e.activation(out=ss2[:, 1:2], in_=ss2[:, 0:1], func=AF.Sqrt, bias=eps_t[:, 0:1], scale=1.0 / D),
                             reads=[bss2, b_consts], writes=[bss2])
                        k.op("dve", lambda e: e.reciprocal(ss2[:, 2:3], ss2[:, 1:2]), reads=[bss2], writes=[bss2])
                        k.op("dve", lambda e, s_=s_: e.tensor_scalar(out=xn2[:], in0=x1[s_][:], scalar1=ss2[:, 2:3], scalar2=None, op0=ALU.mult),
                             reads=[bx1[s_], bss2], writes=[bxn2])
                        for kc in range(8):
                            k.op("pe", lambda e, kc=kc: e.transpose(out=ptf[:, kc, :], in_=xn2[:, kc * 128:(kc + 1) * 128], identity=consts[:, C_ID:C_ID + 128]),
                                 reads=[bxn2, b_consts], writes=[bptf])
                        for kc in range(8):
                            k.op("act", lambda e, kc=kc, b=b: e.activation(out=h2f[:, kc, :], in_=ptf[:, kc, :], func=AF.Identity,
                                                                           bias=B2[:, kc, b:b + 1], scale=A2[:, kc, b:b + 1]),
                                 reads=[bptf, b_mod_], writes=[bh2f])
                        k.op("pool", lambda e, s_=s_: e.tensor_copy(h2b[s_][:], h2f[:]), reads=[bh2f], writes=[bh2b[s_]])
                        k.dma(ds_o1[s_], h2T_d[b, :, xp:xp + 128].rearrange("(c p) t -> p c t", p=128), h2b[s_][:], reads=[bh2b[s_]])
                        for kc in range(8):
                            k.op("pe", lambda e, kc=kc: e.matmul(pl[:], h2f[:, kc, :], wrt[:, kc, :], start=(kc == 0), stop=(kc == 7)),
                                 reads=[bh2f, bw], writes=[bpl])
                        k.op("dve", lambda e: e.tensor_tensor(out=Lg[:], in0=pl[:], in1=brt[:], op=ALU.add), reads=[bpl, bw], writes=[bL])
                        R_ = [bL, brt_]
                        k.op("dve", lambda e: e.tensor_reduce(out=rt[:, 0:1], in_=Lg[:, 0:4], axis=AX.X, op=ALU.max), reads=R_, writes=[brt_])
                        k.op("dve", lambda e: e.tensor_scalar(out=goh[:], in0=Lg[:, 0:4], scalar1=rt[:, 0:1], scalar2=None, op0=ALU.subtract), reads=R_, writes=[brt_])
                        k.op("act", lambda e: e.activation(out=em2[:, 0:4], in_=goh[:], func=AF.Exp), reads=R_, writes=[brt_])
                        k.op("dve", lambda e: e.tensor_reduce(out=rt[:, 1:2], in_=em2[:, 0:4], axis=AX.X, op=ALU.add), reads=R_, writes=[brt_])
                        k.op("dve", lambda e: e.reciprocal(rt[:, 2:3], rt[:, 1:2]), reads=R_, writes=[brt_])
                        k.op("dve", lambda e: e.tensor_scalar(out=goh[:], in0=Lg[:, 0:4], scalar1=rt[:, 0:1], scalar2=None, op0=ALU.is_equal), reads=R_, writes=[brt_])
                        k.op("dve", lambda e: e.tensor_scalar(out=goh[:], in0=goh[:], scalar1=-1.0, scalar2=1e30, op0=ALU.add, op1=ALU.mult), reads=R_, writes=[brt_])
                        k.op("dve", lambda e: e.tensor_tensor(out=em[:], in0=Lg[:, 4:36].rearrange("p (g x) -> p g x", x=8),
                                                              in1=_bc(goh[:].unsqueeze(2), [128, 4, 8]), op=ALU.add), reads=R_, writes=[brt_])
                        emf = em[:].rearrange("p g x -> p (g x)")
                        k.op("dve", lambda e: e.tensor_reduce(out=rt[:, 3:4], in_=emf, axis=AX.X, op=ALU.max), reads=R_, writes=[brt_])
                        k.op("dve", lambda e: e.tensor_scalar(out=m1[:], in0=emf, scalar1=rt[:, 3:4], scalar2=None, op0=ALU.is_equal), reads=R_, writes=[brt_])
                        k.op("dve", lambda e: e.scalar_tensor_tensor(out=em2[:], in0=m1[:], scalar=-1e30, in1=emf, op0=ALU.mult, op1=ALU.add), reads=R_, writes=[brt_])
                        k.op("dve", lambda e: e.tensor_reduce(out=rt[:, 4:5], in_=em2[:], axis=AX.X, op=ALU.max), reads=R_, writes=[brt_])
                        k.op("dve", lambda e: e.tensor_scalar(out=m2[:], in0=em2[:], scalar1=rt[:, 4:5], scalar2=None, op0=ALU.is_equal), reads=R_, writes=[brt_])
                        k.op("dve", lambda e: e.tensor_tensor(out=rt[:, 5:6], in0=rt[:, 4:5], in1=rt[:, 3:4], op=ALU.subtract), reads=R_, writes=[brt_])
                        k.op("act", lambda e: e.activation(out=rt[:, 6:7], in_=rt[:, 5:6], func=AF.Exp), reads=R_, writes=[brt_])
                        k.op("dve", lambda e: e.tensor_scalar(out=rt[:, 7:8], in0=rt[:, 6:7], scalar1=1.0, scalar2=None, op0=ALU.add), reads=R_, writes=[brt_])
                        k.op("dve", lambda e: e.reciprocal(rt[:, 8:9], rt[:, 7:8]), reads=R_, writes=[brt_])
                        k.op("dve", lambda e: e.tensor_tensor(out=rt[:, 9:10], in0=rt[:, 8:9], in1=rt[:, 2:3], op=ALU.mult), reads=R_, writes=[brt_])
                        k.op("dve", lambda e: e.tensor_tensor(out=rt[:, 10:11], in0=rt[:, 9:10], in1=rt[:, 6:7], op=ALU.mult), reads=R_, writes=[brt_])
                        k.op("dve", lambda e: e.tensor_scalar(out=m1[:], in0=m1[:], scalar1=rt[:, 9:10], scalar2=None, op0=ALU.mult), reads=R_, writes=[brt_])
                        k.op("dve", lambda e, s_=s_: e.scalar_tensor_tensor(out=Wdt[s_][:], in0=m2[:], scalar=rt[:, 10:11], in1=m1[:], op0=ALU.mult, op1=ALU.add),
                             reads=R_, writes=[bWd[s_]])
                        k.dma(ds_o1[s_], wd_d[b, xp:xp + 128, :], Wdt[s_][:], reads=[bWd[s_]])
                k.phase_end(es)

            with ExitStack() as pes:
                k.phase_begin(pes)
                TB = min(1024, TX)
                TBC = TB // 128
                NTB = TB // 512
                h2T = k.sb("h2T", [128, 8, TB], BF16)
                acc = k.sb("acc", [128, TBC, D], F32)
                Wd = k.sb("Wd", [128, TBC, NE], F32)
                x1h = k.sb("x1h", [128, 4, D], F32)
                stg = [k.sb("stg%d" % i, [128, 2048], F32) for i in range(3)]
                wgb = [k.sb("wgb%d" % i, [128, 8, FF], BF16) for i in range(2)]
                wub = [k.sb("wub%d" % i, [128, 8, FF], BF16) for i in range(2)]
                wdb = [k.sb("wdb%d" % i, [128, 4, D], BF16) for i in range(2)]
                sg = [k.sb("sg%d" % i, [128, 512], F32) for i in range(2)]
                hid = [k.sb("hid%d" % i, [128, 4, 512], BF16) for i in range(2)]
                pg = [k.ps("pg%d" % i, [128, 512]) for i in range(2)]
                pu = [k.ps("pu%d" % i, [128, 512]) for i in range(2)]
                py = [k.ps("py%d" % i, [128, 512]) for i in range(2)]
                bh2T, bacc, bWd_, bx1h = (k.buf() for _ in range(4))
                bstg = [k.buf() for _ in range(3)]
                bwg, bwu, bwd_, bsg, bhid, bpg, bpu, bpy = ([k.buf(), k.buf()] for _ in range(8))
                ds_m = k.dsem("sp", "mi")
                ds_s = [k.dsem("sp", "ms%d" % i) for i in range(3)]
                ds_x1 = k.dsem("sp", "mx")
                ds_out = k.dsem("pool", "mo")

                def dsl2(start, size):
                    if isinstance(start, int):
                        return slice(start, start + size)
                    return bass.ds(start, size)

                def moe_body(b):
                    def body(it):
                        off = it * TB
                        k.dma(ds_m, h2T[:], h2T_d[b, :, dsl2(off, TB)].rearrange("(c p) t -> p c t", p=128), writes=[bh2T])
                        k.dma(ds_m, Wd[:], wd_d[b, dsl2(off, TB), :].rearrange("(n p) e -> p n e", p=128), writes=[bWd_])
                        k.op("pool", lambda e: e.memset(acc[:], 0.0), writes=[bacc])
                        ist = 0
                        cnt = [0, 0, 0]
                        for ex in range(NE):
                            ws = ex % 2
                            srcs = []
                            gv = moe_g[ex].rearrange("(kc p) f -> p kc f", p=128)
                            uv = moe_u[ex].rearrange("(kc p) f -> p kc f", p=128)
                            dv = moe_d[ex].rearrange("(fc p) n -> p fc n", p=128)
                            for hf in range(2):
                                srcs.append((gv[:, hf * 4:(hf + 1) * 4, :], wgb[ws][:, hf * 4:(hf + 1) * 4, :], bwg[ws], "p (a f) -> p a f", 4))
                                srcs.append((uv[:, hf * 4:(hf + 1) * 4, :], wub[ws][:, hf * 4:(hf + 1) * 4, :], bwu[ws], "p (a f) -> p a f", 4))
                            for hf in range(2):
                                srcs.append((dv[:, hf * 2:(hf + 1) * 2, :], wdb[ws][:, hf * 2:(hf + 1) * 2, :], bwd_[ws], "p (a f) -> p a f", 2))
                            for (src, dst, bdst, pat, a_) in srcs:
                                si = ist % 3
                                ist += 1
                                k.dma(ds_s[si], stg[si][:].rearrange(pat, a=a_), src, writes=[bstg[si]])
                                if ist % 2 == 0:
                                    k.op("pool", lambda e, si=si, dst=dst, pat=pat, a_=a_: e.tensor_copy(dst, stg[si][:].rearrange(pat, a=a_)),
                                         reads=[bstg[si]], writes=[bdst])
                                else:
                                    k.op("act", lambda e, si=si, dst=dst, pat=pat, a_=a_: e.activation(out=dst, in_=stg[si][:].rearrange(pat, a=a_), func=AF.Copy),
                                         reads=[bstg[si]], writes=[bdst])
                            for tb in range(NTB):
                                tsl = slice(tb * 512, (tb + 1) * 512)
                                hs_ = cnt[0] % 2
                                cnt[0] += 1
                                for fc in range(4):
                                    pi = cnt[1] % 2
                                    cnt[1] += 1
                                    fsl = slice(fc * 128, (fc + 1) * 128)
                                    for kc in range(8):
                                        k.op("pe", lambda e, pi=pi, ws=ws, kc=kc, fsl=fsl, tsl=tsl: e.matmul(pg[pi][:], wgb[ws][:, kc, fsl], h2T[:, kc, tsl],
                                                                                                             start=(kc == 0), stop=(kc == 7)),
                                             reads=[bwg[ws], bh2T], writes=[bpg[pi]])
                                    for kc in range(8):
                                        k.op("pe", lambda e, pi=pi, ws=ws, kc=kc, fsl=fsl, tsl=tsl: e.matmul(pu[pi][:], wub[ws][:, kc, fsl], h2T[:, kc, tsl],
                                                                                                             start=(kc == 0), stop=(kc == 7)),
                                             reads=[bwu[ws], bh2T], writes=[bpu[pi]])
                                    k.op("act", lambda e, pi=pi: e.activation(out=sg[pi][:], in_=pg[pi][:], func=AF.Silu), reads=[bpg[pi]], writes=[bsg[pi]])
                                    k.op("dve", lambda e, pi=pi, hs_=hs_, fc=fc: e.tensor_tensor(out=hid[hs_][:, fc, :], in0=sg[pi][:], in1=pu[pi][:], op=ALU.mult),
                                         reads=[bsg[pi], bpu[pi]], writes=[bhid[hs_]])
                                for tc in range(4):
                                    ch = tb * 4 + tc
                                    for hf in range(2):
                                        yi = cnt[2] % 2
                                        cnt[2] += 1
                                        for fc in range(4):
                                            k.op("pe", lambda e, yi=yi, hs_=hs_, fc=fc, tc=tc, ws=ws, hf=hf: e.matmul(
                                                py[yi][:], hid[hs_][:, fc, tc * 128:(tc + 1) * 128], wdb[ws][:, fc, hf * 512:(hf + 1) * 512],
                                                start=(fc == 0), stop=(fc == 3)), reads=[bhid[hs_], bwd_[ws]], writes=[bpy[yi]])
                                        k.op("dve", lambda e, yi=yi, ch=ch, hf=hf, ex=ex: e.scalar_tensor_tensor(
                                            out=acc[:, ch, hf * 512:(hf + 1) * 512], in0=py[yi][:], scalar=Wd[:, ch, ex:ex + 1],
                                            in1=acc[:, ch, hf * 512:(hf + 1) * 512], op0=ALU.mult, op1=ALU.add),
                                            reads=[bpy[yi], bWd_, bacc], writes=[bacc])
                        for hq in range(TBC // 4):
                            k.dma(ds_x1, x1h[:], x1_d[b, dsl2(off + hq * 512, 512), :].rearrange("(n p) d -> p n d", p=128), writes=[bx1h])
                            asl = acc[:, hq * 4:(hq + 1) * 4, :]
                            k.op("dve", lambda e, asl=asl: e.tensor_tensor(out=asl, in0=asl, in1=_bc(G2[:, b, :].unsqueeze(1), [128, 4, D]), op=ALU.mult),
                                 reads=[bacc, b_mod_], writes=[bacc])
                            k.op("pool", lambda e, asl=asl: e.tensor_tensor(out=asl, in0=asl, in1=x1h[:], op=ALU.add), reads=[bacc, bx1h], writes=[bacc])
                            k.dma(ds_out, out_d[b, dsl2(off + hq * 512, 512), :].rearrange("(n p) d -> p n d", p=128), asl, reads=[bacc])
                    return body

                for b in range(NB):
                    k.loop(TX // TB, moe_body(b), static=True)
                k.phase_end(es)
        k.barrier()
    return nc, dram


def core_inputs(inp, b0, TX, TCX, shared=None):
    f = lambda a: np.ascontiguousarray(np.asarray(a, np.float32))
    if shared is None:
        shared = {}
        cs, sn = rope_tables(TX, TCX)
        shared["ropec"], shared["ropes"] = cs, sn
        shared["consts"] = make_consts()
        shared["w_mod"] = f(inp["w_mod"][0])
        shared["b_mod"] = f(inp["b_mod"][0]).reshape(1, -1)
        shared["n1w"] = colform(inp["norm1_w"][0], 8)
        shared["n2w"] = colform(inp["norm2_w"][0], 8)
        shared["w_in"] = f(inp["w_in"][0])
        shared["shift_w"] = f(inp["shift_w"][0])
        shared["rw_w0"] = f(np.asarray(inp["rwkv_w0"][0]).reshape(2, 4, 128).transpose(2, 0, 1))
        shared["rw_a0"] = f(np.asarray(inp["rwkv_a0"][0]).reshape(2, 4, 128).transpose(2, 0, 1))
        shared["rw_wup"] = f(inp["rwkv_w_up"][0])
        shared["rw_aup"] = f(inp["rwkv_a_up"][0])
        shared["rw_gup"] = f(inp["rwkv_g_up"][0])
        shared["rw_kk"] = colform(inp["rwkv_k_k"][0], 4)
        shared["rw_ka"] = colform(inp["rwkv_k_a"][0], 4)
        shared["rw_rk"] = colform(np.asarray(inp["rwkv_r_k"][0]).reshape(-1), 4)
        shared["rw_lnw"] = colform(inp["rwkv_ln_w"][0], 4)
        shared["rw_lnb"] = colform(inp["rwkv_ln_b"][0], 4)
        shared["qnw"] = f(np.tile(np.asarray(inp["q_norm_w"][0]), 2).reshape(128, 1))
        shared["knw"] = f(np.tile(np.asarray(inp["k_norm_w"][0]), 2).reshape(128, 1))
        shared["lamv"] = f(np.concatenate([np.asarray(inp[n][0]) for n in ("lam_q1", "lam_k1", "lam_q2", "lam_k2")]).reshape(1, 256))
        shared["sublnw"] = f(np.asarray(inp["subln_w"][0]).reshape(128, 1))
        shared["w_out"] = f(inp["w_out"][0])
        shared["w_rt"] = f(np.concatenate([np.asarray(inp["w_group"][0]), np.asarray(inp["w_expert"][0])], axis=1))
        shared["b_rt"] = f(np.concatenate([np.asarray(inp["b_group"][0]), np.asarray(inp["b_expert"][0])]).reshape(1, 36))
        shared["moe_g"] = f(inp["moe_w_gate"][0])
        shared["moe_u"] = f(inp["moe_w_up"][0])
        shared["moe_d"] = f(inp["moe_w_down"][0])
    m = dict(shared)
    x = np.asarray(inp["x"][b0:b0 + NB], np.float32)
    ctx = np.asarray(inp["ctx"][b0:b0 + NB], np.float32)
    m["seq"] = np.ascontiguousarray(np.concatenate([ctx, x], axis=1))
    cc = np.concatenate([np.asarray(inp["c"][b0:b0 + NB], np.float32), np.asarray(inp["c_ctx"], np.float32)[None]], axis=0)
    m["csT"] = np.ascontiguousarray(cc.reshape(3, 8, 128).transpose(2, 1, 0))
    return m, shared


TX_FULL, TCX_FULL = 4096, 256
_CACHE = {}


def kernel(**inputs):
    inp = {k_: np.asarray(v) for k_, v in inputs.items()}
    B = inp["x"].shape[0]
    ncores = B // NB
    if "nc" not in _CACHE:
        _CACHE["nc"] = build_program(TX_FULL, TCX_FULL)
    nc, dram = _CACHE["nc"]
    in_maps = []
    shared = None
    for c in range(ncores):
        m, shared = core_inputs(inp, c * NB, TX_FULL, TCX_FULL, shared)
        in_maps.append({k_: v for k_, v in m.items() if k_ in dram})
    res = run_bass_kernel_spmd(nc, in_maps, core_ids=list(range(ncores)))
    out = np.concatenate([np.asarray(r["out"]) for r in res.results], axis=0)
    return out.astype(np.float32, copy=False)
```

```python
import copy
import math
from contextlib import ExitStack

import numpy as np
import concourse.bass as bass
import concourse.mybir as mybir
from concourse.bass_utils import run_bass_kernel_spmd

F32 = mybir.dt.float32
BF16 = mybir.dt.bfloat16
AF = mybir.ActivationFunctionType
ALU = mybir.AluOpType
AX = mybir.AxisListType

D = 1024
NB = 2
RW = 512
INW = 3584
NE = 32
FF = 512
SUB = 4


class Buf:
    __slots__ = ("name", "w", "r")

    def __init__(self, name):
        self.name = name
        self.w = None
        self.r = []


class DSem:
    def __init__(self, h, q):
        self.h = h
        self.q = q
        self.total = 0


class K:
    def __init__(self, nc, es):
        self.nc = nc
        self.es = es
        self.E = {"pe": nc.tensor, "act": nc.scalar, "dve": nc.vector, "pool": nc.gpsimd, "sp": nc.sync}
        self.sem = {e: es.enter_context(nc.semaphore("c_" + e)) for e in ("pe", "act", "dve", "pool")}
        self.cnt = {e: 0 for e in self.sem}
        self.seen = {e: {} for e in self.E}
        self.dsems = []
        self.bufs = []
        self.dry = False
        self.inloop = False
        self.used = set()
        self.nbuf = 0
        self.phase_no = 0

    def buf(self, name=None):
        self.nbuf += 1
        b = Buf(name or ("b%d" % self.nbuf))
        self.bufs.append(b)
        return b

    def dsem(self, q, name):
        d = DSem(self.es.enter_context(self.nc.semaphore("d_%s_%d" % (name, self.phase_no))), q)
        self.dsems.append(d)
        return d

    def sb(self, name, shape, dt):
        return self.es.enter_context(self.nc.sbuf_tensor("%s_%d" % (name, self.phase_no), shape, dt))

    def ps(self, name, shape, dt=F32):
        return self.es.enter_context(self.nc.psum_tensor("%s_%d" % (name, self.phase_no), shape, dt))

    def _wait(self, e, tok):
        kind, src, n = tok
        key = src if kind == "E" else id(src)
        if kind == "E" and src == e and e == "pe":
            return
        if kind == "D":
            n = src.total
        if self.seen[e].get(key, -1) >= n:
            return
        self.seen[e][key] = n
        if self.dry:
            self.used.add((e, key))
            return
        h = self.sem[src] if kind == "E" else src.h
        if self.inloop:
            R = self.regs[(e, key)]
            delta = n - self.cur[(e, key)]
            if delta != 0:
                self.E[e].reg_add(R, R, delta)
            self.cur[(e, key)] = n
            self.E[e].wait_ge(h, R)
        else:
            self.E[e].wait_ge(h, n)

    def _sync(self, e, reads, writes):
        best = {}
        def add(tok):
            kind, src, n = tok
            key = (kind, src if kind == "E" else id(src))
            if key not in best or best[key][2] < n:
                best[key] = tok
        for b in reads:
            if b.w is not None:
                add(b.w)
        for b in writes:
            if b.w is not None:
                add(b.w)
            for t in b.r:
                add(t)
        for key in sorted(best, key=str):
            self._wait(e, best[key])

    def op(self, e, fn, reads=(), writes=()):
        self._sync(e, reads, writes)
        self.cnt[e] += 1
        tok = ("E", e, self.cnt[e])
        if not self.dry:
            fn(self.E[e]).then_inc(self.sem[e], 1)
        self.seen[e][e] = max(self.seen[e].get(e, -1), 0)
        for b in reads:
            b.r = [t for t in b.r if not (t[0] == "E" and t[1] == e)] + [tok]
        for b in writes:
            b.w = tok
            b.r = []
        return tok

    def dma(self, ds, out, in_, reads=(), writes=()):
        q = ds.q
        self._sync(q, reads, writes)
        ds.total += 16
        tok = ("D", ds, ds.total)
        if not self.dry:
            self.E[q].dma_start(out=out, in_=in_).then_inc(ds.h, 16)
        for b in reads:
            b.r.append(tok)
        for b in writes:
            b.w = tok
            b.r = []
        return tok

    def drain(self):
        for d in self.dsems:
            if d.total > 0:
                self._wait(d.q, ("D", d, d.total))

    def barrier(self):
        self.drain()
        if not self.dry:
            self.nc.all_engine_barrier()
        for b in self.bufs:
            b.w = None
            b.r = []

    def _keycount(self, key):
        if isinstance(key, str):
            return self.cnt[key]
        for d in self.dsems:
            if id(d) == key:
                return d.total
        raise KeyError(key)

    def _snap_bufs(self, shift):
        def sh(tok):
            kind, src, n = tok
            return (kind, src, n - shift[src if kind == "E" else id(src)])
        return [(None if b.w is None else sh(b.w), [sh(t) for t in b.r]) for b in self.bufs]

    def _load_bufs(self, states):
        for b, (w, r) in zip(self.bufs, states):
            b.w = w
            b.r = list(r)

    def loop(self, n_iter, body, static=False):
        self.barrier()
        for e in self.seen:
            self.seen[e] = {}
        if n_iter == 1 or static:
            for i in range(n_iter):
                body(i)
                self.barrier()
            return
        c0 = dict(self.cnt)
        d0 = [d.total for d in self.dsems]
        nb0 = len(self.bufs)

        def rewind():
            self.cnt = dict(c0)
            for d, t in zip(self.dsems, d0):
                d.total = t
            for e in self.seen:
                self.seen[e] = {}

        self.dry = True
        self.used = set()
        body(0)
        P = {e: self.cnt[e] - c0[e] for e in self.cnt}
        for d, t in zip(self.dsems, d0):
            P[id(d)] = d.total - t
        carried = self._snap_bufs(P)
        rewind()
        self._load_bufs(carried)
        self.used = set()
        body(0)
        self.drain()
        used = sorted(self.used, key=str)
        rewind()
        self._load_bufs(carried)
        self.dry = False
        self.regs = {}
        self.cur = {}
        base = {}
        for (e, key) in used:
            self.nbuf += 1
            R = self.E[e].alloc_register("w_%s_%d" % (e, self.nbuf))
            base[(e, key)] = self._keycount(key)
            self.E[e].reg_mov(R, base[(e, key)])
            self.regs[(e, key)] = R
            self.cur[(e, key)] = base[(e, key)]
        with self.nc.Fori(0, n_iter, hint_back_edge=True) as it:
            self.inloop = True
            body(it)
            for (e, key) in used:
                delta = base[(e, key)] + P[key] - self.cur[(e, key)]
                if delta != 0:
                    self.E[e].reg_add(self.regs[(e, key)], self.regs[(e, key)], delta)
            self.inloop = False
        for (e, key) in used:
            self.E[e].free_register(self.regs[(e, key)])
        for e in self.cnt:
            self.cnt[e] += (n_iter - 1) * P[e]
        for d in self.dsems:
            d.total += (n_iter - 1) * P[id(d)]
        shift = {key: -(n_iter - 1) * P[key] for key in P}
        self._load_bufs(self._snap_bufs(shift))
        for e in self.seen:
            self.seen[e] = {}
        self.barrier()

    def phase_begin(self, pes):
        self.es = pes
        self.phase_no += 1
        self._mark = (len(self.dsems), len(self.bufs))

    def phase_end(self, es):
        self.barrier()
        self.es = es
        del self.dsems[self._mark[0]:]
        del self.bufs[self._mark[1]:]


def _bc(ap, shape):
    return ap.to_broadcast(shape)


C_ID = 0
C_IDA = 128
C_IDB = 256
C_BONES = 384
C_ONES = 512
C_ROT = 640
C_SEL = 768
C_ID3 = 1152
NCONST = 1160


def make_consts():
    c = np.zeros((128, NCONST), np.float32)
    c[:, C_ID:C_ID + 128] = np.eye(128)
    c[:64, C_IDA:C_IDA + 64] = np.eye(64)
    c[64:, C_IDB + 64:C_IDB + 128] = np.eye(64)
    c[:64, C_BONES:C_BONES + 64] = 1.0
    c[64:, C_BONES + 64:C_BONES + 128] = 1.0
    c[:, C_ONES:C_ONES + 128] = 1.0
    R = np.zeros((128, 128), np.float32)
    for blk in range(2):
        o = blk * 64
        for i in range(16):
            R[o + 16 + i, o + i] = -1.0
            R[o + i, o + 16 + i] = 1.0
            R[o + 48 + i, o + 32 + i] = -1.0
            R[o + 32 + i, o + 48 + i] = 1.0
    c[:, C_ROT:C_ROT + 128] = R
    for b in range(3):
        c[b, C_SEL + b * 128:C_SEL + (b + 1) * 128] = 1.0
    c[:3, C_ID3:C_ID3 + 3] = np.eye(3)
    return c


def rope_tables(TX, TCX):
    T = TX + TCX
    rows = TX // 64
    row_id = np.repeat(np.arange(rows), 64).astype(np.float32)
    col_id = np.tile(np.arange(64), rows).astype(np.float32)
    inv = (10000.0 ** (-np.arange(0, 32, 2, dtype=np.float32) / 32)).astype(np.float32)
    ar = row_id[:, None] * inv
    ac = col_id[:, None] * inv
    ang = np.concatenate([ar, ar, ac, ac], axis=-1)
    cos = np.ones((T, 64), np.float32)
    sin = np.zeros((T, 64), np.float32)
    cos[TCX:] = np.cos(ang)
    sin[TCX:] = np.sin(ang)
    cs = np.concatenate([cos.T, cos.T], axis=0)
    sn = np.concatenate([sin.T, sin.T], axis=0)
    return np.ascontiguousarray(cs), np.ascontiguousarray(sn)


def colform(v, n):
    return np.ascontiguousarray(np.asarray(v, np.float32).reshape(n, 128).T)


def geom(TX, TCX):
    T = TX + TCX
    NT = T // 128
    TP = T + 4
    blocks = []
    p = 0
    while p < TCX:
        n = min(512, TCX - p)
        blocks.append((p, n, p + 1))
        p += n
    p = 0
    while p < TX:
        n = min(512, TX - p)
        blocks.append((TCX + p, n, TCX + 3 + p))
        p += n
    return T, NT, TP, blocks


def build_program(TX, TCX, phases="ABCDEFG", debug=()):
    T, NT, TP, blocks = geom(TX, TCX)
    NTC = TCX // 128
    nc = bass.Bass("TRN2", target_bir_lowering=False)
    dram = {}

    def din(name, shape, dt=F32):
        dram[name] = nc.dram_tensor(name, list(shape), dt, kind="ExternalInput").ap()
        return dram[name]

    def dscr(name, shape, dt=F32):
        kind = "ExternalOutput" if name in debug else "Internal"
        dram[name] = nc.dram_tensor(name, list(shape), dt, kind=kind).ap()
        return dram[name]

    seq = din("seq", [NB, T, D])
    csT = din("csT", [128, 8, 3])
    consts_d = din("consts", [128, NCONST])
    w_mod = din("w_mod", [D, 6 * D])
    b_mod = din("b_mod", [1, 6 * D])
    n1w = din("n1w", [128, 8])
    n2w = din("n2w", [128, 8])
    w_in = din("w_in", [D, INW])
    shift_w = din("shift_w", [3, 2048])
    rw_w0 = din("rw_w0", [128, 2, 4])
    rw_a0 = din("rw_a0", [128, 2, 4])
    rw_wup = din("rw_wup", [2, 64, RW])
    rw_aup = din("rw_aup", [2, 64, RW])
    rw_gup = din("rw_gup", [2, 128, RW])
    rw_kk = din("rw_kk", [128, 4])
    rw_ka = din("rw_ka", [128, 4])
    rw_rk = din("rw_rk", [128, 4])
    rw_lnw = din("rw_lnw", [128, 4])
    rw_lnb = din("rw_lnb", [128, 4])
    qnw = din("qnw", [128, 1])
    knw = din("knw", [128, 1])
    lamv = din("lamv", [1, 256])
    sublnw = din("sublnw", [128, 1])
    w_out = din("w_out", [D, D])
    w_rt = din("w_rt", [D, 36])
    b_rt = din("b_rt", [1, 36])
    moe_g = din("moe_g", [NE, D, FF])
    moe_u = din("moe_u", [NE, D, FF])
    moe_d = din("moe_d", [NE, FF, D])
    ropec = din("ropec", [128, T])
    ropes = din("ropes", [128, T])
    out_d = nc.dram_tensor("out", [NB, TX, D], F32, kind="ExternalOutput").ap()

    P_d = dscr("P_d", [NB, INW, T])
    NCH = T // 16
    cols_d = dscr("cols_d", [2, 128, NCH + 1, NB, 4, 16, 6], BF16)
    rows_d = dscr("rows_d", [2, 6, T, NB * 4, 128], BF16)
    v_d = dscr("v_d", [2, T, NB * 4, 64], BF16)
    g_d = dscr("g_d", [2, NB, 4, 128, T], BF16)
    bon_d = dscr("bon_d", [2, NB, 4, 128, T], BF16)
    y_d = dscr("y_d", [2, 2, T + 2, NB * 4, 64], BF16)
    qT_d = dscr("qT_d", [NB, 4, 128, T], BF16)
    kT_d = dscr("kT_d", [NB, 4, 128, T], BF16)
    vt_d = dscr("vt_d", [NB, T, 512], BF16)
    cat_d = dscr("cat_d", [NB, D, TX], BF16)
    x1_d = dscr("x1_d", [NB, TX, D])
    h2T_d = dscr("h2T_d", [NB, D, TX], BF16)
    wd_d = dscr("wd_d", [NB, TX, NE])

    with ExitStack() as es:
        k = K(nc, es)
        consts = k.sb("consts_sb", [128, NCONST], F32)
        cbf = k.sb("cbf", [128, 768], BF16)
        modT = k.sb("modT", [128, 48, 3], F32)
        A1 = k.sb("A1", [128, 8, 3], F32)
        A2 = k.sb("A2", [128, 8, 3], F32)
        G1 = k.sb("G1", [128, NB, D], F32)
        G2 = k.sb("G2", [128, NB, D], F32)
        eps_t = k.sb("eps_t", [128, 1], F32)
        b_consts = k.buf("consts")
        b_mod_ = k.buf("mod")
        ds_c = k.dsem("sp", "c")
        k.dma(ds_c, consts[:], consts_d[:, :], writes=[b_consts])
        k.op("dve", lambda e: e.tensor_copy(cbf[:], consts[:, 0:768]), reads=[b_consts], writes=[b_consts])
        k.op("dve", lambda e: e.memset(eps_t[:], 1e-6), writes=[b_consts])
        ident_bf = cbf[:, C_ID:C_ID + 128]
        identA_bf = cbf[:, C_IDA:C_IDA + 128]
        identB_bf = cbf[:, C_IDB:C_IDB + 128]
        bones_bf = cbf[:, C_BONES:C_BONES + 128]
        ones_bf = cbf[:, C_ONES:C_ONES + 128]
        rot_bf = cbf[:, C_ROT:C_ROT + 128]
        k.barrier()

        if "A" in phases:
            with ExitStack() as pes:
                k.phase_begin(pes)
                silT = k.sb("silT", [128, 8, 3], F32)
                modrow = k.sb("modrow", [3, 6 * D], F32)
                bmr = k.sb("bmr", [3, 6 * D], F32)
                n1c = k.sb("n1c", [128, 8], F32)
                n2c = k.sb("n2c", [128, 8], F32)
                wm = [k.sb("wm%d" % i, [128, 8, 1024], F32) for i in range(2)]
                pa = [k.ps("pa%d" % i, [3, 512]) for i in range(2)]
                pc = k.ps("pc", [128, 48, 3])
                pg = [k.ps("pg%d" % i, [128, 512]) for i in range(2)]
                b_sil, b_bmr, b_pc = k.buf(), k.buf(), k.buf()
                b_wm = [k.buf(), k.buf()]
                b_pa = [k.buf(), k.buf()]
                b_pg = [k.buf(), k.buf()]
                ds_a = k.dsem("sp", "a")
                ds_w = [k.dsem("sp", "wm0"), k.dsem("sp", "wm1")]
                k.dma(ds_a, silT[:], csT[:, :, :], writes=[b_sil])
                k.dma(ds_a, bmr[:], b_mod.partition_broadcast(3), writes=[b_bmr])
                k.dma(ds_a, n1c[:], n1w[:, :], writes=[b_bmr])
                k.dma(ds_a, n2c[:], n2w[:, :], writes=[b_bmr])
                k.op("act", lambda e: e.activation(out=silT[:], in_=silT[:], func=AF.Silu), reads=[b_sil], writes=[b_sil])
                wmv = w_mod.rearrange("(kc p) n -> p kc n", p=128)
                for m in range(6):
                    s = m % 2
                    k.dma(ds_w[s], wm[s][:], wmv[:, :, m * 1024:(m + 1) * 1024], writes=[b_wm[s]])
                    for blk in range(2):
                        pb = (m * 2 + blk) % 2
                        for kc in range(8):
                            k.op("pe", lambda e, kc=kc, s=s, blk=blk, pb=pb: e.matmul(
                                pa[pb][:], silT[:, kc, :], wm[s][:, kc, blk * 512:(blk + 1) * 512],
                                start=(kc == 0), stop=(kc == 7)), reads=[b_sil, b_wm[s]], writes=[b_pa[pb]])
                        c0 = m * 1024 + blk * 512
                        k.op("dve", lambda e, pb=pb, c0=c0: e.tensor_tensor(
                            out=modrow[:, c0:c0 + 512], in0=pa[pb][:], in1=bmr[:, c0:c0 + 512], op=ALU.add),
                            reads=[b_pa[pb], b_bmr], writes=[b_mod_])
                for f in range(48):
                    k.op("pe", lambda e, f=f: e.matmul(pc[:, f, :], modrow[0:3, f * 128:(f + 1) * 128],
                                                      consts[0:3, C_ID3:C_ID3 + 3], start=True, stop=True),
                         reads=[b_mod_, b_consts], writes=[b_pc])
                k.op("dve", lambda e: e.tensor_copy(modT[:], pc[:]), reads=[b_pc], writes=[b_mod_])
                k.op("dve", lambda e: e.scalar_tensor_tensor(
                    out=A1[:], in0=modT[:, 8:16, :], scalar=1.0, in1=_bc(n1c[:].unsqueeze(2), [128, 8, 3]),
                    op0=ALU.add, op1=ALU.mult), reads=[b_mod_, b_bmr], writes=[b_mod_])
                k.op("dve", lambda e: e.scalar_tensor_tensor(
                    out=A2[:], in0=modT[:, 32:40, :], scalar=1.0, in1=_bc(n2c[:].unsqueeze(2), [128, 8, 3]),
                    op0=ALU.add, op1=ALU.mult), reads=[b_mod_, b_bmr], writes=[b_mod_])
                i = 0
                for gi, Gt in ((2, G1), (5, G2)):
                    for b in range(NB):
                        for blk in range(2):
                            pb = i % 2
                            i += 1
                            c0 = gi * 1024 + blk * 512
                            k.op("pe", lambda e, b=b, c0=c0, pb=pb: e.matmul(
                                pg[pb][:], consts[0:3, C_SEL + b * 128:C_SEL + (b + 1) * 128],
                                modrow[0:3, c0:c0 + 512], start=True, stop=True),
                                reads=[b_mod_, b_consts], writes=[b_pg[pb]])
                            k.op("act", lambda e, Gt=Gt, b=b, blk=blk, pb=pb: e.activation(
                                out=Gt[:, b, blk * 512:(blk + 1) * 512], in_=pg[pb][:], func=AF.Copy),
                                reads=[b_pg[pb]], writes=[b_mod_])
                k.barrier()
                k.phase_end(es)
        B1 = modT[:, 0:8, :]
        B2 = modT[:, 24:32, :]

        if "dbgA" in debug:
            pass

        if "B" in phases:
            with ExitStack() as pes:
                k.phase_begin(pes)
                hT = k.sb("hT", [128, 8, TP], BF16)
                xt = [k.sb("xt%d" % i, [128, D], F32) for i in range(2)]
                sq = k.sb("sq", [128, D], F32)
                ss = k.sb("ss", [128, 4], F32)
                xn = [k.sb("xn%d" % i, [128, D], BF16) for i in range(2)]
                pt = [k.ps("pt%d" % i, [128, 8, 128], BF16) for i in range(2)]
                wst = [k.sb("wst%d" % i, [128, 8, 128], F32) for i in range(2)]
                wbf = [k.sb("wbf%d" % i, [128, 8, 3, 128], BF16) for i in range(2)]
                swb = k.sb("swb", [128, 3, 2048], F32)
                pp = [k.ps("pp%d" % i, [128, 512]) for i in range(4)]
                ev = [k.sb("ev%d" % i, [128, 512], F32) for i in range(4)]
                b_hT = k.buf("hT")
                b_xt, b_xn, b_pt = [k.buf(), k.buf()], [k.buf(), k.buf()], [k.buf(), k.buf()]
                b_sq, b_ss, b_swb = k.buf(), k.buf(), k.buf()
                b_wst, b_wbf = [k.buf(), k.buf()], [k.buf(), k.buf()]
                b_pp, b_ev = [k.buf() for _ in range(4)], [k.buf() for _ in range(4)]
                ds_x = [k.dsem("sp", "x0"), k.dsem("sp", "x1")]
                ds_ws = [k.dsem("sp", "ws0"), k.dsem("sp", "ws1")]
                ds_sw = k.dsem("sp", "sw")
                ds_ev = [k.dsem("pool", "ev%d" % i) for i in range(4)]
                k.dma(ds_sw, swb[:].rearrange("p a b -> p (a b)"),
                      shift_w.rearrange("a b -> (a b)").unsqueeze(0).partition_broadcast(128)
                      if False else shift_w.rearrange("(o a) b -> o (a b)", o=1).partition_broadcast(128),
                      writes=[b_swb])
                k.op("pool", lambda e: e.memset(hT[:], 0.0), writes=[b_hT])
                w_in_v = w_in.rearrange("(kc p) n -> p kc n", p=128)
                for b in range(NB):
                    for q in range(NT):
                        s = q % 2
                        sel = 2 if q < NTC else b
                        col0 = (q * 128 + 1) if q < NTC else (q * 128 + 3)
                        k.dma(ds_x[s], xt[s][:], seq[b, q * 128:(q + 1) * 128, :], writes=[b_xt[s]])
                        k.op("act", lambda e, s=s: e.activation(out=sq[:], in_=xt[s][:], func=AF.Square),
                             reads=[b_xt[s]], writes=[b_sq])
                        k.op("dve", lambda e: e.tensor_reduce(out=ss[:, 0:1], in_=sq[:], axis=AX.X, op=ALU.add),
                             reads=[b_sq], writes=[b_ss])
                        k.op("act", lambda e: e.activation(out=ss[:, 1:2], in_=ss[:, 0:1], func=AF.Sqrt,
                                                           bias=eps_t[:, 0:1], scale=1.0 / D),
                             reads=[b_ss, b_consts], writes=[b_ss])
                        k.op("dve", lambda e: e.reciprocal(ss[:, 2:3], ss[:, 1:2]), reads=[b_ss], writes=[b_ss])
                        k.op("dve", lambda e, s=s: e.tensor_scalar(out=xn[s][:], in0=xt[s][:], scalar1=ss[:, 2:3],
                                                                   scalar2=None, op0=ALU.mult),
                             reads=[b_xt[s], b_ss], writes=[b_xn[s]])
                        for kc in range(8):
                            k.op("pe", lambda e, s=s, kc=kc: e.transpose(out=pt[s][:, kc, :],
                                                                         in_=xn[s][:, kc * 128:(kc + 1) * 128],
                                                                         identity=ident_bf),
                                 reads=[b_xn[s], b_consts], writes=[b_pt[s]])
                        for kc in range(8):
                            k.op("act", lambda e, s=s, kc=kc, sel=sel, col0=col0: e.activation(
                                out=hT[:, kc, col0:col0 + 128], in_=pt[s][:, kc, :], func=AF.Identity,
                                bias=B1[:, kc, sel:sel + 1], scale=A1[:, kc, sel:sel + 1]),
                                reads=[b_pt[s], b_mod_], writes=[b_hT])
                    ie = 0
                    for c in range(28):
                        s = c % 2
                        k.dma(ds_ws[s], wst[s][:], w_in_v[:, :, c * 128:(c + 1) * 128], writes=[b_wst[s]])
                        ntap = 3 if c < 16 else 1
                        if c < 16:
                            for j in range(3):
                                eng = "pool" if j == 1 else "dve"
                                k.op(eng, lambda e, s=s, j=j, c=c: e.tensor_tensor(
                                    out=wbf[s][:, :, j, :], in0=wst[s][:],
                                    in1=_bc(swb[:, j, c * 128:(c + 1) * 128].unsqueeze(1), [128, 8, 128]),
                                    op=ALU.mult), reads=[b_wst[s], b_swb], writes=[b_wbf[s]])
                        else:
                            k.op("dve", lambda e, s=s: e.tensor_copy(wbf[s][:, :, 1, :], wst[s][:]),
                                 reads=[b_wst[s]], writes=[b_wbf[s]])
                        for (pos0, N, colb) in blocks:
                            pi = ie % 4
                            ie += 1
                            taps = (0, 1, 2) if c < 16 else (1,)
                            nmm = len(taps) * 8
                            im = 0
                            for j in taps:
                                for kc in range(8):
                                    k.op("pe", lambda e, pi=pi, s=s, kc=kc, j=j, colb=colb, N=N, im=im, nmm=nmm: e.matmul(
                                        pp[pi][:, 0:N], wbf[s][:, kc, j, :], hT[:, kc, colb + j - 1:colb + j - 1 + N],
                                        start=(im == 0), stop=(im == nmm - 1)),
                                        reads=[b_wbf[s], b_hT], writes=[b_pp[pi]])
                                    im += 1
                            eng = "act" if pi % 2 == 0 else "dve"
                            if eng == "act":
                                k.op("act", lambda e, pi=pi, N=N: e.activation(out=ev[pi][:, 0:N], in_=pp[pi][:, 0:N], func=AF.Copy),
                                     reads=[b_pp[pi]], writes=[b_ev[pi]])
                            else:
                                k.op("dve", lambda e, pi=pi, N=N: e.tensor_copy(ev[pi][:, 0:N], pp[pi][:, 0:N]),
                                     reads=[b_pp[pi]], writes=[b_ev[pi]])
                            k.dma(ds_ev[pi], P_d[b, c * 128:(c + 1) * 128, pos0:pos0 + N], ev[pi][:, 0:N],
                                  reads=[b_ev[pi]])
                    k.barrier()
                k.phase_end(es)

        if "C" in phases:
            with ExitStack() as pes:
                k.phase_begin(pes)
                PB = [k.sb("PB%d" % i, [128, 16, 512], F32) for i in range(2)]
                b_PB = [k.buf(), k.buf()]
                ds_pb = [k.dsem("sp", "pb0"), k.dsem("sp", "pb1")]
                ds_st = k.dsem("sp", "cst")
                ds_o = [k.dsem("pool", "co%d" % i) for i in range(4)]
                wst_c = k.sb("wst_c", [128, 512], F32)
                Wwa = k.sb("Wwa", [128, 2, 512], BF16)
                Wg = k.sb("Wg", [128, 2, 512], BF16)
                colc = k.sb("colc", [128, 40], F32)
                b_w = k.buf("cw")
                for d in range(2):
                    k.dma(ds_st, wst_c[0:64, :], rw_wup[d], writes=[b_w])
                    k.dma(ds_st, wst_c[64:128, :], rw_aup[d], writes=[b_w])
                    k.op("dve", lambda e, d=d: e.tensor_copy(Wwa[:, d, :], wst_c[:]), reads=[b_w], writes=[b_w])
                    k.dma(ds_st, wst_c[:], rw_gup[d], writes=[b_w])
                    k.op("dve", lambda e, d=d: e.tensor_copy(Wg[:, d, :], wst_c[:]), reads=[b_w], writes=[b_w])
                k.dma(ds_st, colc[:, 0:8], rw_w0.rearrange("p a b -> p (a b)"), writes=[b_w])
                k.dma(ds_st, colc[:, 8:16], rw_a0.rearrange("p a b -> p (a b)"), writes=[b_w])
                k.dma(ds_st, colc[:, 16:20], rw_kk[:, :], writes=[b_w])
                k.dma(ds_st, colc[:, 20:24], rw_ka[:, :], writes=[b_w])
                k.dma(ds_st, colc[:, 24:28], rw_rk[:, :], writes=[b_w])
                k.op("dve", lambda e: e.tensor_scalar(out=colc[:, 28:32], in0=colc[:, 20:24], scalar1=-1.0, scalar2=1.0,
                                                      op0=ALU.mult, op1=ALU.add), reads=[b_w], writes=[b_w])
                Vbf = k.sb("Vbf", [128, 4, 512], BF16)
                RH = k.sb("RH", [128, 4, 514], F32)
                CAx = k.sb("CAx", [128, 6], BF16)
                b_RH = k.buf("RH")
                ds_rh = k.dsem("sp", "rh")
                k.op("pool", lambda e: e.memset(CAx[:], 0.0), writes=[b_RH])
                TLs = [k.sb("TL%d" % i, [128, 512], BF16) for i in range(2)]
                SGs = [k.sb("SG%d" % i, [128, 512], BF16) for i in range(2)]
                f32ts = [{n: k.sb("c_%s%d" % (n, i), [128, 512], F32) for n in ("sgw", "dec", "Aa", "kkf", "sd", "kkn", "tmpk", "kd")} for i in range(2)]
                bfts = [{n: k.sb("c_%s%d" % (n, i), [128, 512], BF16) for n in ("Gg", "kk2", "bsc", "kdb", "rkr", "bon")} for i in range(2)]
                CAb = k.sb("CAb", [128, 32, 4, 16, 6], BF16)
                CAw = CAb[:].bitcast(F32)
                bCA = k.buf("CAb")
                rowsbs = [k.sb("rowsb%d" % i, [128, 4, 128], BF16) for i in range(2)]
                vrow = k.sb("vrow", [128, 4, 128], BF16)
                pw, pa_, pg_, pss, pbo = (k.ps(n, [128, 512]) for n in ("pw", "pa_", "pg_", "pss", "pbo"))
                prow = k.ps("prow", [128, 4, 128])
                pv = k.ps("pv", [128, 4, 128])
                bbs = [{n: k.buf(n) for n in ("TL", "SG", "sgw", "dec", "Aa", "kkf", "sd", "kkn", "tmpk", "kd", "Gg", "kk2",
                                              "bsc", "kdb", "rkr", "bon", "CA", "rowsb")} for i in range(2)]
                bbg = {n: k.buf(n) for n in ("Vbf", "vrow", "pw", "pa_", "pg_", "pss", "pbo", "prow", "pv")}
                for i in range(2):
                    bbs[i].update(bbg)
                bb = bbs[0]
                k.op("pool", lambda e: e.memset(CAb[:], 0.0), writes=[bCA])
                ihp = 0
                idd = 0
                irow = 0
                ztile = k.sb("ztile", [128, 1024], BF16)
                b_z = k.buf("z")
                k.op("pool", lambda e: e.memset(ztile[:], 0.0), writes=[b_z])
                for d in range(2):
                    zv = rows_d[d, 2:4].rearrange("r t s c -> (r t) (s c)")
                    for i in range(2 * T // 128):
                        k.dma(ds_o[i % 4], zv[i * 128:(i + 1) * 128, :], ztile[:], reads=[b_z])
                ib = 0
                for b in range(NB):
                    for (pos0, N, _c) in blocks:
                        s = ib % 2
                        ib += 1
                        P = PB[s]
                        bP = b_PB[s]
                        k.dma(ds_pb[s], P[:, :, 0:N], P_d[b, 0:2048, pos0:pos0 + N].rearrange("(c p) n -> p c n", p=128),
                              writes=[bP])
                        k.op("pool", lambda e: e.memset(RH[:], 0.0), writes=[b_RH])
                        lo = max(pos0 - 1, 0)
                        hi = min(pos0 + N + 1, T)
                        co = lo - (pos0 - 1)
                        k.dma(ds_rh, RH[:, :, co:co + hi - lo], P_d[b, 0:512, lo:hi].rearrange("(c p) n -> p c n", p=128), writes=[b_RH])
                        k.op("pool", lambda e, P=P, N=N: e.tensor_copy(Vbf[:, :, 0:N], P[:, 8:12, 0:N]), reads=[bP], writes=[bb["Vbf"]])
                        for j in range(N // 128):
                            for hp in range(4):
                                k.op("pe", lambda e, hp=hp, j=j: e.matmul(pv[:, hp, :], Vbf[:, hp, j * 128:(j + 1) * 128], ident_bf,
                                                                         start=True, stop=True),
                                     reads=[bb["Vbf"], b_consts], writes=[bb["pv"]])
                            k.op("act", lambda e: e.activation(out=vrow[:], in_=pv[:], func=AF.Copy), reads=[bb["pv"]], writes=[bb["vrow"]])
                            p0 = pos0 + j * 128
                            for ab in range(2):
                                k.dma(ds_o[ab], v_d[ab, p0:p0 + 128, b * 4:(b + 1) * 4, :], vrow[:, :, ab * 64:(ab + 1) * 64],
                                      reads=[bb["vrow"]])
                        for d in range(2):
                            TL = TLs[idd % 2]
                            SG = SGs[idd % 2]
                            bbd = bbs[idd % 2]
                            idd += 1
                            k.op("act", lambda e, P=P, N=N, d=d, TL=TL: e.activation(out=TL[0:64, 0:N], in_=P[0:64, 12 + 2 * d, 0:N], func=AF.Tanh),
                                 reads=[bP], writes=[bbd["TL"]])
                            k.op("dve", lambda e, P=P, N=N, d=d: e.tensor_copy(TL[64:128, 0:N], P[64:128, 12 + 2 * d, 0:N]),
                                 reads=[bP], writes=[bbd["TL"]])
                            k.op("act", lambda e, P=P, N=N, d=d: e.activation(out=SG[:, 0:N], in_=P[:, 13 + 2 * d, 0:N], func=AF.Sigmoid),
                                 reads=[bP], writes=[bbd["SG"]])
                            for hp in range(4):
                                hs = slice(hp * 128, (hp + 1) * 128)
                                t = f32ts[ihp % 2]
                                u = bfts[ihp % 2]
                                bb = dict(bbs[ihp % 2])
                                bb["TL"] = bbd["TL"]
                                bb["SG"] = bbd["SG"]
                                bb["CA"] = bCA
                                nch = N // 16
                                c16 = lambda ap: ap.rearrange("p (c s) -> p c s", s=16)
                                ihp += 1
                                k.op("pe", lambda e, d=d, hs=hs, N=N: e.matmul(pw[:, 0:N], Wwa[0:64, d, hs], TL[0:64, 0:N], start=True, stop=True),
                                     reads=[b_w, bb["TL"]], writes=[bb["pw"]])
                                k.op("pe", lambda e, d=d, hs=hs, N=N: e.matmul(pa_[:, 0:N], Wwa[64:128, d, hs], TL[64:128, 0:N], start=True, stop=True),
                                     reads=[b_w, bb["TL"]], writes=[bb["pa_"]])
                                k.op("pe", lambda e, d=d, hs=hs, N=N: e.matmul(pg_[:, 0:N], Wg[:, d, hs], SG[:, 0:N], start=True, stop=True),
                                     reads=[b_w, bb["SG"]], writes=[bb["pg_"]])
                                ci = d * 4 + hp
                                k.op("act", lambda e, N=N, ci=ci: e.activation(out=t["sgw"][:, 0:N], in_=pw[:, 0:N], func=AF.Sigmoid,
                                                                               bias=colc[:, ci:ci + 1], scale=1.0),
                                     reads=[bb["pw"], b_w], writes=[bb["sgw"]])
                                k.op("act", lambda e, N=N: e.activation(out=t["dec"][:, 0:N], in_=t["sgw"][:, 0:N], func=AF.Exp,
                                                                        scale=-math.exp(-0.5)),
                                     reads=[bb["sgw"]], writes=[bb["dec"]])
                                k.op("dve", lambda e, N=N: e.tensor_copy(CAw[:, 0:nch, hp, :, 2], c16(t["dec"][:, 0:N])),
                                     reads=[bb["dec"]], writes=[bCA])
                                k.op("act", lambda e, N=N, ci=ci: e.activation(out=t["Aa"][:, 0:N], in_=pa_[:, 0:N], func=AF.Sigmoid,
                                                                               bias=colc[:, 8 + ci:9 + ci], scale=1.0),
                                     reads=[bb["pa_"], b_w], writes=[bb["Aa"]])
                                k.op("act", lambda e, N=N: e.activation(out=u["Gg"][:, 0:N], in_=pg_[:, 0:N], func=AF.Copy),
                                     reads=[bb["pg_"]], writes=[bb["Gg"]])
                                k.dma(ds_o[3], g_d[d, b, hp, :, pos0:pos0 + N], u["Gg"][:, 0:N], reads=[bb["Gg"]])
                                kk_ = P[:, 4 + hp, 0:N]
                                r_ = P[:, hp, 0:N]
                                v_ = P[:, 8 + hp, 0:N]
                                k.op("dve", lambda e, N=N, hp=hp, kk_=kk_: e.tensor_scalar(out=t["kkf"][:, 0:N], in0=kk_, scalar1=colc[:, 16 + hp:17 + hp],
                                                                                        scalar2=None, op0=ALU.mult),
                                     reads=[bP, b_w], writes=[bb["kkf"]])
                                k.op("act", lambda e, N=N: e.activation(out=u["kk2"][:, 0:N], in_=t["kkf"][:, 0:N], func=AF.Square),
                                     reads=[bb["kkf"]], writes=[bb["kk2"]])
                                k.op("pe", lambda e, N=N: e.matmul(pss[:, 0:N], bones_bf, u["kk2"][:, 0:N], start=True, stop=True),
                                     reads=[bb["kk2"], b_consts], writes=[bb["pss"]])
                                k.op("act", lambda e, N=N: e.activation(out=t["sd"][:, 0:N], in_=pss[:, 0:N], func=AF.Sqrt),
                                     reads=[bb["pss"]], writes=[bb["sd"]])
                                k.op("dve", lambda e, N=N: e.tensor_scalar(out=t["sd"][:, 0:N], in0=t["sd"][:, 0:N], scalar1=1e-12, scalar2=None, op0=ALU.max),
                                     reads=[bb["sd"]], writes=[bb["sd"]])
                                k.op("dve", lambda e, N=N: e.reciprocal(t["sd"][:, 0:N], t["sd"][:, 0:N]), reads=[bb["sd"]], writes=[bb["sd"]])
                                k.op("dve", lambda e, N=N: e.tensor_tensor(out=t["kkn"][:, 0:N], in0=t["kkf"][:, 0:N], in1=t["sd"][:, 0:N], op=ALU.mult),
                                     reads=[bb["kkf"], bb["sd"]], writes=[bb["kkn"]])
                                k.op("dve", lambda e, N=N: e.tensor_tensor(out=u["bsc"][:, 0:N], in0=t["kkn"][:, 0:N], in1=t["Aa"][:, 0:N], op=ALU.mult),
                                     reads=[bb["kkn"], bb["Aa"]], writes=[bb["bsc"]])
                                k.op("dve", lambda e, N=N, hp=hp: e.tensor_scalar(out=t["tmpk"][:, 0:N], in0=t["Aa"][:, 0:N], scalar1=colc[:, 20 + hp:21 + hp],
                                                                                 scalar2=colc[:, 28 + hp:29 + hp], op0=ALU.mult, op1=ALU.add),
                                     reads=[bb["Aa"], b_w], writes=[bb["tmpk"]])
                                k.op("dve", lambda e, N=N, kk_=kk_: e.tensor_tensor(out=t["kd"][:, 0:N], in0=kk_, in1=t["tmpk"][:, 0:N], op=ALU.mult),
                                     reads=[bP, bb["tmpk"]], writes=[bb["kd"]])
                                k.op("act", lambda e, N=N: e.activation(out=u["kdb"][:, 0:N], in_=t["kd"][:, 0:N], func=AF.Copy),
                                     reads=[bb["kd"]], writes=[bb["kdb"]])
                                k.op("dve", lambda e, N=N, hp=hp, r_=r_: e.scalar_tensor_tensor(out=u["rkr"][:, 0:N], in0=r_, scalar=colc[:, 24 + hp:25 + hp],
                                                                                             in1=t["kd"][:, 0:N], op0=ALU.mult, op1=ALU.mult),
                                     reads=[bP, bb["kd"], b_w], writes=[bb["rkr"]])
                                k.op("pe", lambda e, N=N: e.matmul(pbo[:, 0:N], bones_bf, u["rkr"][:, 0:N], start=True, stop=True),
                                     reads=[bb["rkr"], b_consts], writes=[bb["pbo"]])
                                k.op("dve", lambda e, N=N, v_=v_: e.tensor_tensor(out=u["bon"][:, 0:N], in0=pbo[:, 0:N], in1=v_, op=ALU.mult),
                                     reads=[bb["pbo"], bP], writes=[bb["bon"]])
                                k.dma(ds_o[0], bon_d[d, b, hp, :, pos0:pos0 + N], u["bon"][:, 0:N], reads=[bb["bon"]])
                                k.op("dve", lambda e, N=N: e.tensor_scalar(out=CAb[0:64, 0:nch, hp, :, 0], in0=c16(t["kkn"][0:64, 0:N]), scalar1=-1.0, scalar2=None, op0=ALU.mult),
                                     reads=[bb["kkn"]], writes=[bb["CA"]])
                                k.op("dve", lambda e, N=N: e.tensor_scalar(out=CAb[64:128, 0:nch, hp, :, 1], in0=c16(t["kkn"][64:128, 0:N]), scalar1=-1.0, scalar2=None, op0=ALU.mult),
                                     reads=[bb["kkn"]], writes=[bb["CA"]])
                                ro = 0 if d == 0 else 2
                                k.op("act", lambda e, N=N, hp=hp, ro=ro: e.activation(out=CAb[0:64, 0:nch, hp, :, 2], in_=c16(RH[0:64, hp, ro:ro + N]), func=AF.Copy),
                                     reads=[b_RH], writes=[bb["CA"]])
                                k.op("act", lambda e, N=N, hp=hp, ro=ro: e.activation(out=CAb[64:128, 0:nch, hp, :, 3], in_=c16(RH[64:128, hp, ro:ro + N]), func=AF.Copy),
                                     reads=[b_RH], writes=[bb["CA"]])
                                if d == 0 and pos0 + N == T:
                                    k.op("act", lambda e, N=N, hp=hp: e.activation(out=CAx[0:64, 2:3], in_=RH[0:64, hp, N:N + 1], func=AF.Copy),
                                         reads=[b_RH], writes=[b_RH])
                                    k.op("act", lambda e, N=N, hp=hp: e.activation(out=CAx[64:128, 3:4], in_=RH[64:128, hp, N:N + 1], func=AF.Copy),
                                         reads=[b_RH], writes=[b_RH])
                                    k.dma(ds_rh, cols_d[0, :, NCH, b, hp, 0, :], CAx[:], reads=[b_RH])
                                if hp == 3:
                                    k.dma(ds_o[1], cols_d[d, :, pos0 // 16:pos0 // 16 + nch, b, :, :, :], CAb[:, 0:nch], reads=[bCA])
                                for j in range(N // 128):
                                    js = slice(j * 128, (j + 1) * 128)
                                    rowsb = rowsbs[irow % 2]
                                    bb["rowsb"] = bbs[irow % 2]["rowsb"]
                                    irow += 1
                                    k.op("pe", lambda e, js=js: e.matmul(prow[:, 0, :], u["bsc"][:, js], identA_bf, start=True, stop=True),
                                         reads=[bb["bsc"], b_consts], writes=[bb["prow"]])
                                    k.op("pe", lambda e, js=js: e.matmul(prow[:, 1, :], u["bsc"][:, js], identB_bf, start=True, stop=True),
                                         reads=[bb["bsc"], b_consts], writes=[bb["prow"]])
                                    k.op("pe", lambda e, js=js: e.matmul(prow[:, 2, :], u["kdb"][:, js], identA_bf, start=True, stop=True),
                                         reads=[bb["kdb"], b_consts], writes=[bb["prow"]])
                                    k.op("pe", lambda e, js=js: e.matmul(prow[:, 3, :], u["kdb"][:, js], identB_bf, start=True, stop=True),
                                         reads=[bb["kdb"], b_consts], writes=[bb["prow"]])
                                    k.op("dve", lambda e: e.tensor_copy(rowsb[:], prow[:]), reads=[bb["prow"]], writes=[bb["rowsb"]])
                                    p0 = pos0 + j * 128
                                    k.dma(ds_o[2], rows_d[d, 0:2, p0:p0 + 128, b * 4 + hp, :].rearrange("r t c -> t r c"), rowsb[:, 0:2, :],
                                          reads=[bb["rowsb"]])
                                    k.dma(ds_o[3], rows_d[d, 4:6, p0:p0 + 128, b * 4 + hp, :].rearrange("r t c -> t r c"), rowsb[:, 2:4, :],
                                          reads=[bb["rowsb"]])
                k.barrier()
                k.phase_end(es)

        if "D" in phases:
            with ExitStack() as pes:
                k.phase_begin(pes)
                CH = 16
                ST = k.sb("ST", [128, 2, 8, 64], F32)
                T1 = k.sb("T1", [128, 2, 8, 64], F32)
                STb = k.sb("STb", [128, 2, 8, 64], BF16)
                colsAR = [k.sb("colsAR%d" % g, [128, 1, 8, CH, 6], BF16) for g in range(2)]
                wv = [colsAR[g][:].bitcast(F32) for g in range(2)]
                rowsL = [k.sb("rowsL%d" % g, [6, CH, 8, 128], BF16) for g in range(2)]
                stage = [k.sb("stage%d" % g, [6, CH, 8, 64], BF16) for g in range(2)]
                ps1 = [k.ps("ps1%d" % g, [4, 8, 64]) for g in range(2)]
                ps2 = [k.ps("ps2%d" % g, [128, 8, 64]) for g in range(2)]
                b_ST, b_T1, b_STb = [k.buf(), k.buf()], [k.buf(), k.buf()], [k.buf(), k.buf()]
                b_cols, b_wc = [k.buf(), k.buf()], [k.buf(), k.buf()]
                b_rows, b_stv, b_sty = [k.buf(), k.buf()], [k.buf(), k.buf()], [k.buf(), k.buf()]
                b_ps1, b_ps2 = [k.buf(), k.buf()], [k.buf(), k.buf()]
                ds_g = [k.dsem("sp", "dg0"), k.dsem("pool", "dg1")]
                ds_gr = [k.dsem("sp", "dgr0"), k.dsem("pool", "dgr1")]
                ds_gv = [k.dsem("sp", "dgv0"), k.dsem("pool", "dgv1")]
                ds_y = [k.dsem("sp", "dy0"), k.dsem("pool", "dy1")]
                k.op("dve", lambda e: e.memset(ST[:], 0.0), writes=b_ST)
                k.op("dve", lambda e: e.memset(STb[:], 0.0), writes=b_STb)
                for g in range(2):
                    k.op("pool", lambda e, g=g: e.memset(stage[g][:], 0.0), writes=[b_stv[g], b_sty[g]])
                cols_v = [cols_d[g].rearrange("p c b h s x -> p c (b h) s x") for g in range(2)]

                def dsl(start, size):
                    if isinstance(start, int):
                        return slice(start, start + size)
                    return bass.ds(start, size)

                def scan_body(cbase, n):
                    def body(it):
                        cidx = [cbase + it, (cbase + n - 1) - it]
                        for g in range(2):
                            p0 = cidx[g] * CH
                            k.dma(ds_g[g], colsAR[g][:], cols_v[g][:, dsl(cidx[g], 1)], writes=[b_cols[g], b_wc[g]])
                            k.dma(ds_gr[g], rowsL[g][:], rows_d[g, :, dsl(p0, CH), :, :], writes=[b_rows[g]])
                            k.dma(ds_gv[g], stage[g][4:6], v_d[:, dsl(p0, CH), :, :], writes=[b_stv[g]])
                        for st_ in range(CH):
                            tl = [st_, CH - 1 - st_]
                            for g in range(2):
                                for pr in range(8):
                                    k.op("pe", lambda e, g=g, pr=pr, t_=tl[g]: e.matmul(
                                        ps1[g][0:4, pr, :], colsAR[g][:, 0, pr, t_, 0:4], STb[:, g, pr, :], start=True, stop=True),
                                        reads=[b_cols[g], b_STb[g]], writes=[b_ps1[g]])
                            for g in range(2):
                                k.op("act", lambda e, g=g, t_=tl[g]: e.activation(
                                    out=stage[g][0:4, t_, :, :], in_=ps1[g][0:4, :, :], func=AF.Copy),
                                    reads=[b_ps1[g]], writes=[b_sty[g]])
                            for g in range(2):
                                k.op("pool", lambda e, g=g, t_=tl[g]: e.tensor_tensor(
                                    out=T1[:, g], in0=ST[:, g], in1=_bc(wv[g][:, 0, :, t_, 2:3], [128, 8, 64]), op=ALU.mult),
                                    reads=[b_ST[g], b_wc[g]], writes=[b_T1[g]])
                            for g in range(2):
                                for pr in range(8):
                                    k.op("pe", lambda e, g=g, pr=pr, t_=tl[g]: e.matmul(
                                        ps2[g][:, pr, :], rowsL[g][0:6, t_, pr, :], stage[g][0:6, t_, pr, :],
                                        start=True, stop=True),
                                        reads=[b_rows[g], b_stv[g], b_sty[g]], writes=[b_ps2[g]])
                            for g in range(2):
                                k.op("dve", lambda e, g=g: e.tensor_tensor(out=STb[:, g], in0=T1[:, g], in1=ps2[g][:], op=ALU.add),
                                     reads=[b_T1[g], b_ps2[g]], writes=[b_STb[g]])
                                k.op("dve", lambda e, g=g: e.tensor_tensor(out=ST[:, g], in0=T1[:, g], in1=ps2[g][:], op=ALU.add),
                                     reads=[b_T1[g], b_ps2[g]], writes=[b_ST[g]])
                        for g in range(2):
                            p0 = cidx[g] * CH
                            k.dma(ds_y[g], y_d[g, :, dsl(p0 + 1, CH), :, :], stage[g][2:4], reads=[b_sty[g]])
                    return body

                k.loop(TCX // CH, scan_body(0, TCX // CH))
                k.loop(TX // CH, scan_body(TCX // CH, TX // CH))
                for g, pv_ in ((0, T), (1, TCX - 1)):
                    k.dma(ds_g[g], colsAR[g][:], cols_v[g][:, pv_ // 16:pv_ // 16 + 1], writes=[b_cols[g]])
                    for pr in range(8):
                        k.op("pe", lambda e, g=g, pr=pr: e.matmul(ps1[g][0:4, pr, :], colsAR[g][:, 0, pr, pv_ % 16, 0:4], STb[:, g, pr, :],
                                                                  start=True, stop=True),
                             reads=[b_cols[g], b_STb[g]], writes=[b_ps1[g]])
                    k.op("act", lambda e, g=g: e.activation(out=stage[g][0:4, 0, :, :], in_=ps1[g][0:4, :, :], func=AF.Copy),
                         reads=[b_ps1[g]], writes=[b_sty[g]])
                    k.dma(ds_y[g], y_d[g, :, pv_ + 1:pv_ + 2, :, :], stage[g][2:4, 0:1], reads=[b_sty[g]])
                k.phase_end(es)

        xblocks = [(p, n) for (p, n, _c) in blocks if p >= TCX]
        if "E" in phases:
            with ExitStack() as pes:
                k.phase_begin(pes)
                Gt = k.sb("Gt", [128, 2, 4, 512], BF16)
                Bt = k.sb("Bt", [128, 2, 4, 512], BF16)
                Yt = k.sb("Yt", [128, 2, 4, 2, 64], BF16)
                Yf = k.sb("Yf", [128, 16, 64], F32)
                cen = k.sb("cen", [128, 16, 64], F32)
                sqe = k.sb("sqe", [128, 16, 64], F32)
                st4 = k.sb("st4", [128, 4, 16], F32)
                yh = k.sb("yh", [128, 2, 512], BF16)
                Zn = k.sb("Zn", [128, 2, 4, 128], F32)
                catR = k.sb("catR", [128, 4, 512], BF16)
                lnc = k.sb("lnc", [128, 8], F32)
                gne = k.sb("gne", [128, 1], F32)
                pte = k.ps("pte", [128, 8, 128], BF16)
                bG, bB, bY, bYf, bcen, bsq, bst, byh, bZn, bcat, bln, bpte = (k.buf() for _ in range(12))
                ds_e = [k.dsem("sp", "e%d" % i) for i in range(3)]
                ds_eo = k.dsem("pool", "eo")
                k.dma(ds_e[2], lnc[:, 0:4], rw_lnw[:, :], writes=[bln])
                k.dma(ds_e[2], lnc[:, 4:8], rw_lnb[:, :], writes=[bln])
                k.op("dve", lambda e: e.memset(gne[:], 64e-5), writes=[bln])
                for b in range(NB):
                    for (pos0, N) in xblocks:
                        for d in range(2):
                            k.dma(ds_e[0], Gt[:, d, :, 0:N], g_d[d, b, :, :, pos0:pos0 + N].rearrange("h p t -> p h t"), writes=[bG])
                            k.dma(ds_e[0], Bt[:, d, :, 0:N], bon_d[d, b, :, :, pos0:pos0 + N].rearrange("h p t -> p h t"), writes=[bB])
                        for j in range(N // 128):
                            pos = pos0 + j * 128
                            for d in range(2):
                                sl0 = pos + 2 if d == 0 else pos
                                for ab in range(2):
                                    k.dma(ds_e[1], Yt[:, d, :, ab, :], y_d[d, ab, sl0:sl0 + 128, b * 4:(b + 1) * 4, :], writes=[bY])
                            k.op("act", lambda e: e.activation(out=Yf[:], in_=Yt[:].rearrange("p d h a i -> p (d h a) i"), func=AF.Copy),
                                 reads=[bY], writes=[bYf])
                            k.op("dve", lambda e: e.tensor_reduce(out=st4[:, 0, :], in_=Yf[:], axis=AX.X, op=ALU.add), reads=[bYf], writes=[bst])
                            k.op("dve", lambda e: e.tensor_scalar(out=st4[:, 1, :], in0=st4[:, 0, :], scalar1=-1.0 / 64, scalar2=None, op0=ALU.mult),
                                 reads=[bst], writes=[bst])
                            k.op("dve", lambda e: e.tensor_tensor(out=cen[:], in0=Yf[:], in1=_bc(st4[:, 1, :].unsqueeze(2), [128, 16, 64]), op=ALU.add),
                                 reads=[bYf, bst], writes=[bcen])
                            k.op("act", lambda e: e.activation(out=sqe[:], in_=cen[:], func=AF.Square), reads=[bcen], writes=[bsq])
                            k.op("dve", lambda e: e.tensor_reduce(out=st4[:, 2, :], in_=sqe[:], axis=AX.X, op=ALU.add), reads=[bsq], writes=[bst])
                            k.op("act", lambda e: e.activation(out=st4[:, 3, :], in_=st4[:, 2, :], func=AF.Sqrt, bias=gne[:, 0:1], scale=1.0 / 64),
                                 reads=[bst, bln], writes=[bst])
                            k.op("dve", lambda e: e.reciprocal(st4[:, 3, :], st4[:, 3, :]), reads=[bst], writes=[bst])
                            k.op("dve", lambda e: e.tensor_tensor(out=yh[:].rearrange("p d (g i) -> p (d g) i", i=64), in0=cen[:],
                                                                  in1=_bc(st4[:, 3, :].unsqueeze(2), [128, 16, 64]), op=ALU.mult),
                                 reads=[bcen, bst], writes=[byh])
                            for d in range(2):
                                for hp in range(4):
                                    k.op("pe", lambda e, d=d, hp=hp: e.transpose(out=pte[:, d * 4 + hp, :], in_=yh[:, d, hp * 128:(hp + 1) * 128],
                                                                                 identity=ident_bf), reads=[byh, b_consts], writes=[bpte])
                            for d in range(2):
                                for hp in range(4):
                                    k.op("act", lambda e, d=d, hp=hp: e.activation(out=Zn[:, d, hp, :], in_=pte[:, d * 4 + hp, :], func=AF.Identity,
                                                                                   bias=lnc[:, 4 + hp:5 + hp], scale=lnc[:, hp:hp + 1]),
                                         reads=[bpte, bln], writes=[bZn])
                            js = slice(j * 128, (j + 1) * 128)
                            k.op("dve", lambda e, js=js: e.tensor_tensor(out=Zn[:], in0=Zn[:], in1=Bt[:, :, :, js], op=ALU.add), reads=[bZn, bB], writes=[bZn])
                            k.op("dve", lambda e, js=js: e.tensor_tensor(out=Zn[:], in0=Zn[:], in1=Gt[:, :, :, js], op=ALU.mult), reads=[bZn, bG], writes=[bZn])
                            k.op("dve", lambda e, js=js: e.tensor_tensor(out=catR[:, :, js], in0=Zn[:, 0], in1=Zn[:, 1], op=ALU.add), reads=[bZn], writes=[bcat])
                        k.dma(ds_eo, cat_d[b, 0:512, pos0 - TCX:pos0 - TCX + N].rearrange("(h p) t -> p h t", p=128), catR[:, :, 0:N], reads=[bcat])
                k.phase_end(es)

        if "F" in phases:
            with ExitStack() as pes:
                k.phase_begin(pes)
                PD = [k.sb("PD%d" % i, [128, 12, 512], F32) for i in range(2)]
                cosb = [k.sb("cosb%d" % i, [128, 512], F32) for i in range(2)]
                sinb = [k.sb("sinb%d" % i, [128, 512], F32) for i in range(2)]
                x2 = k.sb("x2", [128, 512], BF16)
                sdf = k.sb("sdf", [128, 512], F32)
                XQ = k.sb("XQ", [128, 512], F32)
                XQb = k.sb("XQb", [128, 512], BF16)
                t1f = k.sb("t1f", [128, 512], F32)
                t2f = k.sb("t2f", [128, 512], F32)
                qo = [k.sb("qo%d" % i, [128, 512], BF16) for i in range(2)]
                Vb = k.sb("Vb", [128, 4, 512], BF16)
                vtk = [k.sb("vtk%d" % i, [128, 512], BF16) for i in range(2)]
                nwc = k.sb("nwc", [128, 2], F32)
                pssf = k.ps("pssf", [128, 512])
                prot = k.ps("prot", [128, 512])
                pvf = k.ps("pvf", [128, 4, 128])
                bPD, bcs = [k.buf(), k.buf()], [k.buf(), k.buf()]
                bx2, bsd, bXQ, bXQb, bt1, bt2, bVb, bnw, bpss, bprot, bpv = (k.buf() for _ in range(11))
                bqo, bvtk = [k.buf(), k.buf()], [k.buf(), k.buf()]
                ds_f = [k.dsem("sp", "f0"), k.dsem("sp", "f1")]
                ds_fw = k.dsem("sp", "fw")
                ds_fo = [k.dsem("pool", "fo0"), k.dsem("pool", "fo1")]
                ds_fv = [k.dsem("pool", "fv0"), k.dsem("pool", "fv1")]
                k.dma(ds_fw, nwc[:, 0:1], qnw[:, :], writes=[bnw])
                k.dma(ds_fw, nwc[:, 1:2], knw[:, :], writes=[bnw])
                ib = 0
                iq = 0
                iv = 0
                for b in range(NB):
                    for (pos0, N, _c) in blocks:
                        s_ = ib % 2
                        ib += 1
                        P = PD[s_]
                        k.dma(ds_f[s_], P[:, :, 0:N], P_d[b, 2048:3584, pos0:pos0 + N].rearrange("(c p) n -> p c n", p=128), writes=[bPD[s_]])
                        k.dma(ds_f[s_], cosb[s_][:, 0:N], ropec[:, pos0:pos0 + N], writes=[bcs[s_]])
                        k.dma(ds_f[s_], sinb[s_][:, 0:N], ropes[:, pos0:pos0 + N], writes=[bcs[s_]])
                        for c in range(8):
                            if c < 4 and pos0 < TCX:
                                continue
                            X = P[:, c, 0:N]
                            wi = 0 if c < 4 else 1
                            k.op("act", lambda e, X=X, N=N: e.activation(out=x2[:, 0:N], in_=X, func=AF.Square), reads=[bPD[s_]], writes=[bx2])
                            k.op("pe", lambda e, N=N: e.matmul(pssf[:, 0:N], bones_bf, x2[:, 0:N], start=True, stop=True), reads=[bx2, b_consts], writes=[bpss])
                            k.op("act", lambda e, N=N: e.activation(out=sdf[:, 0:N], in_=pssf[:, 0:N], func=AF.Sqrt, bias=eps_t[:, 0:1], scale=1.0 / 64),
                                 reads=[bpss, b_consts], writes=[bsd])
                            k.op("dve", lambda e, N=N: e.reciprocal(sdf[:, 0:N], sdf[:, 0:N]), reads=[bsd], writes=[bsd])
                            k.op("dve", lambda e, X=X, N=N, wi=wi: e.scalar_tensor_tensor(out=XQ[:, 0:N], in0=X, scalar=nwc[:, wi:wi + 1], in1=sdf[:, 0:N],
                                                                                         op0=ALU.mult, op1=ALU.mult),
                                 reads=[bPD[s_], bsd, bnw], writes=[bXQ])
                            k.op("act", lambda e, N=N: e.activation(out=XQb[:, 0:N], in_=XQ[:, 0:N], func=AF.Copy), reads=[bXQ], writes=[bXQb])
                            k.op("pe", lambda e, N=N: e.matmul(prot[:, 0:N], rot_bf, XQb[:, 0:N], start=True, stop=True), reads=[bXQb, b_consts], writes=[bprot])
                            k.op("pool", lambda e, N=N, s_=s_: e.tensor_tensor(out=t1f[:, 0:N], in0=XQ[:, 0:N], in1=cosb[s_][:, 0:N], op=ALU.mult),
                                 reads=[bXQ, bcs[s_]], writes=[bt1])
                            k.op("dve", lambda e, N=N, s_=s_: e.tensor_tensor(out=t2f[:, 0:N], in0=prot[:, 0:N], in1=sinb[s_][:, 0:N], op=ALU.mult),
                                 reads=[bprot, bcs[s_]], writes=[bt2])
                            qs = iq % 2
                            iq += 1
                            k.op("dve", lambda e, N=N, qs=qs: e.tensor_tensor(out=qo[qs][:, 0:N], in0=t1f[:, 0:N], in1=t2f[:, 0:N], op=ALU.add),
                                 reads=[bt1, bt2], writes=[bqo[qs]])
                            dst = qT_d[b, c, :, pos0:pos0 + N] if c < 4 else kT_d[b, c - 4, :, pos0:pos0 + N]
                            k.dma(ds_fo[qs], dst, qo[qs][:, 0:N], reads=[bqo[qs]])
                        k.op("act", lambda e, P=P, N=N: e.activation(out=Vb[:, :, 0:N], in_=P[:, 8:12, 0:N], func=AF.Copy), reads=[bPD[s_]], writes=[bVb])
                        for j in range(N // 128):
                            for h in range(4):
                                k.op("pe", lambda e, h=h, j=j: e.matmul(pvf[:, h, :], Vb[:, h, j * 128:(j + 1) * 128], ident_bf, start=True, stop=True),
                                     reads=[bVb, b_consts], writes=[bpv])
                            vs = iv % 2
                            iv += 1
                            k.op("act", lambda e, vs=vs: e.activation(out=vtk[vs][:], in_=pvf[:].rearrange("p h c -> p (h c)"), func=AF.Copy),
                                 reads=[bpv], writes=[bvtk[vs]])
                            p0 = pos0 + j * 128
                            k.dma(ds_fv[vs], vt_d[b, p0:p0 + 128, :], vtk[vs][:], reads=[bvtk[vs]])
                k.phase_end(es)

            with ExitStack() as pes:
                k.phase_begin(pes)
                LAM_INIT = 0.8 - 0.6 * math.exp(-0.3 * 0)
                KT = [k.sb("KT%d" % i, [128, T], BF16) for i in range(2)]
                VT = [k.sb("VT%d" % i, [128, NT, 128], BF16) for i in range(2)]
                QT = [k.sb("QT%d" % i, [128, 512], BF16) for i in range(2)]
                pT2 = [k.sb("pT2%d" % i, [128, 2, 512], BF16) for i in range(2)]
                lamt = k.sb("lamt", [1, 256], F32)
                lamw = k.sb("lamw", [1, 136], F32)
                nlamc = k.sb("nlamc", [128, 1], F32)
                slw = k.sb("slw", [128, 1], F32)
                o0 = k.sb("o0", [128, 512], F32)
                o1 = k.sb("o1", [128, 512], F32)
                rz = k.sb("rz", [128, 512], F32)
                od2 = k.sb("od2", [128, 512], BF16)
                res = [k.sb("res%d" % i, [128, 512], BF16) for i in range(2)]
                sT2 = [k.ps("sT2%d" % i, [128, 2, 512]) for i in range(2)]
                Oa = [k.ps("Oa%d" % m, [128, 512]) for m in range(2)]
                Za = [k.ps("Za%d" % m, [128, 512]) for m in range(2)]
                bKT, bVT, bQT = [k.buf(), k.buf()], [k.buf(), k.buf()], [k.buf(), k.buf()]
                bpT2 = [k.buf(), k.buf()]
                bsT2 = [k.buf(), k.buf()]
                bO, bZ = [k.buf(), k.buf()], [k.buf(), k.buf()]
                blam, bo0, bo1, brz, bod2 = (k.buf() for _ in range(5))
                bres = [k.buf(), k.buf()]
                ds_kv = [k.dsem("sp", "kv0"), k.dsem("sp", "kv1")]
                ds_q = [k.dsem("sp", "q0"), k.dsem("sp", "q1")]
                ds_l = k.dsem("sp", "lam")
                ds_ro = [k.dsem("pool", "ro0"), k.dsem("pool", "ro1")]
                k.dma(ds_l, lamt[:], lamv[:, :], writes=[blam])
                k.dma(ds_l, slw[:], sublnw[:, :], writes=[blam])
                k.op("dve", lambda e: e.tensor_tensor(out=lamw[:, 0:64], in0=lamt[:, 0:64], in1=lamt[:, 64:128], op=ALU.mult), reads=[blam], writes=[blam])
                k.op("dve", lambda e: e.tensor_tensor(out=lamw[:, 64:128], in0=lamt[:, 128:192], in1=lamt[:, 192:256], op=ALU.mult), reads=[blam], writes=[blam])
                k.op("dve", lambda e: e.tensor_reduce(out=lamw[:, 128:129], in_=lamw[:, 0:64], axis=AX.X, op=ALU.add), reads=[blam], writes=[blam])
                k.op("dve", lambda e: e.tensor_reduce(out=lamw[:, 129:130], in_=lamw[:, 64:128], axis=AX.X, op=ALU.add), reads=[blam], writes=[blam])
                k.op("act", lambda e: e.activation(out=lamw[:, 130:132], in_=lamw[:, 128:130], func=AF.Exp), reads=[blam], writes=[blam])
                k.op("dve", lambda e: e.tensor_tensor(out=lamw[:, 132:133], in0=lamw[:, 131:132], in1=lamw[:, 130:131], op=ALU.subtract), reads=[blam], writes=[blam])
                k.op("dve", lambda e: e.tensor_scalar(out=lamw[:, 133:134], in0=lamw[:, 132:133], scalar1=-LAM_INIT, scalar2=None, op0=ALU.add),
                     reads=[blam], writes=[blam])
                k.op("pe", lambda e: e.matmul(Za[0][:, 0:1], consts[0:1, C_ONES:C_ONES + 128], lamw[0:1, 133:134], start=True, stop=True),
                     reads=[blam, b_consts], writes=[bZ[0]])
                k.op("dve", lambda e: e.tensor_copy(nlamc[:], Za[0][:, 0:1]), reads=[bZ[0]], writes=[blam])
                k.op("dve", lambda e: e.tensor_scalar(out=slw[:], in0=slw[:], scalar1=1.0 - LAM_INIT, scalar2=None, op0=ALU.mult), reads=[blam], writes=[blam])
                ih = 0
                iqb = 0
                for b in range(NB):
                    for h in range(4):
                        hs = ih % 2
                        ih += 1
                        k.dma(ds_kv[hs], KT[hs][:], kT_d[b, h, :, :], writes=[bKT[hs]])
                        k.dma(ds_kv[hs], VT[hs][:], vt_d[b, :, h * 128:(h + 1) * 128].rearrange("(n p) c -> p n c", p=128), writes=[bVT[hs]])
                        for (pos0, N) in xblocks:
                            qs = iqb % 2
                            iqb += 1
                            k.dma(ds_q[qs], QT[qs][:, 0:N], qT_d[b, h, :, pos0:pos0 + N], writes=[bQT[qs]])
                            def score(kt):
                                i_ = kt % 2
                                for m in range(2):
                                    ms = slice(64 * m, 64 * m + 64)
                                    k.op("pe", lambda e, m=m, ms=ms: e.matmul(sT2[i_][:, m, 0:N], KT[hs][ms, kt * 128:(kt + 1) * 128], QT[qs][ms, 0:N],
                                                                          start=True, stop=True),
                                         reads=[bKT[hs], bQT[qs]], writes=[bsT2[i_]])

                            LOOK = 1
                            for kt in range(min(LOOK, NT)):
                                score(kt)
                            for kt in range(NT):
                                i_ = kt % 2
                                k.op("act", lambda e: e.activation(out=pT2[i_][:, :, 0:N], in_=sT2[i_][:, :, 0:N], func=AF.Exp, scale=0.125),
                                     reads=[bsT2[i_]], writes=[bpT2[i_]])
                                if kt + LOOK < NT:
                                    score(kt + LOOK)
                                for m in range(2):
                                    k.op("pe", lambda e, m=m: e.matmul(Oa[m][:, 0:N], VT[hs][:, kt, :], pT2[i_][:, m, 0:N], start=(kt == 0), stop=(kt == NT - 1)),
                                         reads=[bVT[hs], bpT2[i_]], writes=[bO[m]])
                                    k.op("pe", lambda e, m=m: e.matmul(Za[m][:, 0:N], ones_bf, pT2[i_][:, m, 0:N], start=(kt == 0), stop=(kt == NT - 1)),
                                         reads=[bpT2[i_], b_consts], writes=[bZ[m]])
                            k.op("dve", lambda e, N=N: e.reciprocal(rz[:, 0:N], Za[0][:, 0:N]), reads=[bZ[0]], writes=[brz])
                            k.op("dve", lambda e, N=N: e.tensor_tensor(out=o0[:, 0:N], in0=Oa[0][:, 0:N], in1=rz[:, 0:N], op=ALU.mult), reads=[bO[0], brz], writes=[bo0])
                            k.op("dve", lambda e, N=N: e.reciprocal(rz[:, 0:N], Za[1][:, 0:N]), reads=[bZ[1]], writes=[brz])
                            k.op("dve", lambda e, N=N: e.tensor_tensor(out=o1[:, 0:N], in0=Oa[1][:, 0:N], in1=rz[:, 0:N], op=ALU.mult), reads=[bO[1], brz], writes=[bo1])
                            k.op("dve", lambda e, N=N: e.scalar_tensor_tensor(out=o0[:, 0:N], in0=o1[:, 0:N], scalar=nlamc[:, 0:1], in1=o0[:, 0:N],
                                                                               op0=ALU.mult, op1=ALU.add), reads=[bo1, bo0, blam], writes=[bo0])
                            k.op("pool", lambda e, N=N: e.tensor_tensor(out=od2[:, 0:N], in0=o0[:, 0:N], in1=o0[:, 0:N], op=ALU.mult), reads=[bo0], writes=[bod2])
                            k.op("pe", lambda e, N=N: e.matmul(sT2[0][:, 0, 0:N], ones_bf, od2[:, 0:N], start=True, stop=True),
                                 reads=[bod2, b_consts], writes=[bsT2[0]])
                            k.op("act", lambda e, N=N: e.activation(out=rz[:, 0:N], in_=sT2[0][:, 0, 0:N], func=AF.Sqrt, bias=eps_t[:, 0:1], scale=1.0 / 128),
                                 reads=[bsT2[0], b_consts], writes=[brz])
                            k.op("dve", lambda e, N=N: e.reciprocal(rz[:, 0:N], rz[:, 0:N]), reads=[brz], writes=[brz])
                            rs = iqb % 2
                            k.op("dve", lambda e, N=N, rs=rs: e.scalar_tensor_tensor(out=res[rs][:, 0:N], in0=o0[:, 0:N], scalar=slw[:, 0:1], in1=rz[:, 0:N],
                                                                                     op0=ALU.mult, op1=ALU.mult), reads=[bo0, brz, blam], writes=[bres[rs]])
                            k.dma(ds_ro[rs], cat_d[b, 512 + h * 128:512 + (h + 1) * 128, pos0 - TCX:pos0 - TCX + N], res[rs][:, 0:N], reads=[bres[rs]])
                k.phase_end(es)

        if "G" in phases:
            with ExitStack() as pes:
                k.phase_begin(pes)
                wo_st = k.sb("wo_st", [128, 4, D], F32)
                woutb = k.sb("woutb", [128, 8, D], BF16)
                wrt = k.sb("wrt", [128, 8, 36], F32)
                brt = k.sb("brt", [128, 36], F32)
                xt2 = [k.sb("xt2%d" % i, [128, D], F32) for i in range(2)]
                catT = [k.sb("catT%d" % i, [128, 8, 128], BF16) for i in range(2)]
                tmpo = k.sb("tmpo", [128, D], F32)
                x1 = [k.sb("x1%d" % i, [128, D], F32) for i in range(2)]
                sq2 = k.sb("sq2", [128, D], F32)
                ss2 = k.sb("ss2", [128, 4], F32)
                xn2 = k.sb("xn2", [128, D], F32)
                h2f = k.sb("h2f", [128, 8, 128], F32)
                h2b = [k.sb("h2b%d" % i, [128, 8, 128], BF16) for i in range(2)]
                Lg = k.sb("Lg", [128, 36], F32)
                rt = k.sb("rt", [128, 16], F32)
                goh = k.sb("goh", [128, 4], F32)
                em = k.sb("em", [128, 4, 8], F32)
                em2 = k.sb("em2", [128, 32], F32)
                m1 = k.sb("m1", [128, 32], F32)
                m2 = k.sb("m2", [128, 32], F32)
                Wdt = [k.sb("Wdt%d" % i, [128, 32], F32) for i in range(2)]
                po = [k.ps("po%d" % i, [128, 512]) for i in range(2)]
                ptf = k.ps("ptf", [128, 8, 128])
                pl = k.ps("pl", [128, 36])
                bw, bpo, bptf, bpl, btmp, bsq2, bss2, bxn2, bh2f, bL, brt_ = (k.buf() for _ in range(11))
                bxt, bcatT, bx1, bh2b, bWd = ([k.buf(), k.buf()] for _ in range(5))
                bpo = [k.buf(), k.buf()]
                ds_w = k.dsem("sp", "gw")
                ds_i = [k.dsem("sp", "gi0"), k.dsem("sp", "gi1")]
                ds_o1 = [k.dsem("pool", "go0"), k.dsem("pool", "go1")]
                wov = w_out.rearrange("(kc p) n -> p kc n", p=128)
                for hf in range(2):
                    k.dma(ds_w, wo_st[:], wov[:, hf * 4:(hf + 1) * 4, :], writes=[bw])
                    k.op("dve", lambda e, hf=hf: e.tensor_copy(woutb[:, hf * 4:(hf + 1) * 4, :], wo_st[:]), reads=[bw], writes=[bw])
                k.dma(ds_w, wrt[:], w_rt.rearrange("(kc p) n -> p kc n", p=128), writes=[bw])
                k.dma(ds_w, brt[:], b_rt.partition_broadcast(128), writes=[bw])
                it_ = 0
                for b in range(NB):
                    for xp in range(0, TX, 128):
                        s_ = it_ % 2
                        it_ += 1
                        k.dma(ds_i[s_], xt2[s_][:], seq[b, TCX + xp:TCX + xp + 128, :], writes=[bxt[s_]])
                        k.dma(ds_i[s_], catT[s_][:], cat_d[b, :, xp:xp + 128].rearrange("(c p) t -> p c t", p=128), writes=[bcatT[s_]])
                        for hf in range(2):
                            for kc in range(8):
                                k.op("pe", lambda e, hf=hf, kc=kc, s_=s_: e.matmul(po[hf][:], catT[s_][:, kc, :], woutb[:, kc, hf * 512:(hf + 1) * 512],
                                                                                   start=(kc == 0), stop=(kc == 7)),
                                     reads=[bcatT[s_], bw], writes=[bpo[hf]])
                            hsl = slice(hf * 512, (hf + 1) * 512)
                            k.op("dve", lambda e, hf=hf, hsl=hsl, b=b: e.tensor_tensor(out=tmpo[:, hsl], in0=po[hf][:], in1=G1[:, b, hsl], op=ALU.mult),
                                 reads=[bpo[hf], b_mod_], writes=[btmp])
                            k.op("dve", lambda e, hsl=hsl, s_=s_: e.tensor_tensor(out=x1[s_][:, hsl], in0=tmpo[:, hsl], in1=xt2[s_][:, hsl], op=ALU.add),
                                 reads=[btmp, bxt[s_]], writes=[bx1[s_]])
                        k.dma(ds_o1[s_], x1_d[b, xp:xp + 128, :], x1[s_][:], reads=[bx1[s_]])
                        k.op("act", lambda e, s_=s_: e.activation(out=sq2[:], in_=x1[s_][:], func=AF.Square), reads=[bx1[s_]], writes=[bsq2])
                        k.op("dve", lambda e: e.tensor_reduce(out=ss2[:, 0:1], in_=sq2[:], axis=AX.X, op=ALU.add), reads=[bsq2], writes=[bss2])
                        k.op("act", lambda e: e.activation(out=ss2[:, 1:2], in_=ss2[:, 0:1], func=AF.Sqrt, bias=eps_t[:, 0:1], scale=1.0 / D),
                             reads=[bss2, b_consts], writes=[bss2])
                        k.op("dve", lambda e: e.reciprocal(ss2[:, 2:3], ss2[:, 1:2]), reads=[bss2], writes=[bss2])
                        k.op("dve", lambda e, s_=s_: e.tensor_scalar(out=xn2[:], in0=x1[s_][:], scalar1=ss2[:, 2:3], scalar2=None, op0=ALU.mult),
                             reads=[bx1[s_], bss2], writes=[bxn2])
                        for kc in range(8):
                            k.op("pe", lambda e, kc=kc: e.transpose(out=ptf[:, kc, :], in_=xn2[:, kc * 128:(kc + 1) * 128], identity=consts[:, C_ID:C_ID + 128]),
                                 reads=[bxn2, b_consts], writes=[bptf])
                        for kc in range(8):
                            k.op("act", lambda e, kc=kc, b=b: e.activation(out=h2f[:, kc, :], in_=ptf[:, kc, :], func=AF.Identity,
                                                                           bias=B2[:, kc, b:b + 1], scale=A2[:, kc, b:b + 1]),
                                 reads=[bptf, b_mod_], writes=[bh2f])
                        k.op("act", lambda e, s_=s_: e.activation(out=h2b[s_][:], in_=h2f[:], func=AF.Copy), reads=[bh2f], writes=[bh2b[s_]])
                        k.dma(ds_o1[s_], h2T_d[b, :, xp:xp + 128].rearrange("(c p) t -> p c t", p=128), h2b[s_][:], reads=[bh2b[s_]])
                        for kc in range(8):
                            k.op("pe", lambda e, kc=kc: e.matmul(pl[:], h2f[:, kc, :], wrt[:, kc, :], start=(kc == 0), stop=(kc == 7)),
                                 reads=[bh2f, bw], writes=[bpl])
                        k.op("dve", lambda e: e.tensor_tensor(out=Lg[:], in0=pl[:], in1=brt[:], op=ALU.add), reads=[bpl, bw], writes=[bL])
                        R_ = [bL, brt_]
                        k.op("dve", lambda e: e.tensor_reduce(out=rt[:, 0:1], in_=Lg[:, 0:4], axis=AX.X, op=ALU.max), reads=R_, writes=[brt_])
                        k.op("dve", lambda e: e.tensor_scalar(out=goh[:], in0=Lg[:, 0:4], scalar1=rt[:, 0:1], scalar2=None, op0=ALU.subtract), reads=R_, writes=[brt_])
                        k.op("act", lambda e: e.activation(out=em2[:, 0:4], in_=goh[:], func=AF.Exp), reads=R_, writes=[brt_])
                        k.op("dve", lambda e: e.tensor_reduce(out=rt[:, 1:2], in_=em2[:, 0:4], axis=AX.X, op=ALU.add), reads=R_, writes=[brt_])
                        k.op("dve", lambda e: e.reciprocal(rt[:, 2:3], rt[:, 1:2]), reads=R_, writes=[brt_])
                        k.op("dve", lambda e: e.tensor_scalar(out=goh[:], in0=Lg[:, 0:4], scalar1=rt[:, 0:1], scalar2=None, op0=ALU.is_equal), reads=R_, writes=[brt_])
                        k.op("dve", lambda e: e.tensor_scalar(out=goh[:], in0=goh[:], scalar1=-1.0, scalar2=1e30, op0=ALU.add, op1=ALU.mult), reads=R_, writes=[brt_])
                        k.op("dve", lambda e: e.tensor_tensor(out=em[:], in0=Lg[:, 4:36].rearrange("p (g x) -> p g x", x=8),
                                                              in1=_bc(goh[:].unsqueeze(2), [128, 4, 8]), op=ALU.add), reads=R_, writes=[brt_])
                        emf = em[:].rearrange("p g x -> p (g x)")
                        k.op("dve", lambda e: e.tensor_reduce(out=rt[:, 3:4], in_=emf, axis=AX.X, op=ALU.max), reads=R_, writes=[brt_])
                        k.op("dve", lambda e: e.tensor_scalar(out=m1[:], in0=emf, scalar1=rt[:, 3:4], scalar2=None, op0=ALU.is_equal), reads=R_, writes=[brt_])
                        k.op("dve", lambda e: e.scalar_tensor_tensor(out=em2[:], in0=m1[:], scalar=-1e30, in1=emf, op0=ALU.mult, op1=ALU.add), reads=R_, writes=[brt_])
                        k.op("dve", lambda e: e.tensor_reduce(out=rt[:, 4:5], in_=em2[:], axis=AX.X, op=ALU.max), reads=R_, writes=[brt_])
                        k.op("dve", lambda e: e.tensor_scalar(out=m2[:], in0=em2[:], scalar1=rt[:, 4:5], scalar2=None, op0=ALU.is_equal), reads=R_, writes=[brt_])
                        k.op("dve", lambda e: e.tensor_tensor(out=rt[:, 5:6], in0=rt[:, 4:5], in1=rt[:, 3:4], op=ALU.subtract), reads=R_, writes=[brt_])
                        k.op("act", lambda e: e.activation(out=rt[:, 6:7], in_=rt[:, 5:6], func=AF.Exp), reads=R_, writes=[brt_])
                        k.op("dve", lambda e: e.tensor_scalar(out=rt[:, 7:8], in0=rt[:, 6:7], scalar1=1.0, scalar2=None, op0=ALU.add), reads=R_, writes=[brt_])
                        k.op("dve", lambda e: e.reciprocal(rt[:, 8:9], rt[:, 7:8]), reads=R_, writes=[brt_])
                        k.op("dve", lambda e: e.tensor_tensor(out=rt[:, 9:10], in0=rt[:, 8:9], in1=rt[:, 2:3], op=ALU.mult), reads=R_, writes=[brt_])
                        k.op("dve", lambda e: e.tensor_tensor(out=rt[:, 10:11], in0=rt[:, 9:10], in1=rt[:, 6:7], op=ALU.mult), reads=R_, writes=[brt_])
                        k.op("dve", lambda e: e.tensor_scalar(out=m1[:], in0=m1[:], scalar1=rt[:, 9:10], scalar2=None, op0=ALU.mult), reads=R_, writes=[brt_])
                        k.op("dve", lambda e, s_=s_: e.scalar_tensor_tensor(out=Wdt[s_][:], in0=m2[:], scalar=rt[:, 10:11], in1=m1[:], op0=ALU.mult, op1=ALU.add),
                             reads=R_, writes=[bWd[s_]])
                        k.dma(ds_o1[s_], wd_d[b, xp:xp + 128, :], Wdt[s_][:], reads=[bWd[s_]])
                k.phase_end(es)

            with ExitStack() as pes:
                k.phase_begin(pes)
                TB = min(1024, TX)
                TBC = TB // 128
                NTB = TB // 512
                h2T = k.sb("h2T", [128, 8, TB], BF16)
                acc = k.sb("acc", [128, TBC, D], F32)
                Wd = k.sb("Wd", [128, TBC, NE], F32)
                x1h = k.sb("x1h", [128, 4, D], F32)
                stg = [k.sb("stg%d" % i, [128, 2048], F32) for i in range(3)]
                wgb = [k.sb("wgb%d" % i, [128, 8, FF], BF16) for i in range(2)]
                wub = [k.sb("wub%d" % i, [128, 8, FF], BF16) for i in range(2)]
                wdb = [k.sb("wdb%d" % i, [128, 4, D], BF16) for i in range(2)]
                sg = [k.sb("sg%d" % i, [128, 512], F32) for i in range(2)]
                hid = [k.sb("hid%d" % i, [128, 4, 512], BF16) for i in range(2)]
                pg = [k.ps("pg%d" % i, [128, 512]) for i in range(2)]
                pu = [k.ps("pu%d" % i, [128, 512]) for i in range(2)]
                py = [k.ps("py%d" % i, [128, 512]) for i in range(2)]
                bh2T, bacc, bWd_, bx1h = (k.buf() for _ in range(4))
                bstg = [k.buf() for _ in range(3)]
                bwg, bwu, bwd_, bsg, bhid, bpg, bpu, bpy = ([k.buf(), k.buf()] for _ in range(8))
                ds_m = k.dsem("sp", "mi")
                ds_s = [k.dsem("sp", "ms%d" % i) for i in range(3)]
                ds_x1 = k.dsem("sp", "mx")
                ds_out = k.dsem("pool", "mo")

                def dsl2(start, size):
                    if isinstance(start, int):
                        return slice(start, start + size)
                    return bass.ds(start, size)

                def moe_body(b):
                    def body(it):
                        off = it * TB
                        k.dma(ds_m, h2T[:], h2T_d[b, :, dsl2(off, TB)].rearrange("(c p) t -> p c t", p=128), writes=[bh2T])
                        k.dma(ds_m, Wd[:], wd_d[b, dsl2(off, TB), :].rearrange("(n p) e -> p n e", p=128), writes=[bWd_])
                        k.op("pool", lambda e: e.memset(acc[:], 0.0), writes=[bacc])
                        ist = 0
                        cnt = [0, 0, 0]
                        for ex in range(NE):
                            ws = ex % 2
                            srcs = []
                            gv = moe_g[ex].rearrange("(kc p) f -> p kc f", p=128)
                            uv = moe_u[ex].rearrange("(kc p) f -> p kc f", p=128)
                            dv = moe_d[ex].rearrange("(fc p) n -> p fc n", p=128)
                            for hf in range(2):
                                srcs.append((gv[:, hf * 4:(hf + 1) * 4, :], wgb[ws][:, hf * 4:(hf + 1) * 4, :], bwg[ws], "p (a f) -> p a f", 4))
                                srcs.append((uv[:, hf * 4:(hf + 1) * 4, :], wub[ws][:, hf * 4:(hf + 1) * 4, :], bwu[ws], "p (a f) -> p a f", 4))
                            for hf in range(2):
                                srcs.append((dv[:, hf * 2:(hf + 1) * 2, :], wdb[ws][:, hf * 2:(hf + 1) * 2, :], bwd_[ws], "p (a f) -> p a f", 2))
                            for (src, dst, bdst, pat, a_) in srcs:
                                si = ist % 3
                                ist += 1
                                k.dma(ds_s[si], stg[si][:].rearrange(pat, a=a_), src, writes=[bstg[si]])
                                if ist % 2 == 0:
                                    k.op("pool", lambda e, si=si, dst=dst, pat=pat, a_=a_: e.tensor_copy(dst, stg[si][:].rearrange(pat, a=a_)),
                                         reads=[bstg[si]], writes=[bdst])
                                else:
                                    k.op("act", lambda e, si=si, dst=dst, pat=pat, a_=a_: e.activation(out=dst, in_=stg[si][:].rearrange(pat, a=a_), func=AF.Copy),
                                         reads=[bstg[si]], writes=[bdst])
                            for tb in range(NTB):
                                tsl = slice(tb * 512, (tb + 1) * 512)
                                hs_ = cnt[0] % 2
                                cnt[0] += 1
                                for fc in range(4):
                                    pi = cnt[1] % 2
                                    cnt[1] += 1
                                    fsl = slice(fc * 128, (fc + 1) * 128)
                                    for kc in range(8):
                                        k.op("pe", lambda e, pi=pi, ws=ws, kc=kc, fsl=fsl, tsl=tsl: e.matmul(pg[pi][:], wgb[ws][:, kc, fsl], h2T[:, kc, tsl],
                                                                                                             start=(kc == 0), stop=(kc == 7)),
                                             reads=[bwg[ws], bh2T], writes=[bpg[pi]])
                                    for kc in range(8):
                                        k.op("pe", lambda e, pi=pi, ws=ws, kc=kc, fsl=fsl, tsl=tsl: e.matmul(pu[pi][:], wub[ws][:, kc, fsl], h2T[:, kc, tsl],
                                                                                                             start=(kc == 0), stop=(kc == 7)),
                                             reads=[bwu[ws], bh2T], writes=[bpu[pi]])
                                    k.op("act", lambda e, pi=pi: e.activation(out=sg[pi][:], in_=pg[pi][:], func=AF.Silu), reads=[bpg[pi]], writes=[bsg[pi]])
                                    k.op("dve", lambda e, pi=pi, hs_=hs_, fc=fc: e.tensor_tensor(out=hid[hs_][:, fc, :], in0=sg[pi][:], in1=pu[pi][:], op=ALU.mult),
                                         reads=[bsg[pi], bpu[pi]], writes=[bhid[hs_]])
                                for tc in range(4):
                                    ch = tb * 4 + tc
                                    for hf in range(2):
                                        yi = cnt[2] % 2
                                        cnt[2] += 1
                                        for fc in range(4):
                                            k.op("pe", lambda e, yi=yi, hs_=hs_, fc=fc, tc=tc, ws=ws, hf=hf: e.matmul(
                                                py[yi][:], hid[hs_][:, fc, tc * 128:(tc + 1) * 128], wdb[ws][:, fc, hf * 512:(hf + 1) * 512],
                                                start=(fc == 0), stop=(fc == 3)), reads=[bhid[hs_], bwd_[ws]], writes=[bpy[yi]])
                                        k.op("dve", lambda e, yi=yi, ch=ch, hf=hf, ex=ex: e.scalar_tensor_tensor(
                                            out=acc[:, ch, hf * 512:(hf + 1) * 512], in0=py[yi][:], scalar=Wd[:, ch, ex:ex + 1],
                                            in1=acc[:, ch, hf * 512:(hf + 1) * 512], op0=ALU.mult, op1=ALU.add),
                                            reads=[bpy[yi], bWd_, bacc], writes=[bacc])
                        for hq in range(TBC // 4):
                            k.dma(ds_x1, x1h[:], x1_d[b, dsl2(off + hq * 512, 512), :].rearrange("(n p) d -> p n d", p=128), writes=[bx1h])
                            asl = acc[:, hq * 4:(hq + 1) * 4, :]
                            k.op("dve", lambda e, asl=asl: e.tensor_tensor(out=asl, in0=asl, in1=_bc(G2[:, b, :].unsqueeze(1), [128, 4, D]), op=ALU.mult),
                                 reads=[bacc, b_mod_], writes=[bacc])
                            k.op("pool", lambda e, asl=asl: e.tensor_tensor(out=asl, in0=asl, in1=x1h[:], op=ALU.add), reads=[bacc, bx1h], writes=[bacc])
                            k.dma(ds_out, out_d[b, dsl2(off + hq * 512, 512), :].rearrange("(n p) d -> p n d", p=128), asl, reads=[bacc])
                    return body

                for b in range(NB):
                    k.loop(TX // TB, moe_body(b), static=True)
                k.phase_end(es)
        k.barrier()
    return nc, dram


def core_inputs(inp, b0, TX, TCX, shared=None):
    f = lambda a: np.ascontiguousarray(np.asarray(a, np.float32))
    if shared is None:
        shared = {}
        cs, sn = rope_tables(TX, TCX)
        shared["ropec"], shared["ropes"] = cs, sn
        shared["consts"] = make_consts()
        shared["w_mod"] = f(inp["w_mod"][0])
        shared["b_mod"] = f(inp["b_mod"][0]).reshape(1, -1)
        shared["n1w"] = colform(inp["norm1_w"][0], 8)
        shared["n2w"] = colform(inp["norm2_w"][0], 8)
        shared["w_in"] = f(inp["w_in"][0])
        shared["shift_w"] = f(inp["shift_w"][0])
        shared["rw_w0"] = f(np.asarray(inp["rwkv_w0"][0]).reshape(2, 4, 128).transpose(2, 0, 1))
        shared["rw_a0"] = f(np.asarray(inp["rwkv_a0"][0]).reshape(2, 4, 128).transpose(2, 0, 1))
        shared["rw_wup"] = f(inp["rwkv_w_up"][0])
        shared["rw_aup"] = f(inp["rwkv_a_up"][0])
        shared["rw_gup"] = f(inp["rwkv_g_up"][0])
        shared["rw_kk"] = colform(inp["rwkv_k_k"][0], 4)
        shared["rw_ka"] = colform(inp["rwkv_k_a"][0], 4)
        shared["rw_rk"] = colform(np.asarray(inp["rwkv_r_k"][0]).reshape(-1), 4)
        shared["rw_lnw"] = colform(inp["rwkv_ln_w"][0], 4)
        shared["rw_lnb"] = colform(inp["rwkv_ln_b"][0], 4)
        shared["qnw"] = f(np.tile(np.asarray(inp["q_norm_w"][0]), 2).reshape(128, 1))
        shared["knw"] = f(np.tile(np.asarray(inp["k_norm_w"][0]), 2).reshape(128, 1))
        shared["lamv"] = f(np.concatenate([np.asarray(inp[n][0]) for n in ("lam_q1", "lam_k1", "lam_q2", "lam_k2")]).reshape(1, 256))
        shared["sublnw"] = f(np.asarray(inp["subln_w"][0]).reshape(128, 1))
        shared["w_out"] = f(inp["w_out"][0])
        shared["w_rt"] = f(np.concatenate([np.asarray(inp["w_group"][0]), np.asarray(inp["w_expert"][0])], axis=1))
        shared["b_rt"] = f(np.concatenate([np.asarray(inp["b_group"][0]), np.asarray(inp["b_expert"][0])]).reshape(1, 36))
        shared["moe_g"] = f(inp["moe_w_gate"][0])
        shared["moe_u"] = f(inp["moe_w_up"][0])
        shared["moe_d"] = f(inp["moe_w_down"][0])
    m = dict(shared)
    x = np.asarray(inp["x"][b0:b0 + NB], np.float32)
    ctx = np.asarray(inp["ctx"][b0:b0 + NB], np.float32)
    m["seq"] = np.ascontiguousarray(np.concatenate([ctx, x], axis=1))
    cc = np.concatenate([np.asarray(inp["c"][b0:b0 + NB], np.float32), np.asarray(inp["c_ctx"], np.float32)[None]], axis=0)
    m["csT"] = np.ascontiguousarray(cc.reshape(3, 8, 128).transpose(2, 1, 0))
    return m, shared


TX_FULL, TCX_FULL = 4096, 256
_CACHE = {}


def kernel(**inputs):
    inp = {k_: np.asarray(v) for k_, v in inputs.items()}
    B = inp["x"].shape[0]
    ncores = B // NB
    if "nc" not in _CACHE:
        _CACHE["nc"] = build_program(TX_FULL, TCX_FULL)
    nc, dram = _CACHE["nc"]
    in_maps = []
    shared = None
    for c in range(ncores):
        m, shared = core_inputs(inp, c * NB, TX_FULL, TCX_FULL, shared)
        in_maps.append({k_: v for k_, v in m.items() if k_ in dram})
    res = run_bass_kernel_spmd(nc, in_maps, core_ids=list(range(ncores)))
    out = np.concatenate([np.asarray(r["out"]) for r in res.results], axis=0)
    return out.astype(np.float32, copy=False)
```

```python
import copy
import math
from contextlib import ExitStack

import numpy as np
import concourse.bass as bass
import concourse.mybir as mybir
from concourse.bass_utils import run_bass_kernel_spmd

F32 = mybir.dt.float32
BF16 = mybir.dt.bfloat16
AF = mybir.ActivationFunctionType
ALU = mybir.AluOpType
AX = mybir.AxisListType

D = 1024
NB = 2
RW = 512
INW = 3584
NE = 32
FF = 512
SUB = 4


class Buf:
    __slots__ = ("name", "w", "r")

    def __init__(self, name):
        self.name = name
        self.w = None
        self.r = []


class DSem:
    def __init__(self, h, q):
        self.h = h
        self.q = q
        self.total = 0


class K:
    def __init__(self, nc, es):
        self.nc = nc
        self.es = es
        self.E = {"pe": nc.tensor, "act": nc.scalar, "dve": nc.vector, "pool": nc.gpsimd, "sp": nc.sync}
        self.sem = {e: es.enter_context(nc.semaphore("c_" + e)) for e in ("pe", "act", "dve", "pool")}
        self.cnt = {e: 0 for e in self.sem}
        self.seen = {e: {} for e in self.E}
        self.dsems = []
        self.bufs = []
        self.dry = False
        self.inloop = False
        self.used = set()
        self.nbuf = 0
        self.phase_no = 0

    def buf(self, name=None):
        self.nbuf += 1
        b = Buf(name or ("b%d" % self.nbuf))
        self.bufs.append(b)
        return b

    def dsem(self, q, name):
        d = DSem(self.es.enter_context(self.nc.semaphore("d_%s_%d" % (name, self.phase_no))), q)
        self.dsems.append(d)
        return d

    def sb(self, name, shape, dt):
        return self.es.enter_context(self.nc.sbuf_tensor("%s_%d" % (name, self.phase_no), shape, dt))

    def ps(self, name, shape, dt=F32):
        return self.es.enter_context(self.nc.psum_tensor("%s_%d" % (name, self.phase_no), shape, dt))

    def _wait(self, e, tok):
        kind, src, n = tok
        key = src if kind == "E" else id(src)
        if kind == "E" and src == e and e == "pe":
            return
        if kind == "D":
            n = src.total
        if self.seen[e].get(key, -1) >= n:
            return
        self.seen[e][key] = n
        if self.dry:
            self.used.add((e, key))
            return
        h = self.sem[src] if kind == "E" else src.h
        if self.inloop:
            R = self.regs[(e, key)]
            delta = n - self.cur[(e, key)]
            if delta != 0:
                self.E[e].reg_add(R, R, delta)
            self.cur[(e, key)] = n
            self.E[e].wait_ge(h, R)
        else:
            self.E[e].wait_ge(h, n)

    def _sync(self, e, reads, writes):
        best = {}
        def add(tok):
            kind, src, n = tok
            key = (kind, src if kind == "E" else id(src))
            if key not in best or best[key][2] < n:
                best[key] = tok
        for b in reads:
            if b.w is not None:
                add(b.w)
        for b in writes:
            if b.w is not None:
                add(b.w)
            for t in b.r:
                add(t)
        for key in sorted(best, key=str):
            self._wait(e, best[key])

    def op(self, e, fn, reads=(), writes=()):
        self._sync(e, reads, writes)
        self.cnt[e] += 1
        tok = ("E", e, self.cnt[e])
        if not self.dry:
            fn(self.E[e]).then_inc(self.sem[e], 1)
        self.seen[e][e] = max(self.seen[e].get(e, -1), 0)
        for b in reads:
            b.r = [t for t in b.r if not (t[0] == "E" and t[1] == e)] + [tok]
        for b in writes:
            b.w = tok
            b.r = []
        return tok

    def dma(self, ds, out, in_, reads=(), writes=()):
        q = ds.q
        self._sync(q, reads, writes)
        ds.total += 16
        tok = ("D", ds, ds.total)
        if not self.dry:
            self.E[q].dma_start(out=out, in_=in_).then_inc(ds.h, 16)
        for b in reads:
            b.r.append(tok)
        for b in writes:
            b.w = tok
            b.r = []
        return tok

    def drain(self):
        for d in self.dsems:
            if d.total > 0:
                self._wait(d.q, ("D", d, d.total))

    def barrier(self):
        self.drain()
        if not self.dry:
            self.nc.all_engine_barrier()
        for b in self.bufs:
            b.w = None
            b.r = []

    def _keycount(self, key):
        if isinstance(key, str):
            return self.cnt[key]
        for d in self.dsems:
            if id(d) == key:
                return d.total
        raise KeyError(key)

    def _snap_bufs(self, shift):
        def sh(tok):
            kind, src, n = tok
            return (kind, src, n - shift[src if kind == "E" else id(src)])
        return [(None if b.w is None else sh(b.w), [sh(t) for t in b.r]) for b in self.bufs]

    def _load_bufs(self, states):
        for b, (w, r) in zip(self.bufs, states):
            b.w = w
            b.r = list(r)

    def loop(self, n_iter, body, static=False):
        self.barrier()
        for e in self.seen:
            self.seen[e] = {}
        if n_iter == 1 or static:
            for i in range(n_iter):
                body(i)
            self.barrier()
            return
        c0 = dict(self.cnt)
        d0 = [d.total for d in self.dsems]
        nb0 = len(self.bufs)

        def rewind():
            self.cnt = dict(c0)
            for d, t in zip(self.dsems, d0):
                d.total = t
            for e in self.seen:
                self.seen[e] = {}

        self.dry = True
        self.used = set()
        body(0)
        P = {e: self.cnt[e] - c0[e] for e in self.cnt}
        for d, t in zip(self.dsems, d0):
            P[id(d)] = d.total - t
        carried = self._snap_bufs(P)
        rewind()
        self._load_bufs(carried)
        self.used = set()
        body(0)
        self.drain()
        used = sorted(self.used, key=str)
        rewind()
        self._load_bufs(carried)
        self.dry = False
        self.regs = {}
        self.cur = {}
        base = {}
        for (e, key) in used:
            self.nbuf += 1
            R = self.E[e].alloc_register("w_%s_%d" % (e, self.nbuf))
            base[(e, key)] = self._keycount(key)
            self.E[e].reg_mov(R, base[(e, key)])
            self.regs[(e, key)] = R
            self.cur[(e, key)] = base[(e, key)]
        with self.nc.Fori(0, n_iter, hint_back_edge=True) as it:
            self.inloop = True
            body(it)
            for (e, key) in used:
                delta = base[(e, key)] + P[key] - self.cur[(e, key)]
                if delta != 0:
                    self.E[e].reg_add(self.regs[(e, key)], self.regs[(e, key)], delta)
            self.inloop = False
        for (e, key) in used:
            self.E[e].free_register(self.regs[(e, key)])
        for e in self.cnt:
            self.cnt[e] += (n_iter - 1) * P[e]
        for d in self.dsems:
            d.total += (n_iter - 1) * P[id(d)]
        shift = {key: -(n_iter - 1) * P[key] for key in P}
        self._load_bufs(self._snap_bufs(shift))
        for e in self.seen:
            self.seen[e] = {}
        self.barrier()

    def phase_begin(self, pes):
        self.es = pes
        self.phase_no += 1
        self._mark = (len(self.dsems), len(self.bufs))

    def phase_end(self, es):
        self.barrier()
        self.es = es
        del self.dsems[self._mark[0]:]
        del self.bufs[self._mark[1]:]


def _bc(ap, shape):
    return ap.to_broadcast(shape)


C_ID = 0
C_IDA = 128
C_IDB = 256
C_BONES = 384
C_ONES = 512
C_ROT = 640
C_SEL = 768
C_ID3 = 1152
NCONST = 1160


def make_consts():
    c = np.zeros((128, NCONST), np.float32)
    c[:, C_ID:C_ID + 128] = np.eye(128)
    c[:64, C_IDA:C_IDA + 64] = np.eye(64)
    c[64:, C_IDB + 64:C_IDB + 128] = np.eye(64)
    c[:64, C_BONES:C_BONES + 64] = 1.0
    c[64:, C_BONES + 64:C_BONES + 128] = 1.0
    c[:, C_ONES:C_ONES + 128] = 1.0
    R = np.zeros((128, 128), np.float32)
    for blk in range(2):
        o = blk * 64
        for i in range(16):
            R[o + 16 + i, o + i] = -1.0
            R[o + i, o + 16 + i] = 1.0
            R[o + 48 + i, o + 32 + i] = -1.0
            R[o + 32 + i, o + 48 + i] = 1.0
    c[:, C_ROT:C_ROT + 128] = R
    for b in range(3):
        c[b, C_SEL + b * 128:C_SEL + (b + 1) * 128] = 1.0
    c[:3, C_ID3:C_ID3 + 3] = np.eye(3)
    return c


def rope_tables(TX, TCX):
    T = TX + TCX
    rows = TX // 64
    row_id = np.repeat(np.arange(rows), 64).astype(np.float32)
    col_id = np.tile(np.arange(64), rows).astype(np.float32)
    inv = (10000.0 ** (-np.arange(0, 32, 2, dtype=np.float32) / 32)).astype(np.float32)
    ar = row_id[:, None] * inv
    ac = col_id[:, None] * inv
    ang = np.concatenate([ar, ar, ac, ac], axis=-1)
    cos = np.ones((T, 64), np.float32)
    sin = np.zeros((T, 64), np.float32)
    cos[TCX:] = np.cos(ang)
    sin[TCX:] = np.sin(ang)
    cs = np.concatenate([cos.T, cos.T], axis=0)
    sn = np.concatenate([sin.T, sin.T], axis=0)
    return np.ascontiguousarray(cs), np.ascontiguousarray(sn)


def colform(v, n):
    return np.ascontiguousarray(np.asarray(v, np.float32).reshape(n, 128).T)


def geom(TX, TCX):
    T = TX + TCX
    NT = T // 128
    TP = T + 4
    blocks = []
    p = 0
    while p < TCX:
        n = min(512, TCX - p)
        blocks.append((p, n, p + 1))
        p += n
    p = 0
    while p < TX:
        n = min(512, TX - p)
        blocks.append((TCX + p, n, TCX + 3 + p))
        p += n
    return T, NT, TP, blocks


def build_program(TX, TCX, phases="ABCDEFG", debug=()):
    T, NT, TP, blocks = geom(TX, TCX)
    NTC = TCX // 128
    nc = bass.Bass("TRN2", target_bir_lowering=False)
    dram = {}

    def din(name, shape, dt=F32):
        dram[name] = nc.dram_tensor(name, list(shape), dt, kind="ExternalInput").ap()
        return dram[name]

    def dscr(name, shape, dt=F32):
        kind = "ExternalOutput" if name in debug else "Internal"
        dram[name] = nc.dram_tensor(name, list(shape), dt, kind=kind).ap()
        return dram[name]

    seq = din("seq", [NB, T, D])
    csT = din("csT", [128, 8, 3])
    consts_d = din("consts", [128, NCONST])
    w_mod = din("w_mod", [D, 6 * D])
    b_mod = din("b_mod", [1, 6 * D])
    n1w = din("n1w", [128, 8])
    n2w = din("n2w", [128, 8])
    w_in = din("w_in", [D, INW])
    shift_w = din("shift_w", [3, 2048])
    rw_w0 = din("rw_w0", [128, 2, 4])
    rw_a0 = din("rw_a0", [128, 2, 4])
    rw_wup = din("rw_wup", [2, 64, RW])
    rw_aup = din("rw_aup", [2, 64, RW])
    rw_gup = din("rw_gup", [2, 128, RW])
    rw_kk = din("rw_kk", [128, 4])
    rw_ka = din("rw_ka", [128, 4])
    rw_rk = din("rw_rk", [128, 4])
    rw_lnw = din("rw_lnw", [128, 4])
    rw_lnb = din("rw_lnb", [128, 4])
    qnw = din("qnw", [128, 1])
    knw = din("knw", [128, 1])
    lamv = din("lamv", [1, 256])
    sublnw = din("sublnw", [128, 1])
    w_out = din("w_out", [D, D])
    w_rt = din("w_rt", [D, 36])
    b_rt = din("b_rt", [1, 36])
    moe_g = din("moe_g", [NE, D, FF])
    moe_u = din("moe_u", [NE, D, FF])
    moe_d = din("moe_d", [NE, FF, D])
    ropec = din("ropec", [128, T])
    ropes = din("ropes", [128, T])
    out_d = nc.dram_tensor("out", [NB, TX, D], F32, kind="ExternalOutput").ap()

    P_d = dscr("P_d", [NB, INW, T])
    NCH = T // 8
    cols_d = dscr("cols_d", [2, 128, NCH + 4, NB, 4, 8, 6], BF16)
    rows_d = dscr("rows_d", [2, 6, T + 16, NB * 4, 192], BF16)
    g_d = dscr("g_d", [2, NB, 4, 128, T], BF16)
    bon_d = dscr("bon_d", [2, NB, 4, 128, T], BF16)
    y_d = dscr("y_d", [2, 2, T + 2, NB * 4, 64], BF16)
    qT_d = dscr("qT_d", [NB, 4, 128, T], BF16)
    kT_d = dscr("kT_d", [NB, 4, 128, T], BF16)
    vt_d = dscr("vt_d", [NB, T, 512], BF16)
    cat_d = dscr("cat_d", [NB, D, TX], BF16)
    x1_d = dscr("x1_d", [NB, TX, D])
    h2T_d = dscr("h2T_d", [NB, D, TX], BF16)
    wd_d = dscr("wd_d", [NB, TX, NE])

    with ExitStack() as es:
        k = K(nc, es)
        consts = k.sb("consts_sb", [128, NCONST], F32)
        cbf = k.sb("cbf", [128, 768], BF16)
        modT = k.sb("modT", [128, 48, 3], F32)
        A1 = k.sb("A1", [128, 8, 3], F32)
        A2 = k.sb("A2", [128, 8, 3], F32)
        G1 = k.sb("G1", [128, NB, D], F32)
        G2 = k.sb("G2", [128, NB, D], F32)
        eps_t = k.sb("eps_t", [128, 1], F32)
        b_consts = k.buf("consts")
        b_mod_ = k.buf("mod")
        ds_c = k.dsem("sp", "c")
        k.dma(ds_c, consts[:], consts_d[:, :], writes=[b_consts])
        k.op("dve", lambda e: e.tensor_copy(cbf[:], consts[:, 0:768]), reads=[b_consts], writes=[b_consts])
        k.op("dve", lambda e: e.memset(eps_t[:], 1e-6), writes=[b_consts])
        ident_bf = cbf[:, C_ID:C_ID + 128]
        identA_bf = cbf[:, C_IDA:C_IDA + 128]
        identB_bf = cbf[:, C_IDB:C_IDB + 128]
        bones_bf = cbf[:, C_BONES:C_BONES + 128]
        ones_bf = cbf[:, C_ONES:C_ONES + 128]
        rot_bf = cbf[:, C_ROT:C_ROT + 128]
        k.barrier()

        if "A" in phases:
            with ExitStack() as pes:
                k.phase_begin(pes)
                silT = k.sb("silT", [128, 8, 3], F32)
                modrow = k.sb("modrow", [3, 6 * D], F32)
                bmr = k.sb("bmr", [3, 6 * D], F32)
                n1c = k.sb("n1c", [128, 8], F32)
                n2c = k.sb("n2c", [128, 8], F32)
                wm = [k.sb("wm%d" % i, [128, 8, 1024], F32) for i in range(2)]
                pa = [k.ps("pa%d" % i, [3, 512]) for i in range(2)]
                pc = k.ps("pc", [128, 48, 3])
                pg = [k.ps("pg%d" % i, [128, 512]) for i in range(2)]
                b_sil, b_bmr, b_pc = k.buf(), k.buf(), k.buf()
                b_wm = [k.buf(), k.buf()]
                b_pa = [k.buf(), k.buf()]
                b_pg = [k.buf(), k.buf()]
                ds_a = k.dsem("sp", "a")
                ds_w = [k.dsem("sp", "wm0"), k.dsem("sp", "wm1")]
                k.dma(ds_a, silT[:], csT[:, :, :], writes=[b_sil])
                k.dma(ds_a, bmr[:], b_mod.partition_broadcast(3), writes=[b_bmr])
                k.dma(ds_a, n1c[:], n1w[:, :], writes=[b_bmr])
                k.dma(ds_a, n2c[:], n2w[:, :], writes=[b_bmr])
                k.op("act", lambda e: e.activation(out=silT[:], in_=silT[:], func=AF.Silu), reads=[b_sil], writes=[b_sil])
                wmv = w_mod.rearrange("(kc p) n -> p kc n", p=128)
                for m in range(6):
                    s = m % 2
                    k.dma(ds_w[s], wm[s][:], wmv[:, :, m * 1024:(m + 1) * 1024], writes=[b_wm[s]])
                    for blk in range(2):
                        pb = (m * 2 + blk) % 2
                        for kc in range(8):
                            k.op("pe", lambda e, kc=kc, s=s, blk=blk, pb=pb: e.matmul(
                                pa[pb][:], silT[:, kc, :], wm[s][:, kc, blk * 512:(blk + 1) * 512],
                                start=(kc == 0), stop=(kc == 7)), reads=[b_sil, b_wm[s]], writes=[b_pa[pb]])
                        c0 = m * 1024 + blk * 512
                        k.op("dve", lambda e, pb=pb, c0=c0: e.tensor_tensor(
                            out=modrow[:, c0:c0 + 512], in0=pa[pb][:], in1=bmr[:, c0:c0 + 512], op=ALU.add),
                            reads=[b_pa[pb], b_bmr], writes=[b_mod_])
                for f in range(48):
                    k.op("pe", lambda e, f=f: e.matmul(pc[:, f, :], modrow[0:3, f * 128:(f + 1) * 128],
                                                      consts[0:3, C_ID3:C_ID3 + 3], start=True, stop=True),
                         reads=[b_mod_, b_consts], writes=[b_pc])
                k.op("dve", lambda e: e.tensor_copy(modT[:], pc[:]), reads=[b_pc], writes=[b_mod_])
                k.op("dve", lambda e: e.scalar_tensor_tensor(
                    out=A1[:], in0=modT[:, 8:16, :], scalar=1.0, in1=_bc(n1c[:].unsqueeze(2), [128, 8, 3]),
                    op0=ALU.add, op1=ALU.mult), reads=[b_mod_, b_bmr], writes=[b_mod_])
                k.op("dve", lambda e: e.scalar_tensor_tensor(
                    out=A2[:], in0=modT[:, 32:40, :], scalar=1.0, in1=_bc(n2c[:].unsqueeze(2), [128, 8, 3]),
                    op0=ALU.add, op1=ALU.mult), reads=[b_mod_, b_bmr], writes=[b_mod_])
                i = 0
                for gi, Gt in ((2, G1), (5, G2)):
                    for b in range(NB):
                        for blk in range(2):
                            pb = i % 2
                            i += 1
                            c0 = gi * 1024 + blk * 512
                            k.op("pe", lambda e, b=b, c0=c0, pb=pb: e.matmul(
                                pg[pb][:], consts[0:3, C_SEL + b * 128:C_SEL + (b + 1) * 128],
                                modrow[0:3, c0:c0 + 512], start=True, stop=True),
                                reads=[b_mod_, b_consts], writes=[b_pg[pb]])
                            k.op("act", lambda e, Gt=Gt, b=b, blk=blk, pb=pb: e.activation(
                                out=Gt[:, b, blk * 512:(blk + 1) * 512], in_=pg[pb][:], func=AF.Copy),
                                reads=[b_pg[pb]], writes=[b_mod_])
                k.barrier()
                k.phase_end(es)
        B1 = modT[:, 0:8, :]
        B2 = modT[:, 24:32, :]

        if "dbgA" in debug:
            pass

        if "B" in phases:
            with ExitStack() as pes:
                k.phase_begin(pes)
                hT = k.sb("hT", [128, 8, TP], BF16)
                xt = [k.sb("xt%d" % i, [128, D], F32) for i in range(2)]
                sq = k.sb("sq", [128, D], F32)
                ss = k.sb("ss", [128, 4], F32)
                xn = [k.sb("xn%d" % i, [128, D], BF16) for i in range(2)]
                pt = [k.ps("pt%d" % i, [128, 8, 128], BF16) for i in range(2)]
                wst = [k.sb("wst%d" % i, [128, 8, 128], F32) for i in range(2)]
                wbf = [k.sb("wbf%d" % i, [128, 8, 3, 128], BF16) for i in range(2)]
                swb = k.sb("swb", [128, 3, 2048], F32)
                pp = [k.ps("pp%d" % i, [128, 512]) for i in range(4)]
                ev = [k.sb("ev%d" % i, [128, 512], F32) for i in range(4)]
                b_hT = k.buf("hT")
                b_xt, b_xn, b_pt = [k.buf(), k.buf()], [k.buf(), k.buf()], [k.buf(), k.buf()]
                b_sq, b_ss, b_swb = k.buf(), k.buf(), k.buf()
                b_wst, b_wbf = [k.buf(), k.buf()], [k.buf(), k.buf()]
                b_pp, b_ev = [k.buf() for _ in range(4)], [k.buf() for _ in range(4)]
                ds_x = [k.dsem("sp", "x0"), k.dsem("sp", "x1")]
                ds_ws = [k.dsem("sp", "ws0"), k.dsem("sp", "ws1")]
                ds_sw = k.dsem("sp", "sw")
                ds_ev = [k.dsem("pool", "ev%d" % i) for i in range(4)]
                k.dma(ds_sw, swb[:].rearrange("p a b -> p (a b)"),
                      shift_w.rearrange("a b -> (a b)").unsqueeze(0).partition_broadcast(128)
                      if False else shift_w.rearrange("(o a) b -> o (a b)", o=1).partition_broadcast(128),
                      writes=[b_swb])
                k.op("pool", lambda e: e.memset(hT[:], 0.0), writes=[b_hT])
                w_in_v = w_in.rearrange("(kc p) n -> p kc n", p=128)
                for b in range(NB):
                    for q in range(NT):
                        s = q % 2
                        sel = 2 if q < NTC else b
                        col0 = (q * 128 + 1) if q < NTC else (q * 128 + 3)
                        k.dma(ds_x[s], xt[s][:], seq[b, q * 128:(q + 1) * 128, :], writes=[b_xt[s]])
                        k.op("act", lambda e, s=s: e.activation(out=sq[:], in_=xt[s][:], func=AF.Square),
                             reads=[b_xt[s]], writes=[b_sq])
                        k.op("dve", lambda e: e.tensor_reduce(out=ss[:, 0:1], in_=sq[:], axis=AX.X, op=ALU.add),
                             reads=[b_sq], writes=[b_ss])
                        k.op("act", lambda e: e.activation(out=ss[:, 1:2], in_=ss[:, 0:1], func=AF.Sqrt,
                                                           bias=eps_t[:, 0:1], scale=1.0 / D),
                             reads=[b_ss, b_consts], writes=[b_ss])
                        k.op("dve", lambda e: e.reciprocal(ss[:, 2:3], ss[:, 1:2]), reads=[b_ss], writes=[b_ss])
                        k.op("dve", lambda e, s=s: e.tensor_scalar(out=xn[s][:], in0=xt[s][:], scalar1=ss[:, 2:3],
                                                                   scalar2=None, op0=ALU.mult),
                             reads=[b_xt[s], b_ss], writes=[b_xn[s]])
                        for kc in range(8):
                            k.op("pe", lambda e, s=s, kc=kc: e.transpose(out=pt[s][:, kc, :],
                                                                         in_=xn[s][:, kc * 128:(kc + 1) * 128],
                                                                         identity=ident_bf),
                                 reads=[b_xn[s], b_consts], writes=[b_pt[s]])
                        for kc in range(8):
                            k.op("act", lambda e, s=s, kc=kc, sel=sel, col0=col0: e.activation(
                                out=hT[:, kc, col0:col0 + 128], in_=pt[s][:, kc, :], func=AF.Identity,
                                bias=B1[:, kc, sel:sel + 1], scale=A1[:, kc, sel:sel + 1]),
                                reads=[b_pt[s], b_mod_], writes=[b_hT])
                    ie = 0
                    for c in range(28):
                        s = c % 2
                        k.dma(ds_ws[s], wst[s][:], w_in_v[:, :, c * 128:(c + 1) * 128], writes=[b_wst[s]])
                        ntap = 3 if c < 16 else 1
                        if c < 16:
                            for j in range(3):
                                eng = "pool" if j == 1 else "dve"
                                k.op(eng, lambda e, s=s, j=j, c=c: e.tensor_tensor(
                                    out=wbf[s][:, :, j, :], in0=wst[s][:],
                                    in1=_bc(swb[:, j, c * 128:(c + 1) * 128].unsqueeze(1), [128, 8, 128]),
                                    op=ALU.mult), reads=[b_wst[s], b_swb], writes=[b_wbf[s]])
                        else:
                            k.op("dve", lambda e, s=s: e.tensor_copy(wbf[s][:, :, 1, :], wst[s][:]),
                                 reads=[b_wst[s]], writes=[b_wbf[s]])
                        for (pos0, N, colb) in blocks:
                            pi = ie % 4
                            ie += 1
                            taps = (0, 1, 2) if c < 16 else (1,)
                            nmm = len(taps) * 8
                            im = 0
                            for j in taps:
                                for kc in range(8):
                                    k.op("pe", lambda e, pi=pi, s=s, kc=kc, j=j, colb=colb, N=N, im=im, nmm=nmm: e.matmul(
                                        pp[pi][:, 0:N], wbf[s][:, kc, j, :], hT[:, kc, colb + j - 1:colb + j - 1 + N],
                                        start=(im == 0), stop=(im == nmm - 1)),
                                        reads=[b_wbf[s], b_hT], writes=[b_pp[pi]])
                                    im += 1
                            eng = "act" if pi % 2 == 0 else "dve"
                            if eng == "act":
                                k.op("act", lambda e, pi=pi, N=N: e.activation(out=ev[pi][:, 0:N], in_=pp[pi][:, 0:N], func=AF.Copy),
                                     reads=[b_pp[pi]], writes=[b_ev[pi]])
                            else:
                                k.op("dve", lambda e, pi=pi, N=N: e.tensor_copy(ev[pi][:, 0:N], pp[pi][:, 0:N]),
                                     reads=[b_pp[pi]], writes=[b_ev[pi]])
                            k.dma(ds_ev[pi], P_d[b, c * 128:(c + 1) * 128, pos0:pos0 + N], ev[pi][:, 0:N],
                                  reads=[b_ev[pi]])
                    k.barrier()
                k.phase_end(es)

        if "C" in phases:
            with ExitStack() as pes:
                k.phase_begin(pes)
                PB = [k.sb("PB%d" % i, [128, 16, 512], F32) for i in range(2)]
                b_PB = [k.buf(), k.buf()]
                ds_pb = [k.dsem("sp", "pb0"), k.dsem("sp", "pb1")]
                ds_st = k.dsem("sp", "cst")
                ds_o = [k.dsem("pool", "co%d" % i) for i in range(4)]
                wst_c = k.sb("wst_c", [128, 512], F32)
                Wwa = k.sb("Wwa", [128, 2, 512], BF16)
                Wg = k.sb("Wg", [128, 2, 512], BF16)
                colc = k.sb("colc", [128, 40], F32)
                b_w = k.buf("cw")
                for d in range(2):
                    k.dma(ds_st, wst_c[0:64, :], rw_wup[d], writes=[b_w])
                    k.dma(ds_st, wst_c[64:128, :], rw_aup[d], writes=[b_w])
                    k.op("dve", lambda e, d=d: e.tensor_copy(Wwa[:, d, :], wst_c[:]), reads=[b_w], writes=[b_w])
                    k.dma(ds_st, wst_c[:], rw_gup[d], writes=[b_w])
                    k.op("dve", lambda e, d=d: e.tensor_copy(Wg[:, d, :], wst_c[:]), reads=[b_w], writes=[b_w])
                k.dma(ds_st, colc[:, 0:8], rw_w0.rearrange("p a b -> p (a b)"), writes=[b_w])
                k.dma(ds_st, colc[:, 8:16], rw_a0.rearrange("p a b -> p (a b)"), writes=[b_w])
                k.dma(ds_st, colc[:, 16:20], rw_kk[:, :], writes=[b_w])
                k.dma(ds_st, colc[:, 20:24], rw_ka[:, :], writes=[b_w])
                k.dma(ds_st, colc[:, 24:28], rw_rk[:, :], writes=[b_w])
                k.op("dve", lambda e: e.tensor_scalar(out=colc[:, 28:32], in0=colc[:, 20:24], scalar1=-1.0, scalar2=1.0,
                                                      op0=ALU.mult, op1=ALU.add), reads=[b_w], writes=[b_w])
                Vbf = k.sb("Vbf", [128, 4, 512], BF16)
                RH = k.sb("RH", [128, 4, 514], F32)
                CAx = k.sb("CAx", [128, 6], BF16)
                b_RH = k.buf("RH")
                ds_rh = k.dsem("sp", "rh")
                k.op("pool", lambda e: e.memset(CAx[:], 0.0), writes=[b_RH])
                TLs = [k.sb("TL%d" % i, [128, 512], BF16) for i in range(2)]
                SGs = [k.sb("SG%d" % i, [128, 512], BF16) for i in range(2)]
                f32ts = [{n: k.sb("c_%s%d" % (n, i), [128, 512], F32) for n in ("sgw", "dec", "Aa", "kkf", "sd", "kkn", "tmpk", "kd")} for i in range(2)]
                bfts = [{n: k.sb("c_%s%d" % (n, i), [128, 512], BF16) for n in ("Gg", "kk2", "bsc", "kdb", "rkr", "bon")} for i in range(2)]
                CAb = k.sb("CAb", [128, 64, 4, 8, 6], BF16)
                CAw = CAb[:].bitcast(F32)
                bCA = k.buf("CAb")
                rowsbs = [k.sb("rowsb%d" % i, [128, 4, 128], BF16) for i in range(2)]
                vrow = k.sb("vrow", [128, 4, 128], BF16)
                pw, pa_, pg_, pss, pbo = (k.ps(n, [128, 512]) for n in ("pw", "pa_", "pg_", "pss", "pbo"))
                prow = k.ps("prow", [128, 4, 128])
                pv = k.ps("pv", [128, 4, 128])
                bbs = [{n: k.buf(n) for n in ("TL", "SG", "sgw", "dec", "Aa", "kkf", "sd", "kkn", "tmpk", "kd", "Gg", "kk2",
                                              "bsc", "kdb", "rkr", "bon", "CA", "rowsb")} for i in range(2)]
                bbg = {n: k.buf(n) for n in ("Vbf", "vrow", "pw", "pa_", "pg_", "pss", "pbo", "prow", "pv")}
                for i in range(2):
                    bbs[i].update(bbg)
                bb = bbs[0]
                k.op("pool", lambda e: e.memset(CAb[:], 0.0), writes=[bCA])
                ihp = 0
                idd = 0
                irow = 0
                ztile = k.sb("ztile", [128, 1536], BF16)
                b_z = k.buf("z")
                k.op("pool", lambda e: e.memset(ztile[:], 0.0), writes=[b_z])
                for d in range(2):
                    zv = rows_d[d, 2:4, 0:T].rearrange("r t s c -> r t (s c)")
                    for i in range(2 * T // 128):
                        k.dma(ds_o[i % 4], zv[i // (T // 128), (i % (T // 128)) * 128:(i % (T // 128) + 1) * 128, :], ztile[:], reads=[b_z])
                ib = 0
                for b in range(NB):
                    for (pos0, N, _c) in blocks:
                        s = ib % 2
                        ib += 1
                        P = PB[s]
                        bP = b_PB[s]
                        k.dma(ds_pb[s], P[:, :, 0:N], P_d[b, 0:2048, pos0:pos0 + N].rearrange("(c p) n -> p c n", p=128),
                              writes=[bP])
                        k.op("pool", lambda e: e.memset(RH[:], 0.0), writes=[b_RH])
                        lo = max(pos0 - 1, 0)
                        hi = min(pos0 + N + 1, T)
                        co = lo - (pos0 - 1)
                        k.dma(ds_rh, RH[:, :, co:co + hi - lo], P_d[b, 0:512, lo:hi].rearrange("(c p) n -> p c n", p=128), writes=[b_RH])
                        k.op("pool", lambda e, P=P, N=N: e.tensor_copy(Vbf[:, :, 0:N], P[:, 8:12, 0:N]), reads=[bP], writes=[bb["Vbf"]])
                        for j in range(N // 128):
                            for hp in range(4):
                                k.op("pe", lambda e, hp=hp, j=j: e.matmul(pv[:, hp, :], Vbf[:, hp, j * 128:(j + 1) * 128], ident_bf,
                                                                         start=True, stop=True),
                                     reads=[bb["Vbf"], b_consts], writes=[bb["pv"]])
                            k.op("act", lambda e: e.activation(out=vrow[:], in_=pv[:], func=AF.Copy), reads=[bb["pv"]], writes=[bb["vrow"]])
                            p0 = pos0 + j * 128
                            for ab in range(2):
                                for dd in range(2):
                                    k.dma(ds_o[ab * 2 + dd], rows_d[dd, 4 + ab, p0:p0 + 128, b * 4:(b + 1) * 4, 128:192],
                                          vrow[:, :, ab * 64:(ab + 1) * 64], reads=[bb["vrow"]])
                        for d in range(2):
                            TL = TLs[idd % 2]
                            SG = SGs[idd % 2]
                            bbd = bbs[idd % 2]
                            idd += 1
                            k.op("act", lambda e, P=P, N=N, d=d, TL=TL: e.activation(out=TL[0:64, 0:N], in_=P[0:64, 12 + 2 * d, 0:N], func=AF.Tanh),
                                 reads=[bP], writes=[bbd["TL"]])
                            k.op("dve", lambda e, P=P, N=N, d=d: e.tensor_copy(TL[64:128, 0:N], P[64:128, 12 + 2 * d, 0:N]),
                                 reads=[bP], writes=[bbd["TL"]])
                            k.op("act", lambda e, P=P, N=N, d=d: e.activation(out=SG[:, 0:N], in_=P[:, 13 + 2 * d, 0:N], func=AF.Sigmoid),
                                 reads=[bP], writes=[bbd["SG"]])
                            for hp in range(4):
                                hs = slice(hp * 128, (hp + 1) * 128)
                                t = f32ts[ihp % 2]
                                u = bfts[ihp % 2]
                                bb = dict(bbs[ihp % 2])
                                bb["TL"] = bbd["TL"]
                                bb["SG"] = bbd["SG"]
                                bb["CA"] = bCA
                                nch = N // 8
                                c16 = lambda ap: ap.rearrange("p (c s) -> p c s", s=8)
                                ihp += 1
                                k.op("pe", lambda e, d=d, hs=hs, N=N: e.matmul(pw[:, 0:N], Wwa[0:64, d, hs], TL[0:64, 0:N], start=True, stop=True),
                                     reads=[b_w, bb["TL"]], writes=[bb["pw"]])
                                k.op("pe", lambda e, d=d, hs=hs, N=N: e.matmul(pa_[:, 0:N], Wwa[64:128, d, hs], TL[64:128, 0:N], start=True, stop=True),
                                     reads=[b_w, bb["TL"]], writes=[bb["pa_"]])
                                k.op("pe", lambda e, d=d, hs=hs, N=N: e.matmul(pg_[:, 0:N], Wg[:, d, hs], SG[:, 0:N], start=True, stop=True),
                                     reads=[b_w, bb["SG"]], writes=[bb["pg_"]])
                                ci = d * 4 + hp
                                k.op("act", lambda e, N=N, ci=ci: e.activation(out=t["sgw"][:, 0:N], in_=pw[:, 0:N], func=AF.Sigmoid,
                                                                               bias=colc[:, ci:ci + 1], scale=1.0),
                                     reads=[bb["pw"], b_w], writes=[bb["sgw"]])
                                k.op("act", lambda e, N=N: e.activation(out=t["dec"][:, 0:N], in_=t["sgw"][:, 0:N], func=AF.Exp,
                                                                        scale=-math.exp(-0.5)),
                                     reads=[bb["sgw"]], writes=[bb["dec"]])
                                k.op("dve", lambda e, N=N: e.tensor_copy(CAw[:, 0:nch, hp, :, 2], c16(t["dec"][:, 0:N])),
                                     reads=[bb["dec"]], writes=[bCA])
                                k.op("act", lambda e, N=N, ci=ci: e.activation(out=t["Aa"][:, 0:N], in_=pa_[:, 0:N], func=AF.Sigmoid,
                                                                               bias=colc[:, 8 + ci:9 + ci], scale=1.0),
                                     reads=[bb["pa_"], b_w], writes=[bb["Aa"]])
                                k.op("act", lambda e, N=N: e.activation(out=u["Gg"][:, 0:N], in_=pg_[:, 0:N], func=AF.Copy),
                                     reads=[bb["pg_"]], writes=[bb["Gg"]])
                                k.dma(ds_o[3], g_d[d, b, hp, :, pos0:pos0 + N], u["Gg"][:, 0:N], reads=[bb["Gg"]])
                                kk_ = P[:, 4 + hp, 0:N]
                                r_ = P[:, hp, 0:N]
                                v_ = P[:, 8 + hp, 0:N]
                                k.op("dve", lambda e, N=N, hp=hp, kk_=kk_: e.tensor_scalar(out=t["kkf"][:, 0:N], in0=kk_, scalar1=colc[:, 16 + hp:17 + hp],
                                                                                        scalar2=None, op0=ALU.mult),
                                     reads=[bP, b_w], writes=[bb["kkf"]])
                                k.op("act", lambda e, N=N: e.activation(out=u["kk2"][:, 0:N], in_=t["kkf"][:, 0:N], func=AF.Square),
                                     reads=[bb["kkf"]], writes=[bb["kk2"]])
                                k.op("pe", lambda e, N=N: e.matmul(pss[:, 0:N], bones_bf, u["kk2"][:, 0:N], start=True, stop=True),
                                     reads=[bb["kk2"], b_consts], writes=[bb["pss"]])
                                k.op("act", lambda e, N=N: e.activation(out=t["sd"][:, 0:N], in_=pss[:, 0:N], func=AF.Sqrt),
                                     reads=[bb["pss"]], writes=[bb["sd"]])
                                k.op("dve", lambda e, N=N: e.tensor_scalar(out=t["sd"][:, 0:N], in0=t["sd"][:, 0:N], scalar1=1e-12, scalar2=None, op0=ALU.max),
                                     reads=[bb["sd"]], writes=[bb["sd"]])
                                k.op("dve", lambda e, N=N: e.reciprocal(t["sd"][:, 0:N], t["sd"][:, 0:N]), reads=[bb["sd"]], writes=[bb["sd"]])
                                k.op("dve", lambda e, N=N: e.tensor_tensor(out=t["kkn"][:, 0:N], in0=t["kkf"][:, 0:N], in1=t["sd"][:, 0:N], op=ALU.mult),
                                     reads=[bb["kkf"], bb["sd"]], writes=[bb["kkn"]])
                                k.op("dve", lambda e, N=N: e.tensor_tensor(out=u["bsc"][:, 0:N], in0=t["kkn"][:, 0:N], in1=t["Aa"][:, 0:N], op=ALU.mult),
                                     reads=[bb["kkn"], bb["Aa"]], writes=[bb["bsc"]])
                                k.op("dve", lambda e, N=N, hp=hp: e.tensor_scalar(out=t["tmpk"][:, 0:N], in0=t["Aa"][:, 0:N], scalar1=colc[:, 20 + hp:21 + hp],
                                                                                 scalar2=colc[:, 28 + hp:29 + hp], op0=ALU.mult, op1=ALU.add),
                                     reads=[bb["Aa"], b_w], writes=[bb["tmpk"]])
                                k.op("dve", lambda e, N=N, kk_=kk_: e.tensor_tensor(out=t["kd"][:, 0:N], in0=kk_, in1=t["tmpk"][:, 0:N], op=ALU.mult),
                                     reads=[bP, bb["tmpk"]], writes=[bb["kd"]])
                                k.op("act", lambda e, N=N: e.activation(out=u["kdb"][:, 0:N], in_=t["kd"][:, 0:N], func=AF.Copy),
                                     reads=[bb["kd"]], writes=[bb["kdb"]])
                                k.op("dve", lambda e, N=N, hp=hp, r_=r_: e.scalar_tensor_tensor(out=u["rkr"][:, 0:N], in0=r_, scalar=colc[:, 24 + hp:25 + hp],
                                                                                             in1=t["kd"][:, 0:N], op0=ALU.mult, op1=ALU.mult),
                                     reads=[bP, bb["kd"], b_w], writes=[bb["rkr"]])
                                k.op("pe", lambda e, N=N: e.matmul(pbo[:, 0:N], bones_bf, u["rkr"][:, 0:N], start=True, stop=True),
                                     reads=[bb["rkr"], b_consts], writes=[bb["pbo"]])
                                k.op("dve", lambda e, N=N, v_=v_: e.tensor_tensor(out=u["bon"][:, 0:N], in0=pbo[:, 0:N], in1=v_, op=ALU.mult),
                                     reads=[bb["pbo"], bP], writes=[bb["bon"]])
                                k.dma(ds_o[0], bon_d[d, b, hp, :, pos0:pos0 + N], u["bon"][:, 0:N], reads=[bb["bon"]])
                                k.op("dve", lambda e, N=N: e.tensor_scalar(out=CAb[0:64, 0:nch, hp, :, 0], in0=c16(t["kkn"][0:64, 0:N]), scalar1=-1.0, scalar2=None, op0=ALU.mult),
                                     reads=[bb["kkn"]], writes=[bb["CA"]])
                                k.op("dve", lambda e, N=N: e.tensor_scalar(out=CAb[64:128, 0:nch, hp, :, 1], in0=c16(t["kkn"][64:128, 0:N]), scalar1=-1.0, scalar2=None, op0=ALU.mult),
                                     reads=[bb["kkn"]], writes=[bb["CA"]])
                                ro = 0 if d == 0 else 2
                                k.op("act", lambda e, N=N, hp=hp, ro=ro: e.activation(out=CAb[0:64, 0:nch, hp, :, 2], in_=c16(RH[0:64, hp, ro:ro + N]), func=AF.Copy),
                                     reads=[b_RH], writes=[bb["CA"]])
                                k.op("act", lambda e, N=N, hp=hp, ro=ro: e.activation(out=CAb[64:128, 0:nch, hp, :, 3], in_=c16(RH[64:128, hp, ro:ro + N]), func=AF.Copy),
                                     reads=[b_RH], writes=[bb["CA"]])
                                if d == 0 and pos0 + N == T:
                                    k.op("act", lambda e, N=N, hp=hp: e.activation(out=CAx[0:64, 2:3], in_=RH[0:64, hp, N:N + 1], func=AF.Copy),
                                         reads=[b_RH], writes=[b_RH])
                                    k.op("act", lambda e, N=N, hp=hp: e.activation(out=CAx[64:128, 3:4], in_=RH[64:128, hp, N:N + 1], func=AF.Copy),
                                         reads=[b_RH], writes=[b_RH])
                                    k.dma(ds_rh, cols_d[0, :, NCH, b, hp, 0, :], CAx[:], reads=[b_RH])
                                if hp == 3:
                                    k.dma(ds_o[1], cols_d[d, :, pos0 // 8:pos0 // 8 + nch, b, :, :, :], CAb[:, 0:nch], reads=[bCA])
                                for j in range(N // 128):
                                    js = slice(j * 128, (j + 1) * 128)
                                    rowsb = rowsbs[irow % 2]
                                    bb["rowsb"] = bbs[irow % 2]["rowsb"]
                                    irow += 1
                                    k.op("pe", lambda e, js=js: e.matmul(prow[:, 0, :], u["bsc"][:, js], identA_bf, start=True, stop=True),
                                         reads=[bb["bsc"], b_consts], writes=[bb["prow"]])
                                    k.op("pe", lambda e, js=js: e.matmul(prow[:, 1, :], u["bsc"][:, js], identB_bf, start=True, stop=True),
                                         reads=[bb["bsc"], b_consts], writes=[bb["prow"]])
                                    k.op("pe", lambda e, js=js: e.matmul(prow[:, 2, :], u["kdb"][:, js], identA_bf, start=True, stop=True),
                                         reads=[bb["kdb"], b_consts], writes=[bb["prow"]])
                                    k.op("pe", lambda e, js=js: e.matmul(prow[:, 3, :], u["kdb"][:, js], identB_bf, start=True, stop=True),
                                         reads=[bb["kdb"], b_consts], writes=[bb["prow"]])
                                    k.op("dve", lambda e: e.tensor_copy(rowsb[:], prow[:]), reads=[bb["prow"]], writes=[bb["rowsb"]])
                                    p0 = pos0 + j * 128
                                    k.dma(ds_o[2], rows_d[d, 0:2, p0:p0 + 128, b * 4 + hp, 0:128].rearrange("r t c -> t r c"), rowsb[:, 0:2, :],
                                          reads=[bb["rowsb"]])
                                    k.dma(ds_o[3], rows_d[d, 4:6, p0:p0 + 128, b * 4 + hp, 0:128].rearrange("r t c -> t r c"), rowsb[:, 2:4, :],
                                          reads=[bb["rowsb"]])
                k.barrier()
                k.phase_end(es)

        if "D" in phases:
            with ExitStack() as pes:
                k.phase_begin(pes)
                CH = 16
                HS = 8
                ST = k.sb("ST", [128, 2, 8, 64], F32)
                T1 = k.sb("T1", [128, 2, 8, 64], F32)
                STb = k.sb("STb", [128, 2, 8, 64], BF16)
                G2_ = range(2)
                colsAR = [[k.sb("colsAR%d%d" % (g, h), [128, 1, 8, HS, 6], BF16) for h in G2_] for g in G2_]
                wv = [[colsAR[g][h][:].bitcast(F32) for h in G2_] for g in G2_]
                RS = [[k.sb("RS%d%d" % (g, h), [6, HS, 8, 192], BF16) for h in G2_] for g in G2_]
                ps1 = [k.ps("ps1%d" % g, [4, 8, 64]) for g in G2_]
                ps2 = [k.ps("ps2%d" % g, [128, 8, 64]) for g in G2_]
                b_ST, b_T1, b_STb = [k.buf(), k.buf()], [k.buf(), k.buf()], [k.buf(), k.buf()]
                b_cols = [[k.buf(), k.buf()] for g in G2_]
                b_rows = [[k.buf(), k.buf()] for g in G2_]
                b_stv = [[k.buf(), k.buf()] for g in G2_]
                b_sty = [[k.buf(), k.buf()] for g in G2_]
                b_ps1, b_ps2 = [k.buf(), k.buf()], [k.buf(), k.buf()]
                qn = ["sp", "pool"]
                ds_c = [[k.dsem(qn[g], "dc%d%d" % (g, h)) for h in G2_] for g in G2_]
                ds_r = [[k.dsem(qn[g], "dr%d%d" % (g, h)) for h in G2_] for g in G2_]
                ds_y = [[k.dsem(qn[g], "dy%d%d" % (g, h)) for h in G2_] for g in G2_]
                k.op("dve", lambda e: e.memset(ST[:], 0.0), writes=b_ST)
                k.op("dve", lambda e: e.memset(STb[:], 0.0), writes=b_STb)
                for g in G2_:
                    for h in G2_:
                        k.op("pool", lambda e, g=g, h=h: e.memset(RS[g][h][:], 0.0), writes=[b_rows[g][h], b_sty[g][h]])
                cols_v = [cols_d[g].rearrange("p c b h s x -> p c (b h) s x") for g in G2_]

                def dsl(start, size):
                    if isinstance(start, int):
                        return slice(start, start + size)
                    return bass.ds(start, size)

                def c8idx(g, h, cbase, n, it):
                    if g == 0:
                        return (cbase + it) * 2 + h
                    return ((cbase + n - 1) - it) * 2 + (1 - h)

                def load_half(g, h, c8):
                    p0 = c8 * HS
                    k.dma(ds_c[g][h], colsAR[g][h][:], cols_v[g][:, dsl(c8, 1)], writes=[b_cols[g][h]])
                    k.dma(ds_r[g][h], RS[g][h][:], rows_d[g, :, dsl(p0, HS), :, :], writes=[b_rows[g][h], b_sty[g][h]])

                def scan_body(cbase, n, static):
                    def body(it):
                        for h in G2_:
                            for k8 in range(HS):
                                tl = [k8, HS - 1 - k8]
                                for g in G2_:
                                    for pr in range(8):
                                        k.op("pe", lambda e, g=g, pr=pr, t_=tl[g]: e.matmul(
                                            ps1[g][0:4, pr, :], colsAR[g][h][:, 0, pr, t_, 0:4], STb[:, g, pr, :], start=True, stop=True),
                                            reads=[b_cols[g][h], b_STb[g]], writes=[b_ps1[g]])
                                for g in G2_:
                                    k.op("act", lambda e, g=g, t_=tl[g]: e.activation(
                                        out=RS[g][h][0:4, t_, :, 128:192], in_=ps1[g][0:4, :, :], func=AF.Copy),
                                        reads=[b_ps1[g]], writes=[b_sty[g][h]])
                                for g in G2_:
                                    k.op("pool", lambda e, g=g, t_=tl[g]: e.tensor_tensor(
                                        out=T1[:, g], in0=ST[:, g], in1=_bc(wv[g][h][:, 0, :, t_, 2:3], [128, 8, 64]), op=ALU.mult),
                                        reads=[b_ST[g], b_cols[g][h]], writes=[b_T1[g]])
                                for g in G2_:
                                    for pr in range(8):
                                        k.op("pe", lambda e, g=g, pr=pr, t_=tl[g]: e.matmul(
                                            ps2[g][:, pr, :], RS[g][h][0:6, t_, pr, 0:128], RS[g][h][0:6, t_, pr, 128:192],
                                            start=True, stop=True),
                                            reads=[b_rows[g][h], b_sty[g][h]], writes=[b_ps2[g]])
                                for g in G2_:
                                    k.op("dve", lambda e, g=g: e.tensor_tensor(out=STb[:, g], in0=T1[:, g], in1=ps2[g][:], op=ALU.add),
                                         reads=[b_T1[g], b_ps2[g]], writes=[b_STb[g]])
                                    k.op("dve", lambda e, g=g: e.tensor_tensor(out=ST[:, g], in0=T1[:, g], in1=ps2[g][:], op=ALU.add),
                                         reads=[b_T1[g], b_ps2[g]], writes=[b_ST[g]])
                                if k8 == 2:
                                    ito = it + 1 if h == 1 else it
                                    for g in G2_:
                                        if static and h == 1 and (it + 1 >= n):
                                            continue
                                        load_half(g, 1 - h, c8idx(g, 1 - h, cbase, n, ito))
                            for g in G2_:
                                p0 = c8idx(g, h, cbase, n, it) * HS
                                k.dma(ds_y[g][h], y_d[g, :, dsl(p0 + 1, HS), :, :], RS[g][h][2:4, :, :, 128:192], reads=[b_sty[g][h]])
                    return body

                def prologue(cbase, n):
                    for g in G2_:
                        load_half(g, 0, c8idx(g, 0, cbase, n, 0))

                nctx, nx = TCX // CH, TX // CH
                prologue(0, nctx)
                k.loop(nctx, scan_body(0, nctx, True), static=True)
                prologue(nctx, nx)
                k.loop(nx, scan_body(nctx, nx, False))
                for g, pv_ in ((0, T), (1, TCX - 1)):
                    k.dma(ds_c[g][0], colsAR[g][0][:], cols_v[g][:, pv_ // 8:pv_ // 8 + 1], writes=[b_cols[g][0]])
                    for pr in range(8):
                        k.op("pe", lambda e, g=g, pr=pr: e.matmul(ps1[g][0:4, pr, :], colsAR[g][0][:, 0, pr, pv_ % 8, 0:4], STb[:, g, pr, :],
                                                                  start=True, stop=True),
                             reads=[b_cols[g][0], b_STb[g]], writes=[b_ps1[g]])
                    k.op("act", lambda e, g=g: e.activation(out=RS[g][0][0:4, 0, :, 128:192], in_=ps1[g][0:4, :, :], func=AF.Copy),
                         reads=[b_ps1[g]], writes=[b_sty[g][0]])
                    k.dma(ds_y[g][0], y_d[g, :, pv_ + 1:pv_ + 2, :, :], RS[g][0][2:4, 0:1, :, 128:192], reads=[b_sty[g][0]])
                k.phase_end(es)

        xblocks = [(p, n) for (p, n, _c) in blocks if p >= TCX]
        if "E" in phases:
            with ExitStack() as pes:
                k.phase_begin(pes)
                Gt = k.sb("Gt", [128, 2, 4, 512], BF16)
                Bt = k.sb("Bt", [128, 2, 4, 512], BF16)
                Yt = k.sb("Yt", [128, 2, 4, 2, 64], BF16)
                Yf = k.sb("Yf", [128, 16, 64], F32)
                cen = k.sb("cen", [128, 16, 64], F32)
                sqe = k.sb("sqe", [128, 16, 64], F32)
                st4 = k.sb("st4", [128, 4, 16], F32)
                yh = k.sb("yh", [128, 2, 512], BF16)
                Zn = k.sb("Zn", [128, 2, 4, 128], F32)
                catR = k.sb("catR", [128, 4, 512], BF16)
                lnc = k.sb("lnc", [128, 8], F32)
                gne = k.sb("gne", [128, 1], F32)
                pte = k.ps("pte", [128, 8, 128], BF16)
                bG, bB, bY, bYf, bcen, bsq, bst, byh, bZn, bcat, bln, bpte = (k.buf() for _ in range(12))
                ds_e = [k.dsem("sp", "e%d" % i) for i in range(3)]
                ds_eo = k.dsem("pool", "eo")
                k.dma(ds_e[2], lnc[:, 0:4], rw_lnw[:, :], writes=[bln])
                k.dma(ds_e[2], lnc[:, 4:8], rw_lnb[:, :], writes=[bln])
                k.op("dve", lambda e: e.memset(gne[:], 64e-5), writes=[bln])
                for b in range(NB):
                    for (pos0, N) in xblocks:
                        for d in range(2):
                            k.dma(ds_e[0], Gt[:, d, :, 0:N], g_d[d, b, :, :, pos0:pos0 + N].rearrange("h p t -> p h t"), writes=[bG])
                            k.dma(ds_e[0], Bt[:, d, :, 0:N], bon_d[d, b, :, :, pos0:pos0 + N].rearrange("h p t -> p h t"), writes=[bB])
                        for j in range(N // 128):
                            pos = pos0 + j * 128
                            for d in range(2):
                                sl0 = pos + 2 if d == 0 else pos
                                for ab in range(2):
                                    k.dma(ds_e[1], Yt[:, d, :, ab, :], y_d[d, ab, sl0:sl0 + 128, b * 4:(b + 1) * 4, :], writes=[bY])
                            k.op("act", lambda e: e.activation(out=Yf[:], in_=Yt[:].rearrange("p d h a i -> p (d h a) i"), func=AF.Copy),
                                 reads=[bY], writes=[bYf])
                            k.op("dve", lambda e: e.tensor_reduce(out=st4[:, 0, :], in_=Yf[:], axis=AX.X, op=ALU.add), reads=[bYf], writes=[bst])
                            k.op("dve", lambda e: e.tensor_scalar(out=st4[:, 1, :], in0=st4[:, 0, :], scalar1=-1.0 / 64, scalar2=None, op0=ALU.mult),
                                 reads=[bst], writes=[bst])
                            k.op("dve", lambda e: e.tensor_tensor(out=cen[:], in0=Yf[:], in1=_bc(st4[:, 1, :].unsqueeze(2), [128, 16, 64]), op=ALU.add),
                                 reads=[bYf, bst], writes=[bcen])
                            k.op("act", lambda e: e.activation(out=sqe[:], in_=cen[:], func=AF.Square), reads=[bcen], writes=[bsq])
                            k.op("dve", lambda e: e.tensor_reduce(out=st4[:, 2, :], in_=sqe[:], axis=AX.X, op=ALU.add), reads=[bsq], writes=[bst])
                            k.op("act", lambda e: e.activation(out=st4[:, 3, :], in_=st4[:, 2, :], func=AF.Sqrt, bias=gne[:, 0:1], scale=1.0 / 64),
                                 reads=[bst, bln], writes=[bst])
                            k.op("dve", lambda e: e.reciprocal(st4[:, 3, :], st4[:, 3, :]), reads=[bst], writes=[bst])
                            k.op("dve", lambda e: e.tensor_tensor(out=yh[:].rearrange("p d (g i) -> p (d g) i", i=64), in0=cen[:],
                                                                  in1=_bc(st4[:, 3, :].unsqueeze(2), [128, 16, 64]), op=ALU.mult),
                                 reads=[bcen, bst], writes=[byh])
                            for d in range(2):
                                for hp in range(4):
                                    k.op("pe", lambda e, d=d, hp=hp: e.transpose(out=pte[:, d * 4 + hp, :], in_=yh[:, d, hp * 128:(hp + 1) * 128],
                                                                                 identity=ident_bf), reads=[byh, b_consts], writes=[bpte])
                            for d in range(2):
                                for hp in range(4):
                                    k.op("act", lambda e, d=d, hp=hp: e.activation(out=Zn[:, d, hp, :], in_=pte[:, d * 4 + hp, :], func=AF.Identity,
                                                                                   bias=lnc[:, 4 + hp:5 + hp], scale=lnc[:, hp:hp + 1]),
                                         reads=[bpte, bln], writes=[bZn])
                            js = slice(j * 128, (j + 1) * 128)
                            k.op("dve", lambda e, js=js: e.tensor_tensor(out=Zn[:], in0=Zn[:], in1=Bt[:, :, :, js], op=ALU.add), reads=[bZn, bB], writes=[bZn])
                            k.op("dve", lambda e, js=js: e.tensor_tensor(out=Zn[:], in0=Zn[:], in1=Gt[:, :, :, js], op=ALU.mult), reads=[bZn, bG], writes=[bZn])
                            k.op("dve", lambda e, js=js: e.tensor_tensor(out=catR[:, :, js], in0=Zn[:, 0], in1=Zn[:, 1], op=ALU.add), reads=[bZn], writes=[bcat])
                        k.dma(ds_eo, cat_d[b, 0:512, pos0 - TCX:pos0 - TCX + N].rearrange("(h p) t -> p h t", p=128), catR[:, :, 0:N], reads=[bcat])
                k.phase_end(es)

        if "F" in phases:
            with ExitStack() as pes:
                k.phase_begin(pes)
                PD = [k.sb("PD%d" % i, [128, 12, 512], F32) for i in range(2)]
                cosb = [k.sb("cosb%d" % i, [128, 512], F32) for i in range(2)]
                sinb = [k.sb("sinb%d" % i, [128, 512], F32) for i in range(2)]
                x2 = k.sb("x2", [128, 512], BF16)
                sdf = k.sb("sdf", [128, 512], F32)
                XQ = k.sb("XQ", [128, 512], F32)
                XQb = k.sb("XQb", [128, 512], BF16)
                t1f = k.sb("t1f", [128, 512], F32)
                t2f = k.sb("t2f", [128, 512], F32)
                qo = [k.sb("qo%d" % i, [128, 512], BF16) for i in range(2)]
                Vb = k.sb("Vb", [128, 4, 512], BF16)
                vtk = [k.sb("vtk%d" % i, [128, 512], BF16) for i in range(2)]
                nwc = k.sb("nwc", [128, 2], F32)
                pssf = k.ps("pssf", [128, 512])
                prot = k.ps("prot", [128, 512])
                pvf = k.ps("pvf", [128, 4, 128])
                bPD, bcs = [k.buf(), k.buf()], [k.buf(), k.buf()]
                bx2, bsd, bXQ, bXQb, bt1, bt2, bVb, bnw, bpss, bprot, bpv = (k.buf() for _ in range(11))
                bqo, bvtk = [k.buf(), k.buf()], [k.buf(), k.buf()]
                ds_f = [k.dsem("sp", "f0"), k.dsem("sp", "f1")]
                ds_fw = k.dsem("sp", "fw")
                ds_fo = [k.dsem("pool", "fo0"), k.dsem("pool", "fo1")]
                ds_fv = [k.dsem("pool", "fv0"), k.dsem("pool", "fv1")]
                k.dma(ds_fw, nwc[:, 0:1], qnw[:, :], writes=[bnw])
                k.dma(ds_fw, nwc[:, 1:2], knw[:, :], writes=[bnw])
                ib = 0
                iq = 0
                iv = 0
                for b in range(NB):
                    for (pos0, N, _c) in blocks:
                        s_ = ib % 2
                        ib += 1
                        P = PD[s_]
                        k.dma(ds_f[s_], P[:, :, 0:N], P_d[b, 2048:3584, pos0:pos0 + N].rearrange("(c p) n -> p c n", p=128), writes=[bPD[s_]])
                        k.dma(ds_f[s_], cosb[s_][:, 0:N], ropec[:, pos0:pos0 + N], writes=[bcs[s_]])
                        k.dma(ds_f[s_], sinb[s_][:, 0:N], ropes[:, pos0:pos0 + N], writes=[bcs[s_]])
                        for c in range(8):
                            if c < 4 and pos0 < TCX:
                                continue
                            X = P[:, c, 0:N]
                            wi = 0 if c < 4 else 1
                            k.op("act", lambda e, X=X, N=N: e.activation(out=x2[:, 0:N], in_=X, func=AF.Square), reads=[bPD[s_]], writes=[bx2])
                            k.op("pe", lambda e, N=N: e.matmul(pssf[:, 0:N], bones_bf, x2[:, 0:N], start=True, stop=True), reads=[bx2, b_consts], writes=[bpss])
                            k.op("act", lambda e, N=N: e.activation(out=sdf[:, 0:N], in_=pssf[:, 0:N], func=AF.Sqrt, bias=eps_t[:, 0:1], scale=1.0 / 64),
                                 reads=[bpss, b_consts], writes=[bsd])
                            k.op("dve", lambda e, N=N: e.reciprocal(sdf[:, 0:N], sdf[:, 0:N]), reads=[bsd], writes=[bsd])
                            k.op("dve", lambda e, X=X, N=N, wi=wi: e.scalar_tensor_tensor(out=XQ[:, 0:N], in0=X, scalar=nwc[:, wi:wi + 1], in1=sdf[:, 0:N],
                                                                                         op0=ALU.mult, op1=ALU.mult),
                                 reads=[bPD[s_], bsd, bnw], writes=[bXQ])
                            k.op("act", lambda e, N=N: e.activation(out=XQb[:, 0:N], in_=XQ[:, 0:N], func=AF.Copy), reads=[bXQ], writes=[bXQb])
                            k.op("pe", lambda e, N=N: e.matmul(prot[:, 0:N], rot_bf, XQb[:, 0:N], start=True, stop=True), reads=[bXQb, b_consts], writes=[bprot])
                            k.op("pool", lambda e, N=N, s_=s_: e.tensor_tensor(out=t1f[:, 0:N], in0=XQ[:, 0:N], in1=cosb[s_][:, 0:N], op=ALU.mult),
                                 reads=[bXQ, bcs[s_]], writes=[bt1])
                            k.op("dve", lambda e, N=N, s_=s_: e.tensor_tensor(out=t2f[:, 0:N], in0=prot[:, 0:N], in1=sinb[s_][:, 0:N], op=ALU.mult),
                                 reads=[bprot, bcs[s_]], writes=[bt2])
                            qs = iq % 2
                            iq += 1
                            k.op("dve", lambda e, N=N, qs=qs: e.tensor_tensor(out=qo[qs][:, 0:N], in0=t1f[:, 0:N], in1=t2f[:, 0:N], op=ALU.add),
                                 reads=[bt1, bt2], writes=[bqo[qs]])
                            dst = qT_d[b, c, :, pos0:pos0 + N] if c < 4 else kT_d[b, c - 4, :, pos0:pos0 + N]
                            k.dma(ds_fo[qs], dst, qo[qs][:, 0:N], reads=[bqo[qs]])
                        k.op("act", lambda e, P=P, N=N: e.activation(out=Vb[:, :, 0:N], in_=P[:, 8:12, 0:N], func=AF.Copy), reads=[bPD[s_]], writes=[bVb])
                        for j in range(N // 128):
                            for h in range(4):
                                k.op("pe", lambda e, h=h, j=j: e.matmul(pvf[:, h, :], Vb[:, h, j * 128:(j + 1) * 128], ident_bf, start=True, stop=True),
                                     reads=[bVb, b_consts], writes=[bpv])
                            vs = iv % 2
                            iv += 1
                            k.op("act", lambda e, vs=vs: e.activation(out=vtk[vs][:], in_=pvf[:].rearrange("p h c -> p (h c)"), func=AF.Copy),
                                 reads=[bpv], writes=[bvtk[vs]])
                            p0 = pos0 + j * 128
                            k.dma(ds_fv[vs], vt_d[b, p0:p0 + 128, :], vtk[vs][:], reads=[bvtk[vs]])
                k.phase_end(es)

            with ExitStack() as pes:
                k.phase_begin(pes)
                LAM_INIT = 0.8 - 0.6 * math.exp(-0.3 * 0)
                KT = [k.sb("KT%d" % i, [128, T], BF16) for i in range(2)]
                VT = [k.sb("VT%d" % i, [128, NT, 128], BF16) for i in range(2)]
                QT = [k.sb("QT%d" % i, [128, 512], BF16) for i in range(2)]
                pT2 = [k.sb("pT2%d" % i, [128, 2, 512], BF16) for i in range(2)]
                lamt = k.sb("lamt", [1, 256], F32)
                lamw = k.sb("lamw", [1, 136], F32)
                nlamc = k.sb("nlamc", [128, 1], F32)
                slw = k.sb("slw", [128, 1], F32)
                o0 = k.sb("o0", [128, 512], F32)
                o1 = k.sb("o1", [128, 512], F32)
                rz = k.sb("rz", [128, 512], F32)
                od2 = k.sb("od2", [128, 512], BF16)
                res = [k.sb("res%d" % i, [128, 512], BF16) for i in range(2)]
                sT2 = [k.ps("sT2%d" % i, [128, 2, 512]) for i in range(2)]
                Oa = [k.ps("Oa%d" % m, [128, 512]) for m in range(2)]
                Za = [k.ps("Za%d" % m, [128, 512]) for m in range(2)]
                bKT, bVT, bQT = [k.buf(), k.buf()], [k.buf(), k.buf()], [k.buf(), k.buf()]
                bpT2 = [k.buf(), k.buf()]
                bsT2 = [k.buf(), k.buf()]
                bO, bZ = [k.buf(), k.buf()], [k.buf(), k.buf()]
                blam, bo0, bo1, brz, bod2 = (k.buf() for _ in range(5))
                bres = [k.buf(), k.buf()]
                ds_kv = [k.dsem("sp", "kv0"), k.dsem("sp", "kv1")]
                ds_q = [k.dsem("sp", "q0"), k.dsem("sp", "q1")]
                ds_l = k.dsem("sp", "lam")
                ds_ro = [k.dsem("pool", "ro0"), k.dsem("pool", "ro1")]
                k.dma(ds_l, lamt[:], lamv[:, :], writes=[blam])
                k.dma(ds_l, slw[:], sublnw[:, :], writes=[blam])
                k.op("dve", lambda e: e.tensor_tensor(out=lamw[:, 0:64], in0=lamt[:, 0:64], in1=lamt[:, 64:128], op=ALU.mult), reads=[blam], writes=[blam])
                k.op("dve", lambda e: e.tensor_tensor(out=lamw[:, 64:128], in0=lamt[:, 128:192], in1=lamt[:, 192:256], op=ALU.mult), reads=[blam], writes=[blam])
                k.op("dve", lambda e: e.tensor_reduce(out=lamw[:, 128:129], in_=lamw[:, 0:64], axis=AX.X, op=ALU.add), reads=[blam], writes=[blam])
                k.op("dve", lambda e: e.tensor_reduce(out=lamw[:, 129:130], in_=lamw[:, 64:128], axis=AX.X, op=ALU.add), reads=[blam], writes=[blam])
                k.op("act", lambda e: e.activation(out=lamw[:, 130:132], in_=lamw[:, 128:130], func=AF.Exp), reads=[blam], writes=[blam])
                k.op("dve", lambda e: e.tensor_tensor(out=lamw[:, 132:133], in0=lamw[:, 131:132], in1=lamw[:, 130:131], op=ALU.subtract), reads=[blam], writes=[blam])
                k.op("dve", lambda e: e.tensor_scalar(out=lamw[:, 133:134], in0=lamw[:, 132:133], scalar1=-LAM_INIT, scalar2=None, op0=ALU.add),
                     reads=[blam], writes=[blam])
                k.op("pe", lambda e: e.matmul(Za[0][:, 0:1], consts[0:1, C_ONES:C_ONES + 128], lamw[0:1, 133:134], start=True, stop=True),
                     reads=[blam, b_consts], writes=[bZ[0]])
                k.op("dve", lambda e: e.tensor_copy(nlamc[:], Za[0][:, 0:1]), reads=[bZ[0]], writes=[blam])
                k.op("dve", lambda e: e.tensor_scalar(out=slw[:], in0=slw[:], scalar1=1.0 - LAM_INIT, scalar2=None, op0=ALU.mult), reads=[blam], writes=[blam])
                ih = 0
                iqb = 0
                for b in range(NB):
                    for h in range(4):
                        hs = ih % 2
                        ih += 1
                        k.dma(ds_kv[hs], KT[hs][:], kT_d[b, h, :, :], writes=[bKT[hs]])
                        k.dma(ds_kv[hs], VT[hs][:], vt_d[b, :, h * 128:(h + 1) * 128].rearrange("(n p) c -> p n c", p=128), writes=[bVT[hs]])
                        for (pos0, N) in xblocks:
                            qs = iqb % 2
                            iqb += 1
                            k.dma(ds_q[qs], QT[qs][:, 0:N], qT_d[b, h, :, pos0:pos0 + N], writes=[bQT[qs]])
                            def score(kt):
                                i_ = kt % 2
                                for m in range(2):
                                    ms = slice(64 * m, 64 * m + 64)
                                    k.op("pe", lambda e, m=m, ms=ms: e.matmul(sT2[i_][:, m, 0:N], KT[hs][ms, kt * 128:(kt + 1) * 128], QT[qs][ms, 0:N],
                                                                          start=True, stop=True),
                                         reads=[bKT[hs], bQT[qs]], writes=[bsT2[i_]])

                            LOOK = 1
                            for kt in range(min(LOOK, NT)):
                                score(kt)
                            for kt in range(NT):
                                i_ = kt % 2
                                k.op("act", lambda e: e.activation(out=pT2[i_][:, :, 0:N], in_=sT2[i_][:, :, 0:N], func=AF.Exp, scale=0.125),
                                     reads=[bsT2[i_]], writes=[bpT2[i_]])
                                if kt + LOOK < NT:
                                    score(kt + LOOK)
                                for m in range(2):
                                    k.op("pe", lambda e, m=m: e.matmul(Oa[m][:, 0:N], VT[hs][:, kt, :], pT2[i_][:, m, 0:N], start=(kt == 0), stop=(kt == NT - 1)),
                                         reads=[bVT[hs], bpT2[i_]], writes=[bO[m]])
                                    k.op("pe", lambda e, m=m: e.matmul(Za[m][:, 0:N], ones_bf, pT2[i_][:, m, 0:N], start=(kt == 0), stop=(kt == NT - 1)),
                                         reads=[bpT2[i_], b_consts], writes=[bZ[m]])
                            k.op("dve", lambda e, N=N: e.reciprocal(rz[:, 0:N], Za[0][:, 0:N]), reads=[bZ[0]], writes=[brz])
                            k.op("dve", lambda e, N=N: e.tensor_tensor(out=o0[:, 0:N], in0=Oa[0][:, 0:N], in1=rz[:, 0:N], op=ALU.mult), reads=[bO[0], brz], writes=[bo0])
                            k.op("dve", lambda e, N=N: e.reciprocal(rz[:, 0:N], Za[1][:, 0:N]), reads=[bZ[1]], writes=[brz])
                            k.op("dve", lambda e, N=N: e.tensor_tensor(out=o1[:, 0:N], in0=Oa[1][:, 0:N], in1=rz[:, 0:N], op=ALU.mult), reads=[bO[1], brz], writes=[bo1])
                            k.op("dve", lambda e, N=N: e.scalar_tensor_tensor(out=o0[:, 0:N], in0=o1[:, 0:N], scalar=nlamc[:, 0:1], in1=o0[:, 0:N],
                                                                               op0=ALU.mult, op1=ALU.add), reads=[bo1, bo0, blam], writes=[bo0])
                            k.op("pool", lambda e, N=N: e.tensor_tensor(out=od2[:, 0:N], in0=o0[:, 0:N], in1=o0[:, 0:N], op=ALU.mult), reads=[bo0], writes=[bod2])
                            k.op("pe", lambda e, N=N: e.matmul(sT2[0][:, 0, 0:N], ones_bf, od2[:, 0:N], start=True, stop=True),
                                 reads=[bod2, b_consts], writes=[bsT2[0]])
                            k.op("act", lambda e, N=N: e.activation(out=rz[:, 0:N], in_=sT2[0][:, 0, 0:N], func=AF.Sqrt, bias=eps_t[:, 0:1], scale=1.0 / 128),
                                 reads=[bsT2[0], b_consts], writes=[brz])
                            k.op("dve", lambda e, N=N: e.reciprocal(rz[:, 0:N], rz[:, 0:N]), reads=[brz], writes=[brz])
                            rs = iqb % 2
                            k.op("dve", lambda e, N=N, rs=rs: e.scalar_tensor_tensor(out=res[rs][:, 0:N], in0=o0[:, 0:N], scalar=slw[:, 0:1], in1=rz[:, 0:N],
                                                                                     op0=ALU.mult, op1=ALU.mult), reads=[bo0, brz, blam], writes=[bres[rs]])
                            k.dma(ds_ro[rs], cat_d[b, 512 + h * 128:512 + (h + 1) * 128, pos0 - TCX:pos0 - TCX + N], res[rs][:, 0:N], reads=[bres[rs]])
                k.phase_end(es)

        if "G" in phases:
            with ExitStack() as pes:
                k.phase_begin(pes)
                wo_st = k.sb("wo_st", [128, 4, D], F32)
                woutb = k.sb("woutb", [128, 8, D], BF16)
                wrt = k.sb("wrt", [128, 8, 36], F32)
                brt = k.sb("brt", [128, 36], F32)
                xt2 = [k.sb("xt2%d" % i, [128, D], F32) for i in range(2)]
                catT = [k.sb("catT%d" % i, [128, 8, 128], BF16) for i in range(2)]
                tmpo = k.sb("tmpo", [128, D], F32)
                x1 = [k.sb("x1%d" % i, [128, D], F32) for i in range(2)]
                sq2 = k.sb("sq2", [128, D], F32)
                ss2 = k.sb("ss2", [128, 4], F32)
                xn2 = k.sb("xn2", [128, D], F32)
                h2f = k.sb("h2f", [128, 8, 128], F32)
                h2b = [k.sb("h2b%d" % i, [128, 8, 128], BF16) for i in range(2)]
                Lg = k.sb("Lg", [128, 36], F32)
                rt = k.sb("rt", [128, 16], F32)
                goh = k.sb("goh", [128, 4], F32)
                em = k.sb("em", [128, 4, 8], F32)
                em2 = k.sb("em2", [128, 32], F32)
                m1 = k.sb("m1", [128, 32], F32)
                m2 = k.sb("m2", [128, 32], F32)
                Wdt = [k.sb("Wdt%d" % i, [128, 32], F32) for i in range(2)]
                po = [k.ps("po%d" % i, [128, 512]) for i in range(2)]
                ptf = k.ps("ptf", [128, 8, 128])
                pl = k.ps("pl", [128, 36])
                bw, bpo, bptf, bpl, btmp, bsq2, bss2, bxn2, bh2f, bL, brt_ = (k.buf() for _ in range(11))
                bxt, bcatT, bx1, bh2b, bWd = ([k.buf(), k.buf()] for _ in range(5))
                bpo = [k.buf(), k.buf()]
                ds_w = k.dsem("sp", "gw")
                ds_i = [k.dsem("sp", "gi0"), k.dsem("sp", "gi1")]
                ds_o1 = [k.dsem("pool", "go0"), k.dsem("pool", "go1")]
                wov = w_out.rearrange("(kc p) n -> p kc n", p=128)
                for hf in range(2):
                    k.dma(ds_w, wo_st[:], wov[:, hf * 4:(hf + 1) * 4, :], writes=[bw])
                    k.op("dve", lambda e, hf=hf: e.tensor_copy(woutb[:, hf * 4:(hf + 1) * 4, :], wo_st[:]), reads=[bw], writes=[bw])
                k.dma(ds_w, wrt[:], w_rt.rearrange("(kc p) n -> p kc n", p=128), writes=[bw])
                k.dma(ds_w, brt[:], b_rt.partition_broadcast(128), writes=[bw])
                it_ = 0
                for b in range(NB):
                    for xp in range(0, TX, 128):
                        s_ = it_ % 2
                        it_ += 1
                        k.dma(ds_i[s_], xt2[s_][:], seq[b, TCX + xp:TCX + xp + 128, :], writes=[bxt[s_]])
                        k.dma(ds_i[s_], catT[s_][:], cat_d[b, :, xp:xp + 128].rearrange("(c p) t -> p c t", p=128), writes=[bcatT[s_]])
                        for hf in range(2):
                            for kc in range(8):
                                k.op("pe", lambda e, hf=hf, kc=kc, s_=s_: e.matmul(po[hf][:], catT[s_][:, kc, :], woutb[:, kc, hf * 512:(hf + 1) * 512],
                                                                                   start=(kc == 0), stop=(kc == 7)),
                                     reads=[bcatT[s_], bw], writes=[bpo[hf]])
                            hsl = slice(hf * 512, (hf + 1) * 512)
                            k.op("dve", lambda e, hf=hf, hsl=hsl, b=b: e.tensor_tensor(out=tmpo[:, hsl], in0=po[hf][:], in1=G1[:, b, hsl], op=ALU.mult),
                                 reads=[bpo[hf], b_mod_], writes=[btmp])
                            k.op("dve", lambda e, hsl=hsl, s_=s_: e.tensor_tensor(out=x1[s_][:, hsl], in0=tmpo[:, hsl], in1=xt2[s_][:, hsl], op=ALU.add),
                                 reads=[btmp, bxt[s_]], writes=[bx1[s_]])
                        k.dma(ds_o1[s_], x1_d[b, xp:xp + 128, :], x1[s_][:], reads=[bx1[s_]])
                        k.op("act", lambda e, s_=s_: e.activation(out=sq2[:], in_=x1[s_][:], func=AF.Square), reads=[bx1[s_]], writes=[bsq2])
                        k.op("dve", lambda e: e.tensor_reduce(out=ss2[:, 0:1], in_=sq2[:], axis=AX.X, op=ALU.add), reads=[bsq2], writes=[bss2])
                        k.op("act", lambda e: e.activation(out=ss2[:, 1:2], in_=ss2[:, 0:1], func=AF.Sqrt, bias=eps_t[:, 0:1], scale=1.0 / D),
                             reads=[bss2, b_consts], writes=[bss2])
                        k.op("dve", lambda e: e.reciprocal(ss2[:, 2:3], ss2[:, 1:2]), reads=[bss2], writes=[bss2])
                        k.op("dve", lambda e, s_=s_: e.tensor_scalar(out=xn2[:], in0=x1[s_][:], scalar1=ss2[:, 2:3], scalar2=None, op0=ALU.mult),
                             reads=[bx1[s_], bss2], writes=[bxn2])
                        for kc in range(8):
                            k.op("pe", lambda e, kc=kc: e.transpose(out=ptf[:, kc, :], in_=xn2[:, kc * 128:(kc + 1) * 128], identity=consts[:, C_ID:C_ID + 128]),
                                 reads=[bxn2, b_consts], writes=[bptf])
                        for kc in range(8):
                            k.op("act", lambda e, kc=kc, b=b: e.activation(out=h2f[:, kc, :], in_=ptf[:, kc, :], func=AF.Identity,
                                                                           bias=B2[:, kc, b:b + 1], scale=A2[:, kc, b:b + 1]),
                                 reads=[bptf, b_mod_], writes=[bh2f])
                        k.op("act", lambda e, s_=s_: e.activation(out=h2b[s_][:], in_=h2f[:], func=AF.Copy), reads=[bh2f], writes=[bh2b[s_]])
                        k.dma(ds_o1[s_], h2T_d[b, :, xp:xp + 128].rearrange("(c p) t -> p c t", p=128), h2b[s_][:], reads=[bh2b[s_]])
                        for kc in range(8):
                            k.op("pe", lambda e, kc=kc: e.matmul(pl[:], h2f[:, kc, :], wrt[:, kc, :], start=(kc == 0), stop=(kc == 7)),
                                 reads=[bh2f, bw], writes=[bpl])
                        k.op("dve", lambda e: e.tensor_tensor(out=Lg[:], in0=pl[:], in1=brt[:], op=ALU.add), reads=[bpl, bw], writes=[bL])
                        R_ = [bL, brt_]
                        k.op("dve", lambda e: e.tensor_reduce(out=rt[:, 0:1], in_=Lg[:, 0:4], axis=AX.X, op=ALU.max), reads=R_, writes=[brt_])
                        k.op("dve", lambda e: e.tensor_scalar(out=goh[:], in0=Lg[:, 0:4], scalar1=rt[:, 0:1], scalar2=None, op0=ALU.subtract), reads=R_, writes=[brt_])
                        k.op("act", lambda e: e.activation(out=em2[:, 0:4], in_=goh[:], func=AF.Exp), reads=R_, writes=[brt_])
                        k.op("dve", lambda e: e.tensor_reduce(out=rt[:, 1:2], in_=em2[:, 0:4], axis=AX.X, op=ALU.add), reads=R_, writes=[brt_])
                        k.op("dve", lambda e: e.reciprocal(rt[:, 2:3], rt[:, 1:2]), reads=R_, writes=[brt_])
                        k.op("dve", lambda e: e.tensor_scalar(out=goh[:], in0=Lg[:, 0:4], scalar1=rt[:, 0:1], scalar2=None, op0=ALU.is_equal), reads=R_, writes=[brt_])
                        k.op("dve", lambda e: e.tensor_scalar(out=goh[:], in0=goh[:], scalar1=-1.0, scalar2=1e30, op0=ALU.add, op1=ALU.mult), reads=R_, writes=[brt_])
                        k.op("dve", lambda e: e.tensor_tensor(out=em[:], in0=Lg[:, 4:36].rearrange("p (g x) -> p g x", x=8),
                                                              in1=_bc(goh[:].unsqueeze(2), [128, 4, 8]), op=ALU.add), reads=R_, writes=[brt_])
                        emf = em[:].rearrange("p g x -> p (g x)")
                        k.op("dve", lambda e: e.tensor_reduce(out=rt[:, 3:4], in_=emf, axis=AX.X, op=ALU.max), reads=R_, writes=[brt_])
                        k.op("dve", lambda e: e.tensor_scalar(out=m1[:], in0=emf, scalar1=rt[:, 3:4], scalar2=None, op0=ALU.is_equal), reads=R_, writes=[brt_])
                        k.op("dve", lambda e: e.scalar_tensor_tensor(out=em2[:], in0=m1[:], scalar=-1e30, in1=emf, op0=ALU.mult, op1=ALU.add), reads=R_, writes=[brt_])
                        k.op("dve", lambda e: e.tensor_reduce(out=rt[:, 4:5], in_=em2[:], axis=AX.X, op=ALU.max), reads=R_, writes=[brt_])
                        k.op("dve", lambda e: e.tensor_scalar(out=m2[:], in0=em2[:], scalar1=rt[:, 4:5], scalar2=None, op0=ALU.is_equal), reads=R_, writes=[brt_])
                        k.op("dve", lambda e: e.tensor_tensor(out=rt[:, 5:6], in0=rt[:, 4:5], in1=rt[:, 3:4], op=ALU.subtract), reads=R_, writes=[brt_])
                        k.op("act", lambda e: e.activation(out=rt[:, 6:7], in_=rt[:, 5:6], func=AF.Exp), reads=R_, writes=[brt_])
                        k.op("dve", lambda e: e.tensor_scalar(out=rt[:, 7:8], in0=rt[:, 6:7], scalar1=1.0, scalar2=None, op0=ALU.add), reads=R_, writes=[brt_])
                        k.op("dve", lambda e: e.reciprocal(rt[:, 8:9], rt[:, 7:8]), reads=R_, writes=[brt_])
                        k.op("dve", lambda e: e.tensor_tensor(out=rt[:, 9:10], in0=rt[:, 8:9], in1=rt[:, 2:3], op=ALU.mult), reads=R_, writes=[brt_])
                        k.op("dve", lambda e: e.tensor_tensor(out=rt[:, 10:11], in0=rt[:, 9:10], in1=rt[:, 6:7], op=ALU.mult), reads=R_, writes=[brt_])
                        k.op("dve", lambda e: e.tensor_scalar(out=m1[:], in0=m1[:], scalar1=rt[:, 9:10], scalar2=None, op0=ALU.mult), reads=R_, writes=[brt_])
                        k.op("dve", lambda e, s_=s_: e.scalar_tensor_tensor(out=Wdt[s_][:], in0=m2[:], scalar=rt[:, 10:11], in1=m1[:], op0=ALU.mult, op1=ALU.add),
                             reads=R_, writes=[bWd[s_]])
                        k.dma(ds_o1[s_], wd_d[b, xp:xp + 128, :], Wdt[s_][:], reads=[bWd[s_]])
                k.phase_end(es)

            with ExitStack() as pes:
                k.phase_begin(pes)
                TB = min(1024, TX)
                TBC = TB // 128
                NTB = TB // 512
                h2T = k.sb("h2T", [128, 8, TB], BF16)
                acc = k.sb("acc", [128, TBC, D], F32)
                Wd = k.sb("Wd", [128, TBC, NE], F32)
                x1h = k.sb("x1h", [128, 4, D], F32)
                stg = [k.sb("stg%d" % i, [128, 2048], F32) for i in range(3)]
                wgb = [k.sb("wgb%d" % i, [128, 8, FF], BF16) for i in range(2)]
                wub = [k.sb("wub%d" % i, [128, 8, FF], BF16) for i in range(2)]
                wdb = [k.sb("wdb%d" % i, [128, 4, D], BF16) for i in range(2)]
                sg = [k.sb("sg%d" % i, [128, 512], F32) for i in range(2)]
                hid = [k.sb("hid%d" % i, [128, 4, 512], BF16) for i in range(2)]
                pg = [k.ps("pg%d" % i, [128, 512]) for i in range(2)]
                pu = [k.ps("pu%d" % i, [128, 512]) for i in range(2)]
                py = [k.ps("py%d" % i, [128, 512]) for i in range(2)]
                bh2T, bacc, bWd_, bx1h = (k.buf() for _ in range(4))
                bstg = [k.buf() for _ in range(3)]
                bwg, bwu, bwd_, bsg, bhid, bpg, bpu, bpy = ([k.buf(), k.buf()] for _ in range(8))
                ds_m = k.dsem("sp", "mi")
                ds_s = [k.dsem("sp", "ms%d" % i) for i in range(3)]
                ds_x1 = k.dsem("sp", "mx")
                ds_out = k.dsem("pool", "mo")

                def dsl2(start, size):
                    if isinstance(start, int):
                        return slice(start, start + size)
                    return bass.ds(start, size)

                def moe_body(b):
                    def body(it):
                        off = it * TB
                        k.dma(ds_m, h2T[:], h2T_d[b, :, dsl2(off, TB)].rearrange("(c p) t -> p c t", p=128), writes=[bh2T])
                        k.dma(ds_m, Wd[:], wd_d[b, dsl2(off, TB), :].rearrange("(n p) e -> p n e", p=128), writes=[bWd_])
                        k.op("pool", lambda e: e.memset(acc[:], 0.0), writes=[bacc])
                        ist = 0
                        cnt = [0, 0, 0]
                        for ex in range(NE):
                            ws = ex % 2
                            srcs = []
                            gv = moe_g[ex].rearrange("(kc p) f -> p kc f", p=128)
                            uv = moe_u[ex].rearrange("(kc p) f -> p kc f", p=128)
                            dv = moe_d[ex].rearrange("(fc p) n -> p fc n", p=128)
                            for hf in range(2):
                                srcs.append((gv[:, hf * 4:(hf + 1) * 4, :], wgb[ws][:, hf * 4:(hf + 1) * 4, :], bwg[ws], "p (a f) -> p a f", 4))
                                srcs.append((uv[:, hf * 4:(hf + 1) * 4, :], wub[ws][:, hf * 4:(hf + 1) * 4, :], bwu[ws], "p (a f) -> p a f", 4))
                            for hf in range(2):
                                srcs.append((dv[:, hf * 2:(hf + 1) * 2, :], wdb[ws][:, hf * 2:(hf + 1) * 2, :], bwd_[ws], "p (a f) -> p a f", 2))
                            for (src, dst, bdst, pat, a_) in srcs:
                                si = ist % 3
                                ist += 1
                                k.dma(ds_s[si], stg[si][:].rearrange(pat, a=a_), src, writes=[bstg[si]])
                                if ist % 2 == 0:
                                    k.op("pool", lambda e, si=si, dst=dst, pat=pat, a_=a_: e.tensor_copy(dst, stg[si][:].rearrange(pat, a=a_)),
                                         reads=[bstg[si]], writes=[bdst])
                                else:
                                    k.op("act", lambda e, si=si, dst=dst, pat=pat, a_=a_: e.activation(out=dst, in_=stg[si][:].rearrange(pat, a=a_), func=AF.Copy),
                                         reads=[bstg[si]], writes=[bdst])
                            for tb in range(NTB):
                                tsl = slice(tb * 512, (tb + 1) * 512)
                                hs_ = cnt[0] % 2
                                cnt[0] += 1
                                for fc in range(4):
                                    pi = cnt[1] % 2
                                    cnt[1] += 1
                                    fsl = slice(fc * 128, (fc + 1) * 128)
                                    for kc in range(8):
                                        k.op("pe", lambda e, pi=pi, ws=ws, kc=kc, fsl=fsl, tsl=tsl: e.matmul(pg[pi][:], wgb[ws][:, kc, fsl], h2T[:, kc, tsl],
                                                                                                             start=(kc == 0), stop=(kc == 7)),
                                             reads=[bwg[ws], bh2T], writes=[bpg[pi]])
                                    for kc in range(8):
                                        k.op("pe", lambda e, pi=pi, ws=ws, kc=kc, fsl=fsl, tsl=tsl: e.matmul(pu[pi][:], wub[ws][:, kc, fsl], h2T[:, kc, tsl],
                                                                                                             start=(kc == 0), stop=(kc == 7)),
                                             reads=[bwu[ws], bh2T], writes=[bpu[pi]])
                                    k.op("act", lambda e, pi=pi: e.activation(out=sg[pi][:], in_=pg[pi][:], func=AF.Silu), reads=[bpg[pi]], writes=[bsg[pi]])
                                    k.op("dve", lambda e, pi=pi, hs_=hs_, fc=fc: e.tensor_tensor(out=hid[hs_][:, fc, :], in0=sg[pi][:], in1=pu[pi][:], op=ALU.mult),
                                         reads=[bsg[pi], bpu[pi]], writes=[bhid[hs_]])
                                for tc in range(4):
                                    ch = tb * 4 + tc
                                    for hf in range(2):
                                        yi = cnt[2] % 2
                                        cnt[2] += 1
                                        for fc in range(4):
                                            k.op("pe", lambda e, yi=yi, hs_=hs_, fc=fc, tc=tc, ws=ws, hf=hf: e.matmul(
                                                py[yi][:], hid[hs_][:, fc, tc * 128:(tc + 1) * 128], wdb[ws][:, fc, hf * 512:(hf + 1) * 512],
                                                start=(fc == 0), stop=(fc == 3)), reads=[bhid[hs_], bwd_[ws]], writes=[bpy[yi]])
                                        k.op("dve", lambda e, yi=yi, ch=ch, hf=hf, ex=ex: e.scalar_tensor_tensor(
                                            out=acc[:, ch, hf * 512:(hf + 1) * 512], in0=py[yi][:], scalar=Wd[:, ch, ex:ex + 1],
                                            in1=acc[:, ch, hf * 512:(hf + 1) * 512], op0=ALU.mult, op1=ALU.add),
                                            reads=[bpy[yi], bWd_, bacc], writes=[bacc])
                        for hq in range(TBC // 4):
                            k.dma(ds_x1, x1h[:], x1_d[b, dsl2(off + hq * 512, 512), :].rearrange("(n p) d -> p n d", p=128), writes=[bx1h])
                            asl = acc[:, hq * 4:(hq + 1) * 4, :]
                            k.op("dve", lambda e, asl=asl: e.tensor_tensor(out=asl, in0=asl, in1=_bc(G2[:, b, :].unsqueeze(1), [128, 4, D]), op=ALU.mult),
                                 reads=[bacc, b_mod_], writes=[bacc])
                            k.op("pool", lambda e, asl=asl: e.tensor_tensor(out=asl, in0=asl, in1=x1h[:], op=ALU.add), reads=[bacc, bx1h], writes=[bacc])
                            k.dma(ds_out, out_d[b, dsl2(off + hq * 512, 512), :].rearrange("(n p) d -> p n d", p=128), asl, reads=[bacc])
                    return body

                for b in range(NB):
                    k.loop(TX // TB, moe_body(b), static=True)
                k.phase_end(es)
        k.barrier()
    return nc, dram


def core_inputs(inp, b0, TX, TCX, shared=None):
    f = lambda a: np.ascontiguousarray(np.asarray(a, np.float32))
    if shared is None:
        shared = {}
        cs, sn = rope_tables(TX, TCX)
        shared["ropec"], shared["ropes"] = cs, sn
        shared["consts"] = make_consts()
        shared["w_mod"] = f(inp["w_mod"][0])
        shared["b_mod"] = f(inp["b_mod"][0]).reshape(1, -1)
        shared["n1w"] = colform(inp["norm1_w"][0], 8)
        shared["n2w"] = colform(inp["norm2_w"][0], 8)
        shared["w_in"] = f(inp["w_in"][0])
        shared["shift_w"] = f(inp["shift_w"][0])
        shared["rw_w0"] = f(np.asarray(inp["rwkv_w0"][0]).reshape(2, 4, 128).transpose(2, 0, 1))
        shared["rw_a0"] = f(np.asarray(inp["rwkv_a0"][0]).reshape(2, 4, 128).transpose(2, 0, 1))
        shared["rw_wup"] = f(inp["rwkv_w_up"][0])
        shared["rw_aup"] = f(inp["rwkv_a_up"][0])
        shared["rw_gup"] = f(inp["rwkv_g_up"][0])
        shared["rw_kk"] = colform(inp["rwkv_k_k"][0], 4)
        shared["rw_ka"] = colform(inp["rwkv_k_a"][0], 4)
        shared["rw_rk"] = colform(np.asarray(inp["rwkv_r_k"][0]).reshape(-1), 4)
        shared["rw_lnw"] = colform(inp["rwkv_ln_w"][0], 4)
        shared["rw_lnb"] = colform(inp["rwkv_ln_b"][0], 4)
        shared["qnw"] = f(np.tile(np.asarray(inp["q_norm_w"][0]), 2).reshape(128, 1))
        shared["knw"] = f(np.tile(np.asarray(inp["k_norm_w"][0]), 2).reshape(128, 1))
        shared["lamv"] = f(np.concatenate([np.asarray(inp[n][0]) for n in ("lam_q1", "lam_k1", "lam_q2", "lam_k2")]).reshape(1, 256))
        shared["sublnw"] = f(np.asarray(inp["subln_w"][0]).reshape(128, 1))
        shared["w_out"] = f(inp["w_out"][0])
        shared["w_rt"] = f(np.concatenate([np.asarray(inp["w_group"][0]), np.asarray(inp["w_expert"][0])], axis=1))
        shared["b_rt"] = f(np.concatenate([np.asarray(inp["b_group"][0]), np.asarray(inp["b_expert"][0])]).reshape(1, 36))
        shared["moe_g"] = f(inp["moe_w_gate"][0])
        shared["moe_u"] = f(inp["moe_w_up"][0])
        shared["moe_d"] = f(inp["moe_w_down"][0])
    m = dict(shared)
    x = np.asarray(inp["x"][b0:b0 + NB], np.float32)
    ctx = np.asarray(inp["ctx"][b0:b0 + NB], np.float32)
    m["seq"] = np.ascontiguousarray(np.concatenate([ctx, x], axis=1))
    cc = np.concatenate([np.asarray(inp["c"][b0:b0 + NB], np.float32), np.asarray(inp["c_ctx"], np.float32)[None]], axis=0)
    m["csT"] = np.ascontiguousarray(cc.reshape(3, 8, 128).transpose(2, 1, 0))
    return m, shared


TX_FULL, TCX_FULL = 4096, 256
_CACHE = {}


def kernel(**inputs):
    inp = {k_: np.asarray(v) for k_, v in inputs.items()}
    B = inp["x"].shape[0]
    ncores = B // NB
    if "nc" not in _CACHE:
        _CACHE["nc"] = build_program(TX_FULL, TCX_FULL)
    nc, dram = _CACHE["nc"]
    in_maps = []
    shared = None
    for c in range(ncores):
        m, shared = core_inputs(inp, c * NB, TX_FULL, TCX_FULL, shared)
        in_maps.append({k_: v for k_, v in m.items() if k_ in dram})
    res = run_bass_kernel_spmd(nc, in_maps, core_ids=list(range(ncores)))
    out = np.concatenate([np.asarray(r["out"]) for r in res.results], axis=0)
    return out.astype(np.float32, copy=False)
```
